# Optimizing a Trainium2 kernel written in Bass

```python
import math
import jax, jax.numpy as jnp
from jax import lax
import numpy as np

D_MODEL = 2048
BATCH = 2
SEQ = 4096
DEPTH = 2

D_MIX = D_MODEL
HEAD_DIM = 128
GROUP_W = D_MIX // 4
FOX_HEADS = GROUP_W // HEAD_DIM
GDN_HEADS = GROUP_W // HEAD_DIM
GDN_CHUNK = 64
CONV_W = 4
LRU_W = GROUP_W
LRU_BLOCKS = 8
LRU_C = 8.0
NSA_HEADS = GROUP_W // HEAD_DIM
CMP_LEN = 32
CMP_STRIDE = 16
SLC_LEN = 64
N_SELECT = 16
WINDOW = 512
FORCED_SCORE = 1.0e6
N_BUCKETS = 32
MAX_EXACT = 16
MAX_DIST = 1024
D_FF = 5632
Q_BLOCK = 128
EPS = 1e-6

IN_SPLITS = (
    ("fox_q", GROUP_W), ("fox_k", GROUP_W), ("fox_v", GROUP_W), ("fox_f", FOX_HEADS),
    ("gdn_q", GROUP_W), ("gdn_k", GROUP_W), ("gdn_v", GROUP_W), ("gdn_a", GDN_HEADS), ("gdn_b", GDN_HEADS), ("gdn_z", GROUP_W),
    ("lru_x", LRU_W), ("lru_gate", LRU_W),
    ("nsa_q", GROUP_W), ("nsa_kc", HEAD_DIM), ("nsa_vc", HEAD_DIM), ("nsa_ks", HEAD_DIM), ("nsa_vs", HEAD_DIM),
    ("nsa_kw", HEAD_DIM), ("nsa_vw", HEAD_DIM), ("nsa_g", 3 * NSA_HEADS),
)
D_IN = sum(w for _, w in IN_SPLITS)

kernel_name = "hymba_fox_gdn_rglru_nsa_macaron"


def rmsnorm(x, g):
    xf = x.astype(jnp.float32)
    y = xf * lax.rsqrt(jnp.mean(xf * xf, axis=-1, keepdims=True) + EPS)
    return (y * g.astype(jnp.float32)).astype(x.dtype)


def l2norm(x):
    return x * lax.rsqrt(jnp.sum(x * x, axis=-1, keepdims=True) + EPS)


def swiglu(h, w_g, w_u, w_d):
    return (jax.nn.silu(h @ w_g) * (h @ w_u)) @ w_d


def split_cols(z):
    out = []
    off = 0
    for _, w in IN_SPLITS:
        out.append(z[..., off:off + w])
        off += w
    return out


def causal_dwconv(x, w):
    K, C = w.shape
    xp = jnp.pad(x, ((0, 0), (K - 1, 0), (0, 0)))
    return lax.conv_general_dilated(xp, w[:, None, :].astype(x.dtype), window_strides=(1,), padding="VALID",
                                    dimension_numbers=("NWC", "WIO", "NWC"), feature_group_count=C)


def t5_bucket(dist):
    n = jnp.maximum(dist, 0)
    nf = jnp.maximum(n, 1).astype(jnp.float32)
    large = MAX_EXACT + (jnp.log(nf / MAX_EXACT) / math.log(MAX_DIST / MAX_EXACT)
                         * (N_BUCKETS - MAX_EXACT)).astype(jnp.int32)
    large = jnp.minimum(large, N_BUCKETS - 1)
    return jnp.where(n < MAX_EXACT, n, large)


def masked_softmax(s, mask):
    s = jnp.where(mask, s, -jnp.inf)
    m = jnp.max(s, axis=-1, keepdims=True)
    m = jnp.where(jnp.isfinite(m), m, 0.0)
    e = jnp.where(mask, jnp.exp(s - m), 0.0)
    return e / jnp.maximum(jnp.sum(e, axis=-1, keepdims=True), 1e-30)


def fox_attention(q, k, v, log_f):
    B, T, H, D = q.shape
    nb = T // Q_BLOCK
    c = jnp.cumsum(log_f, axis=1)
    c_k = c.transpose(0, 2, 1)[:, :, None, :]
    kpos = jnp.arange(T)
    scale = D ** -0.5
    qb = q.reshape(B, nb, Q_BLOCK, H, D).transpose(1, 0, 2, 3, 4)
    cb = c.reshape(B, nb, Q_BLOCK, H).transpose(1, 0, 2, 3)
    starts = jnp.arange(nb) * Q_BLOCK

    def block(args):
        q_blk, c_blk, s0 = args
        s = jnp.einsum('bqhd,bkhd->bhqk', q_blk, k, preferred_element_type=jnp.float32) * scale
        s = s + c_blk.transpose(0, 2, 1)[..., None] - c_k
        qpos = s0 + jnp.arange(Q_BLOCK)
        mask = kpos[None, :] <= qpos[:, None]
        p = jax.nn.softmax(jnp.where(mask, s, -jnp.inf), axis=-1)
        return jnp.einsum('bhqk,bkhd->bqhd', p.astype(v.dtype), v)

    o = lax.map(block, (qb, cb, starts))
    return o.transpose(1, 0, 2, 3, 4).reshape(B, T, H, D)


def gated_delta_rule(q, k, v, g, beta):
    B, H, T, Dk = q.shape
    Dv = v.shape[-1]
    C = GDN_CHUNK
    N = T // C
    q = q * Dk ** -0.5
    rs = lambda t: t.reshape(B, H, N, C, *t.shape[3:])
    q, k, v, g, beta = rs(q), rs(k), rs(v), rs(g), rs(beta)
    g = jnp.cumsum(g, axis=-1)
    idx = jnp.arange(C)
    tril = idx[:, None] >= idx[None, :]
    strict = idx[:, None] > idx[None, :]
    decay = jnp.where(tril, jnp.exp(jnp.where(tril, g[..., :, None] - g[..., None, :], 0.0)), 0.0)
    kb = k * beta[..., None]
    A = jnp.where(strict, jnp.einsum('bhnid,bhnjd->bhnij', kb, k) * decay, 0.0)
    eye = jnp.eye(C, dtype=jnp.float32)
    Tm = lax.linalg.triangular_solve(eye + A, jnp.broadcast_to(eye, A.shape), left_side=True,
                                     lower=True, unit_diagonal=True)
    U = Tm @ (v * beta[..., None])
    W = Tm @ (kb * jnp.exp(g)[..., None])
    qk = jnp.einsum('bhnid,bhnjd->bhnij', q, k) * decay
    g_last = g[..., -1]

    def step(S, xs):
        q_c, k_c, U_c, W_c, qk_c, g_c, gl_c = xs
        v_new = U_c - W_c @ S
        o = (q_c * jnp.exp(g_c)[..., None]) @ S + qk_c @ v_new
        k_dec = k_c * jnp.exp(gl_c[..., None] - g_c)[..., None]
        S = S * jnp.exp(gl_c)[..., None, None] + jnp.swapaxes(k_dec, -1, -2) @ v_new
        return S, o

    mv = lambda t: jnp.moveaxis(t, 2, 0)
    xs = (mv(q), mv(k), mv(U), mv(W), mv(qk), mv(g), mv(g_last))
    S0 = jnp.zeros((B, H, Dk, Dv), jnp.float32)
    _, o = lax.scan(step, S0, xs)
    return jnp.moveaxis(o, 0, 2).reshape(B, H, T, Dv)


def rg_lru(x, w_a, b_a, w_x, b_x, lam):
    B, T, W = x.shape
    xr = x.reshape(B, T, LRU_BLOCKS, W // LRU_BLOCKS)
    r = jax.nn.sigmoid(jnp.einsum('btnd,nde->btne', xr, w_a).reshape(B, T, W) + b_a)
    i = jax.nn.sigmoid(jnp.einsum('btnd,nde->btne', xr, w_x).reshape(B, T, W) + b_x)
    log_a = -LRU_C * r * jax.nn.softplus(-lam)
    a = jnp.exp(log_a)
    u = jnp.sqrt(-jnp.expm1(2.0 * log_a)) * (i * x)

    def comb(left, right):
        a1, b1 = left
        a2, b2 = right
        return a1 * a2, a2 * b1 + b2

    _, h = lax.associative_scan(comb, (a, u), axis=1)
    return h


def nsa_compress(k, pe, w1, w2):
    B, T, D = k.shape
    n_cmp = (T - CMP_LEN) // CMP_STRIDE + 1
    idx = jnp.arange(n_cmp)[:, None] * CMP_STRIDE + jnp.arange(CMP_LEN)[None, :]
    blocks = k[:, idx] + pe
    hid = jax.nn.gelu(blocks.reshape(B, n_cmp, CMP_LEN * D) @ w1)
    return hid @ w2


def nsa_attention(q, kc, vc, ks, vs, kw, vw, gates, rel_bias):
    B, T, H, D = q.shape
    nb = T // Q_BLOCK
    n_cmp = kc.shape[1]
    n_slc = T // SLC_LEN
    n_sel = min(N_SELECT, n_slc)
    scale = D ** -0.5
    c_start = jnp.arange(n_cmp) * CMP_STRIDE
    c_end = c_start + CMP_LEN - 1
    s_start = jnp.arange(n_slc) * SLC_LEN
    s_end = s_start + SLC_LEN - 1
    overlap = ((c_start[:, None] <= s_end[None, :]) & (c_end[:, None] >= s_start[None, :])).astype(jnp.float32)
    ks_blk = ks.reshape(B, n_slc, SLC_LEN, D)
    vs_blk = vs.reshape(B, n_slc, SLC_LEN, D)
    kw_pad = jnp.pad(kw, ((0, 0), (WINDOW, 0), (0, 0)))
    vw_pad = jnp.pad(vw, ((0, 0), (WINDOW, 0), (0, 0)))
    bidx = jnp.arange(B)[:, None, None]
    jsl = jnp.arange(n_slc)
    qb = q.reshape(B, nb, Q_BLOCK, H, D).transpose(1, 0, 2, 3, 4)
    gb = gates.reshape(B, nb, Q_BLOCK, H, 3).transpose(1, 0, 2, 3, 4)
    starts = jnp.arange(nb) * Q_BLOCK

    def block(args):
        q_blk, g_blk, s0 = args
        qpos = s0 + jnp.arange(Q_BLOCK)
        dist_c = qpos[:, None] - c_end[None, :]
        s_c = jnp.einsum('bqhd,bnd->bhqn', q_blk, kc, preferred_element_type=jnp.float32) * scale
        s_c = s_c + rel_bias[t5_bucket(dist_c)].transpose(2, 0, 1)
        p_c = masked_softmax(s_c, dist_c >= 0)
        o_c = jnp.einsum('bhqn,bnd->bqhd', p_c.astype(vc.dtype), vc)
        imp = jnp.einsum('bhqn,nj->bqj', p_c, overlap)
        cur = qpos // SLC_LEN
        forced = (jsl[None, :] == 0) | (jsl[None, :] == cur[:, None]) | (jsl[None, :] == cur[:, None] - 1)
        imp = jnp.where(forced[None], FORCED_SCORE, imp)
        imp = jnp.where((jsl[None, :] > cur[:, None])[None], -1.0, imp)
        _, sel = lax.top_k(imp, n_sel)
        k_sel = ks_blk[bidx, sel].reshape(B, Q_BLOCK, n_sel * SLC_LEN, D)
        v_sel = vs_blk[bidx, sel].reshape(B, Q_BLOCK, n_sel * SLC_LEN, D)
        pos_s = (sel[..., None] * SLC_LEN + jnp.arange(SLC_LEN)).reshape(B, Q_BLOCK, n_sel * SLC_LEN)
        dist_s = qpos[None, :, None] - pos_s
        s_s = jnp.einsum('bqhd,bqkd->bhqk', q_blk, k_sel, preferred_element_type=jnp.float32) * scale
        s_s = s_s + rel_bias[t5_bucket(dist_s)].transpose(0, 3, 1, 2)
        p_s = masked_softmax(s_s, (dist_s >= 0)[:, None])
        o_s = jnp.einsum('bhqk,bqkd->bqhd', p_s.astype(v_sel.dtype), v_sel)
        k_win = lax.dynamic_slice_in_dim(kw_pad, s0, WINDOW + Q_BLOCK, axis=1)
        v_win = lax.dynamic_slice_in_dim(vw_pad, s0, WINDOW + Q_BLOCK, axis=1)
        pos_w = s0 - WINDOW + jnp.arange(WINDOW + Q_BLOCK)
        dist_w = qpos[:, None] - pos_w[None, :]
        mask_w = (dist_w >= 0) & (dist_w < WINDOW) & (pos_w[None, :] >= 0)
        s_w = jnp.einsum('bqhd,bkd->bhqk', q_blk, k_win, preferred_element_type=jnp.float32) * scale
        s_w = s_w + rel_bias[t5_bucket(dist_w)].transpose(2, 0, 1)
        p_w = masked_softmax(s_w, mask_w)
        o_w = jnp.einsum('bhqk,bkd->bqhd', p_w.astype(v_win.dtype), v_win)
        out = g_blk[..., 0:1] * o_c + g_blk[..., 1:2] * o_s + g_blk[..., 2:3] * o_w
        return out.astype(q.dtype)

    o = lax.map(block, (qb, gb, starts))
    return o.transpose(1, 0, 2, 3, 4).reshape(B, T, H, D)


def hybrid_mixer(h, w_in, b_in, gdn_conv_w, gdn_a_log, gdn_dt_bias, gdn_norm_g,
                 lru_conv_w, lru_conv_b, lru_w_a, lru_b_a, lru_w_x, lru_b_x, lru_lambda,
                 nsa_pe_k, nsa_w1_k, nsa_w2_k, nsa_pe_v, nsa_w1_v, nsa_w2_v, rel_bias,
                 out_norm_g, w_out):
    B, T, _ = h.shape
    f32 = jnp.float32
    z = h @ w_in + b_in
    (fq, fk, fv, ff, gq, gk, gv, ga, gbeta, gz, lx, lg,
     nq, nkc, nvc, nks, nvs, nkw, nvw, ng) = split_cols(z)
    heads = lambda t, n: t.reshape(B, T, n, HEAD_DIM)
    log_f = jax.nn.log_sigmoid(ff.astype(f32))
    o_a = fox_attention(heads(fq, FOX_HEADS), heads(fk, FOX_HEADS), heads(fv, FOX_HEADS), log_f)
    o_a = o_a.reshape(B, T, GROUP_W)
    qkv = jax.nn.silu(causal_dwconv(jnp.concatenate([gq, gk, gv], axis=-1), gdn_conv_w)).astype(f32)
    dq = l2norm(heads(qkv[..., :GROUP_W], GDN_HEADS))
    dk = l2norm(heads(qkv[..., GROUP_W:2 * GROUP_W], GDN_HEADS))
    dv = heads(qkv[..., 2 * GROUP_W:], GDN_HEADS)
    beta = jax.nn.sigmoid(gbeta.astype(f32))
    g = -jnp.exp(gdn_a_log.astype(f32)) * jax.nn.softplus(ga.astype(f32) + gdn_dt_bias.astype(f32))
    tr = lambda t: t.transpose(0, 2, 1, 3)
    o_b = gated_delta_rule(tr(dq), tr(dk), tr(dv), g.transpose(0, 2, 1), beta.transpose(0, 2, 1))
    o_b = rmsnorm(tr(o_b), gdn_norm_g) * jax.nn.silu(heads(gz, GDN_HEADS).astype(f32))
    o_b = o_b.reshape(B, T, GROUP_W)
    xc = (causal_dwconv(lx, lru_conv_w) + lru_conv_b).astype(f32)
    hc = rg_lru(xc, lru_w_a.astype(f32), lru_b_a.astype(f32), lru_w_x.astype(f32), lru_b_x.astype(f32),
                lru_lambda.astype(f32))
    o_c = hc * jax.nn.gelu(lg.astype(f32))
    kc = nsa_compress(nkc, nsa_pe_k, nsa_w1_k, nsa_w2_k)
    vc = nsa_compress(nvc, nsa_pe_v, nsa_w1_v, nsa_w2_v)
    gates = jax.nn.sigmoid(ng.astype(f32)).reshape(B, T, NSA_HEADS, 3)
    o_d = nsa_attention(heads(nq, NSA_HEADS), kc, vc, nks, nvs, nkw, nvw, gates, rel_bias)
    o_d = o_d.reshape(B, T, GROUP_W)
    y = jnp.concatenate([rmsnorm(o_a, out_norm_g[0]).astype(f32), o_b.astype(f32),
                         rmsnorm(o_c, out_norm_g[1]).astype(f32), rmsnorm(o_d, out_norm_g[2]).astype(f32)], axis=-1)
    return y.astype(h.dtype) @ w_out


def setup_inputs(seed: int = 0) -> dict:
    key = jax.random.key(seed)
    keys = iter(list(jax.random.split(key, 64)))
    nrm = lambda shape, scale: jax.random.normal(next(keys), shape, jnp.float32) * scale
    gain = lambda shape: 1.0 + nrm(shape, 0.02)
    L, D, F = DEPTH, D_MODEL, D_FF
    bw = LRU_W // LRU_BLOCKS
    x = nrm((BATCH, SEQ, D), 1.0)
    ffn1_norm_g = gain((L, D))
    ffn1_w_gate = nrm((L, D, F), D ** -0.5)
    ffn1_w_up = nrm((L, D, F), D ** -0.5)
    ffn1_w_down = nrm((L, F, D), F ** -0.5)
    mix_norm_g = gain((L, D))
    w_in = nrm((L, D, D_IN), D ** -0.5)
    b_in = nrm((L, D_IN), 0.02)
    gdn_conv_w = nrm((L, CONV_W, 3 * GROUP_W), CONV_W ** -0.5)
    gdn_a_log = jnp.log(jax.random.uniform(next(keys), (L, GDN_HEADS), jnp.float32, 1.0, 16.0))
    dt = jnp.exp(jax.random.uniform(next(keys), (L, GDN_HEADS), jnp.float32, math.log(1e-3), math.log(1e-1)))
    gdn_dt_bias = dt + jnp.log(-jnp.expm1(-dt))
    gdn_norm_g = gain((L, HEAD_DIM))
    lru_conv_w = nrm((L, CONV_W, LRU_W), CONV_W ** -0.5)
    lru_conv_b = nrm((L, LRU_W), 0.02)
    lru_w_a = nrm((L, LRU_BLOCKS, bw, bw), bw ** -0.5)
    lru_b_a = nrm((L, LRU_W), 0.02)
    lru_w_x = nrm((L, LRU_BLOCKS, bw, bw), bw ** -0.5)
    lru_b_x = nrm((L, LRU_W), 0.02)
    a_c = jax.random.uniform(next(keys), (L, LRU_W), jnp.float32, 0.9, 0.999)
    s = a_c ** (1.0 / LRU_C)
    lru_lambda = jnp.log(s) - jnp.log1p(-s)
    nsa_pe_k = nrm((L, CMP_LEN, HEAD_DIM), 0.02)
    nsa_w1_k = nrm((L, CMP_LEN * HEAD_DIM, HEAD_DIM), (CMP_LEN * HEAD_DIM) ** -0.5)
    nsa_w2_k = nrm((L, HEAD_DIM, HEAD_DIM), HEAD_DIM ** -0.5)
    nsa_pe_v = nrm((L, CMP_LEN, HEAD_DIM), 0.02)
    nsa_w1_v = nrm((L, CMP_LEN * HEAD_DIM, HEAD_DIM), (CMP_LEN * HEAD_DIM) ** -0.5)
    nsa_w2_v = nrm((L, HEAD_DIM, HEAD_DIM), HEAD_DIM ** -0.5)
    rel_bias = nrm((N_BUCKETS, NSA_HEADS), 0.2)
    out_norm_g = gain((L, 3, GROUP_W))
    w_out = nrm((L, D_MIX, D), D_MIX ** -0.5)
    ffn2_norm_g = gain((L, D))
    ffn2_w_gate = nrm((L, D, F), D ** -0.5)
    ffn2_w_up = nrm((L, D, F), D ** -0.5)
    ffn2_w_down = nrm((L, F, D), F ** -0.5)
    final_norm_g = gain((D,))
    return {"x": x, "ffn1_norm_g": ffn1_norm_g, "ffn1_w_gate": ffn1_w_gate, "ffn1_w_up": ffn1_w_up,
            "ffn1_w_down": ffn1_w_down, "mix_norm_g": mix_norm_g, "w_in": w_in, "b_in": b_in,
            "gdn_conv_w": gdn_conv_w, "gdn_a_log": gdn_a_log, "gdn_dt_bias": gdn_dt_bias, "gdn_norm_g": gdn_norm_g,
            "lru_conv_w": lru_conv_w, "lru_conv_b": lru_conv_b, "lru_w_a": lru_w_a, "lru_b_a": lru_b_a,
            "lru_w_x": lru_w_x, "lru_b_x": lru_b_x, "lru_lambda": lru_lambda,
            "nsa_pe_k": nsa_pe_k, "nsa_w1_k": nsa_w1_k, "nsa_w2_k": nsa_w2_k,
            "nsa_pe_v": nsa_pe_v, "nsa_w1_v": nsa_w1_v, "nsa_w2_v": nsa_w2_v, "rel_bias": rel_bias,
            "out_norm_g": out_norm_g, "w_out": w_out, "ffn2_norm_g": ffn2_norm_g, "ffn2_w_gate": ffn2_w_gate,
            "ffn2_w_up": ffn2_w_up, "ffn2_w_down": ffn2_w_down, "final_norm_g": final_norm_g}


def reference(x, ffn1_norm_g, ffn1_w_gate, ffn1_w_up, ffn1_w_down, mix_norm_g, w_in, b_in,
              gdn_conv_w, gdn_a_log, gdn_dt_bias, gdn_norm_g, lru_conv_w, lru_conv_b, lru_w_a, lru_b_a,
              lru_w_x, lru_b_x, lru_lambda, nsa_pe_k, nsa_w1_k, nsa_w2_k, nsa_pe_v, nsa_w1_v, nsa_w2_v,
              rel_bias, out_norm_g, w_out, ffn2_norm_g, ffn2_w_gate, ffn2_w_up, ffn2_w_down, final_norm_g):
    h = x
    for l in range(DEPTH):
        h = h + 0.5 * swiglu(rmsnorm(h, ffn1_norm_g[l]), ffn1_w_gate[l], ffn1_w_up[l], ffn1_w_down[l])
        h = h + hybrid_mixer(rmsnorm(h, mix_norm_g[l]), w_in[l], b_in[l], gdn_conv_w[l], gdn_a_log[l],
                             gdn_dt_bias[l], gdn_norm_g[l], lru_conv_w[l], lru_conv_b[l], lru_w_a[l], lru_b_a[l],
                             lru_w_x[l], lru_b_x[l], lru_lambda[l], nsa_pe_k[l], nsa_w1_k[l], nsa_w2_k[l],
                             nsa_pe_v[l], nsa_w1_v[l], nsa_w2_v[l], rel_bias, out_norm_g[l], w_out[l])
        h = h + 0.5 * swiglu(rmsnorm(h, ffn2_norm_g[l]), ffn2_w_gate[l], ffn2_w_up[l], ffn2_w_down[l])
    return rmsnorm(h, final_norm_g)
```

```python
import numpy as np
import ml_dtypes
from contextlib import ExitStack
import concourse.bass as bass
import concourse.mybir as mybir
from concourse.bass_utils import run_bass_kernel_spmd

F32 = mybir.dt.float32
BF16 = mybir.dt.bfloat16
AF = mybir.ActivationFunctionType
ALU = mybir.AluOpType
AX = mybir.AxisListType

D = 2048
NCH = 16
DFF = 5632
NF = 44
TOK = 1024
TG = 512
DIN = 5912
NZC = 47
EPS = 1e-6
NEG = -30000.0


class St:
    __slots__ = ("w", "r")

    def __init__(self, w=None, r=None):
        self.w = w
        self.r = list(r) if r else []

    def copy(self):
        return St(self.w, self.r)


class T:
    def __init__(self, handle, name):
        self.h = handle
        self.name = name
        self.whole = St()
        self.cells = {}

    def __getitem__(self, idx):
        return self.h[idx]

    def states(self, key):
        if key is None:
            return [self.whole] + list(self.cells.values())
        if key not in self.cells:
            self.cells[key] = self.whole.copy()
        return [self.cells[key]]


class K:
    NSLOT = 6

    def __init__(self, nc, es):
        self.nc = nc
        self.es = es
        self.eng = {"pe": nc.tensor, "dve": nc.vector, "act": nc.scalar, "pool": nc.gpsimd, "sp": nc.sync}
        self.sem = {}
        self.cnt = {}
        for e in self.eng:
            self.sem[e] = es.enter_context(nc.semaphore("s_" + e))
            self.cnt[e] = 0
        self.known = {e: {} for e in self.eng}
        self.dsem = {}
        self.duse = {}
        self.dnext = {}
        for q in ("sp", "pool"):
            self.dnext[q] = 0
            for s in range(self.NSLOT):
                key = ("d", q, s)
                self.dsem[key] = es.enter_context(nc.semaphore("d_%s_%d" % (q, s)))
                self.duse[key] = 0
        self.ntile = 0

    def sb(self, shape, dt, name=None):
        self.ntile += 1
        name = name or ("t%d" % self.ntile)
        h = self.es.enter_context(self.nc.sbuf_tensor(name, list(shape), dt))
        return T(h, name)

    def ps(self, shape, dt, name=None):
        self.ntile += 1
        name = name or ("p%d" % self.ntile)
        h = self.es.enter_context(self.nc.psum_tensor(name, list(shape), dt))
        return T(h, name)

    def din(self, name, shape, dt=F32):
        return T(self.nc.dram_tensor(name, list(shape), dt, kind="ExternalInput").ap(), name)

    def dout(self, name, shape, dt=F32):
        return T(self.nc.dram_tensor(name, list(shape), dt, kind="ExternalOutput").ap(), name)

    def semh(self, key):
        return self.sem[key] if key in self.sem else self.dsem[key]

    def _deps(self, reads, writes):
        deps = set()
        for (t, key) in reads:
            for st in t.states(key):
                if st.w is not None:
                    deps.add(st.w)
        for (t, key) in writes:
            for st in t.states(key):
                if st.w is not None:
                    deps.add(st.w)
                deps.update(st.r)
        return deps

    def _wait(self, eng, deps):
        need = {}
        kn = self.known[eng]
        for (sk, val) in deps:
            if sk == "pe" and eng == "pe":
                continue
            if kn.get(sk, 0) < val and need.get(sk, 0) < val:
                need[sk] = val
        for sk, val in need.items():
            self.eng[eng].wait_ge(self.semh(sk), val)
            kn[sk] = val

    def _commit(self, ev, reads, writes):
        for (t, key) in reads:
            for st in t.states(key):
                st.r.append(ev)
        for (t, key) in writes:
            if key is None:
                t.cells.clear()
                t.whole.w = ev
                t.whole.r = []
            else:
                st = t.states(key)[0]
                st.w = ev
                st.r = []

    @staticmethod
    def _norm(lst):
        out = []
        for x in lst or []:
            out.append(x if isinstance(x, tuple) else (x, None))
        return out

    def op(self, eng, fn, reads=None, writes=None):
        reads = self._norm(reads)
        writes = self._norm(writes)
        self._wait(eng, self._deps(reads, writes))
        ins = fn()
        self.cnt[eng] += 1
        ins.then_inc(self.sem[eng], 1)
        ev = (eng, self.cnt[eng])
        self._commit(ev, reads, writes)
        return ev

    def dma(self, q, out_ap, in_ap, reads=None, writes=None, **kw):
        reads = self._norm(reads)
        writes = self._norm(writes)
        deps = self._deps(reads, writes)
        s = self.dnext[q]
        self.dnext[q] = (s + 1) % self.NSLOT
        key = ("d", q, s)
        if self.duse[key] > 0:
            deps.add((key, 16 * self.duse[key]))
        self._wait(q, deps)
        ins = self.eng[q].dma_start(out=out_ap, in_=in_ap, **kw)
        self.duse[key] += 1
        ins.then_inc(self.dsem[key], 16)
        ev = (key, 16 * self.duse[key])
        self._commit(ev, reads, writes)
        return ev

    def wait_all(self, eng="sp"):
        kn = self.known[eng]
        for e in self.eng:
            if self.cnt[e] > kn.get(e, 0):
                self.eng[eng].wait_ge(self.sem[e], self.cnt[e])
                kn[e] = self.cnt[e]
        for key, n in self.duse.items():
            if 16 * n > kn.get(key, 0):
                self.eng[eng].wait_ge(self.dsem[key], 16 * n)
                kn[key] = 16 * n


class Dense:
    def __init__(self, k):
        self.k = k
        nc = k.nc
        self.nc = nc
        self.h = k.sb([128, NCH, TOK], F32, "h")
        self.hn = k.sb([128, NCH, TOK], BF16, "hn")
        self.sq = [k.sb([128, TG], BF16, "sq%d" % i) for i in range(2)]
        self.rstd = k.sb([128, TG], F32, "rstd")
        self.onesm = k.sb([128, 128], BF16, "onesm")
        self.ones4 = k.sb([128, 128], BF16, "ones4")
        self.gcol = k.sb([128, NCH], F32, "gcol")
        self.wg = [k.sb([128, NCH, 256], BF16, "wg%d" % i) for i in range(2)]
        self.wu = [k.sb([128, NCH, 256], BF16, "wu%d" % i) for i in range(2)]
        self.wd = [k.sb([128, 2, D], BF16, "wd%d" % i) for i in range(2)]
        self.act = [k.sb([128, 2, TOK], BF16, "act%d" % i) for i in range(2)]
        self.sg = [k.sb([128, TG], F32, "sg%d" % i) for i in range(2)]
        self.psA = [k.ps([128, TG], F32, "psA%d" % i) for i in range(4)]
        self.psB = [k.ps([128, TG], F32, "psB%d" % i) for i in range(3)]
        self.psN = k.ps([128, TG], F32, "psN")
        self.ia = 0
        self.ib = 0
        self.epsc = k.sb([128, 1], F32, "epsc")
        k.op("dve", lambda: nc.vector.memset(self.epsc[:], EPS), writes=[self.epsc])
        k.op("dve", lambda: nc.vector.memset(self.onesm[:], 1.0 / D), writes=[self.onesm])
        k.op("dve", lambda: nc.vector.memset(self.ones4[:], 1.0 / 512), writes=[self.ones4])

    def rstd_from(self, ps):
        k, nc = self.k, self.nc
        k.op("act", lambda: nc.scalar.activation(out=self.rstd[:], in_=ps[:], func=AF.Ln, bias=self.epsc[:]),
             reads=[ps, self.epsc], writes=[self.rstd])
        k.op("act", lambda: nc.scalar.activation(out=self.rstd[:], in_=self.rstd[:], func=AF.Exp, scale=-0.5),
             reads=[self.rstd], writes=[self.rstd])

    def load_h(self, src):
        k = self.k
        v = src.h.rearrange("(c p) t -> p c t", p=128)
        for c in range(0, NCH, 4):
            k.dma("sp", self.h[:, c:c + 4, :], v[:, c:c + 4, :], reads=[src],
                  writes=[(self.h, (cc, tg)) for cc in range(c, c + 4) for tg in range(2)])

    def store_h(self, dst):
        k = self.k
        v = dst.h.rearrange("(c p) t -> p c t", p=128)
        for c in range(0, NCH, 4):
            k.dma("sp", v[:, c:c + 4, :], self.h[:, c:c + 4, :],
                  reads=[(self.h, (cc, tg)) for cc in range(c, c + 4) for tg in range(2)], writes=[(dst, c)])

    def rmsnorm(self, gsrc, out_t=None, out_f32=None):
        k, nc = self.k, self.nc
        k.dma("sp", self.gcol[:], gsrc.h, reads=[gsrc], writes=[self.gcol])
        for tg in range(2):
            ts = slice(tg * TG, (tg + 1) * TG)
            for c in range(NCH):
                sq = self.sq[c % 2]
                k.op("act", lambda sq=sq, c=c: nc.scalar.activation(out=sq[:], in_=self.h[:, c, ts], func=AF.Square),
                     reads=[(self.h, (c, tg))], writes=[sq])
                k.op("pe", lambda sq=sq, c=c: nc.tensor.matmul(self.psN[:], lhsT=self.onesm[:], rhs=sq[:],
                                                               start=(c == 0), stop=(c == NCH - 1)),
                     reads=[sq, self.onesm], writes=[self.psN])
            self.rstd_from(self.psN)
            for c in range(NCH):
                if out_f32 is None:
                    k.op("dve", lambda c=c: nc.vector.scalar_tensor_tensor(
                        out=self.hn[:, c, ts], in0=self.h[:, c, ts], scalar=self.gcol[:, c:c + 1], in1=self.rstd[:],
                        op0=ALU.mult, op1=ALU.mult),
                        reads=[(self.h, (c, tg)), self.gcol, self.rstd], writes=[(self.hn, tg)])
                else:
                    k.op("dve", lambda c=c: nc.vector.scalar_tensor_tensor(
                        out=out_f32[:, c, ts], in0=self.h[:, c, ts], scalar=self.gcol[:, c:c + 1], in1=self.rstd[:],
                        op0=ALU.mult, op1=ALU.mult),
                        reads=[(self.h, (c, tg)), self.gcol, self.rstd], writes=[(out_f32, (c, tg))])

    def ffn(self, wg_d, wu_d, wd_d):
        k, nc = self.k, self.nc
        wgv = wg_d.h.rearrange("(c p) f -> p c f", p=128)
        wuv = wu_d.h.rearrange("(c p) f -> p c f", p=128)
        wdv = wd_d.h.rearrange("(c p) d -> p c d", p=128)
        NG = NF // 2

        def load(g):
            b = g % 2
            k.dma("pool", self.wg[b][:], wgv[:, :, g * 256:(g + 1) * 256], reads=[wg_d], writes=[self.wg[b]])
            k.dma("pool", self.wu[b][:], wuv[:, :, g * 256:(g + 1) * 256], reads=[wu_d], writes=[self.wu[b]])
            k.dma("pool", self.wd[b][:], wdv[:, 2 * g:2 * g + 2, :], reads=[wd_d], writes=[self.wd[b]])

        load(0)
        for g in range(NG):
            if g + 1 < NG:
                load(g + 1)
            b = g % 2
            wg, wu, wd, act = self.wg[b], self.wu[b], self.wd[b], self.act[b]
            for fcl in range(2):
                fs = slice(fcl * 128, (fcl + 1) * 128)
                for tg in range(2):
                    ts = slice(tg * TG, (tg + 1) * TG)
                    pg = self.psA[self.ia % 4]
                    pu = self.psA[(self.ia + 1) % 4]
                    self.ia += 2

                    def mm(p, w):
                        ins = None
                        for c in range(NCH):
                            ins = nc.tensor.matmul(p[:], lhsT=w[:, c, fs], rhs=self.hn[:, c, ts],
                                                   start=(c == 0), stop=(c == NCH - 1))
                        return ins
                    k.op("pe", lambda: mm(pg, wg), reads=[wg, (self.hn, tg)], writes=[pg])
                    k.op("pe", lambda: mm(pu, wu), reads=[wu, (self.hn, tg)], writes=[pu])
                    sg = self.sg[tg]
                    k.op("act", lambda: nc.scalar.activation(out=sg[:], in_=pg[:], func=AF.Silu), reads=[pg], writes=[sg])
                    k.op("dve", lambda: nc.vector.tensor_tensor(out=act[:, fcl, ts], in0=pu[:], in1=sg[:], op=ALU.mult),
                         reads=[pu, sg], writes=[(act, (fcl, tg))])
            for dc in range(NCH):
                ds = slice(dc * 128, (dc + 1) * 128)
                for tg in range(2):
                    ts = slice(tg * TG, (tg + 1) * TG)
                    pd = self.psB[self.ib % 3]
                    self.ib += 1

                    def mmd():
                        ins = None
                        for fcl in range(2):
                            ins = nc.tensor.matmul(pd[:], lhsT=wd[:, fcl, ds], rhs=act[:, fcl, ts],
                                                   start=(fcl == 0), stop=(fcl == 1))
                        return ins
                    k.op("pe", mmd, reads=[wd, (act, (0, tg)), (act, (1, tg))], writes=[pd])
                    k.op("dve", lambda: nc.vector.scalar_tensor_tensor(
                        out=self.h[:, dc, ts], in0=pd[:], scalar=0.5, in1=self.h[:, dc, ts], op0=ALU.mult, op1=ALU.add),
                        reads=[pd, (self.h, (dc, tg))], writes=[(self.h, (dc, tg))])

    def proj(self, w_d, ncols, rhs_t, emit):
        k, nc = self.k, self.nc
        wv = w_d.h.rearrange("(c p) f -> p c f", p=128)
        ngr = (ncols + 255) // 256

        def load(g):
            b = g % 2
            c0 = g * 256
            c1 = min(ncols, c0 + 256)
            k.dma("pool", self.wg[b][:, :, 0:c1 - c0], wv[:, :, c0:c1], reads=[w_d], writes=[self.wg[b]])

        load(0)
        for g in range(ngr):
            if g + 1 < ngr:
                load(g + 1)
            w = self.wg[g % 2]
            for ml in range(2):
                m = 2 * g + ml
                M = min(128, ncols - m * 128)
                if M <= 0:
                    continue
                for tg in range(2):
                    ts = slice(tg * TG, (tg + 1) * TG)
                    pd = self.psB[self.ib % 3]
                    self.ib += 1

                    def mm():
                        ins = None
                        for c in range(NCH):
                            ins = nc.tensor.matmul(pd[0:M, :], lhsT=w[:, c, ml * 128:ml * 128 + M], rhs=rhs_t[:, c, ts],
                                                   start=(c == 0), stop=(c == NCH - 1))
                        return ins
                    k.op("pe", mm, reads=[w, (rhs_t, tg)], writes=[pd])
                    emit(m, M, tg, ts, pd)


def build_A():
    nc = bass.Bass("TRN2", target_bir_lowering=False)
    with ExitStack() as es:
        k = K(nc, es)
        hT = k.din("hT", [D, TOK])
        g1 = k.din("g1", [128, NCH])
        wg = k.din("wg", [D, DFF]); wu = k.din("wu", [D, DFF]); wd = k.din("wd", [DFF, D])
        g2 = k.din("g2", [128, NCH])
        win = k.din("win", [D, DIN]); bin_ = k.din("bin", [128, NZC])
        h1T = k.dout("h1T", [D, TOK]); zT = k.dout("zT", [DIN, TOK])
        dn = Dense(k)
        bcol = k.sb([128, NZC], F32, "bcol")
        zs = [k.sb([128, TG], F32, "zs%d" % i) for i in range(3)]
        k.dma("sp", bcol[:], bin_.h, reads=[bin_], writes=[bcol])
        dn.load_h(hT)
        dn.rmsnorm(g1)
        dn.ffn(wg, wu, wd)
        dn.store_h(h1T)
        dn.rmsnorm(g2)
        cnt = [0]

        def emit(m, M, tg, ts, pd):
            z = zs[cnt[0] % 3]
            cnt[0] += 1
            k.op("act", lambda: nc.scalar.activation(out=z[0:M, :], in_=pd[0:M, :], func=AF.Identity,
                                                     bias=bcol[0:M, m:m + 1]), reads=[pd, bcol], writes=[z])
            k.dma("sp", zT.h[m * 128:m * 128 + M, ts], z[0:M, :], reads=[z], writes=[(zT, (m, tg))])
        dn.proj(win, DIN, dn.hn, emit)
        k.wait_all("sp")
    return nc


def build_C(final):
    nc = bass.Bass("TRN2", target_bir_lowering=False)
    with ExitStack() as es:
        k = K(nc, es)
        hT = k.din("hT", [D, TOK])
        yT = k.din("yT", [D, TOK])
        ong = k.din("ong", [128, NCH])
        wout = k.din("wout", [D, D])
        g1 = k.din("g1", [128, NCH])
        wg = k.din("wg", [D, DFF]); wu = k.din("wu", [D, DFF]); wd = k.din("wd", [DFF, D])
        if final:
            gf = k.din("gf", [128, NCH])
        oT = k.dout("oT", [D, TOK])
        dn = Dense(k)
        ys = [k.sb([128, 4, TG], F32, "ys%d" % i) for i in range(2)]
        ocol = k.sb([128, NCH], F32, "ocol")
        k.dma("sp", ocol[:], ong.h, reads=[ong], writes=[ocol])
        dn.load_h(hT)
        yv = yT.h.rearrange("(c p) t -> p c t", p=128)
        i = 0
        for grp in range(4):
            for tg in range(2):
                ts = slice(tg * TG, (tg + 1) * TG)
                y = ys[i % 2]
                i += 1
                k.dma("sp", y[:], yv[:, grp * 4:grp * 4 + 4, ts], reads=[yT], writes=[y])
                if grp == 1:
                    for cl in range(4):
                        k.op("dve", lambda cl=cl: nc.vector.tensor_copy(out=dn.hn[:, 4 + cl, ts], in_=y[:, cl, :]),
                             reads=[y], writes=[(dn.hn, tg)])
                    continue
                for cl in range(4):
                    sq = dn.sq[cl % 2]
                    k.op("act", lambda sq=sq, cl=cl: nc.scalar.activation(out=sq[:], in_=y[:, cl, :], func=AF.Square),
                         reads=[y], writes=[sq])
                    k.op("pe", lambda sq=sq, cl=cl: nc.tensor.matmul(dn.psN[:], lhsT=dn.ones4[:], rhs=sq[:],
                                                                     start=(cl == 0), stop=(cl == 3)),
                         reads=[sq, dn.ones4], writes=[dn.psN])
                dn.rstd_from(dn.psN)
                for cl in range(4):
                    c = grp * 4 + cl
                    k.op("dve", lambda cl=cl, c=c: nc.vector.scalar_tensor_tensor(
                        out=dn.hn[:, c, ts], in0=y[:, cl, :], scalar=ocol[:, c:c + 1], in1=dn.rstd[:],
                        op0=ALU.mult, op1=ALU.mult), reads=[y, ocol, dn.rstd], writes=[(dn.hn, tg)])

        def emit(m, M, tg, ts, pd):
            k.op("dve", lambda: nc.vector.tensor_tensor(out=dn.h[:, m, ts], in0=pd[:], in1=dn.h[:, m, ts], op=ALU.add),
                 reads=[pd, (dn.h, (m, tg))], writes=[(dn.h, (m, tg))])
        dn.proj(wout, D, dn.hn, emit)
        dn.rmsnorm(g1)
        dn.ffn(wg, wu, wd)
        if final:
            ov = oT.h.rearrange("(c p) t -> p c t", p=128)
            k.dma("sp", dn.gcol[:], gf.h, reads=[gf], writes=[dn.gcol])
            for tg in range(2):
                ts = slice(tg * TG, (tg + 1) * TG)
                for c in range(NCH):
                    sq = dn.sq[c % 2]
                    k.op("act", lambda sq=sq, c=c: nc.scalar.activation(out=sq[:], in_=dn.h[:, c, ts], func=AF.Square),
                         reads=[(dn.h, (c, tg))], writes=[sq])
                    k.op("pe", lambda sq=sq, c=c: nc.tensor.matmul(dn.psN[:], lhsT=dn.onesm[:], rhs=sq[:],
                                                                   start=(c == 0), stop=(c == NCH - 1)),
                         reads=[sq, dn.onesm], writes=[dn.psN])
                dn.rstd_from(dn.psN)
                for c4 in range(4):
                    y = ys[(c4 + tg * 4) % 2]
                    for cl in range(4):
                        c = c4 * 4 + cl
                        k.op("dve", lambda cl=cl, c=c: nc.vector.scalar_tensor_tensor(
                            out=y[:, cl, :], in0=dn.h[:, c, ts], scalar=dn.gcol[:, c:c + 1], in1=dn.rstd[:],
                            op0=ALU.mult, op1=ALU.mult), reads=[(dn.h, (c, tg)), dn.gcol, dn.rstd], writes=[y])
                    k.dma("sp", ov[:, c4 * 4:c4 * 4 + 4, ts], y[:], reads=[y], writes=[(oT, (c4, tg))])
        else:
            dn.store_h(oT)
        k.wait_all("sp")
    return nc


def col16(g):
    return np.ascontiguousarray(np.asarray(g, np.float32).reshape(NCH, 128).T)


_PROGS = {}


def prog(name, fn):
    if name not in _PROGS:
        _PROGS[name] = fn()
    return _PROGS[name]


def run(nc, in_maps):
    res = run_bass_kernel_spmd(nc, in_maps, core_ids=list(range(8)))
    return res.results


def run_A(hT_sh, inp, l):
    binp = np.zeros(NZC * 128, np.float32)
    binp[:DIN] = inp["b_in"][l]
    common = {"g1": col16(inp["ffn1_norm_g"][l]), "wg": inp["ffn1_w_gate"][l], "wu": inp["ffn1_w_up"][l],
              "wd": inp["ffn1_w_down"][l], "g2": col16(inp["mix_norm_g"][l]), "win": inp["w_in"][l],
              "bin": np.ascontiguousarray(binp.reshape(NZC, 128).T)}
    nc = prog("A", build_A)
    res = run(nc, [dict(common, hT=hT_sh[c]) for c in range(8)])
    return [r["h1T"] for r in res], [r["zT"] for r in res]


T_ = 4096
NT = 32
SCALE = 128 ** -0.5


class Mix:
    def __init__(self, k, nbig=7, nbigb=4):
        self.k = k
        nc = self.nc = k.nc
        self.cident = k.din("c_ident", [128, 128])
        self.identF = k.sb([128, 128], F32, "identF")
        self.identB = k.sb([128, 128], BF16, "identB")
        self.onesF = k.sb([128, 128], F32, "onesF")
        self.onesB = k.sb([128, 128], BF16, "onesB")
        k.dma("sp", self.identF[:], self.cident.h, reads=[self.cident], writes=[self.identF])
        k.op("dve", lambda: nc.vector.tensor_copy(self.identB[:], self.identF[:]), reads=[self.identF], writes=[self.identB])
        k.op("dve", lambda: nc.vector.memset(self.onesF[:], 1.0), writes=[self.onesF])
        k.op("dve", lambda: nc.vector.memset(self.onesB[:], 1.0), writes=[self.onesB])
        self.big = [k.sb([128, T_], F32, "big%d" % i) for i in range(nbig)]
        self.bigb = [k.sb([128, T_], BF16, "bigb%d" % i) for i in range(nbigb)]
        self.psS = [k.ps([128, 512], F32, "psS%d" % i) for i in range(3)]
        self.psO = [k.ps([128, 512], F32, "psO%d" % i) for i in range(2)]
        self.psL = [k.ps([128, 512], F32, "psL%d" % i) for i in range(2)]
        self.psX = k.ps([128, 512], F32, "psX")
        self.iS = 0
        self.pT = [k.sb([128, 512], BF16, "pT%d" % i) for i in range(3)]
        self.tmpA = [k.sb([128, 512], F32, "tmpA%d" % i) for i in range(4)]

    def ld(self, dst, src_t, src_ap=None, q="sp", dst_ap=None):
        self.k.dma(q, dst[:] if dst_ap is None else dst_ap, src_t.h if src_ap is None else src_ap, reads=[src_t], writes=[dst])

    def lru(self, yout):
        k, nc = self.k, self.nc
        lx = k.din("lru_x", [128, T_]); lg = k.din("lru_g", [128, T_])
        cw = k.din("lru_cw", [128, 4]); cb = k.din("lru_cb", [128, 1])
        wa = k.din("lru_wa", [128, 128]); wx = k.din("lru_wx", [128, 128])
        ba = k.din("lru_ba", [128, 1]); bx = k.din("lru_bx", [128, 1]); lam = k.din("lru_lam", [128, 1])
        xs, xc, aa, uu, hs, gg, tt = self.big[0:7]
        xcb = self.bigb[0]
        cws = k.sb([128, 4], F32, "l_cw"); cbs = k.sb([128, 1], F32, "l_cb")
        was = k.sb([128, 128], BF16, "l_wa"); wxs = k.sb([128, 128], BF16, "l_wx")
        bas = k.sb([128, 1], F32, "l_ba"); bxs = k.sb([128, 1], F32, "l_bx"); lams = k.sb([128, 1], F32, "l_lam")
        nsp = k.sb([128, 1], F32, "l_nsp")
        self.ld(xs, lx); self.ld(gg, lg); self.ld(cws, cw); self.ld(cbs, cb); self.ld(bas, ba); self.ld(bxs, bx); self.ld(lams, lam)
        self.ld(was, wa, q="pool"); self.ld(wxs, wx, q="pool")
        k.op("act", lambda: nc.scalar.activation(out=nsp[:], in_=lams[:], func=AF.Exp, scale=-1.0), reads=[lams], writes=[nsp])
        k.op("act", lambda: nc.scalar.activation(out=nsp[:], in_=nsp[:], func=AF.Ln, bias=self.onesF[:, 0:1]),
             reads=[nsp, self.onesF], writes=[nsp])
        k.op("dve", lambda: nc.vector.tensor_scalar(out=nsp[:], in0=nsp[:], scalar1=-8.0, scalar2=None, op0=ALU.mult),
             reads=[nsp], writes=[nsp])
        k.op("dve", lambda: nc.vector.tensor_scalar(out=xc[:], in0=xs[:], scalar1=cws[:, 3:4], scalar2=cbs[:, 0:1],
                                                    op0=ALU.mult, op1=ALU.add), reads=[xs, cws, cbs], writes=[xc])
        for i in range(3):
            sh = 3 - i
            k.op("dve", lambda i=i, sh=sh: nc.vector.scalar_tensor_tensor(
                out=xc[:, sh:], in0=xs[:, 0:T_ - sh], scalar=cws[:, i:i + 1], in1=xc[:, sh:], op0=ALU.mult, op1=ALU.add),
                reads=[xs, cws, xc], writes=[xc])
        k.op("act", lambda: nc.scalar.copy(out=xcb[:], in_=xc[:]), reads=[xc], writes=[xcb])
        for tb in range(8):
            ts = slice(tb * 512, (tb + 1) * 512)
            pr = self.psS[0]; pi = self.psS[1]
            k.op("pe", lambda: nc.tensor.matmul(pr[:], lhsT=was[:], rhs=xcb[:, ts], start=True, stop=True), reads=[was, xcb], writes=[pr])
            k.op("pe", lambda: nc.tensor.matmul(pi[:], lhsT=wxs[:], rhs=xcb[:, ts], start=True, stop=True), reads=[wxs, xcb], writes=[pi])
            r = self.tmpA[0]; ii = self.tmpA[1]
            k.op("act", lambda: nc.scalar.activation(out=r[:], in_=pr[:], func=AF.Sigmoid, bias=bas[:, 0:1]), reads=[pr, bas], writes=[r])
            k.op("act", lambda: nc.scalar.activation(out=ii[:], in_=pi[:], func=AF.Sigmoid, bias=bxs[:, 0:1]), reads=[pi, bxs], writes=[ii])
            k.op("act", lambda: nc.scalar.activation(out=aa[:, ts], in_=r[:], func=AF.Exp, scale=nsp[:, 0:1]),
                 reads=[r, nsp], writes=[(aa, tb)])
            t1 = self.tmpA[2]
            k.op("dve", lambda: nc.vector.tensor_tensor(out=t1[:], in0=aa[:, ts], in1=aa[:, ts], op=ALU.mult), reads=[(aa, tb)], writes=[t1])
            k.op("dve", lambda: nc.vector.tensor_scalar(out=t1[:], in0=t1[:], scalar1=-1.0, scalar2=1.0, op0=ALU.mult, op1=ALU.add),
                 reads=[t1], writes=[t1])
            k.op("act", lambda: nc.scalar.activation(out=t1[:], in_=t1[:], func=AF.Sqrt), reads=[t1], writes=[t1])
            k.op("dve", lambda: nc.vector.tensor_tensor(out=ii[:], in0=ii[:], in1=xc[:, ts], op=ALU.mult), reads=[ii, xc], writes=[ii])
            k.op("dve", lambda: nc.vector.tensor_tensor(out=uu[:, ts], in0=ii[:], in1=t1[:], op=ALU.mult), reads=[ii, t1], writes=[(uu, tb)])
        k.op("dve", lambda: nc.vector.tensor_tensor_scan(out=hs[:], data0=aa[:], data1=uu[:], initial=0.0, op0=ALU.mult, op1=ALU.add),
             reads=[aa, uu], writes=[hs])
        self.gelu_tanh(tt, gg, xs)
        k.op("dve", lambda: nc.vector.tensor_tensor(out=hs[:], in0=hs[:], in1=tt[:], op=ALU.mult), reads=[hs, tt], writes=[hs])
        k.dma("sp", yout.h, hs[:], reads=[hs], writes=[yout])

    def gelu_tanh(self, out, x, tmp):
        k, nc = self.k, self.nc
        k.op("dve", lambda: nc.vector.tensor_tensor(out=tmp[:], in0=x[:], in1=x[:], op=ALU.mult), reads=[x], writes=[tmp])
        k.op("dve", lambda: nc.vector.tensor_scalar(out=tmp[:], in0=tmp[:], scalar1=0.044715, scalar2=1.0, op0=ALU.mult, op1=ALU.add),
             reads=[tmp], writes=[tmp])
        k.op("dve", lambda: nc.vector.tensor_tensor(out=tmp[:], in0=tmp[:], in1=x[:], op=ALU.mult), reads=[tmp, x], writes=[tmp])
        k.op("act", lambda: nc.scalar.activation(out=tmp[:], in_=tmp[:], func=AF.Sigmoid, scale=1.5957691216057308),
             reads=[tmp], writes=[tmp])
        k.op("dve", lambda: nc.vector.tensor_tensor(out=out[:], in0=tmp[:], in1=x[:], op=ALU.mult), reads=[tmp, x], writes=[out])

    def attn_unit(self, kT, kt, qT, q0, extra, bias_ap, bias_reads, V, vslice, O, L, first, last, nkeys=128, kT_t=None, qT_t=None):
        k, nc = self.k, self.nc
        S = self.psS[self.iS % 3]
        P = self.pT[self.iS % 3]
        self.iS += 1
        nk = nkeys

        def mm():
            n = len(extra)
            ins = nc.tensor.matmul(S[0:nk, :], lhsT=kT[:, kt * 128:kt * 128 + nk], rhs=qT[:, q0:q0 + 512], start=True, stop=(n == 0))
            for i, (oa, l, r) in enumerate(extra):
                ins = nc.tensor.matmul(oa(S), lhsT=l, rhs=r, start=False, stop=(i == n - 1))
            return ins
        k.op("pe", mm, reads=[kT_t or kT, qT_t or qT] + self._xr, writes=[S])
        if bias_ap is None:
            k.op("act", lambda: nc.scalar.activation(out=P[0:nk, :], in_=S[0:nk, :], func=AF.Exp), reads=[S], writes=[P])
        else:
            k.op("act", lambda: nc.scalar.activation(out=P[0:nk, :], in_=S[0:nk, :], func=AF.Exp, bias=bias_ap),
                 reads=[S] + bias_reads, writes=[P])
        k.op("pe", lambda: nc.tensor.matmul(O[:], lhsT=vslice[0:nk], rhs=P[0:nk, :], start=first, stop=last), reads=[V, P], writes=[O])
        k.op("pe", lambda: nc.tensor.matmul(L[:], lhsT=self.onesB[0:nk, :], rhs=P[0:nk, :], start=first, stop=last),
             reads=[self.onesB, P], writes=[L])

    def fox(self, yout):
        k, nc = self.k, self.nc
        fq = k.din("fox_q", [128, T_]); fk = k.din("fox_k", [128, T_]); fv = k.din("fox_v", [128, NT, 128])
        ff = k.din("fox_f", [128, NT])
        cut = k.din("c_ut", [128, 128]); csu = k.din("c_su", [32, 32]); cmb = k.din("c_mbfox", [128, 4, 512])
        qf = self.big[0]
        qb, kb = self.bigb[0], self.bigb[1]
        vb = self.bigb[2]
        mb = k.sb([128, 4, 512], BF16, "f_mb")
        ut = k.sb([128, 128], F32, "f_ut"); su = k.sb([32, 32], F32, "f_su")
        lf = k.sb([128, NT], F32, "f_lf"); negc = k.sb([128, NT], F32, "f_negc")
        totc = k.sb([32, 1], F32, "f_totc"); am = k.sb([32, 128], F32, "f_am")
        dg = self.big[1]
        self.ld(qf, fq)
        self.ld(kb, fk, q="pool")
        k.dma("pool", vb[:], fv.h.rearrange("p a b -> p (a b)"), reads=[fv], writes=[vb])
        k.dma("pool", mb[:], cmb.h, reads=[cmb], writes=[mb])
        self.ld(ut, cut); self.ld(su, csu); self.ld(lf, ff)
        k.op("dve", lambda: nc.vector.tensor_scalar(out=qb[:], in0=qf[:], scalar1=SCALE, scalar2=None, op0=ALU.mult), reads=[qf], writes=[qb])
        k.op("act", lambda: nc.scalar.activation(out=lf[:], in_=lf[:], func=AF.Exp, scale=-1.0), reads=[lf], writes=[lf])
        k.op("act", lambda: nc.scalar.activation(out=lf[:], in_=lf[:], func=AF.Ln, bias=self.onesF[:, 0:1]), reads=[lf, self.onesF], writes=[lf])
        px = self.psX
        k.op("pe", lambda: nc.tensor.matmul(px[0:32, 0:1], lhsT=lf[:], rhs=self.onesF[:, 0:1], start=True, stop=True),
             reads=[lf, self.onesF], writes=[px])
        k.op("dve", lambda: nc.vector.tensor_copy(totc[:], px[0:32, 0:1]), reads=[px], writes=[totc])
        k.op("dve", lambda: nc.vector.tensor_scalar(out=am[:], in0=self.onesF[0:32, :], scalar1=totc[:, 0:1], scalar2=None, op0=ALU.mult),
             reads=[self.onesF, totc], writes=[am])

        def mmc():
            nc.tensor.matmul(px[:, 0:NT], lhsT=ut[:], rhs=lf[:], start=True, stop=False)
            return nc.tensor.matmul(px[:, 0:NT], lhsT=am[:], rhs=su[:], start=False, stop=True)
        k.op("pe", mmc, reads=[ut, lf, am, su], writes=[px])
        k.op("dve", lambda: nc.vector.tensor_copy(negc[:], px[:, 0:NT]), reads=[px], writes=[negc])
        for qt in range(NT):
            k.op("dve", lambda qt=qt: nc.vector.tensor_scalar(out=dg[:, qt * 128:(qt + 1) * 128], in0=self.identF[:],
                                                              scalar1=negc[:, qt:qt + 1], scalar2=-1.0, op0=ALU.mult, op1=ALU.mult),
                 reads=[self.identF, negc], writes=[(dg, qt)])
        for i in range(8):
            q0 = 512 * i
            O = self.psO[i % 2]; L = self.psL[i % 2]
            nk = 4 * i + 4
            for kt in range(nk):
                extra = []
                for jq in range(4):
                    qt = 4 * i + jq
                    extra.append((lambda S, jq=jq: S[:, jq * 128:(jq + 1) * 128], self.onesF[:], dg[:, qt * 128:(qt + 1) * 128]))
                self._xr = [self.onesF] + [(dg, 4 * i + jq) for jq in range(4)]
                if kt >= 4 * i:
                    extra.append((lambda S: S[:], self.identB[:], mb[:, kt - 4 * i, :]))
                    self._xr += [self.identB, mb]
                self.attn_unit(kb, kt, qb, q0, extra, negc[:, kt:kt + 1], [negc], vb, vb[:, kt * 128:(kt + 1) * 128], O, L,
                               kt == 0, kt == nk - 1)
            R = self.tmpA[i % 2]; ob = self.tmpA[2 + i % 2]
            k.op("dve", lambda: nc.vector.reciprocal(out=R[:], in_=L[:]), reads=[L], writes=[R])
            k.op("dve", lambda: nc.vector.tensor_tensor(out=ob[:], in0=O[:], in1=R[:], op=ALU.mult), reads=[O, R], writes=[ob])
            k.dma("sp", yout.h[:, q0:q0 + 512], ob[:], reads=[ob], writes=[(yout, i)])


    def bfv(self, t):
        return t.h[:].bitcast(BF16)

    def nsa(self, yout):
        k, nc = self.k, self.nc
        dq = k.din("nsa_q", [128, 4, T_]); dkc = k.din("nsa_kc", [128, T_]); dvc = k.din("nsa_vc", [128, T_])
        dks = k.din("nsa_ks", [128, T_]); dkw = k.din("nsa_kw", [128, T_])
        dvs = k.din("nsa_vs", [128, NT, 128]); dvw = k.din("nsa_vw", [128, NT, 128])
        dg = k.din("nsa_gb", [128, 3, T_])
        dw1k = k.din("nsa_w1k", [128, 32, 128]); dw1v = k.din("nsa_w1v", [128, 32, 128])
        dw2k = k.din("nsa_w2k", [128, 128]); dw2v = k.din("nsa_w2v", [128, 128])
        dpek = k.din("nsa_pek", [128, 32]); dpev = k.din("nsa_pev", [128, 32])
        dbc = k.din("nsa_bc", [128, 2, 4, T_]); dtbs = k.din("nsa_tbs", [128, 12, 512]); dtbw = k.din("nsa_tbw", [128, 8, 512])
        dkeep = k.din("nsa_keep", [128, NT, 64]); dadd = k.din("nsa_add", [128, NT, 64])
        dE = k.din("nsa_E", [64, T_]); dov = k.din("nsa_ov", [128, 2, 64])
        B = self.big
        qv = [self.bfv(B[0])[:, 0:T_], self.bfv(B[0])[:, T_:2 * T_], self.bfv(B[1])[:, 0:T_], self.bfv(B[1])[:, T_:2 * T_]]
        qT = [B[0], B[0], B[1], B[1]]
        kcin = self.bfv(B[2])[:, 0:T_]; vcin = self.bfv(B[2])[:, T_:2 * T_]
        ksb = self.bfv(B[3])[:, 0:T_]; kwb = self.bfv(B[3])[:, T_:2 * T_]
        vsb = self.bfv(B[4])[:, 0:T_]; vwb = self.bfv(B[4])[:, T_:2 * T_]
        qf = B[5]
        for h in range(4):
            k.dma("sp", qf[:], dq.h[:, h, :], reads=[dq], writes=[qf])
            k.op("dve", lambda h=h: nc.vector.tensor_scalar(out=qv[h], in0=qf[:], scalar1=SCALE, scalar2=None, op0=ALU.mult),
                 reads=[qf], writes=[(qT[h], h % 2)])
        k.dma("pool", kcin, dkc.h, reads=[dkc], writes=[(B[2], 0)])
        k.dma("pool", vcin, dvc.h, reads=[dvc], writes=[(B[2], 1)])
        k.dma("pool", ksb, dks.h, reads=[dks], writes=[(B[3], 0)])
        k.dma("pool", kwb, dkw.h, reads=[dkw], writes=[(B[3], 1)])
        k.dma("pool", vsb, dvs.h.rearrange("p a b -> p (a b)"), reads=[dvs], writes=[(B[4], 0)])
        k.dma("pool", vwb, dvw.h.rearrange("p a b -> p (a b)"), reads=[dvw], writes=[(B[4], 1)])
        w1 = [k.sb([128, 32, 128], BF16, "n_w1k"), k.sb([128, 32, 128], BF16, "n_w1v")]
        w2 = [k.sb([128, 128], BF16, "n_w2k"), k.sb([128, 128], BF16, "n_w2v")]
        pe = [k.sb([128, 32], BF16, "n_pek"), k.sb([128, 32], BF16, "n_pev")]
        self.ld(w1[0], dw1k, q="pool"); self.ld(w1[1], dw1v, q="pool"); self.ld(w2[0], dw2k, q="pool"); self.ld(w2[1], dw2v, q="pool")
        self.ld(pe[0], dpek, q="pool"); self.ld(pe[1], dpev, q="pool")
        tbs = k.sb([128, 12, 512], BF16, "n_tbs"); tbw = k.sb([128, 8, 512], BF16, "n_tbw")
        self.ld(tbs, dtbs, q="pool"); self.ld(tbw, dtbw, q="pool")
        keep = k.sb([128, 4, 64], F32, "n_keep"); addt = k.sb([128, 4, 64], F32, "n_add")
        Eb = k.sb([64, T_], BF16, "n_E"); ov = k.sb([128, 2, 64], BF16, "n_ov")
        self.ld(Eb, dE, q="pool"); self.ld(ov, dov, q="pool")
        kcT = k.sb([128, 256], BF16, "n_kcT"); vc = k.sb([128, 2, 128], BF16, "n_vc")
        hidb = [k.sb([128, 256], BF16, "n_hidk"), k.sb([128, 256], BF16, "n_hidv")]
        pb = k.sb([128, 1], F32, "n_pb")
        hx = k.sb([128, 256], F32, "n_hx"); hy = k.sb([128, 256], F32, "n_hy"); hz = k.sb([128, 256], F32, "n_hz")
        srcs = [(B[2], 0), (B[2], 1)]
        cin = [kcin, vcin]
        for w in range(2):
            px = self.psX

            def mmb():
                ins = None
                for i in range(32):
                    ins = nc.tensor.matmul(px[:, 0:1], lhsT=w1[w][:, i, :], rhs=pe[w][:, i:i + 1], start=(i == 0), stop=(i == 31))
                return ins
            k.op("pe", mmb, reads=[w1[w], pe[w]], writes=[px])
            k.op("dve", lambda: nc.vector.tensor_copy(pb[:], px[:, 0:1]), reads=[px], writes=[pb])
            ph = self.psS[w]

            def mmh():
                ins = None
                for i in range(32):
                    ins = nc.tensor.matmul(ph[:, 0:255], lhsT=w1[w][:, i, :], rhs=cin[w][:, i:i + 4065:16], start=(i == 0), stop=(i == 31))
                return ins
            k.op("pe", mmh, reads=[w1[w], srcs[w]], writes=[ph])
            k.op("dve", lambda: nc.vector.memset(hx[:], 0.0), writes=[hx])
            k.op("act", lambda: nc.scalar.activation(out=hx[:, 0:255], in_=ph[:, 0:255], func=AF.Identity, bias=pb[:, 0:1]),
                 reads=[ph, pb], writes=[hx])
            self.gelu_tanh(hz, hx, hy)
            k.op("dve", lambda: nc.vector.tensor_copy(hidb[w][:], hz[:]), reads=[hz], writes=[hidb[w]])
        pk = self.psS[2]
        k.op("pe", lambda: nc.tensor.matmul(pk[:, 0:256], lhsT=w2[0][:], rhs=hidb[0][:], start=True, stop=True), reads=[w2[0], hidb[0]], writes=[pk])
        k.op("dve", lambda: nc.vector.tensor_copy(kcT[:], pk[:, 0:256]), reads=[pk], writes=[kcT])
        for nt in range(2):
            pv = self.psO[nt]
            k.op("pe", lambda: nc.tensor.matmul(pv[:, 0:128], lhsT=hidb[1][:, nt * 128:(nt + 1) * 128], rhs=w2[1][:], start=True, stop=True),
                 reads=[hidb[1], w2[1]], writes=[pv])
            k.op("dve", lambda: nc.vector.tensor_copy(vc[:, nt, :], pv[:, 0:128]), reads=[pv], writes=[(vc, nt)])
        bc = [k.sb([128, 2, 4, 512], BF16, "n_bc0")] * 2
        gb = [k.sb([128, 3, 512], F32, "n_gb0")] * 2
        pc = [[k.sb([128, 512], BF16, "n_pc%d%d" % (h, nt)) for nt in range(2)] for h in range(4)]
        Rh = [k.sb([128, 512], F32, "n_R%d" % h) for h in range(4)]
        acc = [k.sb([128, 512], F32, "n_acc%d" % i) for i in range(2)]
        impt = k.sb([128, 4, 64], F32, "n_imp"); wrk = k.sb([128, 64], F32, "n_wrk")
        m8 = k.sb([128, 8], F32, "n_m8"); thr = k.sb([128, 1], F32, "n_thr"); selb = k.sb([128, 64], F32, "n_selb")
        selT = k.sb([64, 512], BF16, "n_selT")
        Wt = k.sb([128, 512], F32, "n_W")
        NKC = [128, 127]
        for i in range(8):
            q0 = 512 * i
            bci = bc[i % 2]; gbi = gb[i % 2]; ac = acc[i % 2]
            k.dma("pool", bci[:], dbc.h[:, :, :, q0:q0 + 512], reads=[dbc], writes=[bci])
            k.dma("sp", gbi[:], dg.h[:, :, q0:q0 + 512], reads=[dg], writes=[gbi])
            k.dma("sp", keep[:], dkeep.h[:, 4 * i:4 * i + 4, :], reads=[dkeep], writes=[keep])
            k.dma("sp", addt[:], dadd.h[:, 4 * i:4 * i + 4, :], reads=[dadd], writes=[addt])
            k.op("act", lambda: nc.scalar.activation(out=gbi[:], in_=gbi[:], func=AF.Sigmoid), reads=[gbi], writes=[gbi])
            for hh in range(4):
                h = hh
                own = (h == 3)
                L = self.psL[hh % 2]; O = self.psO[0]
                for nt in range(2):
                    nk = NKC[nt]
                    S = self.psS[self.iS % 3]; self.iS += 1
                    P = pc[h][nt]

                    def mm():
                        nc.tensor.matmul(S[0:nk, :], lhsT=kcT[:, nt * 128:nt * 128 + nk], rhs=qv[h][:, q0:q0 + 512], start=True, stop=False)
                        return nc.tensor.matmul(S[0:nk, :], lhsT=self.identB[:, 0:nk], rhs=bci[:, nt, h, :], start=False, stop=True)
                    k.op("pe", mm, reads=[kcT, (qT[h], h % 2), self.identB, bci], writes=[S])
                    if nk < 128:
                        k.op("dve", lambda: nc.vector.memset(P[:], 0.0), writes=[P])
                    k.op("act", lambda: nc.scalar.activation(out=P[0:nk, :], in_=S[0:nk, :], func=AF.Exp), reads=[S], writes=[P])
                    k.op("pe", lambda: nc.tensor.matmul(L[:], lhsT=self.onesB[0:nk, :], rhs=P[0:nk, :], start=(nt == 0), stop=(nt == 1)),
                         reads=[self.onesB, P], writes=[L])
                    if own:
                        k.op("pe", lambda: nc.tensor.matmul(O[:], lhsT=vc[0:nk, nt, :], rhs=P[0:nk, :], start=(nt == 0), stop=(nt == 1)),
                             reads=[vc, P], writes=[O])
                k.op("dve", lambda: nc.vector.tensor_scalar(out=Rh[h][:], in0=L[:], scalar1=1e-30, scalar2=None, op0=ALU.max),
                     reads=[L], writes=[Rh[h]])
                k.op("dve", lambda: nc.vector.reciprocal(out=Rh[h][:], in_=Rh[h][:]), reads=[Rh[h]], writes=[Rh[h]])
                if own:
                    k.op("dve", lambda: nc.vector.tensor_tensor(out=Wt[:], in0=Rh[h][:], in1=gbi[:, 0, :], op=ALU.mult), reads=[Rh[h], gbi], writes=[Wt])
                    k.op("dve", lambda: nc.vector.tensor_tensor(out=ac[:], in0=O[:], in1=Wt[:], op=ALU.mult), reads=[O, Wt], writes=[ac])
                for nt in range(2):
                    k.op("dve", lambda: nc.vector.tensor_tensor(out=pc[h][nt][:], in0=pc[h][nt][:], in1=Rh[h][:], op=ALU.mult),
                         reads=[pc[h][nt], Rh[h]], writes=[pc[h][nt]])
            px = self.psX
            for jq in range(4):
                def mmi():
                    ins = None
                    n = 0
                    for h in range(4):
                        for nt in range(2):
                            ins = nc.tensor.matmul(px[:, jq * 64:(jq + 1) * 64], lhsT=pc[h][nt][:, jq * 128:(jq + 1) * 128], rhs=ov[:, nt, :],
                                                   start=(n == 0), stop=(n == 7))
                            n += 1
                    return ins
                k.op("pe", mmi, reads=[ov] + [pc[h][nt] for h in range(4) for nt in range(2)], writes=[(px, jq)])
            k.op("dve", lambda: nc.vector.tensor_tensor(out=impt[:].rearrange("p a b -> p (a b)"), in0=px[:, 0:256],
                                                        in1=keep[:].rearrange("p a b -> p (a b)"), op=ALU.mult),
                 reads=[px, keep], writes=[impt])
            k.op("dve", lambda: nc.vector.tensor_tensor(out=impt[:], in0=impt[:], in1=addt[:], op=ALU.add),
                 reads=[impt, addt], writes=[impt])
            pt = self.psL[0]
            for jq in range(4):
                k.op("dve", lambda: nc.vector.max(out=m8[:], in_=impt[:, jq, :]), reads=[impt], writes=[m8])
                k.op("dve", lambda: nc.vector.match_replace(out=wrk[:], in_to_replace=m8[:], in_values=impt[:, jq, :], imm_value=-1e30),
                     reads=[m8, impt], writes=[wrk])
                k.op("dve", lambda: nc.vector.max(out=m8[:], in_=wrk[:]), reads=[wrk], writes=[m8])
                k.op("dve", lambda: nc.vector.tensor_reduce(out=thr[:], in_=m8[:], axis=AX.X, op=ALU.min), reads=[m8], writes=[thr])
                k.op("dve", lambda: nc.vector.tensor_scalar(out=selb[:], in0=impt[:, jq, :], scalar1=thr[:, 0:1], scalar2=NEG,
                                                            op0=ALU.is_lt, op1=ALU.mult), reads=[impt, thr], writes=[selb])
                k.op("pe", lambda: nc.tensor.matmul(pt[0:64, jq * 128:(jq + 1) * 128], lhsT=selb[:], rhs=self.identF[:], start=True, stop=True),
                     reads=[selb, self.identF], writes=[(pt, jq)])
            k.op("dve", lambda: nc.vector.tensor_copy(selT[:], pt[0:64, :]), reads=[pt], writes=[selT])
            O = self.psO[1]; L = self.psL[1]
            nkt = 4 * i + 4
            for kt in range(nkt):
                idx = min(4 * i - kt + 3, 11)
                extra = [(lambda S: S[:], self.identB[:], tbs[:, idx, :]),
                         (lambda S: S[:], Eb[:, kt * 128:(kt + 1) * 128], selT[:])]
                self._xr = [self.identB, tbs, Eb, selT]
                self.attn_unit(ksb, kt, qv[3], q0, extra, None, [], (B[4], 0), vsb[:, kt * 128:(kt + 1) * 128], O, L, kt == 0, kt == nkt - 1,
                               kT_t=(B[3], 0), qT_t=(qT[3], 1))
            self.nsa_fin(O, L, gbi, 1, ac, Wt)
            O = self.psO[0]; L = self.psL[0]
            kts = list(range(max(0, 4 * i - 4), 4 * i + 4))
            for n, kt in enumerate(kts):
                idx = 4 * i - kt + 3
                extra = [(lambda S: S[:], self.identB[:], tbw[:, idx, :])]
                self._xr = [self.identB, tbw]
                self.attn_unit(kwb, kt, qv[3], q0, extra, None, [], (B[4], 1), vwb[:, kt * 128:(kt + 1) * 128], O, L, n == 0, n == len(kts) - 1,
                               kT_t=(B[3], 1), qT_t=(qT[3], 1))
            self.nsa_fin(O, L, gbi, 2, ac, Wt)
            k.dma("sp", yout.h[:, q0:q0 + 512], ac[:], reads=[ac], writes=[(yout, i)])

    def nsa_fin(self, O, L, gbi, gi, ac, Wt):
        k, nc = self.k, self.nc
        t2 = self.tmpA[0]
        k.op("dve", lambda: nc.vector.reciprocal(out=Wt[:], in_=L[:]), reads=[L], writes=[Wt])
        k.op("dve", lambda: nc.vector.tensor_tensor(out=Wt[:], in0=Wt[:], in1=gbi[:, gi, :], op=ALU.mult), reads=[Wt, gbi], writes=[Wt])
        k.op("dve", lambda: nc.vector.tensor_tensor(out=t2[:], in0=O[:], in1=Wt[:], op=ALU.mult), reads=[O, Wt], writes=[t2])
        k.op("dve", lambda: nc.vector.tensor_tensor(out=ac[:], in0=ac[:], in1=t2[:], op=ALU.add), reads=[ac, t2], writes=[ac])


    def gdn(self, yout):
        k, nc = self.k, self.nc
        dq = k.din("gdn_q", [128, T_]); dk = k.din("gdn_k", [128, T_]); dv = k.din("gdn_v", [128, T_]); dz = k.din("gdn_z", [128, T_])
        dcw = k.din("gdn_cw", [128, 3, 4]); da = k.din("gdn_a", [128, NT]); db = k.din("gdn_b", [128, NT])
        dal = k.din("gdn_alog", [128, 1]); ddt = k.din("gdn_dtb", [128, 1]); dng = k.din("gdn_ng", [128, 1])
        dct = k.din("c_ct", [128, 128]); dsc = k.din("c_sc", [128, 128]); dh0 = k.din("c_h0", [128, 128]); dh1 = k.din("c_h1", [128, 128])
        dmst = k.din("c_mst", [128, 128]); dmit = k.din("c_mit", [128, 128]); dmsn = k.din("c_msn", [128, 128]); dcm = k.din("c_cm", [128, 2])
        B = self.big
        raw, qs, ks, vs, oT, tA, tB = B[0], B[1], B[2], B[3], B[4], B[5], B[6]
        qnb, knb, vsb = self.bigb[0], self.bigb[1], self.bigb[2]
        cw = k.sb([128, 3, 4], F32, "g_cw"); self.ld(cw, dcw)
        cst = {}
        for nm, dd in (("ct", dct), ("sc", dsc), ("h0", dh0), ("h1", dh1), ("mst", dmst), ("mit", dmit), ("msn", dmsn)):
            cst[nm] = k.sb([128, 128], F32, "g_" + nm); self.ld(cst[nm], dd)
        cm = k.sb([128, 2], F32, "g_cm"); self.ld(cm, dcm)
        al = k.sb([128, 1], F32, "g_al"); dtb = k.sb([128, 1], F32, "g_dtb"); ng = k.sb([128, 1], F32, "g_ng")
        self.ld(al, dal); self.ld(dtb, ddt); self.ld(ng, dng)
        epsc = k.sb([128, 1], F32, "g_eps")
        k.op("dve", lambda: nc.vector.memset(epsc[:], EPS), writes=[epsc])
        for wi, (src, dst) in enumerate(((dq, qs), (dk, ks), (dv, vs))):
            self.ld(raw, src)
            k.op("dve", lambda: nc.vector.tensor_scalar(out=dst[:], in0=raw[:], scalar1=cw[:, wi, 3:4], scalar2=None, op0=ALU.mult),
                 reads=[raw, cw], writes=[dst])
            for i in range(3):
                sh = 3 - i
                k.op("dve", lambda: nc.vector.scalar_tensor_tensor(out=dst[:, sh:], in0=raw[:, 0:T_ - sh], scalar=cw[:, wi, i:i + 1],
                                                                   in1=dst[:, sh:], op0=ALU.mult, op1=ALU.add), reads=[raw, cw, dst], writes=[dst])
            k.op("act", lambda: nc.scalar.activation(out=dst[:], in_=dst[:], func=AF.Silu), reads=[dst], writes=[dst])
        k.op("act", lambda: nc.scalar.copy(out=vsb[:], in_=vs[:]), reads=[vs], writes=[vsb])
        for (src, dstb, sc) in ((qs, qnb, SCALE), (ks, knb, 1.0)):
            k.op("dve", lambda: nc.vector.tensor_tensor(out=tA[:], in0=src[:], in1=src[:], op=ALU.mult), reads=[src], writes=[tA])
            for tb in range(8):
                ts = slice(tb * 512, (tb + 1) * 512)
                ps = self.psS[tb % 3]
                k.op("pe", lambda: nc.tensor.matmul(ps[:], lhsT=self.onesF[:], rhs=tA[:, ts], start=True, stop=True), reads=[self.onesF, tA], writes=[ps])
                k.op("act", lambda: nc.scalar.activation(out=tB[:, ts], in_=ps[:], func=AF.Ln, bias=epsc[:, 0:1]), reads=[ps, epsc], writes=[(tB, tb)])
                k.op("act", lambda: nc.scalar.activation(out=tB[:, ts], in_=tB[:, ts], func=AF.Exp, scale=-0.5), reads=[(tB, tb)], writes=[(tB, tb)])
            k.op("dve", lambda: nc.vector.scalar_tensor_tensor(out=dstb[:], in0=src[:], scalar=sc, in1=tB[:], op0=ALU.mult, op1=ALU.mult),
                 reads=[src, tB], writes=[dstb])
        def col(nm):
            return k.sb([128, NT], F32, "g_c_" + nm)
        g = col("g"); beta = col("beta"); gc = col("gc"); ngc = col("ngc"); gl = col("gl"); wcol = col("w")
        skbg = col("skbg"); skd = [col("skd0"), col("skd1")]; egl = [col("egl0"), col("egl1")]; tmpc = col("tmp")
        self.ld(g, da); self.ld(beta, db)
        k.op("act", lambda: nc.scalar.activation(out=g[:], in_=g[:], func=AF.Exp, bias=dtb[:, 0:1]), reads=[g, dtb], writes=[g])
        k.op("act", lambda: nc.scalar.activation(out=g[:], in_=g[:], func=AF.Ln, bias=self.onesF[:, 0:1]), reads=[g, self.onesF], writes=[g])
        k.op("act", lambda: nc.scalar.activation(out=al[:], in_=al[:], func=AF.Exp), reads=[al], writes=[al])
        k.op("dve", lambda: nc.vector.tensor_scalar(out=g[:], in0=g[:], scalar1=al[:, 0:1], scalar2=-1.0, op0=ALU.mult, op1=ALU.mult),
             reads=[g, al], writes=[g])
        k.op("act", lambda: nc.scalar.activation(out=beta[:], in_=beta[:], func=AF.Sigmoid), reads=[beta], writes=[beta])
        px = self.psX

        def colmm(lhs, dst, func=None):
            k.op("pe", lambda: nc.tensor.matmul(px[:, 0:NT], lhsT=lhs[:], rhs=g[:], start=True, stop=True), reads=[lhs, g], writes=[px])
            if func is None:
                k.op("dve", lambda: nc.vector.tensor_copy(dst[:], px[:, 0:NT]), reads=[px], writes=[dst])
            else:
                k.op("act", lambda: nc.scalar.activation(out=dst[:], in_=px[:, 0:NT], func=func), reads=[px], writes=[dst])
        colmm(cst["ct"], gc)
        colmm(cst["sc"], gl)
        colmm(cst["h0"], egl[0], AF.Exp)
        colmm(cst["h1"], egl[1], AF.Exp)
        k.op("dve", lambda: nc.vector.tensor_scalar(out=ngc[:], in0=gc[:], scalar1=-1.0, scalar2=None, op0=ALU.mult), reads=[gc], writes=[ngc])
        k.op("act", lambda: nc.scalar.activation(out=wcol[:], in_=beta[:], func=AF.Ln), reads=[beta], writes=[wcol])
        k.op("dve", lambda: nc.vector.tensor_tensor(out=wcol[:], in0=wcol[:], in1=gc[:], op=ALU.add), reads=[wcol, gc], writes=[wcol])
        k.op("act", lambda: nc.scalar.activation(out=skbg[:], in_=gc[:], func=AF.Exp), reads=[gc], writes=[skbg])
        k.op("dve", lambda: nc.vector.tensor_tensor(out=skbg[:], in0=skbg[:], in1=beta[:], op=ALU.mult), reads=[skbg, beta], writes=[skbg])
        k.op("dve", lambda: nc.vector.tensor_tensor(out=tmpc[:], in0=gl[:], in1=gc[:], op=ALU.subtract), reads=[gl, gc], writes=[tmpc])
        k.op("act", lambda: nc.scalar.activation(out=tmpc[:], in_=tmpc[:], func=AF.Exp), reads=[tmpc], writes=[tmpc])
        for c in range(2):
            k.op("dve", lambda: nc.vector.tensor_scalar(out=skd[c][:], in0=tmpc[:], scalar1=cm[:, c:c + 1], scalar2=None, op0=ALU.mult),
                 reads=[tmpc, cm], writes=[skd[c]])
        S = k.sb([128, 128], F32, "g_S"); Sb = k.sb([128, 128], BF16, "g_Sb")
        k.op("dve", lambda: nc.vector.memset(S[:], 0.0), writes=[S])
        k.op("dve", lambda: nc.vector.memset(Sb[:], 0.0), writes=[Sb])

        def t128(nm, dt=F32):
            return k.sb([128, 128], dt, "g_t_" + nm)
        kbg = t128("kbg", BF16); kd = [t128("kd0", BF16), t128("kd1", BF16)]; vb = t128("vb", BF16)
        dgw = t128("dgw"); dgg = t128("dgg"); dgn = t128("dgn")
        Gs = t128("G"); Y = t128("Y"); X = t128("X"); Pm = t128("P"); Z = t128("Z"); ZT = t128("ZT"); Z2 = t128("Z2"); ZT2 = t128("ZT2")
        E1 = t128("E1"); qkT = t128("qkT", BF16); qgT = t128("qgT", BF16); PTb = t128("PTb", BF16); nWT = t128("nWT", BF16)
        vnb = t128("vnb", BF16)
        pool6 = [self.psS[0], self.psS[1], self.psS[2], self.psO[1], self.psL[0], self.psL[1]]
        ctr = [0]

        def pp():
            ctr[0] += 1
            return pool6[ctr[0] % 6]
        pOg = self.psO[0]
        for t in range(NT):
            cs = slice(t * 128, (t + 1) * 128)
            tc_ = slice(t, t + 1)
            pa = pp()
            k.op("pe", lambda: nc.tensor.matmul(pa[:, 0:128], lhsT=knb[:, cs], rhs=self.identB[:], start=True, stop=True), reads=[knb, self.identB], writes=[(pa, 0)])
            k.op("pe", lambda: nc.tensor.matmul(pa[:, 128:256], lhsT=vsb[:, cs], rhs=self.identB[:], start=True, stop=True), reads=[vsb, self.identB], writes=[(pa, 1)])
            k.op("dve", lambda: nc.vector.tensor_scalar(out=kbg[:], in0=pa[:, 0:128], scalar1=skbg[:, tc_], scalar2=None, op0=ALU.mult), reads=[(pa, 0), skbg], writes=[kbg])
            for c in range(2):
                k.op("dve", lambda: nc.vector.tensor_scalar(out=kd[c][:], in0=pa[:, 0:128], scalar1=skd[c][:, tc_], scalar2=None, op0=ALU.mult),
                     reads=[(pa, 0), skd[c]], writes=[kd[c]])
            k.op("dve", lambda: nc.vector.tensor_scalar(out=vb[:], in0=pa[:, 128:256], scalar1=beta[:, tc_], scalar2=None, op0=ALU.mult), reads=[(pa, 1), beta], writes=[vb])
            k.op("dve", lambda: nc.vector.tensor_scalar(out=dgw[:], in0=self.identF[:], scalar1=wcol[:, tc_], scalar2=None, op0=ALU.mult), reads=[self.identF, wcol], writes=[dgw])
            k.op("dve", lambda: nc.vector.tensor_scalar(out=dgg[:], in0=self.identF[:], scalar1=gc[:, tc_], scalar2=None, op0=ALU.mult), reads=[self.identF, gc], writes=[dgg])
            k.op("dve", lambda: nc.vector.tensor_scalar(out=dgn[:], in0=self.identF[:], scalar1=ngc[:, tc_], scalar2=None, op0=ALU.mult), reads=[self.identF, ngc], writes=[dgn])
            pg = pp()
            k.op("pe", lambda: nc.tensor.matmul(pg[:, 0:128], lhsT=knb[:, cs], rhs=knb[:, cs], start=True, stop=True), reads=[knb], writes=[(pg, 0)])
            k.op("pe", lambda: nc.tensor.matmul(pg[:, 128:256], lhsT=knb[:, cs], rhs=qnb[:, cs], start=True, stop=True), reads=[knb, qnb], writes=[(pg, 1)])
            k.op("dve", lambda: nc.vector.tensor_copy(Gs[:], pg[:, 0:128]), reads=[(pg, 0)], writes=[Gs])

            def expmat(diag, mask, bias_col, bias_t, dst_fn):
                pe_ = pp()

                def mm():
                    ins0 = nc.tensor.matmul(pe_[:, 0:128], lhsT=self.onesF[:], rhs=diag[:], start=True, stop=(mask is None))
                    if mask is None:
                        return ins0
                    return nc.tensor.matmul(pe_[:, 0:128], lhsT=self.identF[:], rhs=mask[:], start=False, stop=True)
                k.op("pe", mm, reads=[self.onesF, diag, self.identF] + ([mask] if mask is not None else []), writes=[pe_])
                if bias_col is None:
                    k.op("act", lambda: nc.scalar.activation(out=E1[:], in_=pe_[:, 0:128], func=AF.Exp), reads=[pe_], writes=[E1])
                else:
                    k.op("act", lambda: nc.scalar.activation(out=E1[:], in_=pe_[:, 0:128], func=AF.Exp, bias=bias_col[:, tc_]),
                         reads=[pe_, bias_t], writes=[E1])
                dst_fn()
            expmat(dgw, cst["mst"], ngc, ngc, lambda: k.op("dve", lambda: nc.vector.tensor_tensor(out=Y[:], in0=Gs[:], in1=E1[:], op=ALU.mult), reads=[Gs, E1], writes=[Y]))
            expmat(dgn, cst["msn"], wcol, wcol, lambda: k.op("dve", lambda: nc.vector.tensor_tensor(out=X[:], in0=Gs[:], in1=E1[:], op=ALU.mult), reads=[Gs, E1], writes=[X]))
            expmat(dgg, cst["mit"], ngc, ngc, lambda: k.op("dve", lambda: nc.vector.tensor_tensor(out=qkT[:], in0=pg[:, 128:256], in1=E1[:], op=ALU.mult), reads=[(pg, 1), E1], writes=[qkT]))
            expmat(dgg, None, None, None, lambda: k.op("dve", lambda: nc.vector.tensor_tensor(out=qgT[:], in0=qnb[:, cs], in1=E1[:], op=ALU.mult), reads=[qnb, E1], writes=[qgT]))
            k.op("dve", lambda: nc.vector.tensor_tensor(out=Pm[:], in0=self.identF[:], in1=Y[:], op=ALU.subtract), reads=[self.identF, Y], writes=[Pm])
            p1 = pp(); p2 = pp()
            k.op("pe", lambda: nc.tensor.matmul(p1[:, 0:128], lhsT=X[:], rhs=Y[:], start=True, stop=True), reads=[X, Y], writes=[p1])
            k.op("pe", lambda: nc.tensor.matmul(p2[:, 0:128], lhsT=Y[:], rhs=X[:], start=True, stop=True), reads=[X, Y], writes=[p2])
            zc, ztc, zn, ztn = Z, ZT, Z2, ZT2
            k.op("dve", lambda: nc.vector.tensor_copy(zc[:], p1[:, 0:128]), reads=[p1], writes=[zc])
            k.op("act", lambda: nc.scalar.copy(out=ztc[:], in_=p2[:, 0:128]), reads=[p2], writes=[ztc])
            for it in range(5):
                p3 = pp()
                k.op("pe", lambda: nc.tensor.matmul(p3[:, 0:128], lhsT=ztc[:], rhs=Pm[:], start=True, stop=True), reads=[ztc, Pm], writes=[p3])
                if it < 4:
                    p1 = pp(); p2 = pp()
                    k.op("pe", lambda: nc.tensor.matmul(p1[:, 0:128], lhsT=ztc[:], rhs=zc[:], start=True, stop=True), reads=[ztc, zc], writes=[p1])
                    k.op("pe", lambda: nc.tensor.matmul(p2[:, 0:128], lhsT=zc[:], rhs=ztc[:], start=True, stop=True), reads=[ztc, zc], writes=[p2])
                k.op("dve", lambda: nc.vector.tensor_tensor(out=Pm[:], in0=Pm[:], in1=p3[:, 0:128], op=ALU.add), reads=[Pm, p3], writes=[Pm])
                if it < 4:
                    k.op("dve", lambda: nc.vector.tensor_copy(zn[:], p1[:, 0:128]), reads=[p1], writes=[zn])
                    k.op("act", lambda: nc.scalar.copy(out=ztn[:], in_=p2[:, 0:128]), reads=[p2], writes=[ztn])
                    zc, ztc, zn, ztn = zn, ztn, zc, ztc
            k.op("act", lambda: nc.scalar.copy(out=PTb[:], in_=Pm[:]), reads=[Pm], writes=[PTb])
            pw = pp()
            k.op("pe", lambda: nc.tensor.matmul(pw[:, 0:128], lhsT=kbg[:], rhs=PTb[:], start=True, stop=True), reads=[kbg, PTb], writes=[pw])
            k.op("dve", lambda: nc.vector.tensor_scalar(out=nWT[:], in0=pw[:, 0:128], scalar1=-1.0, scalar2=None, op0=ALU.mult), reads=[pw], writes=[nWT])
            for c in range(2):
                ccs = slice(64 * c, 64 * c + 64)
                pv = pp()

                def mmv():
                    nc.tensor.matmul(pv[:, 0:128], lhsT=PTb[:], rhs=vb[:], start=True, stop=False)
                    return nc.tensor.matmul(pv[:, 0:128], lhsT=nWT[:], rhs=Sb[:], start=False, stop=True)
                k.op("pe", mmv, reads=[PTb, vb, nWT, Sb], writes=[pv])
                k.op("act", lambda: nc.scalar.copy(out=vnb[:], in_=pv[:, 0:128]), reads=[pv], writes=[vnb])

                def mmo():
                    nc.tensor.matmul(pOg[:, ccs], lhsT=Sb[:], rhs=qgT[:, ccs], start=True, stop=False)
                    return nc.tensor.matmul(pOg[:, ccs], lhsT=vnb[:], rhs=qkT[:, ccs], start=False, stop=True)
                k.op("pe", mmo, reads=[Sb, qgT, vnb, qkT], writes=[(pOg, c)])
                pu = pp()
                k.op("pe", lambda: nc.tensor.matmul(pu[:, 0:128], lhsT=kd[c][:], rhs=vnb[:], start=True, stop=True), reads=[kd[c], vnb], writes=[pu])
                k.op("dve", lambda: nc.vector.scalar_tensor_tensor(out=S[:], in0=S[:], scalar=egl[c][:, tc_], in1=pu[:, 0:128], op0=ALU.mult, op1=ALU.add),
                     reads=[S, egl[c], pu], writes=[S])
                k.op("act", lambda: nc.scalar.copy(out=Sb[:], in_=S[:]), reads=[S], writes=[Sb])
            k.op("dve", lambda: nc.vector.tensor_copy(oT[:, cs], pOg[:, 0:128]), reads=[pOg], writes=[(oT, t)])
        self.ld(raw, dz)
        k.op("act", lambda: nc.scalar.activation(out=raw[:], in_=raw[:], func=AF.Silu), reads=[raw], writes=[raw])
        k.op("dve", lambda: nc.vector.tensor_tensor(out=tA[:], in0=oT[:], in1=oT[:], op=ALU.mult), reads=[oT], writes=[tA])
        k.op("dve", lambda: nc.vector.tensor_scalar(out=tA[:], in0=tA[:], scalar1=1.0 / 128, scalar2=None, op0=ALU.mult), reads=[tA], writes=[tA])
        for tb in range(8):
            ts = slice(tb * 512, (tb + 1) * 512)
            ps = self.psS[tb % 3]
            k.op("pe", lambda: nc.tensor.matmul(ps[:], lhsT=self.onesF[:], rhs=tA[:, ts], start=True, stop=True), reads=[self.onesF, tA], writes=[ps])
            k.op("act", lambda: nc.scalar.activation(out=tB[:, ts], in_=ps[:], func=AF.Ln, bias=epsc[:, 0:1]), reads=[ps, epsc], writes=[(tB, tb)])
            k.op("act", lambda: nc.scalar.activation(out=tB[:, ts], in_=tB[:, ts], func=AF.Exp, scale=-0.5), reads=[(tB, tb)], writes=[(tB, tb)])
        k.op("dve", lambda: nc.vector.scalar_tensor_tensor(out=oT[:], in0=oT[:], scalar=ng[:, 0:1], in1=tB[:], op0=ALU.mult, op1=ALU.mult),
             reads=[oT, ng, tB], writes=[oT])
        k.op("dve", lambda: nc.vector.tensor_tensor(out=oT[:], in0=oT[:], in1=raw[:], op=ALU.mult), reads=[oT, raw], writes=[oT])
        k.dma("sp", yout.h, oT[:], reads=[oT], writes=[yout])


def build_B(which):
    nc = bass.Bass("TRN2", target_bir_lowering=False)
    with ExitStack() as es:
        k = K(nc, es)
        nb = {"fox": (2, 3), "lru": (7, 1), "gdn": (7, 3), "nsa": (6, 0)}
        m = Mix(k, max(nb[w][0] for w in which), max(nb[w][1] for w in which))
        if "fox" in which:
            m.fox(k.dout("y_fox", [128, T_]))
        if "lru" in which:
            m.lru(k.dout("y_lru", [128, T_]))
        if "nsa" in which:
            m.nsa(k.dout("y_nsa", [128, T_]))
        if "gdn" in which:
            m.gdn(k.dout("y_gdn", [128, T_]))
        k.wait_all("sp")
    return nc


def tm_tiles(a):
    t, d = a.shape
    return np.ascontiguousarray(a.reshape(t // 128, 128, d).transpose(1, 0, 2))


def col_tiles(v):
    return np.ascontiguousarray(v.reshape(-1, 128).T)


_OFF = {}
_o = 0
for _n, _w in (("fox_q", 512), ("fox_k", 512), ("fox_v", 512), ("fox_f", 4), ("gdn_q", 512), ("gdn_k", 512), ("gdn_v", 512),
               ("gdn_a", 4), ("gdn_b", 4), ("gdn_z", 512), ("lru_x", 512), ("lru_gate", 512), ("nsa_q", 512), ("nsa_kc", 128),
               ("nsa_vc", 128), ("nsa_ks", 128), ("nsa_vs", 128), ("nsa_kw", 128), ("nsa_vw", 128), ("nsa_g", 12)):
    _OFF[_n] = _o
    _o += _w


def consts_B():
    c = {}
    c["c_ident"] = np.eye(128, dtype=np.float32)
    p = np.arange(128)
    c["c_ut"] = (p[:, None] <= p[None, :]).astype(np.float32)
    q = np.arange(32)
    c["c_su"] = (q[:, None] < q[None, :]).astype(np.float32)
    col = np.arange(512)
    mb = np.zeros((128, 4, 512), np.float32)
    for m in range(4):
        mb[:, m, :] = np.where(p[:, None] + 128 * m <= col[None, :], 0.0, NEG)
    c["c_mbfox"] = mb
    ch = p // 64
    same = ch[:, None] == ch[None, :]
    c["c_ct"] = (same & (p[:, None] <= p[None, :])).astype(np.float32)
    c["c_sc"] = same.astype(np.float32)
    c["c_h0"] = np.broadcast_to((p < 64)[:, None], (128, 128)).astype(np.float32).copy()
    c["c_h1"] = np.broadcast_to((p >= 64)[:, None], (128, 128)).astype(np.float32).copy()
    c["c_mst"] = np.where(same & (p[None, :] > p[:, None]), 0.0, NEG).astype(np.float32)
    c["c_mit"] = np.where(same & (p[None, :] >= p[:, None]), 0.0, NEG).astype(np.float32)
    c["c_msn"] = np.where(same & (p[:, None] > p[None, :]), 0.0, NEG).astype(np.float32)
    c["c_cm"] = np.stack([(p < 64), (p >= 64)], axis=1).astype(np.float32)
    return c


def prep_B(zfull, inp, l, which):
    cs = consts_B()
    maps = []
    for core in range(8):
        b, j = core // 4, core % 4
        z = zfull[b]
        m = {"c_ident": cs["c_ident"]}
        hs = slice(j * 128, (j + 1) * 128)
        if "fox" in which:
            m["fox_q"] = np.ascontiguousarray(z[:, _OFF["fox_q"]:_OFF["fox_q"] + 512][:, hs].T)
            m["fox_k"] = np.ascontiguousarray(z[:, _OFF["fox_k"]:_OFF["fox_k"] + 512][:, hs].T)
            m["fox_v"] = tm_tiles(z[:, _OFF["fox_v"]:_OFF["fox_v"] + 512][:, hs])
            m["fox_f"] = col_tiles(z[:, _OFF["fox_f"] + j])
            m["c_ut"] = cs["c_ut"]; m["c_su"] = cs["c_su"]; m["c_mbfox"] = cs["c_mbfox"]
        if "lru" in which:
            m["lru_x"] = np.ascontiguousarray(z[:, _OFF["lru_x"]:_OFF["lru_x"] + 512][:, hs].T)
            m["lru_g"] = np.ascontiguousarray(z[:, _OFF["lru_gate"]:_OFF["lru_gate"] + 512][:, hs].T)
            m["lru_cw"] = np.ascontiguousarray(inp["lru_conv_w"][l][:, hs].T)
            m["lru_cb"] = np.ascontiguousarray(inp["lru_conv_b"][l][hs].reshape(128, 1))
            for nm, src in (("lru_wa", "lru_w_a"), ("lru_wx", "lru_w_x")):
                bd = np.zeros((128, 128), np.float32)
                bd[0:64, 0:64] = inp[src][l][2 * j]
                bd[64:128, 64:128] = inp[src][l][2 * j + 1]
                m[nm] = bd
            m["lru_ba"] = np.ascontiguousarray(inp["lru_b_a"][l][hs].reshape(128, 1))
            m["lru_bx"] = np.ascontiguousarray(inp["lru_b_x"][l][hs].reshape(128, 1))
            m["lru_lam"] = np.ascontiguousarray(inp["lru_lambda"][l][hs].reshape(128, 1))
        if "gdn" in which:
            for nm in ("q", "k", "v", "z"):
                o = _OFF["gdn_" + nm]
                m["gdn_" + nm] = np.ascontiguousarray(z[:, o:o + 512][:, hs].T)
            cwf = inp["gdn_conv_w"][l]
            m["gdn_cw"] = np.ascontiguousarray(np.stack([cwf[:, g0 * 512:(g0 + 1) * 512][:, hs].T for g0 in range(3)], axis=1))
            m["gdn_a"] = col_tiles(z[:, _OFF["gdn_a"] + j]); m["gdn_b"] = col_tiles(z[:, _OFF["gdn_b"] + j])
            m["gdn_alog"] = np.full((128, 1), inp["gdn_a_log"][l][j], np.float32)
            m["gdn_dtb"] = np.full((128, 1), inp["gdn_dt_bias"][l][j], np.float32)
            m["gdn_ng"] = np.ascontiguousarray(inp["gdn_norm_g"][l].reshape(128, 1))
            for nm in ("c_ct", "c_sc", "c_h0", "c_h1", "c_mst", "c_mit", "c_msn", "c_cm"):
                m[nm] = cs[nm]
        if "nsa" in which:
            order = [(j + 1 + hh) % 4 for hh in range(4)]
            qa = z[:, _OFF["nsa_q"]:_OFF["nsa_q"] + 512]
            m["nsa_q"] = np.ascontiguousarray(np.stack([qa[:, h * 128:(h + 1) * 128].T for h in order], axis=1))
            for nm in ("kc", "vc", "ks", "kw"):
                o = _OFF["nsa_" + nm]
                m["nsa_" + nm] = np.ascontiguousarray(z[:, o:o + 128].T)
            for nm in ("vs", "vw"):
                o = _OFF["nsa_" + nm]
                m["nsa_" + nm] = tm_tiles(z[:, o:o + 128])
            gg = z[:, _OFF["nsa_g"] + 3 * j:_OFF["nsa_g"] + 3 * j + 3]
            m["nsa_gb"] = np.ascontiguousarray(np.broadcast_to(gg.T[None], (128, 3, T_)))
            for nm, src in (("nsa_w1k", "nsa_w1_k"), ("nsa_w1v", "nsa_w1_v")):
                m[nm] = np.ascontiguousarray(inp[src][l].reshape(32, 128, 128).transpose(1, 0, 2))
            m["nsa_w2k"] = inp["nsa_w2_k"][l]; m["nsa_w2v"] = inp["nsa_w2_v"][l]
            m["nsa_pek"] = np.ascontiguousarray(inp["nsa_pe_k"][l].T); m["nsa_pev"] = np.ascontiguousarray(inp["nsa_pe_v"][l].T)
            rb = np.asarray(inp["rel_bias"], np.float32)
            nsc = nsa_static()
            bcs = np.where(nsc["bc_mask"][:, :, None, :], rb[nsc["bc_idx"]][:, :, :, :][..., order].transpose(0, 1, 3, 2), NEG)
            m["nsa_bc"] = np.ascontiguousarray(bcs.astype(np.float32))
            m["nsa_tbs"] = np.where(nsc["tbs_mask"], rb[nsc["tbs_idx"], j], NEG).astype(np.float32)
            m["nsa_tbw"] = np.where(nsc["tbw_mask"], rb[nsc["tbw_idx"], j], NEG).astype(np.float32)
            m["nsa_keep"] = nsc["keep"]; m["nsa_add"] = nsc["add"]; m["nsa_E"] = nsc["E"]; m["nsa_ov"] = nsc["ov"]
        maps.append(m)
    return maps


_NSC = {}


def t5_bucket_static(dist):
    import math
    import jax
    import jax.numpy as jnp
    with jax.default_device(jax.devices("cpu")[0]):
        n = jnp.maximum(jnp.asarray(dist, jnp.int32), 0)
        nf = jnp.maximum(n, 1).astype(jnp.float32)
        large = 16 + (jnp.log(nf / 16) / math.log(1024 / 16) * (32 - 16)).astype(jnp.int32)
        large = jnp.minimum(large, 31)
        return np.asarray(jnp.where(n < 16, n, large))


def nsa_static():
    if _NSC:
        return _NSC
    p = np.arange(128)
    col = np.arange(512)
    q = np.arange(T_)
    n = (np.arange(2)[:, None] * 128 + p[None, :])
    d = q[None, None, :] - (16 * n[:, :, None] + 31)
    msk = (d >= 0) & (n[:, :, None] < 255)
    _NSC["bc_idx"] = t5_bucket_static(d).transpose(1, 0, 2)
    _NSC["bc_mask"] = msk.transpose(1, 0, 2)
    ms = np.arange(12) - 3
    d = 128 * ms[None, :, None] + col[None, None, :] - p[:, None, None]
    _NSC["tbs_idx"] = t5_bucket_static(d); _NSC["tbs_mask"] = d >= 0
    mw = np.arange(8) - 3
    d = 128 * mw[None, :, None] + col[None, None, :] - p[:, None, None]
    _NSC["tbw_idx"] = t5_bucket_static(d); _NSC["tbw_mask"] = (d >= 0) & (d < 512)
    qpos = np.arange(NT)[None, :, None] * 128 + p[:, None, None]
    cur = qpos // 64
    jj = np.arange(64)[None, None, :]
    forced = (jj == 0) | (jj == cur) | (jj == cur - 1)
    fut = jj > cur
    _NSC["keep"] = np.where(forced | fut, 0.0, 1.0).astype(np.float32)
    _NSC["add"] = np.where(fut, -1.0, np.where(forced, 1.0e6, 0.0)).astype(np.float32)
    _NSC["E"] = (np.arange(T_)[None, :] // 64 == np.arange(64)[:, None]).astype(np.float32)
    nn = np.arange(256)
    cst, cen = nn * 16, nn * 16 + 31
    sst, sen = np.arange(64) * 64, np.arange(64) * 64 + 63
    ovl = ((cst[:, None] <= sen[None, :]) & (cen[:, None] >= sst[None, :]) & (nn[:, None] < 255)).astype(np.float32)
    _NSC["ov"] = np.ascontiguousarray(ovl.reshape(2, 128, 64).transpose(1, 0, 2))
    return _NSC


def run_C(h1T_sh, yT_sh, inp, l, final):
    ong = np.ones((D,), np.float32)
    ong[0:512] = inp["out_norm_g"][l][0]
    ong[1024:1536] = inp["out_norm_g"][l][1]
    ong[1536:2048] = inp["out_norm_g"][l][2]
    common = {"ong": col16(ong), "wout": inp["w_out"][l], "g1": col16(inp["ffn2_norm_g"][l]),
              "wg": inp["ffn2_w_gate"][l], "wu": inp["ffn2_w_up"][l], "wd": inp["ffn2_w_down"][l]}
    if final:
        common["gf"] = col16(inp["final_norm_g"])
    nc = prog("CF" if final else "C", lambda: build_C(final))
    res = run(nc, [dict(common, hT=h1T_sh[c], yT=yT_sh[c]) for c in range(8)])
    return [r["oT"] for r in res]


def run_mixers(zT_sh, inp, l):
    z = np.concatenate([a.T for a in zT_sh], axis=0).reshape(2, T_, DIN)
    YT = np.zeros((2, D, T_), np.float32)
    for gi, nm in enumerate(("fox", "gdn", "lru", "nsa")):
        nc = prog("B_" + nm, lambda nm=nm: build_B([nm]))
        res = run(nc, prep_B(z, inp, l, [nm]))
        for c in range(8):
            b, j = c // 4, c % 4
            YT[b, gi * 512 + j * 128:gi * 512 + (j + 1) * 128, :] = res[c]["y_" + nm]
    return [np.ascontiguousarray(YT[c // 4][:, (c % 4) * TOK:(c % 4 + 1) * TOK]) for c in range(8)]


def kernel(**inputs):
    inp = {k_: np.asarray(v, np.float32) for k_, v in inputs.items()}
    x = inp["x"].reshape(8 * TOK, D)
    hT = [np.ascontiguousarray(x[c * TOK:(c + 1) * TOK].T) for c in range(8)]
    for l in range(2):
        h1T, zT = run_A(hT, inp, l)
        yT = run_mixers(zT, inp, l)
        hT = run_C(h1T, yT, inp, l, final=(l == 1))
    out = np.concatenate([a.T for a in hT], axis=0).reshape(2, T_, D)
    return np.ascontiguousarray(out.astype(np.float32))
```

```python
import numpy as np
import ml_dtypes
from contextlib import ExitStack
import concourse.bass as bass
import concourse.mybir as mybir
from concourse.bass_utils import run_bass_kernel_spmd

F32 = mybir.dt.float32
BF16 = mybir.dt.bfloat16
I32 = mybir.dt.int32
SP_POOL = [mybir.EngineType.SP, mybir.EngineType.Pool]
AF = mybir.ActivationFunctionType
ALU = mybir.AluOpType
AX = mybir.AxisListType

D = 2048
NCH = 16
DFF = 5632
NF = 44
TOK = 1024
TG = 512
DIN = 5912
NZC = 47
EPS = 1e-6
NEG = -30000.0


class St:
    __slots__ = ("w", "r")

    def __init__(self, w=None, r=None):
        self.w = w
        self.r = list(r) if r else []

    def copy(self):
        return St(self.w, self.r)


class T:
    def __init__(self, handle, name):
        self.h = handle
        self.name = name
        self.whole = St()
        self.cells = {}

    def __getitem__(self, idx):
        return self.h[idx]

    def states(self, key):
        if key is None:
            return [self.whole] + list(self.cells.values())
        if key not in self.cells:
            self.cells[key] = self.whole.copy()
        return [self.cells[key]]


class K:
    NSLOT = 6

    def __init__(self, nc, es):
        self.nc = nc
        self.es = es
        self.eng = {"pe": nc.tensor, "dve": nc.vector, "act": nc.scalar, "pool": nc.gpsimd, "sp": nc.sync}
        self.sem = {}
        self.cnt = {}
        for e in self.eng:
            self.sem[e] = es.enter_context(nc.semaphore("s_" + e))
            self.cnt[e] = 0
        self.known = {e: {} for e in self.eng}
        self.dsem = {}
        self.duse = {}
        self.dnext = {}
        for q in ("sp", "pool"):
            self.dnext[q] = 0
            for s in range(self.NSLOT):
                key = ("d", q, s)
                self.dsem[key] = es.enter_context(nc.semaphore("d_%s_%d" % (q, s)))
                self.duse[key] = 0
        self.ntile = 0
        self.dins = {}

    def sb(self, shape, dt, name=None):
        self.ntile += 1
        name = "%s_%d" % (name or "t", self.ntile)
        h = self.es.enter_context(self.nc.sbuf_tensor(name, list(shape), dt))
        return T(h, name)

    def ps(self, shape, dt, name=None):
        self.ntile += 1
        name = "%s_%d" % (name or "p", self.ntile)
        h = self.es.enter_context(self.nc.psum_tensor(name, list(shape), dt))
        return T(h, name)

    def din(self, name, shape, dt=F32):
        if name not in self.dins:
            self.dins[name] = T(self.nc.dram_tensor(name, list(shape), dt, kind="ExternalInput").ap(), name)
        return self.dins[name]

    def barrier(self):
        for e in self.eng:
            self.wait_all(e)

    def scope(self):
        return _Scope(self)

    def dout(self, name, shape, dt=F32):
        return T(self.nc.dram_tensor(name, list(shape), dt, kind="ExternalOutput").ap(), name)

    def semh(self, key):
        return self.sem[key] if key in self.sem else self.dsem[key]

    def _deps(self, reads, writes):
        deps = set()
        for (t, key) in reads:
            for st in t.states(key):
                if st.w is not None:
                    deps.add(st.w)
        for (t, key) in writes:
            for st in t.states(key):
                if st.w is not None:
                    deps.add(st.w)
                deps.update(st.r)
        return deps

    def _wait(self, eng, deps):
        need = {}
        kn = self.known[eng]
        for (sk, val) in deps:
            if sk == "pe" and eng == "pe":
                continue
            if kn.get(sk, 0) < val and need.get(sk, 0) < val:
                need[sk] = val
        for sk, val in need.items():
            self.eng[eng].wait_ge(self.semh(sk), val)
            kn[sk] = val

    def _commit(self, ev, reads, writes):
        for (t, key) in reads:
            for st in t.states(key):
                st.r.append(ev)
        for (t, key) in writes:
            if key is None:
                t.cells.clear()
                t.whole.w = ev
                t.whole.r = []
            else:
                st = t.states(key)[0]
                st.w = ev
                st.r = []

    @staticmethod
    def _norm(lst):
        out = []
        for x in lst or []:
            out.append(x if isinstance(x, tuple) else (x, None))
        return out

    def op(self, eng, fn, reads=None, writes=None):
        reads = self._norm(reads)
        writes = self._norm(writes)
        self._wait(eng, self._deps(reads, writes))
        ins = fn()
        self.cnt[eng] += 1
        ins.then_inc(self.sem[eng], 1)
        ev = (eng, self.cnt[eng])
        self._commit(ev, reads, writes)
        return ev

    def dma(self, q, out_ap, in_ap, reads=None, writes=None, **kw):
        reads = self._norm(reads)
        writes = self._norm(writes)
        deps = self._deps(reads, writes)
        s = self.dnext[q]
        self.dnext[q] = (s + 1) % self.NSLOT
        key = ("d", q, s)
        if self.duse[key] > 0:
            deps.add((key, 16 * self.duse[key]))
        self._wait(q, deps)
        ins = self.eng[q].dma_start(out=out_ap, in_=in_ap, **kw)
        self.duse[key] += 1
        ins.then_inc(self.dsem[key], 16)
        ev = (key, 16 * self.duse[key])
        self._commit(ev, reads, writes)
        return ev

    def wait_all(self, eng="sp"):
        kn = self.known[eng]
        for e in self.eng:
            if self.cnt[e] > kn.get(e, 0):
                self.eng[eng].wait_ge(self.sem[e], self.cnt[e])
                kn[e] = self.cnt[e]
        for key, n in self.duse.items():
            if 16 * n > kn.get(key, 0):
                self.eng[eng].wait_ge(self.dsem[key], 16 * n)
                kn[key] = 16 * n


class _Scope:
    def __init__(self, k):
        self.k = k

    def __enter__(self):
        self.old = self.k.es
        self.sub = ExitStack()
        self.sub.__enter__()
        self.k.es = self.sub
        return self

    def __exit__(self, *a):
        self.k.barrier()
        self.k.es = self.old
        return self.sub.__exit__(*a)


class Dense:
    def __init__(self, k, h):
        self.k = k
        nc = k.nc
        self.nc = nc
        self.h = h
        self.hn = k.sb([128, NCH, TOK], BF16, "hn")
        self.sq = [k.sb([128, TG], BF16, "sq%d" % i) for i in range(2)]
        self.rstd = k.sb([128, TG], F32, "rstd")
        self.onesm = k.sb([128, 128], BF16, "onesm")
        self.ones4 = k.sb([128, 128], BF16, "ones4")
        self.gcol = k.sb([128, NCH], F32, "gcol")
        self.wg = [k.sb([128, NCH, 256], BF16, "wg%d" % i) for i in range(2)]
        self.wu = [k.sb([128, NCH, 256], BF16, "wu%d" % i) for i in range(2)]
        self.wd = [k.sb([128, 2, D], BF16, "wd%d" % i) for i in range(2)]
        self.act = [k.sb([128, 2, TOK], BF16, "act%d" % i) for i in range(2)]
        self.sg = [k.sb([128, TG], F32, "sg%d" % i) for i in range(2)]
        self.psA = [k.ps([128, TG], F32, "psA%d" % i) for i in range(4)]
        self.psB = [k.ps([128, TG], F32, "psB%d" % i) for i in range(3)]
        self.psN = k.ps([128, TG], F32, "psN")
        self.ia = 0
        self.ib = 0
        self.epsc = k.sb([128, 1], F32, "epsc")
        k.op("dve", lambda: nc.vector.memset(self.epsc[:], EPS), writes=[self.epsc])
        k.op("dve", lambda: nc.vector.memset(self.onesm[:], 1.0 / D), writes=[self.onesm])
        k.op("dve", lambda: nc.vector.memset(self.ones4[:], 1.0 / 512), writes=[self.ones4])

    def rstd_from(self, ps):
        k, nc = self.k, self.nc
        k.op("act", lambda: nc.scalar.activation(out=self.rstd[:], in_=ps[:], func=AF.Ln, bias=self.epsc[:]),
             reads=[ps, self.epsc], writes=[self.rstd])
        k.op("act", lambda: nc.scalar.activation(out=self.rstd[:], in_=self.rstd[:], func=AF.Exp, scale=-0.5),
             reads=[self.rstd], writes=[self.rstd])

    def load_h(self, src):
        k = self.k
        v = src.h.rearrange("(c p) t -> p c t", p=128)
        for c in range(0, NCH, 4):
            k.dma("sp", self.h[:, c:c + 4, :], v[:, c:c + 4, :], reads=[src],
                  writes=[(self.h, (cc, tg)) for cc in range(c, c + 4) for tg in range(2)])

    def store_h(self, dst):
        k = self.k
        v = dst.h.rearrange("(c p) t -> p c t", p=128)
        for c in range(0, NCH, 4):
            k.dma("sp", v[:, c:c + 4, :], self.h[:, c:c + 4, :],
                  reads=[(self.h, (cc, tg)) for cc in range(c, c + 4) for tg in range(2)], writes=[(dst, c)])

    def rmsnorm(self, gsrc, out_t=None, out_f32=None):
        k, nc = self.k, self.nc
        k.dma("sp", self.gcol[:], gsrc.h, reads=[gsrc], writes=[self.gcol])
        for tg in range(2):
            ts = slice(tg * TG, (tg + 1) * TG)
            for c in range(NCH):
                sq = self.sq[c % 2]
                k.op("act", lambda sq=sq, c=c: nc.scalar.activation(out=sq[:], in_=self.h[:, c, ts], func=AF.Square),
                     reads=[(self.h, (c, tg))], writes=[sq])
                k.op("pe", lambda sq=sq, c=c: nc.tensor.matmul(self.psN[:], lhsT=self.onesm[:], rhs=sq[:],
                                                               start=(c == 0), stop=(c == NCH - 1)),
                     reads=[sq, self.onesm], writes=[self.psN])
            self.rstd_from(self.psN)
            for c in range(NCH):
                if out_f32 is None:
                    k.op("dve", lambda c=c: nc.vector.scalar_tensor_tensor(
                        out=self.hn[:, c, ts], in0=self.h[:, c, ts], scalar=self.gcol[:, c:c + 1], in1=self.rstd[:],
                        op0=ALU.mult, op1=ALU.mult),
                        reads=[(self.h, (c, tg)), self.gcol, self.rstd], writes=[(self.hn, tg)])
                else:
                    k.op("dve", lambda c=c: nc.vector.scalar_tensor_tensor(
                        out=out_f32[:, c, ts], in0=self.h[:, c, ts], scalar=self.gcol[:, c:c + 1], in1=self.rstd[:],
                        op0=ALU.mult, op1=ALU.mult),
                        reads=[(self.h, (c, tg)), self.gcol, self.rstd], writes=[(out_f32, (c, tg))])

    def ffn(self, wg_d, wu_d, wd_d):
        k, nc = self.k, self.nc
        wgv = wg_d.h.rearrange("(c p) f -> p c f", p=128)
        wuv = wu_d.h.rearrange("(c p) f -> p c f", p=128)
        wdv = wd_d.h.rearrange("(c p) d -> p c d", p=128)
        NG = NF // 2

        def load(g):
            b = g % 2
            k.dma("pool", self.wg[b][:], wgv[:, :, g * 256:(g + 1) * 256], reads=[wg_d], writes=[self.wg[b]])
            k.dma("pool", self.wu[b][:], wuv[:, :, g * 256:(g + 1) * 256], reads=[wu_d], writes=[self.wu[b]])
            k.dma("pool", self.wd[b][:], wdv[:, 2 * g:2 * g + 2, :], reads=[wd_d], writes=[self.wd[b]])

        load(0)
        for g in range(NG):
            if g + 1 < NG:
                load(g + 1)
            b = g % 2
            wg, wu, wd, act = self.wg[b], self.wu[b], self.wd[b], self.act[b]
            for fcl in range(2):
                fs = slice(fcl * 128, (fcl + 1) * 128)
                for tg in range(2):
                    ts = slice(tg * TG, (tg + 1) * TG)
                    pg = self.psA[self.ia % 4]
                    pu = self.psA[(self.ia + 1) % 4]
                    self.ia += 2

                    def mm(p, w):
                        ins = None
                        for c in range(NCH):
                            ins = nc.tensor.matmul(p[:], lhsT=w[:, c, fs], rhs=self.hn[:, c, ts],
                                                   start=(c == 0), stop=(c == NCH - 1))
                        return ins
                    k.op("pe", lambda: mm(pg, wg), reads=[wg, (self.hn, tg)], writes=[pg])
                    k.op("pe", lambda: mm(pu, wu), reads=[wu, (self.hn, tg)], writes=[pu])
                    sg = self.sg[tg]
                    k.op("act", lambda: nc.scalar.activation(out=sg[:], in_=pg[:], func=AF.Silu), reads=[pg], writes=[sg])
                    k.op("dve", lambda: nc.vector.tensor_tensor(out=act[:, fcl, ts], in0=pu[:], in1=sg[:], op=ALU.mult),
                         reads=[pu, sg], writes=[(act, (fcl, tg))])
            for dc in range(NCH):
                ds = slice(dc * 128, (dc + 1) * 128)
                for tg in range(2):
                    ts = slice(tg * TG, (tg + 1) * TG)
                    pd = self.psB[self.ib % 3]
                    self.ib += 1

                    def mmd():
                        ins = None
                        for fcl in range(2):
                            ins = nc.tensor.matmul(pd[:], lhsT=wd[:, fcl, ds], rhs=act[:, fcl, ts],
                                                   start=(fcl == 0), stop=(fcl == 1))
                        return ins
                    k.op("pe", mmd, reads=[wd, (act, (0, tg)), (act, (1, tg))], writes=[pd])
                    k.op("dve", lambda: nc.vector.scalar_tensor_tensor(
                        out=self.h[:, dc, ts], in0=pd[:], scalar=0.5, in1=self.h[:, dc, ts], op0=ALU.mult, op1=ALU.add),
                        reads=[pd, (self.h, (dc, tg))], writes=[(self.h, (dc, tg))])

    def proj(self, w_d, ncols, rhs_t, emit):
        k, nc = self.k, self.nc
        wv = w_d.h.rearrange("(c p) f -> p c f", p=128)
        ngr = (ncols + 255) // 256

        def load(g):
            b = g % 2
            c0 = g * 256
            c1 = min(ncols, c0 + 256)
            k.dma("pool", self.wg[b][:, :, 0:c1 - c0], wv[:, :, c0:c1], reads=[w_d], writes=[self.wg[b]])

        load(0)
        for g in range(ngr):
            if g + 1 < ngr:
                load(g + 1)
            w = self.wg[g % 2]
            for ml in range(2):
                m = 2 * g + ml
                M = min(128, ncols - m * 128)
                if M <= 0:
                    continue
                for tg in range(2):
                    ts = slice(tg * TG, (tg + 1) * TG)
                    pd = self.psB[self.ib % 3]
                    self.ib += 1

                    def mm():
                        ins = None
                        for c in range(NCH):
                            ins = nc.tensor.matmul(pd[0:M, :], lhsT=w[:, c, ml * 128:ml * 128 + M], rhs=rhs_t[:, c, ts],
                                                   start=(c == 0), stop=(c == NCH - 1))
                        return ins
                    k.op("pe", mm, reads=[w, (rhs_t, tg)], writes=[pd])
                    emit(m, M, tg, ts, pd)


def dense_A(k, h, l, zsh):
    nc = k.nc
    g1 = k.din("g1a%d" % l, [128, NCH]); g2 = k.din("g2a%d" % l, [128, NCH])
    wg = k.din("f1wg%d" % l, [D, DFF]); wu = k.din("f1wu%d" % l, [D, DFF]); wd = k.din("f1wd%d" % l, [DFF, D])
    win = k.din("win%d" % l, [D, DIN]); bin_ = k.din("bin%d" % l, [128, NZC])
    dn = Dense(k, h)
    bcol = k.sb([128, NZC], F32, "bcol")
    zs = [k.sb([128, TG], F32, "zs%d" % i) for i in range(3)]
    k.dma("sp", bcol[:], bin_.h, reads=[bin_], writes=[bcol])
    dn.rmsnorm(g1)
    dn.ffn(wg, wu, wd)
    dn.rmsnorm(g2)
    cnt = [0]

    def emit(m, M, tg, ts, pd):
        z = zs[cnt[0] % 3]
        cnt[0] += 1
        k.op("act", lambda: nc.scalar.activation(out=z[0:M, :], in_=pd[0:M, :], func=AF.Identity,
                                                 bias=bcol[0:M, m:m + 1]), reads=[pd, bcol], writes=[z])
        k.dma("sp", zsh.h[m * 128:m * 128 + M, ts], z[0:M, :], reads=[z], writes=[(zsh, (m, tg))])
    dn.proj(win, DIN, dn.hn, emit)


def dense_C(k, h, l, yall, rv):
    nc = k.nc
    ong = k.din("ong%d" % l, [128, NCH]); wout = k.din("wout%d" % l, [D, D]); g1 = k.din("g1c%d" % l, [128, NCH])
    wg = k.din("f2wg%d" % l, [D, DFF]); wu = k.din("f2wu%d" % l, [D, DFF]); wd = k.din("f2wd%d" % l, [DFF, D])
    dn = Dense(k, h)
    ys = [k.sb([128, 4, TG], F32, "ys%d" % i) for i in range(2)]
    ocol = k.sb([128, NCH], F32, "ocol")
    k.dma("sp", ocol[:], ong.h, reads=[ong], writes=[ocol])
    yst = [k.sb([128, 4, TG], F32, "yst%d" % i) for i in range(2)]
    mskc = k.sb([128, 16], F32, "mskc")
    dmsk = k.din("msk", [128, 16])
    k.dma("sp", mskc[:], dmsk.h, reads=[dmsk], writes=[mskc])
    i = 0
    for grp in range(4):
        for tg in range(2):
            ts = slice(tg * TG, (tg + 1) * TG)
            y = ys[i % 2]
            i += 1
            for rc in range(4):
                st_ = yst[rc % 2]
                for jp in range(4):
                    for (ct, sap_, d0, cnt) in yall.pieces(jp, grp * 128, 128):
                        k.dma("sp", st_[d0:d0 + cnt, jp, :], sap_[:, rc * TOK + tg * TG:rc * TOK + (tg + 1) * TG], reads=[ct], writes=[st_])
                mc = mskc[:, rc:rc + 1]
                if rc == 0:
                    k.op("dve", lambda: nc.vector.tensor_scalar(out=y[:], in0=st_[:], scalar1=mc, scalar2=None, op0=ALU.mult), reads=[st_, mskc], writes=[y])
                else:
                    k.op("dve", lambda: nc.vector.scalar_tensor_tensor(out=y[:], in0=st_[:], scalar=mc, in1=y[:], op0=ALU.mult, op1=ALU.add),
                         reads=[st_, mskc, y], writes=[y])
            if grp == 1:
                for cl in range(4):
                    k.op("dve", lambda cl=cl: nc.vector.tensor_copy(out=dn.hn[:, 4 + cl, ts], in_=y[:, cl, :]),
                         reads=[y], writes=[(dn.hn, tg)])
                continue
            for cl in range(4):
                sq = dn.sq[cl % 2]
                k.op("act", lambda sq=sq, cl=cl: nc.scalar.activation(out=sq[:], in_=y[:, cl, :], func=AF.Square),
                     reads=[y], writes=[sq])
                k.op("pe", lambda sq=sq, cl=cl: nc.tensor.matmul(dn.psN[:], lhsT=dn.ones4[:], rhs=sq[:],
                                                                 start=(cl == 0), stop=(cl == 3)),
                     reads=[sq, dn.ones4], writes=[dn.psN])
            dn.rstd_from(dn.psN)
            for cl in range(4):
                c = grp * 4 + cl
                k.op("dve", lambda cl=cl, c=c: nc.vector.scalar_tensor_tensor(
                    out=dn.hn[:, c, ts], in0=y[:, cl, :], scalar=ocol[:, c:c + 1], in1=dn.rstd[:],
                    op0=ALU.mult, op1=ALU.mult), reads=[y, ocol, dn.rstd], writes=[(dn.hn, tg)])

    def emit(m, M, tg, ts, pd):
        k.op("dve", lambda: nc.vector.tensor_tensor(out=h[:, m, ts], in0=pd[:], in1=h[:, m, ts], op=ALU.add),
             reads=[pd, (h, (m, tg))], writes=[(h, (m, tg))])
    dn.proj(wout, D, dn.hn, emit)
    dn.rmsnorm(g1)
    dn.ffn(wg, wu, wd)


def final_out(k, h, out):
    nc = k.nc
    sq_ = [k.sb([128, TG], BF16, "sq%d" % i) for i in range(2)]
    rstd = k.sb([128, TG], F32, "rstd")
    onesm = k.sb([128, 128], BF16, "onesm")
    gcol = k.sb([128, NCH], F32, "gcol")
    epsc = k.sb([128, 1], F32, "epsc")
    psN = k.ps([128, TG], F32, "psN")
    psB = [k.ps([128, TG], F32, "psB%d" % i) for i in range(3)]
    ib = [0]
    k.op("dve", lambda: nc.vector.memset(epsc[:], EPS), writes=[epsc])
    k.op("dve", lambda: nc.vector.memset(onesm[:], 1.0 / D), writes=[onesm])

    def rstd_from():
        k.op("act", lambda: nc.scalar.activation(out=rstd[:], in_=psN[:], func=AF.Ln, bias=epsc[:]), reads=[psN, epsc], writes=[rstd])
        k.op("act", lambda: nc.scalar.activation(out=rstd[:], in_=rstd[:], func=AF.Exp, scale=-0.5), reads=[rstd], writes=[rstd])
    gf = k.din("gf", [128, NCH])
    identF = k.sb([128, 128], F32, "identF")
    cid = k.din("c_ident", [128, 128])
    k.dma("sp", identF[:], cid.h, reads=[cid], writes=[identF])
    fin = k.sb([128, NCH, TG], F32, "fin")
    ot = [k.sb([128, D], F32, "ot%d" % i) for i in range(2)]
    k.dma("sp", gcol[:], gf.h, reads=[gf], writes=[gcol])
    for tg in range(2):
        ts = slice(tg * TG, (tg + 1) * TG)
        for c in range(NCH):
            sq = sq_[c % 2]
            k.op("act", lambda sq=sq, c=c: nc.scalar.activation(out=sq[:], in_=h[:, c, ts], func=AF.Square),
                 reads=[(h, (c, tg))], writes=[sq])
            k.op("pe", lambda sq=sq, c=c: nc.tensor.matmul(psN[:], lhsT=onesm[:], rhs=sq[:],
                                                           start=(c == 0), stop=(c == NCH - 1)),
                 reads=[sq, onesm], writes=[psN])
        rstd_from()
        for c in range(NCH):
            k.op("dve", lambda c=c: nc.vector.scalar_tensor_tensor(
                out=fin[:, c, :], in0=h[:, c, ts], scalar=gcol[:, c:c + 1], in1=rstd[:],
                op0=ALU.mult, op1=ALU.mult), reads=[(h, (c, tg)), gcol, rstd], writes=[(fin, c)])
        for tt in range(4):
            o = ot[tt % 2]
            for c4 in range(4):
                pd = psB[ib[0] % 3]
                ib[0] += 1

                def mmT():
                    ins = None
                    for cl in range(4):
                        c = c4 * 4 + cl
                        ins = nc.tensor.matmul(pd[:, cl * 128:(cl + 1) * 128], lhsT=fin[:, c, tt * 128:(tt + 1) * 128],
                                               rhs=identF[:], start=True, stop=True)
                    return ins
                k.op("pe", mmT, reads=[identF] + [(fin, c4 * 4 + cl) for cl in range(4)], writes=[pd])
                k.op("act", lambda: nc.scalar.copy(out=o[:, c4 * 512:(c4 + 1) * 512], in_=pd[:]), reads=[pd], writes=[(o, c4)])
            r0 = tg * TG + tt * 128
            k.dma("sp", out.h[r0:r0 + 128, :], o[:], reads=[o], writes=[(out, r0)])


def load_x(k, h, x):
    nc = k.nc
    identF = k.sb([128, 128], F32, "identF")
    cid = k.din("c_ident", [128, 128])
    k.dma("sp", identF[:], cid.h, reads=[cid], writes=[identF])
    xt = [k.sb([128, D], F32, "xt%d" % i) for i in range(4)]
    pst = [k.ps([128, TG], F32, "pst%d" % i) for i in range(4)]
    n = 0
    for tg in range(2):
        for tt in range(4):
            r0 = tg * TG + tt * 128
            k.dma("sp", xt[tt][:], x.h[r0:r0 + 128, :], reads=[x], writes=[xt[tt]])
        for c in range(NCH):
            pd = pst[n % 4]
            n += 1

            def mmT():
                ins = None
                for tt in range(4):
                    ins = nc.tensor.matmul(pd[:, tt * 128:(tt + 1) * 128], lhsT=xt[tt][:, c * 128:(c + 1) * 128], rhs=identF[:],
                                           start=True, stop=True)
                return ins
            k.op("pe", mmT, reads=[identF] + xt, writes=[pd])
            eng = "act" if c % 2 else "dve"
            if eng == "act":
                k.op("act", lambda: nc.scalar.copy(out=h[:, c, tg * TG:(tg + 1) * TG], in_=pd[:]), reads=[pd], writes=[(h, (c, tg))])
            else:
                k.op("dve", lambda: nc.vector.tensor_copy(h[:, c, tg * TG:(tg + 1) * TG], pd[:]), reads=[pd], writes=[(h, (c, tg))])


def col16(g):
    return np.ascontiguousarray(np.asarray(g, np.float32).reshape(NCH, 128).T)


T_ = 4096
NT = 32
SCALE = 128 ** -0.5


class Mix:
    def __init__(self, k, l, zall, vals, nbig=7, nbigb=4):
        self.k = k
        self.l = l
        self.zall = zall
        nc = self.nc = k.nc
        self.cident = k.din("c_ident", [128, 128])
        self.msk = k.sb([128, 16], F32, "msk")
        dmsk = k.din("msk", [128, 16])
        k.dma("sp", self.msk[:], dmsk.h, reads=[dmsk], writes=[self.msk])
        self.identF = k.sb([128, 128], F32, "identF")
        self.identB = k.sb([128, 128], BF16, "identB")
        self.onesF = k.sb([128, 128], F32, "onesF")
        self.onesB = k.sb([128, 128], BF16, "onesB")
        k.dma("sp", self.identF[:], self.cident.h, reads=[self.cident], writes=[self.identF])
        k.op("dve", lambda: nc.vector.tensor_copy(self.identB[:], self.identF[:]), reads=[self.identF], writes=[self.identB])
        k.op("dve", lambda: nc.vector.memset(self.onesF[:], 1.0), writes=[self.onesF])
        k.op("dve", lambda: nc.vector.memset(self.onesB[:], 1.0), writes=[self.onesB])
        self.big = [k.sb([128, T_], F32, "big%d" % i) for i in range(nbig)]
        self.bigb = [k.sb([128, T_], BF16, "bigb%d" % i) for i in range(nbigb)]
        self.psS = [k.ps([128, 512], F32, "psS%d" % i) for i in range(3)]
        self.psO = [k.ps([128, 512], F32, "psO%d" % i) for i in range(2)]
        self.psL = [k.ps([128, 512], F32, "psL%d" % i) for i in range(2)]
        self.psX = k.ps([128, 512], F32, "psX")
        self.iS = 0
        self.pT = [k.sb([128, 512], BF16, "pT%d" % i) for i in range(3)]
        self.tmpA = [k.sb([128, 512], F32, "tmpA%d" % i) for i in range(4)]

    def zfm(self, dst_t, dst_ap, off, dyn=None, q="sp", n=128, key=None, mul=128, stage=None):
        k, nc = self.k, self.nc
        dap = dst_ap[:] if not hasattr(dst_ap, "ap") else dst_ap
        if dyn is None:
            for s_ in range(4):
                for (ct, sap_, d0, cnt) in self.zall.pieces(s_, off, n):
                    k.dma(q, dap[d0:d0 + cnt, s_ * TOK:(s_ + 1) * TOK], sap_, reads=[ct], writes=[(dst_t, key)])
            return
        sap = stage[:]
        for jc in range(4):
            for s_ in range(4):
                for (ct, sap_, d0, cnt) in self.zall.pieces(s_, off + jc * mul, n):
                    k.dma(q, sap[d0:d0 + cnt, s_ * TOK:(s_ + 1) * TOK], sap_, reads=[ct], writes=[stage])
            mc = self.msk[0:n, dyn + jc:dyn + jc + 1]
            if jc == 0:
                k.op("dve", lambda: nc.vector.tensor_scalar(out=dap[0:n, :], in0=sap[0:n, :], scalar1=mc, scalar2=None, op0=ALU.mult),
                     reads=[stage, self.msk], writes=[(dst_t, key)])
            else:
                k.op("dve", lambda: nc.vector.scalar_tensor_tensor(out=dap[0:n, :], in0=sap[0:n, :], scalar=mc, in1=dap[0:n, :], op0=ALU.mult, op1=ALU.add),
                     reads=[stage, self.msk, (dst_t, key)], writes=[(dst_t, key)])

    def row2col(self, row, dst):
        k, nc = self.k, self.nc
        px = self.psX

        def mm():
            ins = None
            for kt in range(NT):
                ins = nc.tensor.matmul(px[:, kt:kt + 1], lhsT=row[0:1, kt * 128:(kt + 1) * 128], rhs=self.onesF[0:1, 0:1], start=True, stop=True)
            return ins
        k.op("pe", mm, reads=[row, self.onesF], writes=[px])
        k.op("dve", lambda: nc.vector.tensor_copy(dst[:], px[:, 0:NT]), reads=[px], writes=[dst])

    def fm2tm(self, src_ap, src_dep, dst_t, dst_ap, key=None):
        k, nc = self.k, self.nc
        for g4 in range(NT // 4):
            ps = self.psS[g4 % 3]

            def mm():
                ins = None
                for i in range(4):
                    kt = g4 * 4 + i
                    ins = nc.tensor.matmul(ps[:, i * 128:(i + 1) * 128], lhsT=src_ap[:, kt * 128:(kt + 1) * 128], rhs=self.identB[:], start=True, stop=True)
                return ins
            k.op("pe", mm, reads=[src_dep, self.identB], writes=[ps])
            if g4 % 2:
                k.op("act", lambda: nc.scalar.copy(out=dst_ap[:, g4 * 512:(g4 + 1) * 512], in_=ps[:]), reads=[ps], writes=[(dst_t, key)])
            else:
                k.op("dve", lambda: nc.vector.tensor_copy(dst_ap[:, g4 * 512:(g4 + 1) * 512], ps[:]), reads=[ps], writes=[(dst_t, key)])

    def ld(self, dst, src_t, src_ap=None, q="sp", dst_ap=None):
        self.k.dma(q, dst[:] if dst_ap is None else dst_ap, src_t.h if src_ap is None else src_ap, reads=[src_t], writes=[dst])

    def lru(self, yout):
        k, nc = self.k, self.nc
        L_ = "%d" % self.l
        cw = k.din("lru_cw" + L_, [128, 4]); cb = k.din("lru_cb" + L_, [128, 1])
        wa = k.din("lru_wa" + L_, [128, 128]); wx = k.din("lru_wx" + L_, [128, 128])
        ba = k.din("lru_ba" + L_, [128, 1]); bx = k.din("lru_bx" + L_, [128, 1]); lam = k.din("lru_lam" + L_, [128, 1])
        xs, xc, aa, uu, gg, tt = self.big[0:6]
        hs = gg
        xcb = self.bigb[0]
        cws = k.sb([128, 4], F32, "l_cw"); cbs = k.sb([128, 1], F32, "l_cb")
        was = k.sb([128, 128], BF16, "l_wa"); wxs = k.sb([128, 128], BF16, "l_wx")
        bas = k.sb([128, 1], F32, "l_ba"); bxs = k.sb([128, 1], F32, "l_bx"); lams = k.sb([128, 1], F32, "l_lam")
        nsp = k.sb([128, 1], F32, "l_nsp")
        self.zfm(xs, xs.h, _OFF["lru_x"], 0, stage=tt); self.zfm(gg, gg.h, _OFF["lru_gate"], 0, stage=tt); self.ld(cws, cw); self.ld(cbs, cb); self.ld(bas, ba); self.ld(bxs, bx); self.ld(lams, lam)
        self.ld(was, wa, q="pool"); self.ld(wxs, wx, q="pool")
        k.op("act", lambda: nc.scalar.activation(out=nsp[:], in_=lams[:], func=AF.Exp, scale=-1.0), reads=[lams], writes=[nsp])
        k.op("act", lambda: nc.scalar.activation(out=nsp[:], in_=nsp[:], func=AF.Ln, bias=self.onesF[:, 0:1]),
             reads=[nsp, self.onesF], writes=[nsp])
        k.op("dve", lambda: nc.vector.tensor_scalar(out=nsp[:], in0=nsp[:], scalar1=-8.0, scalar2=None, op0=ALU.mult),
             reads=[nsp], writes=[nsp])
        k.op("dve", lambda: nc.vector.tensor_scalar(out=xc[:], in0=xs[:], scalar1=cws[:, 3:4], scalar2=cbs[:, 0:1],
                                                    op0=ALU.mult, op1=ALU.add), reads=[xs, cws, cbs], writes=[xc])
        for i in range(3):
            sh = 3 - i
            k.op("dve", lambda i=i, sh=sh: nc.vector.scalar_tensor_tensor(
                out=xc[:, sh:], in0=xs[:, 0:T_ - sh], scalar=cws[:, i:i + 1], in1=xc[:, sh:], op0=ALU.mult, op1=ALU.add),
                reads=[xs, cws, xc], writes=[xc])
        k.op("act", lambda: nc.scalar.copy(out=xcb[:], in_=xc[:]), reads=[xc], writes=[xcb])
        for tb in range(8):
            ts = slice(tb * 512, (tb + 1) * 512)
            pr = self.psS[0]; pi = self.psS[1]
            k.op("pe", lambda: nc.tensor.matmul(pr[:], lhsT=was[:], rhs=xcb[:, ts], start=True, stop=True), reads=[was, xcb], writes=[pr])
            k.op("pe", lambda: nc.tensor.matmul(pi[:], lhsT=wxs[:], rhs=xcb[:, ts], start=True, stop=True), reads=[wxs, xcb], writes=[pi])
            r = self.tmpA[0]; ii = self.tmpA[1]
            k.op("act", lambda: nc.scalar.activation(out=r[:], in_=pr[:], func=AF.Sigmoid, bias=bas[:, 0:1]), reads=[pr, bas], writes=[r])
            k.op("act", lambda: nc.scalar.activation(out=ii[:], in_=pi[:], func=AF.Sigmoid, bias=bxs[:, 0:1]), reads=[pi, bxs], writes=[ii])
            k.op("act", lambda: nc.scalar.activation(out=aa[:, ts], in_=r[:], func=AF.Exp, scale=nsp[:, 0:1]),
                 reads=[r, nsp], writes=[(aa, tb)])
            t1 = self.tmpA[2]
            k.op("dve", lambda: nc.vector.tensor_tensor(out=t1[:], in0=aa[:, ts], in1=aa[:, ts], op=ALU.mult), reads=[(aa, tb)], writes=[t1])
            k.op("dve", lambda: nc.vector.tensor_scalar(out=t1[:], in0=t1[:], scalar1=-1.0, scalar2=1.0, op0=ALU.mult, op1=ALU.add),
                 reads=[t1], writes=[t1])
            k.op("act", lambda: nc.scalar.activation(out=t1[:], in_=t1[:], func=AF.Sqrt), reads=[t1], writes=[t1])
            k.op("dve", lambda: nc.vector.tensor_tensor(out=ii[:], in0=ii[:], in1=xc[:, ts], op=ALU.mult), reads=[ii, xc], writes=[ii])
            k.op("dve", lambda: nc.vector.tensor_tensor(out=uu[:, ts], in0=ii[:], in1=t1[:], op=ALU.mult), reads=[ii, t1], writes=[(uu, tb)])
        self.gelu_tanh(tt, gg, xs)
        k.op("dve", lambda: nc.vector.tensor_tensor_scan(out=hs[:], data0=aa[:], data1=uu[:], initial=0.0, op0=ALU.mult, op1=ALU.add),
             reads=[aa, uu], writes=[hs])
        k.op("dve", lambda: nc.vector.tensor_tensor(out=hs[:], in0=hs[:], in1=tt[:], op=ALU.mult), reads=[hs, tt], writes=[hs])
        k.dma("sp", yout.h, hs[:], reads=[hs], writes=[yout])

    def gelu_tanh(self, out, x, tmp):
        k, nc = self.k, self.nc
        k.op("dve", lambda: nc.vector.tensor_tensor(out=tmp[:], in0=x[:], in1=x[:], op=ALU.mult), reads=[x], writes=[tmp])
        k.op("dve", lambda: nc.vector.tensor_scalar(out=tmp[:], in0=tmp[:], scalar1=0.044715, scalar2=1.0, op0=ALU.mult, op1=ALU.add),
             reads=[tmp], writes=[tmp])
        k.op("dve", lambda: nc.vector.tensor_tensor(out=tmp[:], in0=tmp[:], in1=x[:], op=ALU.mult), reads=[tmp, x], writes=[tmp])
        k.op("act", lambda: nc.scalar.activation(out=tmp[:], in_=tmp[:], func=AF.Sigmoid, scale=1.5957691216057308),
             reads=[tmp], writes=[tmp])
        k.op("dve", lambda: nc.vector.tensor_tensor(out=out[:], in0=tmp[:], in1=x[:], op=ALU.mult), reads=[tmp, x], writes=[out])

    def attn_unit(self, kT, kt, qT, q0, extra, bias_ap, bias_reads, V, vslice, O, L, first, last, nkeys=128, kT_t=None, qT_t=None):
        k, nc = self.k, self.nc
        S = self.psS[self.iS % 3]
        P = self.pT[self.iS % 3]
        self.iS += 1
        nk = nkeys

        def mm():
            n = len(extra)
            ins = nc.tensor.matmul(S[0:nk, :], lhsT=kT[:, kt * 128:kt * 128 + nk], rhs=qT[:, q0:q0 + 512], start=True, stop=(n == 0))
            for i, (oa, l, r) in enumerate(extra):
                ins = nc.tensor.matmul(oa(S), lhsT=l, rhs=r, start=False, stop=(i == n - 1))
            return ins
        k.op("pe", mm, reads=[kT_t or kT, qT_t or qT] + self._xr, writes=[S])
        if bias_ap is None:
            k.op("act", lambda: nc.scalar.activation(out=P[0:nk, :], in_=S[0:nk, :], func=AF.Exp), reads=[S], writes=[P])
        else:
            k.op("act", lambda: nc.scalar.activation(out=P[0:nk, :], in_=S[0:nk, :], func=AF.Exp, bias=bias_ap),
                 reads=[S] + bias_reads, writes=[P])
        k.op("pe", lambda: nc.tensor.matmul(O[:], lhsT=vslice[0:nk], rhs=P[0:nk, :], start=first, stop=last), reads=[V, P], writes=[O])
        k.op("pe", lambda: nc.tensor.matmul(L[:], lhsT=self.onesB[0:nk, :], rhs=P[0:nk, :], start=first, stop=last),
             reads=[self.onesB, P], writes=[L])

    def fox(self, yout):
        k, nc = self.k, self.nc
        cut = k.din("c_ut", [128, 128]); csu = k.din("c_su", [32, 32]); cmb = k.din("c_mbfox", [128, 4, 512])
        qf = self.big[0]
        qb, kb = self.bigb[0], self.bigb[1]
        vb = self.bigb[2]
        mb = k.sb([128, 4, 512], BF16, "f_mb")
        ut = k.sb([128, 128], F32, "f_ut"); su = k.sb([32, 32], F32, "f_su")
        lf = k.sb([128, NT], F32, "f_lf"); negc = k.sb([128, NT], F32, "f_negc")
        totc = k.sb([32, 1], F32, "f_totc"); am = k.sb([32, 128], F32, "f_am")
        dg = self.big[1]
        frow = k.sb([1, T_], F32, "f_row")
        vT = self.bigb[3]
        frow2 = k.sb([1, T_], F32, "f_row2")
        self.zfm(kb, kb.h, _OFF["fox_k"], 0, q="pool", stage=qb)
        self.zfm(vT, vT.h, _OFF["fox_v"], 0, q="pool", stage=qb)
        self.zfm(qf, qf.h, _OFF["fox_q"], 0, stage=dg)
        self.zfm(frow, frow.h, _OFF["fox_f"], 0, n=1, mul=1, stage=frow2)
        self.fm2tm(vT.h, vT, vb, vb.h)
        k.dma("pool", mb[:], cmb.h, reads=[cmb], writes=[mb])
        self.ld(ut, cut); self.ld(su, csu)
        self.row2col(frow, lf)
        k.op("dve", lambda: nc.vector.tensor_scalar(out=qb[:], in0=qf[:], scalar1=SCALE, scalar2=None, op0=ALU.mult), reads=[qf], writes=[qb])
        k.op("act", lambda: nc.scalar.activation(out=lf[:], in_=lf[:], func=AF.Exp, scale=-1.0), reads=[lf], writes=[lf])
        k.op("act", lambda: nc.scalar.activation(out=lf[:], in_=lf[:], func=AF.Ln, bias=self.onesF[:, 0:1]), reads=[lf, self.onesF], writes=[lf])
        px = self.psX
        k.op("pe", lambda: nc.tensor.matmul(px[0:32, 0:1], lhsT=lf[:], rhs=self.onesF[:, 0:1], start=True, stop=True),
             reads=[lf, self.onesF], writes=[px])
        k.op("dve", lambda: nc.vector.tensor_copy(totc[:], px[0:32, 0:1]), reads=[px], writes=[totc])
        k.op("dve", lambda: nc.vector.tensor_scalar(out=am[:], in0=self.onesF[0:32, :], scalar1=totc[:, 0:1], scalar2=None, op0=ALU.mult),
             reads=[self.onesF, totc], writes=[am])

        def mmc():
            nc.tensor.matmul(px[:, 0:NT], lhsT=ut[:], rhs=lf[:], start=True, stop=False)
            return nc.tensor.matmul(px[:, 0:NT], lhsT=am[:], rhs=su[:], start=False, stop=True)
        k.op("pe", mmc, reads=[ut, lf, am, su], writes=[px])
        k.op("dve", lambda: nc.vector.tensor_copy(negc[:], px[:, 0:NT]), reads=[px], writes=[negc])
        for qt in range(NT):
            k.op("dve", lambda qt=qt: nc.vector.tensor_scalar(out=dg[:, qt * 128:(qt + 1) * 128], in0=self.identF[:],
                                                              scalar1=negc[:, qt:qt + 1], scalar2=-1.0, op0=ALU.mult, op1=ALU.mult),
                 reads=[self.identF, negc], writes=[(dg, qt)])
        for i in range(8):
            q0 = 512 * i
            O = self.psO[i % 2]; L = self.psL[i % 2]
            nk = 4 * i + 4
            for kt in range(nk):
                extra = []
                for jq in range(4):
                    qt = 4 * i + jq
                    extra.append((lambda S, jq=jq: S[:, jq * 128:(jq + 1) * 128], self.onesF[:], dg[:, qt * 128:(qt + 1) * 128]))
                self._xr = [self.onesF] + [(dg, 4 * i + jq) for jq in range(4)]
                if kt >= 4 * i:
                    extra.append((lambda S: S[:], self.identB[:], mb[:, kt - 4 * i, :]))
                    self._xr += [self.identB, mb]
                self.attn_unit(kb, kt, qb, q0, extra, negc[:, kt:kt + 1], [negc], vb, vb[:, kt * 128:(kt + 1) * 128], O, L,
                               kt == 0, kt == nk - 1)
            R = self.tmpA[i % 2]; ob = self.tmpA[2 + i % 2]
            k.op("dve", lambda: nc.vector.reciprocal(out=R[:], in_=L[:]), reads=[L], writes=[R])
            k.op("dve", lambda: nc.vector.tensor_tensor(out=ob[:], in0=O[:], in1=R[:], op=ALU.mult), reads=[O, R], writes=[ob])
            k.dma("sp", yout.h[:, q0:q0 + 512], ob[:], reads=[ob], writes=[(yout, i)])


    def bfv(self, t):
        return t.h[:].bitcast(BF16)

    def nsa(self, yout):
        k, nc = self.k, self.nc
        L_ = "%d" % self.l
        dw1k = k.din("nsa_w1k" + L_, [128, 32, 128]); dw1v = k.din("nsa_w1v" + L_, [128, 32, 128])
        dw2k = k.din("nsa_w2k" + L_, [128, 128]); dw2v = k.din("nsa_w2v" + L_, [128, 128])
        dpek = k.din("nsa_pek" + L_, [128, 32]); dpev = k.din("nsa_pev" + L_, [128, 32])
        dbc = k.din("nsa_bc", [128, 2, 4, T_]); dtbs = k.din("nsa_tbs", [128, 12, 512]); dtbw = k.din("nsa_tbw", [128, 8, 512])
        dkeep = k.din("nsa_keep", [128, NT, 64]); dadd = k.din("nsa_add", [128, NT, 64])
        dE = k.din("nsa_E", [64, T_]); dov = k.din("nsa_ov", [128, 2, 64])
        kcT = k.sb([128, 256], BF16, "n_kcT"); vc = k.sb([128, 2, 128], BF16, "n_vc")
        qown = k.sb([128, T_], BF16, "n_qown")
        ksb = k.sb([128, T_], BF16, "n_ks"); kwb = k.sb([128, T_], BF16, "n_kw")
        vsb = k.sb([128, T_], BF16, "n_vs"); vwb = k.sb([128, T_], BF16, "n_vw")
        grow = k.sb([3, T_], F32, "n_grow")
        with k.scope():
            gstg = k.sb([3, T_], F32, "n_gstg")
            self.zfm(grow, grow.h, _OFF["nsa_g"], 0, n=3, mul=3, stage=gstg)
        gsel = [k.sb([3, 128], F32, "n_gsel%d" % i) for i in range(3)]
        for gi in range(3):
            k.op("dve", lambda gi=gi: nc.vector.tensor_scalar(out=gsel[gi][:], in0=self.onesF[0:3, :], scalar1=self.identF[0:3, gi:gi + 1], scalar2=None, op0=ALU.mult),
                 reads=[self.onesF, self.identF], writes=[gsel[gi]])
        self.zfm(qown, qown.h, _OFF["nsa_q"], 0, q="pool", stage=vsb)
        k.op("dve", lambda: nc.vector.tensor_scalar(out=qown[:], in0=qown[:], scalar1=SCALE, scalar2=None, op0=ALU.mult), reads=[qown], writes=[qown])
        self.zfm(ksb, ksb.h, _OFF["nsa_ks"], None, q="pool")
        self.zfm(kwb, kwb.h, _OFF["nsa_kw"], None, q="pool")
        with k.scope():
            stg = [k.sb([128, T_], BF16, "n_stg%d" % i) for i in range(2)]
            self.zfm(stg[0], stg[0].h, _OFF["nsa_vs"], None, q="pool")
            self.zfm(stg[1], stg[1].h, _OFF["nsa_vw"], None, q="pool")
            self.fm2tm(stg[0].h, stg[0], vsb, vsb.h)
            self.fm2tm(stg[1].h, stg[1], vwb, vwb.h)
        cscope = k.scope()
        cscope.__enter__()
        kcin_t = k.sb([128, T_], BF16, "n_kcin"); vcin_t = k.sb([128, T_], BF16, "n_vcin")
        kcin = kcin_t.h; vcin = vcin_t.h
        self.zfm(kcin_t, kcin, _OFF["nsa_kc"], None, q="pool")
        self.zfm(vcin_t, vcin, _OFF["nsa_vc"], None, q="pool")
        w1 = [k.sb([128, 32, 128], BF16, "n_w1k"), k.sb([128, 32, 128], BF16, "n_w1v")]
        w2 = [k.sb([128, 128], BF16, "n_w2k"), k.sb([128, 128], BF16, "n_w2v")]
        pe = [k.sb([128, 32], BF16, "n_pek"), k.sb([128, 32], BF16, "n_pev")]
        self.ld(w1[0], dw1k, q="pool"); self.ld(w1[1], dw1v, q="pool"); self.ld(w2[0], dw2k, q="pool"); self.ld(w2[1], dw2v, q="pool")
        self.ld(pe[0], dpek, q="pool"); self.ld(pe[1], dpev, q="pool")
        hidb = [k.sb([128, 256], BF16, "n_hidk"), k.sb([128, 256], BF16, "n_hidv")]
        pb = k.sb([128, 1], F32, "n_pb")
        hx = k.sb([128, 256], F32, "n_hx"); hy = k.sb([128, 256], F32, "n_hy"); hz = k.sb([128, 256], F32, "n_hz")
        srcs = [kcin_t, vcin_t]
        cin = [kcin, vcin]
        for w in range(2):
            px = self.psX

            def mmb():
                ins = None
                for i in range(32):
                    ins = nc.tensor.matmul(px[:, 0:1], lhsT=w1[w][:, i, :], rhs=pe[w][:, i:i + 1], start=(i == 0), stop=(i == 31))
                return ins
            k.op("pe", mmb, reads=[w1[w], pe[w]], writes=[px])
            k.op("dve", lambda: nc.vector.tensor_copy(pb[:], px[:, 0:1]), reads=[px], writes=[pb])
            ph = self.psS[w]

            def mmh():
                ins = None
                for i in range(32):
                    ins = nc.tensor.matmul(ph[:, 0:255], lhsT=w1[w][:, i, :], rhs=cin[w][:, i:i + 4065:16], start=(i == 0), stop=(i == 31))
                return ins
            k.op("pe", mmh, reads=[w1[w], srcs[w]], writes=[ph])
            k.op("dve", lambda: nc.vector.memset(hx[:], 0.0), writes=[hx])
            k.op("act", lambda: nc.scalar.activation(out=hx[:, 0:255], in_=ph[:, 0:255], func=AF.Identity, bias=pb[:, 0:1]),
                 reads=[ph, pb], writes=[hx])
            self.gelu_tanh(hz, hx, hy)
            k.op("dve", lambda: nc.vector.tensor_copy(hidb[w][:], hz[:]), reads=[hz], writes=[hidb[w]])
        pk = self.psS[2]
        k.op("pe", lambda: nc.tensor.matmul(pk[:, 0:256], lhsT=w2[0][:], rhs=hidb[0][:], start=True, stop=True), reads=[w2[0], hidb[0]], writes=[pk])
        k.op("dve", lambda: nc.vector.tensor_copy(kcT[:], pk[:, 0:256]), reads=[pk], writes=[kcT])
        for nt in range(2):
            pv = self.psO[nt]
            k.op("pe", lambda: nc.tensor.matmul(pv[:, 0:128], lhsT=hidb[1][:, nt * 128:(nt + 1) * 128], rhs=w2[1][:], start=True, stop=True),
                 reads=[hidb[1], w2[1]], writes=[pv])
            k.op("dve", lambda: nc.vector.tensor_copy(vc[:, nt, :], pv[:, 0:128]), reads=[pv], writes=[(vc, nt)])
        cscope.__exit__(None, None, None)
        tbs = k.sb([128, 12, 512], BF16, "n_tbs"); tbw = k.sb([128, 8, 512], BF16, "n_tbw")
        self.ld(tbs, dtbs, q="pool"); self.ld(tbw, dtbw, q="pool")
        keep = k.sb([128, 4, 64], F32, "n_keep"); addt = k.sb([128, 4, 64], F32, "n_add")
        Eb = k.sb([64, T_], BF16, "n_E"); ov = k.sb([128, 2, 64], BF16, "n_ov")
        self.ld(Eb, dE, q="pool"); self.ld(ov, dov, q="pool")
        qo = k.sb([128, 3, 512], BF16, "n_qo"); qst = k.sb([128, 4, 512], BF16, "n_qst")
        bc = [k.sb([128, 2, 4, 512], BF16, "n_bc0")] * 2
        gb = [k.sb([128, 3, 512], F32, "n_gb0")] * 2
        pc = [[k.sb([128, 512], BF16, "n_pc%d%d" % (h, nt)) for nt in range(2)] for h in range(4)]
        Rh = [k.sb([128, 512], F32, "n_R")] * 4
        acc = [k.sb([128, 512], F32, "n_acc%d" % i) for i in range(2)]
        impt = k.sb([128, 4, 64], F32, "n_imp"); wrk = k.sb([128, 64], F32, "n_wrk")
        m8 = k.sb([128, 8], F32, "n_m8"); thr = k.sb([128, 1], F32, "n_thr"); selb = k.sb([128, 64], F32, "n_selb")
        selT = k.sb([64, 512], BF16, "n_selT")
        Wt = k.sb([128, 512], F32, "n_W")
        NKC = [128, 127]
        for i in range(8):
            q0 = 512 * i
            bci = bc[i % 2]; gbi = gb[i % 2]; ac = acc[i % 2]
            k.dma("pool", bci[:], dbc.h[:, :, :, q0:q0 + 512], reads=[dbc], writes=[bci])
            for gi in range(3):
                pgx = self.psS[self.iS % 3]; self.iS += 1
                k.op("pe", lambda: nc.tensor.matmul(pgx[:], lhsT=gsel[gi][:], rhs=grow[0:3, q0:q0 + 512], start=True, stop=True),
                     reads=[gsel[gi], grow], writes=[pgx])
                k.op("act", lambda: nc.scalar.activation(out=gbi[:, gi, :], in_=pgx[:], func=AF.Sigmoid), reads=[pgx], writes=[(gbi, gi)])
            sq_ = q0 // TOK
            c0 = q0 - sq_ * TOK
            for jc in range(4):
                for (ct, sap_, d0, cnt) in self.zall.pieces(sq_, _OFF["nsa_q"] + jc * 128, 128):
                    k.dma("pool", qst[d0:d0 + cnt, jc, :], sap_[:, c0:c0 + 512], reads=[ct], writes=[qst])
            for hh in range(3):
                for jc in range(4):
                    mc = self.msk[:, 4 + 4 * hh + jc:5 + 4 * hh + jc]
                    if jc == 0:
                        k.op("dve", lambda: nc.vector.tensor_scalar(out=qo[:, hh, :], in0=qst[:, jc, :], scalar1=mc, scalar2=None, op0=ALU.mult),
                             reads=[qst, self.msk], writes=[qo])
                    else:
                        k.op("dve", lambda: nc.vector.scalar_tensor_tensor(out=qo[:, hh, :], in0=qst[:, jc, :], scalar=mc, in1=qo[:, hh, :], op0=ALU.mult, op1=ALU.add),
                             reads=[qst, self.msk, qo], writes=[qo])
            k.op("dve", lambda: nc.vector.tensor_scalar(out=qo[:], in0=qo[:], scalar1=SCALE, scalar2=None, op0=ALU.mult), reads=[qo], writes=[qo])
            k.dma("sp", keep[:], dkeep.h[:, 4 * i:4 * i + 4, :], reads=[dkeep], writes=[keep])
            k.dma("sp", addt[:], dadd.h[:, 4 * i:4 * i + 4, :], reads=[dadd], writes=[addt])
            for hh in range(4):
                h = hh
                own = (h == 3)
                L = self.psL[hh % 2]; O = self.psO[0]
                for nt in range(2):
                    nk = NKC[nt]
                    S = self.psS[self.iS % 3]; self.iS += 1
                    P = pc[h][nt]

                    def mm():
                        nc.tensor.matmul(S[0:nk, :], lhsT=kcT[:, nt * 128:nt * 128 + nk], rhs=(qown[:, q0:q0 + 512] if h == 3 else qo[:, h, :]), start=True, stop=False)
                        return nc.tensor.matmul(S[0:nk, :], lhsT=self.identB[:, 0:nk], rhs=bci[:, nt, h, :], start=False, stop=True)
                    k.op("pe", mm, reads=[kcT, qown, qo, self.identB, bci], writes=[S])
                    if nk < 128:
                        k.op("dve", lambda: nc.vector.memset(P[:], 0.0), writes=[P])
                    k.op("act", lambda: nc.scalar.activation(out=P[0:nk, :], in_=S[0:nk, :], func=AF.Exp), reads=[S], writes=[P])
                    k.op("pe", lambda: nc.tensor.matmul(L[:], lhsT=self.onesB[0:nk, :], rhs=P[0:nk, :], start=(nt == 0), stop=(nt == 1)),
                         reads=[self.onesB, P], writes=[L])
                    if own:
                        k.op("pe", lambda: nc.tensor.matmul(O[:], lhsT=vc[0:nk, nt, :], rhs=P[0:nk, :], start=(nt == 0), stop=(nt == 1)),
                             reads=[vc, P], writes=[O])
                k.op("dve", lambda: nc.vector.tensor_scalar(out=Rh[h][:], in0=L[:], scalar1=1e-30, scalar2=None, op0=ALU.max),
                     reads=[L], writes=[Rh[h]])
                k.op("dve", lambda: nc.vector.reciprocal(out=Rh[h][:], in_=Rh[h][:]), reads=[Rh[h]], writes=[Rh[h]])
                if own:
                    k.op("dve", lambda: nc.vector.tensor_tensor(out=Wt[:], in0=Rh[h][:], in1=gbi[:, 0, :], op=ALU.mult), reads=[Rh[h], gbi], writes=[Wt])
                    k.op("dve", lambda: nc.vector.tensor_tensor(out=ac[:], in0=O[:], in1=Wt[:], op=ALU.mult), reads=[O, Wt], writes=[ac])
                for nt in range(2):
                    k.op("dve", lambda: nc.vector.tensor_tensor(out=pc[h][nt][:], in0=pc[h][nt][:], in1=Rh[h][:], op=ALU.mult),
                         reads=[pc[h][nt], Rh[h]], writes=[pc[h][nt]])
            px = self.psX
            for jq in range(4):
                def mmi():
                    ins = None
                    n = 0
                    for h in range(4):
                        for nt in range(2):
                            ins = nc.tensor.matmul(px[:, jq * 64:(jq + 1) * 64], lhsT=pc[h][nt][:, jq * 128:(jq + 1) * 128], rhs=ov[:, nt, :],
                                                   start=(n == 0), stop=(n == 7))
                            n += 1
                    return ins
                k.op("pe", mmi, reads=[ov] + [pc[h][nt] for h in range(4) for nt in range(2)], writes=[(px, jq)])
            k.op("dve", lambda: nc.vector.tensor_tensor(out=impt[:].rearrange("p a b -> p (a b)"), in0=px[:, 0:256],
                                                        in1=keep[:].rearrange("p a b -> p (a b)"), op=ALU.mult),
                 reads=[px, keep], writes=[impt])
            k.op("dve", lambda: nc.vector.tensor_tensor(out=impt[:], in0=impt[:], in1=addt[:], op=ALU.add),
                 reads=[impt, addt], writes=[impt])
            pt = self.psL[0]
            for jq in range(4):
                k.op("dve", lambda: nc.vector.max(out=m8[:], in_=impt[:, jq, :]), reads=[impt], writes=[m8])
                k.op("dve", lambda: nc.vector.match_replace(out=wrk[:], in_to_replace=m8[:], in_values=impt[:, jq, :], imm_value=-1e30),
                     reads=[m8, impt], writes=[wrk])
                k.op("dve", lambda: nc.vector.max(out=m8[:], in_=wrk[:]), reads=[wrk], writes=[m8])
                k.op("dve", lambda: nc.vector.tensor_reduce(out=thr[:], in_=m8[:], axis=AX.X, op=ALU.min), reads=[m8], writes=[thr])
                k.op("dve", lambda: nc.vector.tensor_scalar(out=selb[:], in0=impt[:, jq, :], scalar1=thr[:, 0:1], scalar2=NEG,
                                                            op0=ALU.is_lt, op1=ALU.mult), reads=[impt, thr], writes=[selb])
                k.op("pe", lambda: nc.tensor.matmul(pt[0:64, jq * 128:(jq + 1) * 128], lhsT=selb[:], rhs=self.identF[:], start=True, stop=True),
                     reads=[selb, self.identF], writes=[(pt, jq)])
            k.op("dve", lambda: nc.vector.tensor_copy(selT[:], pt[0:64, :]), reads=[pt], writes=[selT])
            O = self.psO[1]; L = self.psL[1]
            nkt = 4 * i + 4
            for kt in range(nkt):
                idx = min(4 * i - kt + 3, 11)
                extra = [(lambda S: S[:], self.identB[:], tbs[:, idx, :]),
                         (lambda S: S[:], Eb[:, kt * 128:(kt + 1) * 128], selT[:])]
                self._xr = [self.identB, tbs, Eb, selT]
                self.attn_unit(ksb, kt, qown, q0, extra, None, [], vsb, vsb[:, kt * 128:(kt + 1) * 128], O, L, kt == 0, kt == nkt - 1)
            self.nsa_fin(O, L, gbi, 1, ac, Wt)
            O = self.psO[0]; L = self.psL[0]
            kts = list(range(max(0, 4 * i - 4), 4 * i + 4))
            for n, kt in enumerate(kts):
                idx = 4 * i - kt + 3
                extra = [(lambda S: S[:], self.identB[:], tbw[:, idx, :])]
                self._xr = [self.identB, tbw]
                self.attn_unit(kwb, kt, qown, q0, extra, None, [], vwb, vwb[:, kt * 128:(kt + 1) * 128], O, L, n == 0, n == len(kts) - 1)
            self.nsa_fin(O, L, gbi, 2, ac, Wt)
            k.dma("sp", yout.h[:, q0:q0 + 512], ac[:], reads=[ac], writes=[(yout, i)])

    def nsa_fin(self, O, L, gbi, gi, ac, Wt):
        k, nc = self.k, self.nc
        t2 = self.tmpA[0]
        k.op("dve", lambda: nc.vector.reciprocal(out=Wt[:], in_=L[:]), reads=[L], writes=[Wt])
        k.op("dve", lambda: nc.vector.tensor_tensor(out=Wt[:], in0=Wt[:], in1=gbi[:, gi, :], op=ALU.mult), reads=[Wt, gbi], writes=[Wt])
        k.op("dve", lambda: nc.vector.tensor_tensor(out=t2[:], in0=O[:], in1=Wt[:], op=ALU.mult), reads=[O, Wt], writes=[t2])
        k.op("dve", lambda: nc.vector.tensor_tensor(out=ac[:], in0=ac[:], in1=t2[:], op=ALU.add), reads=[ac, t2], writes=[ac])


    def gdn(self, yout):
        k, nc = self.k, self.nc
        L_ = "%d" % self.l
        dcw = k.din("gdn_cw" + L_, [128, 3, 4])
        dal = k.din("gdn_alog" + L_, [128, 1]); ddt = k.din("gdn_dtb" + L_, [128, 1]); dng = k.din("gdn_ng" + L_, [128, 1])
        dct = k.din("c_ct", [128, 128]); dsc = k.din("c_sc", [128, 128]); dh0 = k.din("c_h0", [128, 128]); dh1 = k.din("c_h1", [128, 128])
        dmst = k.din("c_mst", [128, 128]); dmit = k.din("c_mit", [128, 128]); dmsn = k.din("c_msn", [128, 128]); dcm = k.din("c_cm", [128, 2])
        B = self.big
        raw, W_, tA, tB = B[0], B[1], B[2], B[3]
        qs = ks = vs = oT = W_
        arow = k.sb([1, T_], F32, "g_arow")
        qnb, knb, vsb = self.bigb[0], self.bigb[1], self.bigb[2]
        cw = k.sb([128, 3, 4], F32, "g_cw"); self.ld(cw, dcw)
        cst = {}
        for nm, dd in (("ct", dct), ("sc", dsc), ("h0", dh0), ("h1", dh1), ("mst", dmst), ("mit", dmit), ("msn", dmsn)):
            cst[nm] = k.sb([128, 128], F32, "g_" + nm); self.ld(cst[nm], dd)
        cm = k.sb([128, 2], F32, "g_cm"); self.ld(cm, dcm)
        al = k.sb([128, 1], F32, "g_al"); dtb = k.sb([128, 1], F32, "g_dtb"); ng = k.sb([128, 1], F32, "g_ng")
        self.ld(al, dal); self.ld(dtb, ddt); self.ld(ng, dng)
        epsc = k.sb([128, 1], F32, "g_eps")
        k.op("dve", lambda: nc.vector.memset(epsc[:], EPS), writes=[epsc])
        for wi, (src, dst) in enumerate((("gdn_q", qs), ("gdn_k", ks), ("gdn_v", vs))):
            self.zfm(raw, raw.h, _OFF[src], 0, stage=tA)
            k.op("dve", lambda: nc.vector.tensor_scalar(out=dst[:], in0=raw[:], scalar1=cw[:, wi, 3:4], scalar2=None, op0=ALU.mult),
                 reads=[raw, cw], writes=[dst])
            for i in range(3):
                sh = 3 - i
                k.op("dve", lambda: nc.vector.scalar_tensor_tensor(out=dst[:, sh:], in0=raw[:, 0:T_ - sh], scalar=cw[:, wi, i:i + 1],
                                                                   in1=dst[:, sh:], op0=ALU.mult, op1=ALU.add), reads=[raw, cw, dst], writes=[dst])
            k.op("act", lambda: nc.scalar.activation(out=dst[:], in_=dst[:], func=AF.Silu), reads=[dst], writes=[dst])
            if wi == 2:
                k.op("act", lambda: nc.scalar.copy(out=vsb[:], in_=vs[:]), reads=[vs], writes=[vsb])
                continue
            src, dstb, sc = ((qs, qnb, SCALE), (ks, knb, 1.0))[wi]
            if True:
                k.op("dve", lambda: nc.vector.tensor_tensor(out=tA[:], in0=src[:], in1=src[:], op=ALU.mult), reads=[src], writes=[tA])
                for tb in range(8):
                    ts = slice(tb * 512, (tb + 1) * 512)
                    ps = self.psS[tb % 3]
                    k.op("pe", lambda: nc.tensor.matmul(ps[:], lhsT=self.onesF[:], rhs=tA[:, ts], start=True, stop=True), reads=[self.onesF, tA], writes=[ps])
                    k.op("act", lambda: nc.scalar.activation(out=tB[:, ts], in_=ps[:], func=AF.Ln, bias=epsc[:, 0:1]), reads=[ps, epsc], writes=[(tB, tb)])
                    k.op("act", lambda: nc.scalar.activation(out=tB[:, ts], in_=tB[:, ts], func=AF.Exp, scale=-0.5), reads=[(tB, tb)], writes=[(tB, tb)])
                k.op("dve", lambda: nc.vector.scalar_tensor_tensor(out=dstb[:], in0=src[:], scalar=sc, in1=tB[:], op0=ALU.mult, op1=ALU.mult),
                     reads=[src, tB], writes=[dstb])
        def col(nm):
            return k.sb([128, NT], F32, "g_c_" + nm)
        g = col("g"); beta = col("beta"); gc = col("gc"); ngc = col("ngc"); gl = col("gl"); wcol = col("w")
        skbg = col("skbg"); skd = [col("skd0"), col("skd1")]; egl = [col("egl0"), col("egl1")]; tmpc = col("tmp")
        self.zfm(arow, arow.h, _OFF["gdn_a"], 0, n=1, mul=1, stage=tB)
        self.row2col(arow, g)
        self.zfm(arow, arow.h, _OFF["gdn_b"], 0, n=1, mul=1, stage=tB)
        self.row2col(arow, beta)
        k.op("act", lambda: nc.scalar.activation(out=g[:], in_=g[:], func=AF.Exp, bias=dtb[:, 0:1]), reads=[g, dtb], writes=[g])
        k.op("act", lambda: nc.scalar.activation(out=g[:], in_=g[:], func=AF.Ln, bias=self.onesF[:, 0:1]), reads=[g, self.onesF], writes=[g])
        k.op("act", lambda: nc.scalar.activation(out=al[:], in_=al[:], func=AF.Exp), reads=[al], writes=[al])
        k.op("dve", lambda: nc.vector.tensor_scalar(out=g[:], in0=g[:], scalar1=al[:, 0:1], scalar2=-1.0, op0=ALU.mult, op1=ALU.mult),
             reads=[g, al], writes=[g])
        k.op("act", lambda: nc.scalar.activation(out=beta[:], in_=beta[:], func=AF.Sigmoid), reads=[beta], writes=[beta])
        px = self.psX

        def colmm(lhs, dst, func=None):
            k.op("pe", lambda: nc.tensor.matmul(px[:, 0:NT], lhsT=lhs[:], rhs=g[:], start=True, stop=True), reads=[lhs, g], writes=[px])
            if func is None:
                k.op("dve", lambda: nc.vector.tensor_copy(dst[:], px[:, 0:NT]), reads=[px], writes=[dst])
            else:
                k.op("act", lambda: nc.scalar.activation(out=dst[:], in_=px[:, 0:NT], func=func), reads=[px], writes=[dst])
        colmm(cst["ct"], gc)
        colmm(cst["sc"], gl)
        colmm(cst["h0"], egl[0], AF.Exp)
        colmm(cst["h1"], egl[1], AF.Exp)
        k.op("dve", lambda: nc.vector.tensor_scalar(out=ngc[:], in0=gc[:], scalar1=-1.0, scalar2=None, op0=ALU.mult), reads=[gc], writes=[ngc])
        k.op("act", lambda: nc.scalar.activation(out=wcol[:], in_=beta[:], func=AF.Ln), reads=[beta], writes=[wcol])
        k.op("dve", lambda: nc.vector.tensor_tensor(out=wcol[:], in0=wcol[:], in1=gc[:], op=ALU.add), reads=[wcol, gc], writes=[wcol])
        k.op("act", lambda: nc.scalar.activation(out=skbg[:], in_=gc[:], func=AF.Exp), reads=[gc], writes=[skbg])
        k.op("dve", lambda: nc.vector.tensor_tensor(out=skbg[:], in0=skbg[:], in1=beta[:], op=ALU.mult), reads=[skbg, beta], writes=[skbg])
        k.op("dve", lambda: nc.vector.tensor_tensor(out=tmpc[:], in0=gl[:], in1=gc[:], op=ALU.subtract), reads=[gl, gc], writes=[tmpc])
        k.op("act", lambda: nc.scalar.activation(out=tmpc[:], in_=tmpc[:], func=AF.Exp), reads=[tmpc], writes=[tmpc])
        for c in range(2):
            k.op("dve", lambda: nc.vector.tensor_scalar(out=skd[c][:], in0=tmpc[:], scalar1=cm[:, c:c + 1], scalar2=None, op0=ALU.mult),
                 reads=[tmpc, cm], writes=[skd[c]])
        S = k.sb([128, 128], F32, "g_S"); Sb = k.sb([128, 128], BF16, "g_Sb")
        k.op("dve", lambda: nc.vector.memset(S[:], 0.0), writes=[S])
        k.op("dve", lambda: nc.vector.memset(Sb[:], 0.0), writes=[Sb])

        def t128(nm, dt=F32):
            return k.sb([128, 128], dt, "g_t_" + nm)
        kbg = t128("kbg", BF16); kd = [t128("kd0", BF16), t128("kd1", BF16)]; vb = t128("vb", BF16)
        dgw = t128("dgw"); dgg = t128("dgg"); dgn = t128("dgn")
        Gs = t128("G"); Y = t128("Y"); X = t128("X"); Pm = t128("P"); Z = t128("Z"); ZT = t128("ZT"); Z2 = t128("Z2"); ZT2 = t128("ZT2")
        E1 = t128("E1"); qkT = t128("qkT", BF16); qgT = t128("qgT", BF16); PTb = t128("PTb", BF16); nWT = t128("nWT", BF16)
        vnb = t128("vnb", BF16)
        pool6 = [self.psS[0], self.psS[1], self.psS[2], self.psO[1], self.psL[0], self.psL[1]]
        ctr = [0]

        def pp():
            ctr[0] += 1
            return pool6[ctr[0] % 6]
        pOg = self.psO[0]
        for t in range(NT):
            cs = slice(t * 128, (t + 1) * 128)
            tc_ = slice(t, t + 1)
            pa = pp()
            k.op("pe", lambda: nc.tensor.matmul(pa[:, 0:128], lhsT=knb[:, cs], rhs=self.identB[:], start=True, stop=True), reads=[knb, self.identB], writes=[(pa, 0)])
            k.op("pe", lambda: nc.tensor.matmul(pa[:, 128:256], lhsT=vsb[:, cs], rhs=self.identB[:], start=True, stop=True), reads=[vsb, self.identB], writes=[(pa, 1)])
            k.op("dve", lambda: nc.vector.tensor_scalar(out=kbg[:], in0=pa[:, 0:128], scalar1=skbg[:, tc_], scalar2=None, op0=ALU.mult), reads=[(pa, 0), skbg], writes=[kbg])
            for c in range(2):
                k.op("dve", lambda: nc.vector.tensor_scalar(out=kd[c][:], in0=pa[:, 0:128], scalar1=skd[c][:, tc_], scalar2=None, op0=ALU.mult),
                     reads=[(pa, 0), skd[c]], writes=[kd[c]])
            k.op("dve", lambda: nc.vector.tensor_scalar(out=vb[:], in0=pa[:, 128:256], scalar1=beta[:, tc_], scalar2=None, op0=ALU.mult), reads=[(pa, 1), beta], writes=[vb])
            k.op("dve", lambda: nc.vector.tensor_scalar(out=dgw[:], in0=self.identF[:], scalar1=wcol[:, tc_], scalar2=None, op0=ALU.mult), reads=[self.identF, wcol], writes=[dgw])
            k.op("dve", lambda: nc.vector.tensor_scalar(out=dgg[:], in0=self.identF[:], scalar1=gc[:, tc_], scalar2=None, op0=ALU.mult), reads=[self.identF, gc], writes=[dgg])
            k.op("dve", lambda: nc.vector.tensor_scalar(out=dgn[:], in0=self.identF[:], scalar1=ngc[:, tc_], scalar2=None, op0=ALU.mult), reads=[self.identF, ngc], writes=[dgn])
            pg = pp()
            k.op("pe", lambda: nc.tensor.matmul(pg[:, 0:128], lhsT=knb[:, cs], rhs=knb[:, cs], start=True, stop=True), reads=[knb], writes=[(pg, 0)])
            k.op("pe", lambda: nc.tensor.matmul(pg[:, 128:256], lhsT=knb[:, cs], rhs=qnb[:, cs], start=True, stop=True), reads=[knb, qnb], writes=[(pg, 1)])
            k.op("dve", lambda: nc.vector.tensor_copy(Gs[:], pg[:, 0:128]), reads=[(pg, 0)], writes=[Gs])

            def expmat(diag, mask, bias_col, bias_t, dst_fn):
                pe_ = pp()

                def mm():
                    ins0 = nc.tensor.matmul(pe_[:, 0:128], lhsT=self.onesF[:], rhs=diag[:], start=True, stop=(mask is None))
                    if mask is None:
                        return ins0
                    return nc.tensor.matmul(pe_[:, 0:128], lhsT=self.identF[:], rhs=mask[:], start=False, stop=True)
                k.op("pe", mm, reads=[self.onesF, diag, self.identF] + ([mask] if mask is not None else []), writes=[pe_])
                if bias_col is None:
                    k.op("act", lambda: nc.scalar.activation(out=E1[:], in_=pe_[:, 0:128], func=AF.Exp), reads=[pe_], writes=[E1])
                else:
                    k.op("act", lambda: nc.scalar.activation(out=E1[:], in_=pe_[:, 0:128], func=AF.Exp, bias=bias_col[:, tc_]),
                         reads=[pe_, bias_t], writes=[E1])
                dst_fn()
            expmat(dgw, cst["mst"], ngc, ngc, lambda: k.op("dve", lambda: nc.vector.tensor_tensor(out=Y[:], in0=Gs[:], in1=E1[:], op=ALU.mult), reads=[Gs, E1], writes=[Y]))
            expmat(dgn, cst["msn"], wcol, wcol, lambda: k.op("dve", lambda: nc.vector.tensor_tensor(out=X[:], in0=Gs[:], in1=E1[:], op=ALU.mult), reads=[Gs, E1], writes=[X]))
            expmat(dgg, cst["mit"], ngc, ngc, lambda: k.op("dve", lambda: nc.vector.tensor_tensor(out=qkT[:], in0=pg[:, 128:256], in1=E1[:], op=ALU.mult), reads=[(pg, 1), E1], writes=[qkT]))
            expmat(dgg, None, None, None, lambda: k.op("dve", lambda: nc.vector.tensor_tensor(out=qgT[:], in0=qnb[:, cs], in1=E1[:], op=ALU.mult), reads=[qnb, E1], writes=[qgT]))
            k.op("dve", lambda: nc.vector.tensor_tensor(out=Pm[:], in0=self.identF[:], in1=Y[:], op=ALU.subtract), reads=[self.identF, Y], writes=[Pm])
            p1 = pp(); p2 = pp()
            k.op("pe", lambda: nc.tensor.matmul(p1[:, 0:128], lhsT=X[:], rhs=Y[:], start=True, stop=True), reads=[X, Y], writes=[p1])
            k.op("pe", lambda: nc.tensor.matmul(p2[:, 0:128], lhsT=Y[:], rhs=X[:], start=True, stop=True), reads=[X, Y], writes=[p2])
            zc, ztc, zn, ztn = Z, ZT, Z2, ZT2
            k.op("dve", lambda: nc.vector.tensor_copy(zc[:], p1[:, 0:128]), reads=[p1], writes=[zc])
            k.op("act", lambda: nc.scalar.copy(out=ztc[:], in_=p2[:, 0:128]), reads=[p2], writes=[ztc])
            for it in range(5):
                p3 = pp()
                k.op("pe", lambda: nc.tensor.matmul(p3[:, 0:128], lhsT=ztc[:], rhs=Pm[:], start=True, stop=True), reads=[ztc, Pm], writes=[p3])
                if it < 4:
                    p1 = pp(); p2 = pp()
                    k.op("pe", lambda: nc.tensor.matmul(p1[:, 0:128], lhsT=ztc[:], rhs=zc[:], start=True, stop=True), reads=[ztc, zc], writes=[p1])
                    k.op("pe", lambda: nc.tensor.matmul(p2[:, 0:128], lhsT=zc[:], rhs=ztc[:], start=True, stop=True), reads=[ztc, zc], writes=[p2])
                k.op("dve", lambda: nc.vector.tensor_tensor(out=Pm[:], in0=Pm[:], in1=p3[:, 0:128], op=ALU.add), reads=[Pm, p3], writes=[Pm])
                if it < 4:
                    k.op("dve", lambda: nc.vector.tensor_copy(zn[:], p1[:, 0:128]), reads=[p1], writes=[zn])
                    k.op("act", lambda: nc.scalar.copy(out=ztn[:], in_=p2[:, 0:128]), reads=[p2], writes=[ztn])
                    zc, ztc, zn, ztn = zn, ztn, zc, ztc
            k.op("act", lambda: nc.scalar.copy(out=PTb[:], in_=Pm[:]), reads=[Pm], writes=[PTb])
            pw = pp()
            k.op("pe", lambda: nc.tensor.matmul(pw[:, 0:128], lhsT=kbg[:], rhs=PTb[:], start=True, stop=True), reads=[kbg, PTb], writes=[pw])
            k.op("dve", lambda: nc.vector.tensor_scalar(out=nWT[:], in0=pw[:, 0:128], scalar1=-1.0, scalar2=None, op0=ALU.mult), reads=[pw], writes=[nWT])
            for c in range(2):
                ccs = slice(64 * c, 64 * c + 64)
                pv = pp()

                def mmv():
                    nc.tensor.matmul(pv[:, 0:128], lhsT=PTb[:], rhs=vb[:], start=True, stop=False)
                    return nc.tensor.matmul(pv[:, 0:128], lhsT=nWT[:], rhs=Sb[:], start=False, stop=True)
                k.op("pe", mmv, reads=[PTb, vb, nWT, Sb], writes=[pv])
                k.op("act", lambda: nc.scalar.copy(out=vnb[:], in_=pv[:, 0:128]), reads=[pv], writes=[vnb])

                def mmo():
                    nc.tensor.matmul(pOg[:, ccs], lhsT=Sb[:], rhs=qgT[:, ccs], start=True, stop=False)
                    return nc.tensor.matmul(pOg[:, ccs], lhsT=vnb[:], rhs=qkT[:, ccs], start=False, stop=True)
                k.op("pe", mmo, reads=[Sb, qgT, vnb, qkT], writes=[(pOg, c)])
                pu = pp()
                k.op("pe", lambda: nc.tensor.matmul(pu[:, 0:128], lhsT=kd[c][:], rhs=vnb[:], start=True, stop=True), reads=[kd[c], vnb], writes=[pu])
                k.op("dve", lambda: nc.vector.scalar_tensor_tensor(out=S[:], in0=S[:], scalar=egl[c][:, tc_], in1=pu[:, 0:128], op0=ALU.mult, op1=ALU.add),
                     reads=[S, egl[c], pu], writes=[S])
                k.op("act", lambda: nc.scalar.copy(out=Sb[:], in_=S[:]), reads=[S], writes=[Sb])
            k.op("dve", lambda: nc.vector.tensor_copy(oT[:, cs], pOg[:, 0:128]), reads=[pOg], writes=[(oT, t)])
        self.zfm(raw, raw.h, _OFF["gdn_z"], 0, stage=tA)
        k.op("act", lambda: nc.scalar.activation(out=raw[:], in_=raw[:], func=AF.Silu), reads=[raw], writes=[raw])
        k.op("dve", lambda: nc.vector.tensor_tensor(out=tA[:], in0=oT[:], in1=oT[:], op=ALU.mult), reads=[oT], writes=[tA])
        k.op("dve", lambda: nc.vector.tensor_scalar(out=tA[:], in0=tA[:], scalar1=1.0 / 128, scalar2=None, op0=ALU.mult), reads=[tA], writes=[tA])
        for tb in range(8):
            ts = slice(tb * 512, (tb + 1) * 512)
            ps = self.psS[tb % 3]
            k.op("pe", lambda: nc.tensor.matmul(ps[:], lhsT=self.onesF[:], rhs=tA[:, ts], start=True, stop=True), reads=[self.onesF, tA], writes=[ps])
            k.op("act", lambda: nc.scalar.activation(out=tB[:, ts], in_=ps[:], func=AF.Ln, bias=epsc[:, 0:1]), reads=[ps, epsc], writes=[(tB, tb)])
            k.op("act", lambda: nc.scalar.activation(out=tB[:, ts], in_=tB[:, ts], func=AF.Exp, scale=-0.5), reads=[(tB, tb)], writes=[(tB, tb)])
        k.op("dve", lambda: nc.vector.scalar_tensor_tensor(out=oT[:], in0=oT[:], scalar=ng[:, 0:1], in1=tB[:], op0=ALU.mult, op1=ALU.mult),
             reads=[oT, ng, tB], writes=[oT])
        k.op("dve", lambda: nc.vector.tensor_tensor(out=oT[:], in0=oT[:], in1=raw[:], op=ALU.mult), reads=[oT, raw], writes=[oT])
        k.dma("sp", yout.h, oT[:], reads=[oT], writes=[yout])


class Gathered:
    def __init__(self, nc, name, nrows, ncols, CR):
        self.nrows, self.ncols, self.CR = nrows, ncols, CR
        self.chunks = []
        r = 0
        while r < nrows:
            cr = min(CR, nrows - r)
            self.chunks.append((r, cr, T(nc.dram_tensor("%s_c%d" % (name, len(self.chunks)), [4 * cr, ncols], F32).ap(), "%s_c%d" % (name, len(self.chunks)))))
            r += cr

    def pieces(self, s_, r0, n):
        out = []
        r = r0
        while r < r0 + n:
            ci = r // self.CR
            c0, cr, t = self.chunks[ci]
            cnt = min(r0 + n, c0 + cr) - r
            out.append((t, t.h[s_ * cr + (r - c0):s_ * cr + (r - c0) + cnt, :], r - r0, cnt))
            r += cnt
        return out


def exchange(k, src, dst, sem):
    nc = k.nc
    k.barrier()
    sems = []
    for (c0, cr, t) in dst.chunks:
        cs = sem.enter_context(nc.semaphore("cc_%s" % t.name))
        sems.append(cs)
        nc.gpsimd.collective_compute("AllGather", ALU.bypass, replica_groups=[[0, 1, 2, 3], [4, 5, 6, 7]],
                                     ins=[src.h[c0:c0 + cr, :].opt()], outs=[t.h.opt()]).then_inc(cs)
    for cs in sems:
        for e in k.eng.values():
            e.wait_ge(cs, 1)


NBIG = {"fox": (2, 4), "lru": (6, 1), "gdn": (4, 3), "nsa": (0, 0)}


def build_fused(dbg=False):
    nc = bass.Bass("TRN2", target_bir_lowering=False)
    with ExitStack() as es:
        k = K(nc, es)
        x = k.din("x", [TOK, D])
        out = k.dout("out", [TOK, D])
        h = k.sb([128, NCH, TOK], F32, "h")
        vals = None
        zsh = [T(nc.dram_tensor("zsh%d" % l, [DIN, TOK], F32).ap(), "zsh%d" % l) for l in range(2)]
        zall = [Gathered(nc, "zall%d" % l, DIN, TOK, 256) for l in range(2)]
        ysh = [T(nc.dram_tensor("ysh%d" % l, [4 * 128, T_], F32).ap(), "ysh%d" % l) for l in range(2)]
        yall = [Gathered(nc, "yall%d" % l, 4 * 128, T_, 64) for l in range(2)]
        csem = [es, es, es, es]
        with k.scope():
            load_x(k, h, x)
        for l in range(2):
            with k.scope():
                dense_A(k, h, l, zsh[l])
            exchange(k, zsh[l], zall[l], csem[2 * l])
            for gi, nm in enumerate(("fox", "gdn", "lru", "nsa")):
                with k.scope():
                    m = Mix(k, l, zall[l], vals, NBIG[nm][0], NBIG[nm][1])
                    yout = T(ysh[l].h[gi * 128:(gi + 1) * 128, :], "y_%s%d" % (nm, l))
                    getattr(m, nm)(yout)
            exchange(k, ysh[l], yall[l], csem[2 * l + 1])
            with k.scope():
                dense_C(k, h, l, yall[l], None)
            if dbg and l == 0:
                dh = k.dout("dbg_h", [D, TOK])
                v = dh.h.rearrange("(c p) t -> p c t", p=128)
                for c in range(0, NCH, 4):
                    k.dma("sp", v[:, c:c + 4, :], h[:, c:c + 4, :], reads=[h], writes=[(dh, c)])
        with k.scope():
            final_out(k, h, out)
        k.wait_all("sp")
    return nc


def tm_tiles(a):
    t, d = a.shape
    return np.ascontiguousarray(a.reshape(t // 128, 128, d).transpose(1, 0, 2))


def col_tiles(v):
    return np.ascontiguousarray(v.reshape(-1, 128).T)


_OFF = {}
_o = 0
for _n, _w in (("fox_q", 512), ("fox_k", 512), ("fox_v", 512), ("fox_f", 4), ("gdn_q", 512), ("gdn_k", 512), ("gdn_v", 512),
               ("gdn_a", 4), ("gdn_b", 4), ("gdn_z", 512), ("lru_x", 512), ("lru_gate", 512), ("nsa_q", 512), ("nsa_kc", 128),
               ("nsa_vc", 128), ("nsa_ks", 128), ("nsa_vs", 128), ("nsa_kw", 128), ("nsa_vw", 128), ("nsa_g", 12)):
    _OFF[_n] = _o
    _o += _w


def consts_B():
    c = {}
    c["c_ident"] = np.eye(128, dtype=np.float32)
    p = np.arange(128)
    c["c_ut"] = (p[:, None] <= p[None, :]).astype(np.float32)
    q = np.arange(32)
    c["c_su"] = (q[:, None] < q[None, :]).astype(np.float32)
    col = np.arange(512)
    mb = np.zeros((128, 4, 512), np.float32)
    for m in range(4):
        mb[:, m, :] = np.where(p[:, None] + 128 * m <= col[None, :], 0.0, NEG)
    c["c_mbfox"] = mb
    ch = p // 64
    same = ch[:, None] == ch[None, :]
    c["c_ct"] = (same & (p[:, None] <= p[None, :])).astype(np.float32)
    c["c_sc"] = same.astype(np.float32)
    c["c_h0"] = np.broadcast_to((p < 64)[:, None], (128, 128)).astype(np.float32).copy()
    c["c_h1"] = np.broadcast_to((p >= 64)[:, None], (128, 128)).astype(np.float32).copy()
    c["c_mst"] = np.where(same & (p[None, :] > p[:, None]), 0.0, NEG).astype(np.float32)
    c["c_mit"] = np.where(same & (p[None, :] >= p[:, None]), 0.0, NEG).astype(np.float32)
    c["c_msn"] = np.where(same & (p[:, None] > p[None, :]), 0.0, NEG).astype(np.float32)
    c["c_cm"] = np.stack([(p < 64), (p >= 64)], axis=1).astype(np.float32)
    return c


def prep_mix(inp, l, j):
    L_ = "%d" % l
    m = {}
    hs = slice(j * 128, (j + 1) * 128)
    m["lru_cw" + L_] = np.ascontiguousarray(inp["lru_conv_w"][l][:, hs].T)
    m["lru_cb" + L_] = np.ascontiguousarray(inp["lru_conv_b"][l][hs].reshape(128, 1))
    for nm, src in (("lru_wa", "lru_w_a"), ("lru_wx", "lru_w_x")):
        bd = np.zeros((128, 128), np.float32)
        bd[0:64, 0:64] = inp[src][l][2 * j]
        bd[64:128, 64:128] = inp[src][l][2 * j + 1]
        m[nm + L_] = bd
    m["lru_ba" + L_] = np.ascontiguousarray(inp["lru_b_a"][l][hs].reshape(128, 1))
    m["lru_bx" + L_] = np.ascontiguousarray(inp["lru_b_x"][l][hs].reshape(128, 1))
    m["lru_lam" + L_] = np.ascontiguousarray(inp["lru_lambda"][l][hs].reshape(128, 1))
    cwf = inp["gdn_conv_w"][l]
    m["gdn_cw" + L_] = np.ascontiguousarray(np.stack([cwf[:, g0 * 512:(g0 + 1) * 512][:, hs].T for g0 in range(3)], axis=1))
    m["gdn_alog" + L_] = np.full((128, 1), inp["gdn_a_log"][l][j], np.float32)
    m["gdn_dtb" + L_] = np.full((128, 1), inp["gdn_dt_bias"][l][j], np.float32)
    m["gdn_ng" + L_] = np.ascontiguousarray(inp["gdn_norm_g"][l].reshape(128, 1))
    for nm, src in (("nsa_w1k", "nsa_w1_k"), ("nsa_w1v", "nsa_w1_v")):
        m[nm + L_] = np.ascontiguousarray(inp[src][l].reshape(32, 128, 128).transpose(1, 0, 2))
    m["nsa_w2k" + L_] = inp["nsa_w2_k"][l]; m["nsa_w2v" + L_] = inp["nsa_w2_v"][l]
    m["nsa_pek" + L_] = np.ascontiguousarray(inp["nsa_pe_k"][l].T); m["nsa_pev" + L_] = np.ascontiguousarray(inp["nsa_pe_v"][l].T)
    return m


def prep_shared(inp):
    m = dict(consts_B())
    for l in range(2):
        L_ = "%d" % l
        binp = np.zeros(NZC * 128, np.float32)
        binp[:DIN] = inp["b_in"][l]
        ong = np.ones((D,), np.float32)
        ong[0:512] = inp["out_norm_g"][l][0]
        ong[1024:1536] = inp["out_norm_g"][l][1]
        ong[1536:2048] = inp["out_norm_g"][l][2]
        m.update({"g1a" + L_: col16(inp["ffn1_norm_g"][l]), "g2a" + L_: col16(inp["mix_norm_g"][l]),
                  "f1wg" + L_: inp["ffn1_w_gate"][l], "f1wu" + L_: inp["ffn1_w_up"][l], "f1wd" + L_: inp["ffn1_w_down"][l],
                  "win" + L_: inp["w_in"][l], "bin" + L_: np.ascontiguousarray(binp.reshape(NZC, 128).T),
                  "ong" + L_: col16(ong), "wout" + L_: inp["w_out"][l], "g1c" + L_: col16(inp["ffn2_norm_g"][l]),
                  "f2wg" + L_: inp["ffn2_w_gate"][l], "f2wu" + L_: inp["ffn2_w_up"][l], "f2wd" + L_: inp["ffn2_w_down"][l]})
    m["gf"] = col16(inp["final_norm_g"])
    nsc = nsa_static()
    m["nsa_keep"] = nsc["keep"]; m["nsa_add"] = nsc["add"]; m["nsa_E"] = nsc["E"]; m["nsa_ov"] = nsc["ov"]
    return m


def prep_core(inp, core):
    j = core % 4
    m = {}
    for l in range(2):
        m.update(prep_mix(inp, l, j))
    order = [(j + 1 + hh) % 4 for hh in range(4)]
    rb = np.asarray(inp["rel_bias"], np.float32)
    nsc = nsa_static()
    bcs = np.where(nsc["bc_mask"][:, :, None, :], rb[nsc["bc_idx"]][..., order].transpose(0, 1, 3, 2), NEG)
    m["nsa_bc"] = np.ascontiguousarray(bcs.astype(np.float32))
    m["nsa_tbs"] = np.where(nsc["tbs_mask"], rb[nsc["tbs_idx"], j], NEG).astype(np.float32)
    m["nsa_tbw"] = np.where(nsc["tbw_mask"], rb[nsc["tbw_idx"], j], NEG).astype(np.float32)
    mk = np.zeros((128, 16), np.float32)
    for hh in range(4):
        mk[:, 4 * hh + (j + hh) % 4] = 1.0
    m["msk"] = mk
    return m


_PROG = {}


def run_fused(inputs, dbg=False):
    inp = {k_: np.asarray(v, np.float32) for k_, v in inputs.items()}
    x = inp["x"].reshape(8 * TOK, D)
    key = "dbg" if dbg else "main"
    if key not in _PROG:
        _PROG[key] = build_fused(dbg)
    nc = _PROG[key]
    shared = prep_shared(inp)
    maps = []
    for c in range(8):
        m = dict(shared)
        m.update(prep_core(inp, c))
        m["x"] = np.ascontiguousarray(x[c * TOK:(c + 1) * TOK])
        maps.append(m)
    res = run_bass_kernel_spmd(nc, maps, core_ids=list(range(8)))
    return res.results


def kernel(**inputs):
    res = run_fused(inputs)
    out = np.concatenate([r["out"] for r in res], axis=0).reshape(2, T_, D)
    return np.ascontiguousarray(out.astype(np.float32))


_NSC = {}


def t5_bucket_static(dist):
    import math
    import jax
    import jax.numpy as jnp
    with jax.default_device(jax.devices("cpu")[0]):
        n = jnp.maximum(jnp.asarray(dist, jnp.int32), 0)
        nf = jnp.maximum(n, 1).astype(jnp.float32)
        large = 16 + (jnp.log(nf / 16) / math.log(1024 / 16) * (32 - 16)).astype(jnp.int32)
        large = jnp.minimum(large, 31)
        return np.asarray(jnp.where(n < 16, n, large))


def nsa_static():
    if _NSC:
        return _NSC
    p = np.arange(128)
    col = np.arange(512)
    q = np.arange(T_)
    n = (np.arange(2)[:, None] * 128 + p[None, :])
    d = q[None, None, :] - (16 * n[:, :, None] + 31)
    msk = (d >= 0) & (n[:, :, None] < 255)
    _NSC["bc_idx"] = t5_bucket_static(d).transpose(1, 0, 2)
    _NSC["bc_mask"] = msk.transpose(1, 0, 2)
    ms = np.arange(12) - 3
    d = 128 * ms[None, :, None] + col[None, None, :] - p[:, None, None]
    _NSC["tbs_idx"] = t5_bucket_static(d); _NSC["tbs_mask"] = d >= 0
    mw = np.arange(8) - 3
    d = 128 * mw[None, :, None] + col[None, None, :] - p[:, None, None]
    _NSC["tbw_idx"] = t5_bucket_static(d); _NSC["tbw_mask"] = (d >= 0) & (d < 512)
    qpos = np.arange(NT)[None, :, None] * 128 + p[:, None, None]
    cur = qpos // 64
    jj = np.arange(64)[None, None, :]
    forced = (jj == 0) | (jj == cur) | (jj == cur - 1)
    fut = jj > cur
    _NSC["keep"] = np.where(forced | fut, 0.0, 1.0).astype(np.float32)
    _NSC["add"] = np.where(fut, -1.0, np.where(forced, 1.0e6, 0.0)).astype(np.float32)
    _NSC["E"] = (np.arange(T_)[None, :] // 64 == np.arange(64)[:, None]).astype(np.float32)
    nn = np.arange(256)
    cst, cen = nn * 16, nn * 16 + 31
    sst, sen = np.arange(64) * 64, np.arange(64) * 64 + 63
    ovl = ((cst[:, None] <= sen[None, :]) & (cen[:, None] >= sst[None, :]) & (nn[:, None] < 255)).astype(np.float32)
    _NSC["ov"] = np.ascontiguousarray(ovl.reshape(2, 128, 64).transpose(1, 0, 2))
    return _NSC
```

```python
import numpy as np
import ml_dtypes
from contextlib import ExitStack
import concourse.bass as bass
import concourse.mybir as mybir
from concourse.bass_utils import run_bass_kernel_spmd

F32 = mybir.dt.float32
BF16 = mybir.dt.bfloat16
I32 = mybir.dt.int32
SP_POOL = [mybir.EngineType.SP, mybir.EngineType.Pool]
AF = mybir.ActivationFunctionType
ALU = mybir.AluOpType
AX = mybir.AxisListType

D = 2048
NCH = 16
DFF = 5632
NF = 44
TOK = 1024
TG = 512
DIN = 5912
NZC = 47
EPS = 1e-6
NEG = -30000.0


class St:
    __slots__ = ("w", "r")

    def __init__(self, w=None, r=None):
        self.w = w
        self.r = list(r) if r else []

    def copy(self):
        return St(self.w, self.r)


class T:
    def __init__(self, handle, name):
        self.h = handle
        self.name = name
        self.whole = St()
        self.cells = {}

    def __getitem__(self, idx):
        return self.h[idx]

    def states(self, key):
        if key is None:
            return [self.whole] + list(self.cells.values())
        if key not in self.cells:
            self.cells[key] = self.whole.copy()
        return [self.cells[key]]


class K:
    NSLOT = 6

    def __init__(self, nc, es):
        self.nc = nc
        self.es = es
        self.eng = {"pe": nc.tensor, "dve": nc.vector, "act": nc.scalar, "pool": nc.gpsimd, "sp": nc.sync}
        self.sem = {}
        self.cnt = {}
        for e in self.eng:
            self.sem[e] = es.enter_context(nc.semaphore("s_" + e))
            self.cnt[e] = 0
        self.known = {e: {} for e in self.eng}
        self.dsem = {}
        self.duse = {}
        self.dnext = {}
        for q in ("sp", "pool"):
            self.dnext[q] = 0
            for s in range(self.NSLOT):
                key = ("d", q, s)
                self.dsem[key] = es.enter_context(nc.semaphore("d_%s_%d" % (q, s)))
                self.duse[key] = 0
        self.ntile = 0
        self.dins = {}
        self.es_root = es
        self.ncc = 0

    def sb(self, shape, dt, name=None):
        self.ntile += 1
        name = "%s_%d" % (name or "t", self.ntile)
        h = self.es.enter_context(self.nc.sbuf_tensor(name, list(shape), dt))
        return T(h, name)

    def ps(self, shape, dt, name=None):
        self.ntile += 1
        name = "%s_%d" % (name or "p", self.ntile)
        h = self.es.enter_context(self.nc.psum_tensor(name, list(shape), dt))
        return T(h, name)

    def din(self, name, shape, dt=F32):
        if name not in self.dins:
            self.dins[name] = T(self.nc.dram_tensor(name, list(shape), dt, kind="ExternalInput").ap(), name)
        return self.dins[name]

    def barrier(self):
        for e in self.eng:
            self.wait_all(e)

    def scope(self):
        return _Scope(self)

    def dout(self, name, shape, dt=F32):
        return T(self.nc.dram_tensor(name, list(shape), dt, kind="ExternalOutput").ap(), name)

    def semh(self, key):
        return self.sem[key] if key in self.sem else self.dsem[key]

    def _deps(self, reads, writes):
        deps = set()
        for (t, key) in reads:
            for st in t.states(key):
                if st.w is not None:
                    deps.add(st.w)
        for (t, key) in writes:
            for st in t.states(key):
                if st.w is not None:
                    deps.add(st.w)
                deps.update(st.r)
        return deps

    def _wait(self, eng, deps):
        need = {}
        kn = self.known[eng]
        for (sk, val) in deps:
            if sk == "pe" and eng == "pe":
                continue
            if kn.get(sk, 0) < val and need.get(sk, 0) < val:
                need[sk] = val
        for sk, val in need.items():
            self.eng[eng].wait_ge(self.semh(sk), val)
            kn[sk] = val

    def _commit(self, ev, reads, writes):
        for (t, key) in reads:
            for st in t.states(key):
                st.r.append(ev)
        for (t, key) in writes:
            if key is None:
                t.cells.clear()
                t.whole.w = ev
                t.whole.r = []
            else:
                st = t.states(key)[0]
                st.w = ev
                st.r = []

    @staticmethod
    def _norm(lst):
        out = []
        for x in lst or []:
            out.append(x if isinstance(x, tuple) else (x, None))
        return out

    def op(self, eng, fn, reads=None, writes=None):
        reads = self._norm(reads)
        writes = self._norm(writes)
        self._wait(eng, self._deps(reads, writes))
        ins = fn()
        self.cnt[eng] += 1
        ins.then_inc(self.sem[eng], 1)
        ev = (eng, self.cnt[eng])
        self._commit(ev, reads, writes)
        return ev

    def dma(self, q, out_ap, in_ap, reads=None, writes=None, **kw):
        reads = self._norm(reads)
        writes = self._norm(writes)
        deps = self._deps(reads, writes)
        s = self.dnext[q]
        self.dnext[q] = (s + 1) % self.NSLOT
        key = ("d", q, s)
        if self.duse[key] > 0:
            deps.add((key, 16 * self.duse[key]))
        self._wait(q, deps)
        ins = self.eng[q].dma_start(out=out_ap, in_=in_ap, **kw)
        self.duse[key] += 1
        ins.then_inc(self.dsem[key], 16)
        ev = (key, 16 * self.duse[key])
        self._commit(ev, reads, writes)
        return ev

    def allgather(self, src_ap, dst_t, reads):
        reads = self._norm(reads)
        writes = [(dst_t, None)]
        self._wait("pool", self._deps(reads, writes))
        cs = self.es_root.enter_context(self.nc.semaphore("cc%d" % self.ncc))
        key = ("c", self.ncc)
        self.ncc += 1
        self.dsem[key] = cs
        self.nc.gpsimd.collective_compute("AllGather", ALU.bypass, replica_groups=[[0, 1, 2, 3], [4, 5, 6, 7]],
                                          ins=[src_ap.opt()], outs=[dst_t.h.opt()]).then_inc(cs)
        ev = (key, 1)
        self._commit(ev, reads, writes)
        return ev

    def wait_all(self, eng="sp"):
        kn = self.known[eng]
        for e in self.eng:
            if self.cnt[e] > kn.get(e, 0):
                self.eng[eng].wait_ge(self.sem[e], self.cnt[e])
                kn[e] = self.cnt[e]
        for key, n in self.duse.items():
            if 16 * n > kn.get(key, 0):
                self.eng[eng].wait_ge(self.dsem[key], 16 * n)
                kn[key] = 16 * n


class _Scope:
    def __init__(self, k):
        self.k = k

    def __enter__(self):
        self.old = self.k.es
        self.sub = ExitStack()
        self.sub.__enter__()
        self.k.es = self.sub
        return self

    def __exit__(self, *a):
        self.k.barrier()
        self.k.es = self.old
        return self.sub.__exit__(*a)


class Dense:
    def __init__(self, k, h):
        self.k = k
        nc = k.nc
        self.nc = nc
        self.h = h
        self.hn = k.sb([128, NCH, TOK], BF16, "hn")
        self.sq = [k.sb([128, TG], BF16, "sq%d" % i) for i in range(2)]
        self.rstd = k.sb([128, TG], F32, "rstd")
        self.onesm = k.sb([128, 128], BF16, "onesm")
        self.ones4 = k.sb([128, 128], BF16, "ones4")
        self.gcol = k.sb([128, NCH], F32, "gcol")
        self.wg = [k.sb([128, NCH, 256], BF16, "wg%d" % i) for i in range(2)]
        self.wu = [k.sb([128, NCH, 256], BF16, "wu%d" % i) for i in range(2)]
        self.wd = [k.sb([128, 2, D], BF16, "wd%d" % i) for i in range(2)]
        self.act = [k.sb([128, 2, TOK], BF16, "act%d" % i) for i in range(2)]
        self.sg = [k.sb([128, TG], F32, "sg%d" % i) for i in range(2)]
        self.psA = [k.ps([128, TG], F32, "psA%d" % i) for i in range(4)]
        self.psB = [k.ps([128, TG], F32, "psB%d" % i) for i in range(3)]
        self.psN = k.ps([128, TG], F32, "psN")
        self.ia = 0
        self.ib = 0
        self.epsc = k.sb([128, 1], F32, "epsc")
        k.op("dve", lambda: nc.vector.memset(self.epsc[:], EPS), writes=[self.epsc])
        k.op("dve", lambda: nc.vector.memset(self.onesm[:], 1.0 / D), writes=[self.onesm])
        k.op("dve", lambda: nc.vector.memset(self.ones4[:], 1.0 / 512), writes=[self.ones4])

    def rstd_from(self, ps):
        k, nc = self.k, self.nc
        k.op("act", lambda: nc.scalar.activation(out=self.rstd[:], in_=ps[:], func=AF.Ln, bias=self.epsc[:]),
             reads=[ps, self.epsc], writes=[self.rstd])
        k.op("act", lambda: nc.scalar.activation(out=self.rstd[:], in_=self.rstd[:], func=AF.Exp, scale=-0.5),
             reads=[self.rstd], writes=[self.rstd])

    def load_h(self, src):
        k = self.k
        v = src.h.rearrange("(c p) t -> p c t", p=128)
        for c in range(0, NCH, 4):
            k.dma("sp", self.h[:, c:c + 4, :], v[:, c:c + 4, :], reads=[src],
                  writes=[(self.h, (cc, tg)) for cc in range(c, c + 4) for tg in range(2)])

    def store_h(self, dst):
        k = self.k
        v = dst.h.rearrange("(c p) t -> p c t", p=128)
        for c in range(0, NCH, 4):
            k.dma("sp", v[:, c:c + 4, :], self.h[:, c:c + 4, :],
                  reads=[(self.h, (cc, tg)) for cc in range(c, c + 4) for tg in range(2)], writes=[(dst, c)])

    def rmsnorm(self, gsrc, out_t=None, out_f32=None):
        k, nc = self.k, self.nc
        k.dma("sp", self.gcol[:], gsrc.h, reads=[gsrc], writes=[self.gcol])
        for tg in range(2):
            ts = slice(tg * TG, (tg + 1) * TG)
            for c in range(NCH):
                sq = self.sq[c % 2]
                k.op("act", lambda sq=sq, c=c: nc.scalar.activation(out=sq[:], in_=self.h[:, c, ts], func=AF.Square),
                     reads=[(self.h, (c, tg))], writes=[sq])
                k.op("pe", lambda sq=sq, c=c: nc.tensor.matmul(self.psN[:], lhsT=self.onesm[:], rhs=sq[:],
                                                               start=(c == 0), stop=(c == NCH - 1)),
                     reads=[sq, self.onesm], writes=[self.psN])
            self.rstd_from(self.psN)
            for c in range(NCH):
                if out_f32 is None:
                    k.op("dve", lambda c=c: nc.vector.scalar_tensor_tensor(
                        out=self.hn[:, c, ts], in0=self.h[:, c, ts], scalar=self.gcol[:, c:c + 1], in1=self.rstd[:],
                        op0=ALU.mult, op1=ALU.mult),
                        reads=[(self.h, (c, tg)), self.gcol, self.rstd], writes=[(self.hn, tg)])
                else:
                    k.op("dve", lambda c=c: nc.vector.scalar_tensor_tensor(
                        out=out_f32[:, c, ts], in0=self.h[:, c, ts], scalar=self.gcol[:, c:c + 1], in1=self.rstd[:],
                        op0=ALU.mult, op1=ALU.mult),
                        reads=[(self.h, (c, tg)), self.gcol, self.rstd], writes=[(out_f32, (c, tg))])

    def ffn(self, wg_d, wu_d, wd_d):
        k, nc = self.k, self.nc
        wgv = wg_d.h.rearrange("(c p) f -> p c f", p=128)
        wuv = wu_d.h.rearrange("(c p) f -> p c f", p=128)
        wdv = wd_d.h.rearrange("(c p) d -> p c d", p=128)
        NG = NF // 2

        def load(g):
            b = g % 2
            k.dma("pool", self.wg[b][:], wgv[:, :, g * 256:(g + 1) * 256], reads=[wg_d], writes=[self.wg[b]])
            k.dma("pool", self.wu[b][:], wuv[:, :, g * 256:(g + 1) * 256], reads=[wu_d], writes=[self.wu[b]])
            k.dma("pool", self.wd[b][:], wdv[:, 2 * g:2 * g + 2, :], reads=[wd_d], writes=[self.wd[b]])

        load(0)
        for g in range(NG):
            if g + 1 < NG:
                load(g + 1)
            b = g % 2
            wg, wu, wd, act = self.wg[b], self.wu[b], self.wd[b], self.act[b]
            for fcl in range(2):
                fs = slice(fcl * 128, (fcl + 1) * 128)
                for tg in range(2):
                    ts = slice(tg * TG, (tg + 1) * TG)
                    pg = self.psA[self.ia % 4]
                    pu = self.psA[(self.ia + 1) % 4]
                    self.ia += 2

                    def mm(p, w):
                        ins = None
                        for c in range(NCH):
                            ins = nc.tensor.matmul(p[:], lhsT=w[:, c, fs], rhs=self.hn[:, c, ts],
                                                   start=(c == 0), stop=(c == NCH - 1))
                        return ins
                    k.op("pe", lambda: mm(pg, wg), reads=[wg, (self.hn, tg)], writes=[pg])
                    k.op("pe", lambda: mm(pu, wu), reads=[wu, (self.hn, tg)], writes=[pu])
                    sg = self.sg[tg]
                    k.op("act", lambda: nc.scalar.activation(out=sg[:], in_=pg[:], func=AF.Silu), reads=[pg], writes=[sg])
                    k.op("dve", lambda: nc.vector.tensor_tensor(out=act[:, fcl, ts], in0=pu[:], in1=sg[:], op=ALU.mult),
                         reads=[pu, sg], writes=[(act, (fcl, tg))])
            for dc in range(NCH):
                ds = slice(dc * 128, (dc + 1) * 128)
                for tg in range(2):
                    ts = slice(tg * TG, (tg + 1) * TG)
                    pd = self.psB[self.ib % 3]
                    self.ib += 1

                    def mmd():
                        ins = None
                        for fcl in range(2):
                            ins = nc.tensor.matmul(pd[:], lhsT=wd[:, fcl, ds], rhs=act[:, fcl, ts],
                                                   start=(fcl == 0), stop=(fcl == 1))
                        return ins
                    k.op("pe", mmd, reads=[wd, (act, (0, tg)), (act, (1, tg))], writes=[pd])
                    k.op("dve", lambda: nc.vector.scalar_tensor_tensor(
                        out=self.h[:, dc, ts], in0=pd[:], scalar=0.5, in1=self.h[:, dc, ts], op0=ALU.mult, op1=ALU.add),
                        reads=[pd, (self.h, (dc, tg))], writes=[(self.h, (dc, tg))])

    def proj(self, w_d, ncols, rhs_t, emit):
        k, nc = self.k, self.nc
        wv = w_d.h.rearrange("(c p) f -> p c f", p=128)
        ngr = (ncols + 255) // 256

        def load(g):
            b = g % 2
            c0 = g * 256
            c1 = min(ncols, c0 + 256)
            k.dma("pool", self.wg[b][:, :, 0:c1 - c0], wv[:, :, c0:c1], reads=[w_d], writes=[self.wg[b]])

        load(0)
        for g in range(ngr):
            if g + 1 < ngr:
                load(g + 1)
            w = self.wg[g % 2]
            for ml in range(2):
                m = 2 * g + ml
                M = min(128, ncols - m * 128)
                if M <= 0:
                    continue
                for tg in range(2):
                    ts = slice(tg * TG, (tg + 1) * TG)
                    pd = self.psB[self.ib % 3]
                    self.ib += 1

                    def mm():
                        ins = None
                        for c in range(NCH):
                            ins = nc.tensor.matmul(pd[0:M, :], lhsT=w[:, c, ml * 128:ml * 128 + M], rhs=rhs_t[:, c, ts],
                                                   start=(c == 0), stop=(c == NCH - 1))
                        return ins
                    k.op("pe", mm, reads=[w, (rhs_t, tg)], writes=[pd])
                    emit(m, M, tg, ts, pd)


def dense_A(k, h, l, zsh, zall):
    nc = k.nc
    g1 = k.din("g1a%d" % l, [128, NCH]); g2 = k.din("g2a%d" % l, [128, NCH])
    wg = k.din("f1wg%d" % l, [D, DFF]); wu = k.din("f1wu%d" % l, [D, DFF]); wd = k.din("f1wd%d" % l, [DFF, D])
    win = k.din("win%d" % l, [D, DIN]); bin_ = k.din("bin%d" % l, [128, NZC])
    dn = Dense(k, h)
    bcol = k.sb([128, NZC], F32, "bcol")
    zs = [k.sb([128, TG], F32, "zs%d" % i) for i in range(3)]
    k.dma("sp", bcol[:], bin_.h, reads=[bin_], writes=[bcol])
    dn.rmsnorm(g1)
    dn.ffn(wg, wu, wd)
    dn.rmsnorm(g2)
    cnt = [0]

    def emit(m, M, tg, ts, pd):
        z = zs[cnt[0] % 3]
        cnt[0] += 1
        k.op("act", lambda: nc.scalar.activation(out=z[0:M, :], in_=pd[0:M, :], func=AF.Identity,
                                                 bias=bcol[0:M, m:m + 1]), reads=[pd, bcol], writes=[z])
        k.dma("sp", zsh.h[m * 128:m * 128 + M, ts], z[0:M, :], reads=[z], writes=[(zsh, (m, tg))])
        if tg == 1 and (m % 2 == 1 or m == NZC - 1):
            c0, cr, ct = zall.chunks[m // 2]
            k.allgather(zsh.h[c0:c0 + cr, :], ct, [(zsh, (mm_, t_)) for mm_ in (2 * (m // 2), 2 * (m // 2) + 1) for t_ in (0, 1) if mm_ < NZC])
    assert zall.CR == 256
    dn.proj(win, DIN, dn.hn, emit)


def dense_C(k, h, l, yall, rv):
    nc = k.nc
    ong = k.din("ong%d" % l, [128, NCH]); wout = k.din("wout%d" % l, [D, D]); g1 = k.din("g1c%d" % l, [128, NCH])
    wg = k.din("f2wg%d" % l, [D, DFF]); wu = k.din("f2wu%d" % l, [D, DFF]); wd = k.din("f2wd%d" % l, [DFF, D])
    dn = Dense(k, h)
    ys = [k.sb([128, 4, TG], F32, "ys%d" % i) for i in range(2)]
    ocol = k.sb([128, NCH], F32, "ocol")
    k.dma("sp", ocol[:], ong.h, reads=[ong], writes=[ocol])
    yst = [k.sb([128, 4, TG], F32, "yst%d" % i) for i in range(2)]
    mskc = k.sb([128, 16], F32, "mskc")
    dmsk = k.din("msk", [128, 16])
    k.dma("sp", mskc[:], dmsk.h, reads=[dmsk], writes=[mskc])
    i = 0
    for grp in range(4):
        for tg in range(2):
            ts = slice(tg * TG, (tg + 1) * TG)
            y = ys[i % 2]
            i += 1
            for rc in range(4):
                st_ = yst[rc % 2]
                for jp in range(4):
                    for (ct, sap_, d0, cnt) in yall.pieces(jp, grp * 128, 128):
                        k.dma("sp", st_[d0:d0 + cnt, jp, :], sap_[:, rc * TOK + tg * TG:rc * TOK + (tg + 1) * TG], reads=[ct], writes=[st_])
                mc = mskc[:, rc:rc + 1]
                if rc == 0:
                    k.op("dve", lambda: nc.vector.tensor_scalar(out=y[:], in0=st_[:], scalar1=mc, scalar2=None, op0=ALU.mult), reads=[st_, mskc], writes=[y])
                else:
                    k.op("dve", lambda: nc.vector.scalar_tensor_tensor(out=y[:], in0=st_[:], scalar=mc, in1=y[:], op0=ALU.mult, op1=ALU.add),
                         reads=[st_, mskc, y], writes=[y])
            if grp == 1:
                for cl in range(4):
                    k.op("dve", lambda cl=cl: nc.vector.tensor_copy(out=dn.hn[:, 4 + cl, ts], in_=y[:, cl, :]),
                         reads=[y], writes=[(dn.hn, tg)])
                continue
            for cl in range(4):
                sq = dn.sq[cl % 2]
                k.op("act", lambda sq=sq, cl=cl: nc.scalar.activation(out=sq[:], in_=y[:, cl, :], func=AF.Square),
                     reads=[y], writes=[sq])
                k.op("pe", lambda sq=sq, cl=cl: nc.tensor.matmul(dn.psN[:], lhsT=dn.ones4[:], rhs=sq[:],
                                                                 start=(cl == 0), stop=(cl == 3)),
                     reads=[sq, dn.ones4], writes=[dn.psN])
            dn.rstd_from(dn.psN)
            for cl in range(4):
                c = grp * 4 + cl
                k.op("dve", lambda cl=cl, c=c: nc.vector.scalar_tensor_tensor(
                    out=dn.hn[:, c, ts], in0=y[:, cl, :], scalar=ocol[:, c:c + 1], in1=dn.rstd[:],
                    op0=ALU.mult, op1=ALU.mult), reads=[y, ocol, dn.rstd], writes=[(dn.hn, tg)])

    def emit(m, M, tg, ts, pd):
        k.op("dve", lambda: nc.vector.tensor_tensor(out=h[:, m, ts], in0=pd[:], in1=h[:, m, ts], op=ALU.add),
             reads=[pd, (h, (m, tg))], writes=[(h, (m, tg))])
    dn.proj(wout, D, dn.hn, emit)
    dn.rmsnorm(g1)
    dn.ffn(wg, wu, wd)


def final_out(k, h, out):
    nc = k.nc
    sq_ = [k.sb([128, TG], BF16, "sq%d" % i) for i in range(2)]
    rstd = k.sb([128, TG], F32, "rstd")
    onesm = k.sb([128, 128], BF16, "onesm")
    gcol = k.sb([128, NCH], F32, "gcol")
    epsc = k.sb([128, 1], F32, "epsc")
    psN = k.ps([128, TG], F32, "psN")
    psB = [k.ps([128, TG], F32, "psB%d" % i) for i in range(3)]
    ib = [0]
    k.op("dve", lambda: nc.vector.memset(epsc[:], EPS), writes=[epsc])
    k.op("dve", lambda: nc.vector.memset(onesm[:], 1.0 / D), writes=[onesm])

    def rstd_from():
        k.op("act", lambda: nc.scalar.activation(out=rstd[:], in_=psN[:], func=AF.Ln, bias=epsc[:]), reads=[psN, epsc], writes=[rstd])
        k.op("act", lambda: nc.scalar.activation(out=rstd[:], in_=rstd[:], func=AF.Exp, scale=-0.5), reads=[rstd], writes=[rstd])
    gf = k.din("gf", [128, NCH])
    identF = k.sb([128, 128], F32, "identF")
    cid = k.din("c_ident", [128, 128])
    k.dma("sp", identF[:], cid.h, reads=[cid], writes=[identF])
    fin = k.sb([128, NCH, TG], F32, "fin")
    ot = [k.sb([128, D], F32, "ot%d" % i) for i in range(2)]
    k.dma("sp", gcol[:], gf.h, reads=[gf], writes=[gcol])
    for tg in range(2):
        ts = slice(tg * TG, (tg + 1) * TG)
        for c in range(NCH):
            sq = sq_[c % 2]
            k.op("act", lambda sq=sq, c=c: nc.scalar.activation(out=sq[:], in_=h[:, c, ts], func=AF.Square),
                 reads=[(h, (c, tg))], writes=[sq])
            k.op("pe", lambda sq=sq, c=c: nc.tensor.matmul(psN[:], lhsT=onesm[:], rhs=sq[:],
                                                           start=(c == 0), stop=(c == NCH - 1)),
                 reads=[sq, onesm], writes=[psN])
        rstd_from()
        for c in range(NCH):
            k.op("dve", lambda c=c: nc.vector.scalar_tensor_tensor(
                out=fin[:, c, :], in0=h[:, c, ts], scalar=gcol[:, c:c + 1], in1=rstd[:],
                op0=ALU.mult, op1=ALU.mult), reads=[(h, (c, tg)), gcol, rstd], writes=[(fin, c)])
        for tt in range(4):
            o = ot[tt % 2]
            for c4 in range(4):
                pd = psB[ib[0] % 3]
                ib[0] += 1

                def mmT():
                    ins = None
                    for cl in range(4):
                        c = c4 * 4 + cl
                        ins = nc.tensor.matmul(pd[:, cl * 128:(cl + 1) * 128], lhsT=fin[:, c, tt * 128:(tt + 1) * 128],
                                               rhs=identF[:], start=True, stop=True)
                    return ins
                k.op("pe", mmT, reads=[identF] + [(fin, c4 * 4 + cl) for cl in range(4)], writes=[pd])
                k.op("act", lambda: nc.scalar.copy(out=o[:, c4 * 512:(c4 + 1) * 512], in_=pd[:]), reads=[pd], writes=[(o, c4)])
            r0 = tg * TG + tt * 128
            k.dma("sp", out.h[r0:r0 + 128, :], o[:], reads=[o], writes=[(out, r0)])


def load_x(k, h, x):
    nc = k.nc
    identF = k.sb([128, 128], F32, "identF")
    cid = k.din("c_ident", [128, 128])
    k.dma("sp", identF[:], cid.h, reads=[cid], writes=[identF])
    xt = [k.sb([128, D], F32, "xt%d" % i) for i in range(4)]
    pst = [k.ps([128, TG], F32, "pst%d" % i) for i in range(4)]
    n = 0
    for tg in range(2):
        for tt in range(4):
            r0 = tg * TG + tt * 128
            k.dma("sp", xt[tt][:], x.h[r0:r0 + 128, :], reads=[x], writes=[xt[tt]])
        for c in range(NCH):
            pd = pst[n % 4]
            n += 1

            def mmT():
                ins = None
                for tt in range(4):
                    ins = nc.tensor.matmul(pd[:, tt * 128:(tt + 1) * 128], lhsT=xt[tt][:, c * 128:(c + 1) * 128], rhs=identF[:],
                                           start=True, stop=True)
                return ins
            k.op("pe", mmT, reads=[identF] + xt, writes=[pd])
            eng = "act" if c % 2 else "dve"
            if eng == "act":
                k.op("act", lambda: nc.scalar.copy(out=h[:, c, tg * TG:(tg + 1) * TG], in_=pd[:]), reads=[pd], writes=[(h, (c, tg))])
            else:
                k.op("dve", lambda: nc.vector.tensor_copy(h[:, c, tg * TG:(tg + 1) * TG], pd[:]), reads=[pd], writes=[(h, (c, tg))])


def col16(g):
    return np.ascontiguousarray(np.asarray(g, np.float32).reshape(NCH, 128).T)


T_ = 4096
NT = 32
SCALE = 128 ** -0.5


class Mix:
    def __init__(self, k, l, zall, vals, nbig=7, nbigb=4):
        self.k = k
        self.l = l
        self.zall = zall
        nc = self.nc = k.nc
        self.cident = k.din("c_ident", [128, 128])
        self.msk = k.sb([128, 16], F32, "msk")
        dmsk = k.din("msk", [128, 16])
        k.dma("sp", self.msk[:], dmsk.h, reads=[dmsk], writes=[self.msk])
        self.identF = k.sb([128, 128], F32, "identF")
        self.identB = k.sb([128, 128], BF16, "identB")
        self.onesF = k.sb([128, 128], F32, "onesF")
        self.onesB = k.sb([128, 128], BF16, "onesB")
        k.dma("sp", self.identF[:], self.cident.h, reads=[self.cident], writes=[self.identF])
        k.op("dve", lambda: nc.vector.tensor_copy(self.identB[:], self.identF[:]), reads=[self.identF], writes=[self.identB])
        k.op("dve", lambda: nc.vector.memset(self.onesF[:], 1.0), writes=[self.onesF])
        k.op("dve", lambda: nc.vector.memset(self.onesB[:], 1.0), writes=[self.onesB])
        self.big = [k.sb([128, T_], F32, "big%d" % i) for i in range(nbig)]
        self.bigb = [k.sb([128, T_], BF16, "bigb%d" % i) for i in range(nbigb)]
        self.psS = [k.ps([128, 512], F32, "psS%d" % i) for i in range(3)]
        self.psO = [k.ps([128, 512], F32, "psO%d" % i) for i in range(2)]
        self.psL = [k.ps([128, 512], F32, "psL%d" % i) for i in range(2)]
        self.psX = k.ps([128, 512], F32, "psX")
        self.iS = 0
        self.pT = [k.sb([128, 512], BF16, "pT%d" % i) for i in range(3)]
        self.tmpA = [k.sb([128, 512], F32, "tmpA%d" % i) for i in range(4)]

    def zfm(self, dst_t, dst_ap, off, dyn=None, q="sp", n=128, key=None, mul=128, stage=None):
        k, nc = self.k, self.nc
        dap = dst_ap[:] if not hasattr(dst_ap, "ap") else dst_ap
        if dyn is None:
            for s_ in range(4):
                for (ct, sap_, d0, cnt) in self.zall.pieces(s_, off, n):
                    k.dma(q, dap[d0:d0 + cnt, s_ * TOK:(s_ + 1) * TOK], sap_, reads=[ct], writes=[(dst_t, key)])
            return
        sap = stage[:]
        for jc in range(4):
            for s_ in range(4):
                for (ct, sap_, d0, cnt) in self.zall.pieces(s_, off + jc * mul, n):
                    k.dma(q, sap[d0:d0 + cnt, s_ * TOK:(s_ + 1) * TOK], sap_, reads=[ct], writes=[stage])
            mc = self.msk[0:n, dyn + jc:dyn + jc + 1]
            if jc == 0:
                k.op("dve", lambda: nc.vector.tensor_scalar(out=dap[0:n, :], in0=sap[0:n, :], scalar1=mc, scalar2=None, op0=ALU.mult),
                     reads=[stage, self.msk], writes=[(dst_t, key)])
            else:
                k.op("dve", lambda: nc.vector.scalar_tensor_tensor(out=dap[0:n, :], in0=sap[0:n, :], scalar=mc, in1=dap[0:n, :], op0=ALU.mult, op1=ALU.add),
                     reads=[stage, self.msk, (dst_t, key)], writes=[(dst_t, key)])

    def row2col(self, row, dst):
        k, nc = self.k, self.nc
        px = self.psX

        def mm():
            ins = None
            for kt in range(NT):
                ins = nc.tensor.matmul(px[:, kt:kt + 1], lhsT=row[0:1, kt * 128:(kt + 1) * 128], rhs=self.onesF[0:1, 0:1], start=True, stop=True)
            return ins
        k.op("pe", mm, reads=[row, self.onesF], writes=[px])
        k.op("dve", lambda: nc.vector.tensor_copy(dst[:], px[:, 0:NT]), reads=[px], writes=[dst])

    def fm2tm(self, src_ap, src_dep, dst_t, dst_ap, key=None):
        k, nc = self.k, self.nc
        for g4 in range(NT // 4):
            ps = self.psS[g4 % 3]

            def mm():
                ins = None
                for i in range(4):
                    kt = g4 * 4 + i
                    ins = nc.tensor.matmul(ps[:, i * 128:(i + 1) * 128], lhsT=src_ap[:, kt * 128:(kt + 1) * 128], rhs=self.identB[:], start=True, stop=True)
                return ins
            k.op("pe", mm, reads=[src_dep, self.identB], writes=[ps])
            if g4 % 2:
                k.op("act", lambda: nc.scalar.copy(out=dst_ap[:, g4 * 512:(g4 + 1) * 512], in_=ps[:]), reads=[ps], writes=[(dst_t, key)])
            else:
                k.op("dve", lambda: nc.vector.tensor_copy(dst_ap[:, g4 * 512:(g4 + 1) * 512], ps[:]), reads=[ps], writes=[(dst_t, key)])

    def ld(self, dst, src_t, src_ap=None, q="sp", dst_ap=None):
        self.k.dma(q, dst[:] if dst_ap is None else dst_ap, src_t.h if src_ap is None else src_ap, reads=[src_t], writes=[dst])

    def lru(self, yout):
        k, nc = self.k, self.nc
        L_ = "%d" % self.l
        cw = k.din("lru_cw" + L_, [128, 4]); cb = k.din("lru_cb" + L_, [128, 1])
        wa = k.din("lru_wa" + L_, [128, 128]); wx = k.din("lru_wx" + L_, [128, 128])
        ba = k.din("lru_ba" + L_, [128, 1]); bx = k.din("lru_bx" + L_, [128, 1]); lam = k.din("lru_lam" + L_, [128, 1])
        xs, xc, aa, uu, gg, tt = self.big[0:6]
        hs = gg
        xcb = self.bigb[0]
        cws = k.sb([128, 4], F32, "l_cw"); cbs = k.sb([128, 1], F32, "l_cb")
        was = k.sb([128, 128], BF16, "l_wa"); wxs = k.sb([128, 128], BF16, "l_wx")
        bas = k.sb([128, 1], F32, "l_ba"); bxs = k.sb([128, 1], F32, "l_bx"); lams = k.sb([128, 1], F32, "l_lam")
        nsp = k.sb([128, 1], F32, "l_nsp")
        self.zfm(xs, xs.h, _OFF["lru_x"], 0, stage=tt); self.zfm(gg, gg.h, _OFF["lru_gate"], 0, stage=tt); self.ld(cws, cw); self.ld(cbs, cb); self.ld(bas, ba); self.ld(bxs, bx); self.ld(lams, lam)
        self.ld(was, wa, q="pool"); self.ld(wxs, wx, q="pool")
        k.op("act", lambda: nc.scalar.activation(out=nsp[:], in_=lams[:], func=AF.Exp, scale=-1.0), reads=[lams], writes=[nsp])
        k.op("act", lambda: nc.scalar.activation(out=nsp[:], in_=nsp[:], func=AF.Ln, bias=self.onesF[:, 0:1]),
             reads=[nsp, self.onesF], writes=[nsp])
        k.op("dve", lambda: nc.vector.tensor_scalar(out=nsp[:], in0=nsp[:], scalar1=-8.0, scalar2=None, op0=ALU.mult),
             reads=[nsp], writes=[nsp])
        k.op("dve", lambda: nc.vector.tensor_scalar(out=xc[:], in0=xs[:], scalar1=cws[:, 3:4], scalar2=cbs[:, 0:1],
                                                    op0=ALU.mult, op1=ALU.add), reads=[xs, cws, cbs], writes=[xc])
        for i in range(3):
            sh = 3 - i
            k.op("dve", lambda i=i, sh=sh: nc.vector.scalar_tensor_tensor(
                out=xc[:, sh:], in0=xs[:, 0:T_ - sh], scalar=cws[:, i:i + 1], in1=xc[:, sh:], op0=ALU.mult, op1=ALU.add),
                reads=[xs, cws, xc], writes=[xc])
        k.op("act", lambda: nc.scalar.copy(out=xcb[:], in_=xc[:]), reads=[xc], writes=[xcb])
        for tb in range(8):
            ts = slice(tb * 512, (tb + 1) * 512)
            pr = self.psS[0]; pi = self.psS[1]
            k.op("pe", lambda: nc.tensor.matmul(pr[:], lhsT=was[:], rhs=xcb[:, ts], start=True, stop=True), reads=[was, xcb], writes=[pr])
            k.op("pe", lambda: nc.tensor.matmul(pi[:], lhsT=wxs[:], rhs=xcb[:, ts], start=True, stop=True), reads=[wxs, xcb], writes=[pi])
            r = self.tmpA[0]; ii = self.tmpA[1]
            k.op("act", lambda: nc.scalar.activation(out=r[:], in_=pr[:], func=AF.Sigmoid, bias=bas[:, 0:1]), reads=[pr, bas], writes=[r])
            k.op("act", lambda: nc.scalar.activation(out=ii[:], in_=pi[:], func=AF.Sigmoid, bias=bxs[:, 0:1]), reads=[pi, bxs], writes=[ii])
            k.op("act", lambda: nc.scalar.activation(out=aa[:, ts], in_=r[:], func=AF.Exp, scale=nsp[:, 0:1]),
                 reads=[r, nsp], writes=[(aa, tb)])
            t1 = self.tmpA[2]
            k.op("dve", lambda: nc.vector.tensor_tensor(out=t1[:], in0=aa[:, ts], in1=aa[:, ts], op=ALU.mult), reads=[(aa, tb)], writes=[t1])
            k.op("dve", lambda: nc.vector.tensor_scalar(out=t1[:], in0=t1[:], scalar1=-1.0, scalar2=1.0, op0=ALU.mult, op1=ALU.add),
                 reads=[t1], writes=[t1])
            k.op("act", lambda: nc.scalar.activation(out=t1[:], in_=t1[:], func=AF.Sqrt), reads=[t1], writes=[t1])
            k.op("dve", lambda: nc.vector.tensor_tensor(out=ii[:], in0=ii[:], in1=xc[:, ts], op=ALU.mult), reads=[ii, xc], writes=[ii])
            k.op("dve", lambda: nc.vector.tensor_tensor(out=uu[:, ts], in0=ii[:], in1=t1[:], op=ALU.mult), reads=[ii, t1], writes=[(uu, tb)])
        self.gelu_tanh(tt, gg, xs)
        k.op("dve", lambda: nc.vector.tensor_tensor_scan(out=hs[:], data0=aa[:], data1=uu[:], initial=0.0, op0=ALU.mult, op1=ALU.add),
             reads=[aa, uu], writes=[hs])
        k.op("dve", lambda: nc.vector.tensor_tensor(out=hs[:], in0=hs[:], in1=tt[:], op=ALU.mult), reads=[hs, tt], writes=[hs])
        k.dma("sp", yout.h, hs[:], reads=[hs], writes=[yout])

    def gelu_tanh(self, out, x, tmp):
        k, nc = self.k, self.nc
        k.op("dve", lambda: nc.vector.tensor_tensor(out=tmp[:], in0=x[:], in1=x[:], op=ALU.mult), reads=[x], writes=[tmp])
        k.op("dve", lambda: nc.vector.tensor_scalar(out=tmp[:], in0=tmp[:], scalar1=0.044715, scalar2=1.0, op0=ALU.mult, op1=ALU.add),
             reads=[tmp], writes=[tmp])
        k.op("dve", lambda: nc.vector.tensor_tensor(out=tmp[:], in0=tmp[:], in1=x[:], op=ALU.mult), reads=[tmp, x], writes=[tmp])
        k.op("act", lambda: nc.scalar.activation(out=tmp[:], in_=tmp[:], func=AF.Sigmoid, scale=1.5957691216057308),
             reads=[tmp], writes=[tmp])
        k.op("dve", lambda: nc.vector.tensor_tensor(out=out[:], in0=tmp[:], in1=x[:], op=ALU.mult), reads=[tmp, x], writes=[out])

    def attn_unit(self, kT, kt, qT, q0, extra, bias_ap, bias_reads, V, vslice, O, L, first, last, nkeys=128, kT_t=None, qT_t=None):
        k, nc = self.k, self.nc
        S = self.psS[self.iS % 3]
        P = self.pT[self.iS % 3]
        self.iS += 1
        nk = nkeys

        def mm():
            n = len(extra)
            ins = nc.tensor.matmul(S[0:nk, :], lhsT=kT[:, kt * 128:kt * 128 + nk], rhs=qT[:, q0:q0 + 512], start=True, stop=(n == 0))
            for i, (oa, l, r) in enumerate(extra):
                ins = nc.tensor.matmul(oa(S), lhsT=l, rhs=r, start=False, stop=(i == n - 1))
            return ins
        k.op("pe", mm, reads=[kT_t or kT, qT_t or qT] + self._xr, writes=[S])
        if bias_ap is None:
            k.op("act", lambda: nc.scalar.activation(out=P[0:nk, :], in_=S[0:nk, :], func=AF.Exp), reads=[S], writes=[P])
        else:
            k.op("act", lambda: nc.scalar.activation(out=P[0:nk, :], in_=S[0:nk, :], func=AF.Exp, bias=bias_ap),
                 reads=[S] + bias_reads, writes=[P])
        k.op("pe", lambda: nc.tensor.matmul(O[:], lhsT=vslice[0:nk], rhs=P[0:nk, :], start=first, stop=last), reads=[V, P], writes=[O])
        k.op("pe", lambda: nc.tensor.matmul(L[:], lhsT=self.onesB[0:nk, :], rhs=P[0:nk, :], start=first, stop=last),
             reads=[self.onesB, P], writes=[L])

    def fox(self, yout):
        k, nc = self.k, self.nc
        cut = k.din("c_ut", [128, 128]); csu = k.din("c_su", [32, 32]); cmb = k.din("c_mbfox", [128, 4, 512])
        qf = self.big[0]
        qb, kb = self.bigb[0], self.bigb[1]
        vb = self.bigb[2]
        mb = k.sb([128, 4, 512], BF16, "f_mb")
        ut = k.sb([128, 128], F32, "f_ut"); su = k.sb([32, 32], F32, "f_su")
        lf = k.sb([128, NT], F32, "f_lf"); negc = k.sb([128, NT], F32, "f_negc")
        totc = k.sb([32, 1], F32, "f_totc"); am = k.sb([32, 128], F32, "f_am")
        dg = self.big[1]
        frow = k.sb([1, T_], F32, "f_row")
        vT = self.bigb[3]
        frow2 = k.sb([1, T_], F32, "f_row2")
        self.zfm(kb, kb.h, _OFF["fox_k"], 0, q="pool", stage=qb)
        self.zfm(vT, vT.h, _OFF["fox_v"], 0, q="pool", stage=qb)
        self.zfm(qf, qf.h, _OFF["fox_q"], 0, stage=dg)
        self.zfm(frow, frow.h, _OFF["fox_f"], 0, n=1, mul=1, stage=frow2)
        self.fm2tm(vT.h, vT, vb, vb.h)
        k.dma("pool", mb[:], cmb.h, reads=[cmb], writes=[mb])
        self.ld(ut, cut); self.ld(su, csu)
        self.row2col(frow, lf)
        k.op("dve", lambda: nc.vector.tensor_scalar(out=qb[:], in0=qf[:], scalar1=SCALE, scalar2=None, op0=ALU.mult), reads=[qf], writes=[qb])
        k.op("act", lambda: nc.scalar.activation(out=lf[:], in_=lf[:], func=AF.Exp, scale=-1.0), reads=[lf], writes=[lf])
        k.op("act", lambda: nc.scalar.activation(out=lf[:], in_=lf[:], func=AF.Ln, bias=self.onesF[:, 0:1]), reads=[lf, self.onesF], writes=[lf])
        px = self.psX
        k.op("pe", lambda: nc.tensor.matmul(px[0:32, 0:1], lhsT=lf[:], rhs=self.onesF[:, 0:1], start=True, stop=True),
             reads=[lf, self.onesF], writes=[px])
        k.op("dve", lambda: nc.vector.tensor_copy(totc[:], px[0:32, 0:1]), reads=[px], writes=[totc])
        k.op("dve", lambda: nc.vector.tensor_scalar(out=am[:], in0=self.onesF[0:32, :], scalar1=totc[:, 0:1], scalar2=None, op0=ALU.mult),
             reads=[self.onesF, totc], writes=[am])

        def mmc():
            nc.tensor.matmul(px[:, 0:NT], lhsT=ut[:], rhs=lf[:], start=True, stop=False)
            return nc.tensor.matmul(px[:, 0:NT], lhsT=am[:], rhs=su[:], start=False, stop=True)
        k.op("pe", mmc, reads=[ut, lf, am, su], writes=[px])
        k.op("dve", lambda: nc.vector.tensor_copy(negc[:], px[:, 0:NT]), reads=[px], writes=[negc])
        for qt in range(NT):
            k.op("dve", lambda qt=qt: nc.vector.tensor_scalar(out=dg[:, qt * 128:(qt + 1) * 128], in0=self.identF[:],
                                                              scalar1=negc[:, qt:qt + 1], scalar2=-1.0, op0=ALU.mult, op1=ALU.mult),
                 reads=[self.identF, negc], writes=[(dg, qt)])
        for i in range(8):
            q0 = 512 * i
            O = self.psO[i % 2]; L = self.psL[i % 2]
            nk = 4 * i + 4
            for kt in range(nk):
                extra = []
                for jq in range(4):
                    qt = 4 * i + jq
                    extra.append((lambda S, jq=jq: S[:, jq * 128:(jq + 1) * 128], self.onesF[:], dg[:, qt * 128:(qt + 1) * 128]))
                self._xr = [self.onesF] + [(dg, 4 * i + jq) for jq in range(4)]
                if kt >= 4 * i:
                    extra.append((lambda S: S[:], self.identB[:], mb[:, kt - 4 * i, :]))
                    self._xr += [self.identB, mb]
                self.attn_unit(kb, kt, qb, q0, extra, negc[:, kt:kt + 1], [negc], vb, vb[:, kt * 128:(kt + 1) * 128], O, L,
                               kt == 0, kt == nk - 1)
            R = self.tmpA[i % 2]; ob = self.tmpA[2 + i % 2]
            k.op("dve", lambda: nc.vector.reciprocal(out=R[:], in_=L[:]), reads=[L], writes=[R])
            k.op("dve", lambda: nc.vector.tensor_tensor(out=ob[:], in0=O[:], in1=R[:], op=ALU.mult), reads=[O, R], writes=[ob])
            k.dma("sp", yout.h[:, q0:q0 + 512], ob[:], reads=[ob], writes=[(yout, i)])


    def bfv(self, t):
        return t.h[:].bitcast(BF16)

    def nsa(self, yout):
        k, nc = self.k, self.nc
        L_ = "%d" % self.l
        dw1k = k.din("nsa_w1k" + L_, [128, 32, 128]); dw1v = k.din("nsa_w1v" + L_, [128, 32, 128])
        dw2k = k.din("nsa_w2k" + L_, [128, 128]); dw2v = k.din("nsa_w2v" + L_, [128, 128])
        dpek = k.din("nsa_pek" + L_, [128, 32]); dpev = k.din("nsa_pev" + L_, [128, 32])
        dbc = k.din("nsa_bc", [128, 2, 4, T_]); dtbs = k.din("nsa_tbs", [128, 12, 512]); dtbw = k.din("nsa_tbw", [128, 8, 512])
        dkeep = k.din("nsa_keep", [128, NT, 64]); dadd = k.din("nsa_add", [128, NT, 64])
        dE = k.din("nsa_E", [64, T_]); dov = k.din("nsa_ov", [128, 2, 64])
        kcT = k.sb([128, 256], BF16, "n_kcT"); vc = k.sb([128, 2, 128], BF16, "n_vc")
        qown = k.sb([128, T_], BF16, "n_qown")
        ksb = k.sb([128, T_], BF16, "n_ks"); kwb = k.sb([128, T_], BF16, "n_kw")
        vsb = k.sb([128, T_], BF16, "n_vs"); vwb = k.sb([128, T_], BF16, "n_vw")
        grow = k.sb([3, T_], F32, "n_grow")
        with k.scope():
            gstg = k.sb([3, T_], F32, "n_gstg")
            self.zfm(grow, grow.h, _OFF["nsa_g"], 0, n=3, mul=3, stage=gstg)
        gsel = [k.sb([3, 128], F32, "n_gsel%d" % i) for i in range(3)]
        for gi in range(3):
            k.op("dve", lambda gi=gi: nc.vector.tensor_scalar(out=gsel[gi][:], in0=self.onesF[0:3, :], scalar1=self.identF[0:3, gi:gi + 1], scalar2=None, op0=ALU.mult),
                 reads=[self.onesF, self.identF], writes=[gsel[gi]])
        self.zfm(qown, qown.h, _OFF["nsa_q"], 0, q="pool", stage=vsb)
        k.op("dve", lambda: nc.vector.tensor_scalar(out=qown[:], in0=qown[:], scalar1=SCALE, scalar2=None, op0=ALU.mult), reads=[qown], writes=[qown])
        self.zfm(ksb, ksb.h, _OFF["nsa_ks"], None, q="pool")
        self.zfm(kwb, kwb.h, _OFF["nsa_kw"], None, q="pool")
        with k.scope():
            stg = [k.sb([128, T_], BF16, "n_stg%d" % i) for i in range(2)]
            self.zfm(stg[0], stg[0].h, _OFF["nsa_vs"], None, q="pool")
            self.zfm(stg[1], stg[1].h, _OFF["nsa_vw"], None, q="pool")
            self.fm2tm(stg[0].h, stg[0], vsb, vsb.h)
            self.fm2tm(stg[1].h, stg[1], vwb, vwb.h)
        cscope = k.scope()
        cscope.__enter__()
        kcin_t = k.sb([128, T_], BF16, "n_kcin"); vcin_t = k.sb([128, T_], BF16, "n_vcin")
        kcin = kcin_t.h; vcin = vcin_t.h
        self.zfm(kcin_t, kcin, _OFF["nsa_kc"], None, q="pool")
        self.zfm(vcin_t, vcin, _OFF["nsa_vc"], None, q="pool")
        w1 = [k.sb([128, 32, 128], BF16, "n_w1k"), k.sb([128, 32, 128], BF16, "n_w1v")]
        w2 = [k.sb([128, 128], BF16, "n_w2k"), k.sb([128, 128], BF16, "n_w2v")]
        pe = [k.sb([128, 32], BF16, "n_pek"), k.sb([128, 32], BF16, "n_pev")]
        self.ld(w1[0], dw1k, q="pool"); self.ld(w1[1], dw1v, q="pool"); self.ld(w2[0], dw2k, q="pool"); self.ld(w2[1], dw2v, q="pool")
        self.ld(pe[0], dpek, q="pool"); self.ld(pe[1], dpev, q="pool")
        hidb = [k.sb([128, 256], BF16, "n_hidk"), k.sb([128, 256], BF16, "n_hidv")]
        pb = k.sb([128, 1], F32, "n_pb")
        hx = k.sb([128, 256], F32, "n_hx"); hy = k.sb([128, 256], F32, "n_hy"); hz = k.sb([128, 256], F32, "n_hz")
        srcs = [kcin_t, vcin_t]
        cin = [kcin, vcin]
        for w in range(2):
            px = self.psX

            def mmb():
                ins = None
                for i in range(32):
                    ins = nc.tensor.matmul(px[:, 0:1], lhsT=w1[w][:, i, :], rhs=pe[w][:, i:i + 1], start=(i == 0), stop=(i == 31))
                return ins
            k.op("pe", mmb, reads=[w1[w], pe[w]], writes=[px])
            k.op("dve", lambda: nc.vector.tensor_copy(pb[:], px[:, 0:1]), reads=[px], writes=[pb])
            ph = self.psS[w]

            def mmh():
                ins = None
                for i in range(32):
                    ins = nc.tensor.matmul(ph[:, 0:255], lhsT=w1[w][:, i, :], rhs=cin[w][:, i:i + 4065:16], start=(i == 0), stop=(i == 31))
                return ins
            k.op("pe", mmh, reads=[w1[w], srcs[w]], writes=[ph])
            k.op("dve", lambda: nc.vector.memset(hx[:], 0.0), writes=[hx])
            k.op("act", lambda: nc.scalar.activation(out=hx[:, 0:255], in_=ph[:, 0:255], func=AF.Identity, bias=pb[:, 0:1]),
                 reads=[ph, pb], writes=[hx])
            self.gelu_tanh(hz, hx, hy)
            k.op("dve", lambda: nc.vector.tensor_copy(hidb[w][:], hz[:]), reads=[hz], writes=[hidb[w]])
        pk = self.psS[2]
        k.op("pe", lambda: nc.tensor.matmul(pk[:, 0:256], lhsT=w2[0][:], rhs=hidb[0][:], start=True, stop=True), reads=[w2[0], hidb[0]], writes=[pk])
        k.op("dve", lambda: nc.vector.tensor_copy(kcT[:], pk[:, 0:256]), reads=[pk], writes=[kcT])
        for nt in range(2):
            pv = self.psO[nt]
            k.op("pe", lambda: nc.tensor.matmul(pv[:, 0:128], lhsT=hidb[1][:, nt * 128:(nt + 1) * 128], rhs=w2[1][:], start=True, stop=True),
                 reads=[hidb[1], w2[1]], writes=[pv])
            k.op("dve", lambda: nc.vector.tensor_copy(vc[:, nt, :], pv[:, 0:128]), reads=[pv], writes=[(vc, nt)])
        cscope.__exit__(None, None, None)
        tbs = k.sb([128, 12, 512], BF16, "n_tbs"); tbw = k.sb([128, 8, 512], BF16, "n_tbw")
        self.ld(tbs, dtbs, q="pool"); self.ld(tbw, dtbw, q="pool")
        keep = k.sb([128, 4, 64], F32, "n_keep"); addt = k.sb([128, 4, 64], F32, "n_add")
        Eb = k.sb([64, T_], BF16, "n_E"); ov = k.sb([128, 2, 64], BF16, "n_ov")
        self.ld(Eb, dE, q="pool"); self.ld(ov, dov, q="pool")
        qo = k.sb([128, 3, 512], BF16, "n_qo"); qst = k.sb([128, 4, 512], BF16, "n_qst")
        bc = [k.sb([128, 2, 4, 512], BF16, "n_bc0")] * 2
        gb = [k.sb([128, 3, 512], F32, "n_gb0")] * 2
        pc = [[k.sb([128, 512], BF16, "n_pc%d%d" % (h, nt)) for nt in range(2)] for h in range(4)]
        Rh = [k.sb([128, 512], F32, "n_R")] * 4
        acc = [k.sb([128, 512], F32, "n_acc%d" % i) for i in range(2)]
        impt = k.sb([128, 4, 64], F32, "n_imp"); wrk = k.sb([128, 64], F32, "n_wrk")
        m8 = k.sb([128, 8], F32, "n_m8"); thr = k.sb([128, 1], F32, "n_thr"); selb = k.sb([128, 64], F32, "n_selb")
        selT = k.sb([64, 512], BF16, "n_selT")
        Wt = k.sb([128, 512], F32, "n_W")
        NKC = [128, 127]
        for i in range(8):
            q0 = 512 * i
            bci = bc[i % 2]; gbi = gb[i % 2]; ac = acc[i % 2]
            k.dma("pool", bci[:], dbc.h[:, :, :, q0:q0 + 512], reads=[dbc], writes=[bci])
            for gi in range(3):
                pgx = self.psS[self.iS % 3]; self.iS += 1
                k.op("pe", lambda: nc.tensor.matmul(pgx[:], lhsT=gsel[gi][:], rhs=grow[0:3, q0:q0 + 512], start=True, stop=True),
                     reads=[gsel[gi], grow], writes=[pgx])
                k.op("act", lambda: nc.scalar.activation(out=gbi[:, gi, :], in_=pgx[:], func=AF.Sigmoid), reads=[pgx], writes=[(gbi, gi)])
            sq_ = q0 // TOK
            c0 = q0 - sq_ * TOK
            for jc in range(4):
                for (ct, sap_, d0, cnt) in self.zall.pieces(sq_, _OFF["nsa_q"] + jc * 128, 128):
                    k.dma("pool", qst[d0:d0 + cnt, jc, :], sap_[:, c0:c0 + 512], reads=[ct], writes=[qst])
            for hh in range(3):
                for jc in range(4):
                    mc = self.msk[:, 4 + 4 * hh + jc:5 + 4 * hh + jc]
                    if jc == 0:
                        k.op("dve", lambda: nc.vector.tensor_scalar(out=qo[:, hh, :], in0=qst[:, jc, :], scalar1=mc, scalar2=None, op0=ALU.mult),
                             reads=[qst, self.msk], writes=[qo])
                    else:
                        k.op("dve", lambda: nc.vector.scalar_tensor_tensor(out=qo[:, hh, :], in0=qst[:, jc, :], scalar=mc, in1=qo[:, hh, :], op0=ALU.mult, op1=ALU.add),
                             reads=[qst, self.msk, qo], writes=[qo])
            k.op("dve", lambda: nc.vector.tensor_scalar(out=qo[:], in0=qo[:], scalar1=SCALE, scalar2=None, op0=ALU.mult), reads=[qo], writes=[qo])
            k.dma("sp", keep[:], dkeep.h[:, 4 * i:4 * i + 4, :], reads=[dkeep], writes=[keep])
            k.dma("sp", addt[:], dadd.h[:, 4 * i:4 * i + 4, :], reads=[dadd], writes=[addt])
            for hh in range(4):
                h = hh
                own = (h == 3)
                L = self.psL[hh % 2]; O = self.psO[0]
                for nt in range(2):
                    nk = NKC[nt]
                    S = self.psS[self.iS % 3]; self.iS += 1
                    P = pc[h][nt]

                    def mm():
                        nc.tensor.matmul(S[0:nk, :], lhsT=kcT[:, nt * 128:nt * 128 + nk], rhs=(qown[:, q0:q0 + 512] if h == 3 else qo[:, h, :]), start=True, stop=False)
                        return nc.tensor.matmul(S[0:nk, :], lhsT=self.identB[:, 0:nk], rhs=bci[:, nt, h, :], start=False, stop=True)
                    k.op("pe", mm, reads=[kcT, qown, qo, self.identB, bci], writes=[S])
                    if nk < 128:
                        k.op("dve", lambda: nc.vector.memset(P[:], 0.0), writes=[P])
                    k.op("act", lambda: nc.scalar.activation(out=P[0:nk, :], in_=S[0:nk, :], func=AF.Exp), reads=[S], writes=[P])
                    k.op("pe", lambda: nc.tensor.matmul(L[:], lhsT=self.onesB[0:nk, :], rhs=P[0:nk, :], start=(nt == 0), stop=(nt == 1)),
                         reads=[self.onesB, P], writes=[L])
                    if own:
                        k.op("pe", lambda: nc.tensor.matmul(O[:], lhsT=vc[0:nk, nt, :], rhs=P[0:nk, :], start=(nt == 0), stop=(nt == 1)),
                             reads=[vc, P], writes=[O])
                k.op("dve", lambda: nc.vector.tensor_scalar(out=Rh[h][:], in0=L[:], scalar1=1e-30, scalar2=None, op0=ALU.max),
                     reads=[L], writes=[Rh[h]])
                k.op("dve", lambda: nc.vector.reciprocal(out=Rh[h][:], in_=Rh[h][:]), reads=[Rh[h]], writes=[Rh[h]])
                if own:
                    k.op("dve", lambda: nc.vector.tensor_tensor(out=Wt[:], in0=Rh[h][:], in1=gbi[:, 0, :], op=ALU.mult), reads=[Rh[h], gbi], writes=[Wt])
                    k.op("dve", lambda: nc.vector.tensor_tensor(out=ac[:], in0=O[:], in1=Wt[:], op=ALU.mult), reads=[O, Wt], writes=[ac])
                for nt in range(2):
                    k.op("dve", lambda: nc.vector.tensor_tensor(out=pc[h][nt][:], in0=pc[h][nt][:], in1=Rh[h][:], op=ALU.mult),
                         reads=[pc[h][nt], Rh[h]], writes=[pc[h][nt]])
            px = self.psX
            for jq in range(4):
                def mmi():
                    ins = None
                    n = 0
                    for h in range(4):
                        for nt in range(2):
                            ins = nc.tensor.matmul(px[:, jq * 64:(jq + 1) * 64], lhsT=pc[h][nt][:, jq * 128:(jq + 1) * 128], rhs=ov[:, nt, :],
                                                   start=(n == 0), stop=(n == 7))
                            n += 1
                    return ins
                k.op("pe", mmi, reads=[ov] + [pc[h][nt] for h in range(4) for nt in range(2)], writes=[(px, jq)])
            k.op("dve", lambda: nc.vector.tensor_tensor(out=impt[:].rearrange("p a b -> p (a b)"), in0=px[:, 0:256],
                                                        in1=keep[:].rearrange("p a b -> p (a b)"), op=ALU.mult),
                 reads=[px, keep], writes=[impt])
            k.op("dve", lambda: nc.vector.tensor_tensor(out=impt[:], in0=impt[:], in1=addt[:], op=ALU.add),
                 reads=[impt, addt], writes=[impt])
            pt = self.psL[0]
            for jq in range(4):
                k.op("dve", lambda: nc.vector.max(out=m8[:], in_=impt[:, jq, :]), reads=[impt], writes=[m8])
                k.op("dve", lambda: nc.vector.match_replace(out=wrk[:], in_to_replace=m8[:], in_values=impt[:, jq, :], imm_value=-1e30),
                     reads=[m8, impt], writes=[wrk])
                k.op("dve", lambda: nc.vector.max(out=m8[:], in_=wrk[:]), reads=[wrk], writes=[m8])
                k.op("dve", lambda: nc.vector.tensor_reduce(out=thr[:], in_=m8[:], axis=AX.X, op=ALU.min), reads=[m8], writes=[thr])
                k.op("dve", lambda: nc.vector.tensor_scalar(out=selb[:], in0=impt[:, jq, :], scalar1=thr[:, 0:1], scalar2=NEG,
                                                            op0=ALU.is_lt, op1=ALU.mult), reads=[impt, thr], writes=[selb])
                k.op("pe", lambda: nc.tensor.matmul(pt[0:64, jq * 128:(jq + 1) * 128], lhsT=selb[:], rhs=self.identF[:], start=True, stop=True),
                     reads=[selb, self.identF], writes=[(pt, jq)])
            k.op("dve", lambda: nc.vector.tensor_copy(selT[:], pt[0:64, :]), reads=[pt], writes=[selT])
            O = self.psO[1]; L = self.psL[1]
            nkt = 4 * i + 4
            for kt in range(nkt):
                idx = min(4 * i - kt + 3, 11)
                extra = [(lambda S: S[:], self.identB[:], tbs[:, idx, :]),
                         (lambda S: S[:], Eb[:, kt * 128:(kt + 1) * 128], selT[:])]
                self._xr = [self.identB, tbs, Eb, selT]
                self.attn_unit(ksb, kt, qown, q0, extra, None, [], vsb, vsb[:, kt * 128:(kt + 1) * 128], O, L, kt == 0, kt == nkt - 1)
            self.nsa_fin(O, L, gbi, 1, ac, Wt)
            O = self.psO[0]; L = self.psL[0]
            kts = list(range(max(0, 4 * i - 4), 4 * i + 4))
            for n, kt in enumerate(kts):
                idx = 4 * i - kt + 3
                extra = [(lambda S: S[:], self.identB[:], tbw[:, idx, :])]
                self._xr = [self.identB, tbw]
                self.attn_unit(kwb, kt, qown, q0, extra, None, [], vwb, vwb[:, kt * 128:(kt + 1) * 128], O, L, n == 0, n == len(kts) - 1)
            self.nsa_fin(O, L, gbi, 2, ac, Wt)
            k.dma("sp", yout.h[:, q0:q0 + 512], ac[:], reads=[ac], writes=[(yout, i)])

    def nsa_fin(self, O, L, gbi, gi, ac, Wt):
        k, nc = self.k, self.nc
        t2 = self.tmpA[0]
        k.op("dve", lambda: nc.vector.reciprocal(out=Wt[:], in_=L[:]), reads=[L], writes=[Wt])
        k.op("dve", lambda: nc.vector.tensor_tensor(out=Wt[:], in0=Wt[:], in1=gbi[:, gi, :], op=ALU.mult), reads=[Wt, gbi], writes=[Wt])
        k.op("dve", lambda: nc.vector.tensor_tensor(out=t2[:], in0=O[:], in1=Wt[:], op=ALU.mult), reads=[O, Wt], writes=[t2])
        k.op("dve", lambda: nc.vector.tensor_tensor(out=ac[:], in0=ac[:], in1=t2[:], op=ALU.add), reads=[ac, t2], writes=[ac])


    def gdn(self, yout):
        k, nc = self.k, self.nc
        L_ = "%d" % self.l
        dcw = k.din("gdn_cw" + L_, [128, 3, 4])
        dal = k.din("gdn_alog" + L_, [128, 1]); ddt = k.din("gdn_dtb" + L_, [128, 1]); dng = k.din("gdn_ng" + L_, [128, 1])
        dct = k.din("c_ct", [128, 128]); dsc = k.din("c_sc", [128, 128]); dh0 = k.din("c_h0", [128, 128]); dh1 = k.din("c_h1", [128, 128])
        dmst = k.din("c_mst", [128, 128]); dmit = k.din("c_mit", [128, 128]); dmsn = k.din("c_msn", [128, 128]); dcm = k.din("c_cm", [128, 2])
        B = self.big
        raw, W_, tA, tB = B[0], B[1], B[2], B[3]
        qs = ks = vs = oT = W_
        arow = k.sb([1, T_], F32, "g_arow")
        qnb, knb, vsb = self.bigb[0], self.bigb[1], self.bigb[2]
        cw = k.sb([128, 3, 4], F32, "g_cw"); self.ld(cw, dcw)
        cst = {}
        for nm, dd in (("ct", dct), ("sc", dsc), ("h0", dh0), ("h1", dh1), ("mst", dmst), ("mit", dmit), ("msn", dmsn)):
            cst[nm] = k.sb([128, 128], F32, "g_" + nm); self.ld(cst[nm], dd)
        cm = k.sb([128, 2], F32, "g_cm"); self.ld(cm, dcm)
        al = k.sb([128, 1], F32, "g_al"); dtb = k.sb([128, 1], F32, "g_dtb"); ng = k.sb([128, 1], F32, "g_ng")
        self.ld(al, dal); self.ld(dtb, ddt); self.ld(ng, dng)
        epsc = k.sb([128, 1], F32, "g_eps")
        k.op("dve", lambda: nc.vector.memset(epsc[:], EPS), writes=[epsc])
        for wi, (src, dst) in enumerate((("gdn_q", qs), ("gdn_k", ks), ("gdn_v", vs))):
            self.zfm(raw, raw.h, _OFF[src], 0, stage=tA)
            k.op("dve", lambda: nc.vector.tensor_scalar(out=dst[:], in0=raw[:], scalar1=cw[:, wi, 3:4], scalar2=None, op0=ALU.mult),
                 reads=[raw, cw], writes=[dst])
            for i in range(3):
                sh = 3 - i
                k.op("dve", lambda: nc.vector.scalar_tensor_tensor(out=dst[:, sh:], in0=raw[:, 0:T_ - sh], scalar=cw[:, wi, i:i + 1],
                                                                   in1=dst[:, sh:], op0=ALU.mult, op1=ALU.add), reads=[raw, cw, dst], writes=[dst])
            k.op("act", lambda: nc.scalar.activation(out=dst[:], in_=dst[:], func=AF.Silu), reads=[dst], writes=[dst])
            if wi == 2:
                k.op("act", lambda: nc.scalar.copy(out=vsb[:], in_=vs[:]), reads=[vs], writes=[vsb])
                continue
            src, dstb, sc = ((qs, qnb, SCALE), (ks, knb, 1.0))[wi]
            if True:
                k.op("dve", lambda: nc.vector.tensor_tensor(out=tA[:], in0=src[:], in1=src[:], op=ALU.mult), reads=[src], writes=[tA])
                for tb in range(8):
                    ts = slice(tb * 512, (tb + 1) * 512)
                    ps = self.psS[tb % 3]
                    k.op("pe", lambda: nc.tensor.matmul(ps[:], lhsT=self.onesF[:], rhs=tA[:, ts], start=True, stop=True), reads=[self.onesF, tA], writes=[ps])
                    k.op("act", lambda: nc.scalar.activation(out=tB[:, ts], in_=ps[:], func=AF.Ln, bias=epsc[:, 0:1]), reads=[ps, epsc], writes=[(tB, tb)])
                    k.op("act", lambda: nc.scalar.activation(out=tB[:, ts], in_=tB[:, ts], func=AF.Exp, scale=-0.5), reads=[(tB, tb)], writes=[(tB, tb)])
                k.op("dve", lambda: nc.vector.scalar_tensor_tensor(out=dstb[:], in0=src[:], scalar=sc, in1=tB[:], op0=ALU.mult, op1=ALU.mult),
                     reads=[src, tB], writes=[dstb])
        def col(nm):
            return k.sb([128, NT], F32, "g_c_" + nm)
        g = col("g"); beta = col("beta"); gc = col("gc"); ngc = col("ngc"); gl = col("gl"); wcol = col("w")
        skbg = col("skbg"); skd = [col("skd0"), col("skd1")]; egl = [col("egl0"), col("egl1")]; tmpc = col("tmp")
        self.zfm(arow, arow.h, _OFF["gdn_a"], 0, n=1, mul=1, stage=tB)
        self.row2col(arow, g)
        self.zfm(arow, arow.h, _OFF["gdn_b"], 0, n=1, mul=1, stage=tB)
        self.row2col(arow, beta)
        k.op("act", lambda: nc.scalar.activation(out=g[:], in_=g[:], func=AF.Exp, bias=dtb[:, 0:1]), reads=[g, dtb], writes=[g])
        k.op("act", lambda: nc.scalar.activation(out=g[:], in_=g[:], func=AF.Ln, bias=self.onesF[:, 0:1]), reads=[g, self.onesF], writes=[g])
        k.op("act", lambda: nc.scalar.activation(out=al[:], in_=al[:], func=AF.Exp), reads=[al], writes=[al])
        k.op("dve", lambda: nc.vector.tensor_scalar(out=g[:], in0=g[:], scalar1=al[:, 0:1], scalar2=-1.0, op0=ALU.mult, op1=ALU.mult),
             reads=[g, al], writes=[g])
        k.op("act", lambda: nc.scalar.activation(out=beta[:], in_=beta[:], func=AF.Sigmoid), reads=[beta], writes=[beta])
        px = self.psX

        def colmm(lhs, dst, func=None):
            k.op("pe", lambda: nc.tensor.matmul(px[:, 0:NT], lhsT=lhs[:], rhs=g[:], start=True, stop=True), reads=[lhs, g], writes=[px])
            if func is None:
                k.op("dve", lambda: nc.vector.tensor_copy(dst[:], px[:, 0:NT]), reads=[px], writes=[dst])
            else:
                k.op("act", lambda: nc.scalar.activation(out=dst[:], in_=px[:, 0:NT], func=func), reads=[px], writes=[dst])
        colmm(cst["ct"], gc)
        colmm(cst["sc"], gl)
        colmm(cst["h0"], egl[0], AF.Exp)
        colmm(cst["h1"], egl[1], AF.Exp)
        k.op("dve", lambda: nc.vector.tensor_scalar(out=ngc[:], in0=gc[:], scalar1=-1.0, scalar2=None, op0=ALU.mult), reads=[gc], writes=[ngc])
        k.op("act", lambda: nc.scalar.activation(out=wcol[:], in_=beta[:], func=AF.Ln), reads=[beta], writes=[wcol])
        k.op("dve", lambda: nc.vector.tensor_tensor(out=wcol[:], in0=wcol[:], in1=gc[:], op=ALU.add), reads=[wcol, gc], writes=[wcol])
        k.op("act", lambda: nc.scalar.activation(out=skbg[:], in_=gc[:], func=AF.Exp), reads=[gc], writes=[skbg])
        k.op("dve", lambda: nc.vector.tensor_tensor(out=skbg[:], in0=skbg[:], in1=beta[:], op=ALU.mult), reads=[skbg, beta], writes=[skbg])
        k.op("dve", lambda: nc.vector.tensor_tensor(out=tmpc[:], in0=gl[:], in1=gc[:], op=ALU.subtract), reads=[gl, gc], writes=[tmpc])
        k.op("act", lambda: nc.scalar.activation(out=tmpc[:], in_=tmpc[:], func=AF.Exp), reads=[tmpc], writes=[tmpc])
        for c in range(2):
            k.op("dve", lambda: nc.vector.tensor_scalar(out=skd[c][:], in0=tmpc[:], scalar1=cm[:, c:c + 1], scalar2=None, op0=ALU.mult),
                 reads=[tmpc, cm], writes=[skd[c]])
        S = k.sb([128, 128], F32, "g_S"); Sb = k.sb([128, 128], BF16, "g_Sb")
        k.op("dve", lambda: nc.vector.memset(S[:], 0.0), writes=[S])
        k.op("dve", lambda: nc.vector.memset(Sb[:], 0.0), writes=[Sb])

        def t128(nm, dt=F32):
            return k.sb([128, 128], dt, "g_t_" + nm)
        kbg = t128("kbg", BF16); kd = [t128("kd0", BF16), t128("kd1", BF16)]; vb = t128("vb", BF16)
        dgw = t128("dgw"); dgg = t128("dgg"); dgn = t128("dgn")
        Gs = t128("G"); Y = t128("Y"); X = t128("X"); Pm = t128("P"); Z = t128("Z"); ZT = t128("ZT"); Z2 = t128("Z2"); ZT2 = t128("ZT2")
        E1 = t128("E1"); qkT = t128("qkT", BF16); qgT = t128("qgT", BF16); PTb = t128("PTb", BF16); nWT = t128("nWT", BF16)
        vnb = t128("vnb", BF16)
        pool6 = [self.psS[0], self.psS[1], self.psS[2], self.psO[1], self.psL[0], self.psL[1]]
        ctr = [0]

        def pp():
            ctr[0] += 1
            return pool6[ctr[0] % 6]
        pOg = self.psO[0]
        for t in range(NT):
            cs = slice(t * 128, (t + 1) * 128)
            tc_ = slice(t, t + 1)
            pa = pp()
            k.op("pe", lambda: nc.tensor.matmul(pa[:, 0:128], lhsT=knb[:, cs], rhs=self.identB[:], start=True, stop=True), reads=[knb, self.identB], writes=[(pa, 0)])
            k.op("pe", lambda: nc.tensor.matmul(pa[:, 128:256], lhsT=vsb[:, cs], rhs=self.identB[:], start=True, stop=True), reads=[vsb, self.identB], writes=[(pa, 1)])
            k.op("dve", lambda: nc.vector.tensor_scalar(out=kbg[:], in0=pa[:, 0:128], scalar1=skbg[:, tc_], scalar2=None, op0=ALU.mult), reads=[(pa, 0), skbg], writes=[kbg])
            for c in range(2):
                k.op("dve", lambda: nc.vector.tensor_scalar(out=kd[c][:], in0=pa[:, 0:128], scalar1=skd[c][:, tc_], scalar2=None, op0=ALU.mult),
                     reads=[(pa, 0), skd[c]], writes=[kd[c]])
            k.op("dve", lambda: nc.vector.tensor_scalar(out=vb[:], in0=pa[:, 128:256], scalar1=beta[:, tc_], scalar2=None, op0=ALU.mult), reads=[(pa, 1), beta], writes=[vb])
            k.op("dve", lambda: nc.vector.tensor_scalar(out=dgw[:], in0=self.identF[:], scalar1=wcol[:, tc_], scalar2=None, op0=ALU.mult), reads=[self.identF, wcol], writes=[dgw])
            k.op("dve", lambda: nc.vector.tensor_scalar(out=dgg[:], in0=self.identF[:], scalar1=gc[:, tc_], scalar2=None, op0=ALU.mult), reads=[self.identF, gc], writes=[dgg])
            k.op("dve", lambda: nc.vector.tensor_scalar(out=dgn[:], in0=self.identF[:], scalar1=ngc[:, tc_], scalar2=None, op0=ALU.mult), reads=[self.identF, ngc], writes=[dgn])
            pg = pp()
            k.op("pe", lambda: nc.tensor.matmul(pg[:, 0:128], lhsT=knb[:, cs], rhs=knb[:, cs], start=True, stop=True), reads=[knb], writes=[(pg, 0)])
            k.op("pe", lambda: nc.tensor.matmul(pg[:, 128:256], lhsT=knb[:, cs], rhs=qnb[:, cs], start=True, stop=True), reads=[knb, qnb], writes=[(pg, 1)])
            k.op("dve", lambda: nc.vector.tensor_copy(Gs[:], pg[:, 0:128]), reads=[(pg, 0)], writes=[Gs])

            def expmat(diag, mask, bias_col, bias_t, dst_fn):
                pe_ = pp()

                def mm():
                    ins0 = nc.tensor.matmul(pe_[:, 0:128], lhsT=self.onesF[:], rhs=diag[:], start=True, stop=(mask is None))
                    if mask is None:
                        return ins0
                    return nc.tensor.matmul(pe_[:, 0:128], lhsT=self.identF[:], rhs=mask[:], start=False, stop=True)
                k.op("pe", mm, reads=[self.onesF, diag, self.identF] + ([mask] if mask is not None else []), writes=[pe_])
                if bias_col is None:
                    k.op("act", lambda: nc.scalar.activation(out=E1[:], in_=pe_[:, 0:128], func=AF.Exp), reads=[pe_], writes=[E1])
                else:
                    k.op("act", lambda: nc.scalar.activation(out=E1[:], in_=pe_[:, 0:128], func=AF.Exp, bias=bias_col[:, tc_]),
                         reads=[pe_, bias_t], writes=[E1])
                dst_fn()
            expmat(dgw, cst["mst"], ngc, ngc, lambda: k.op("dve", lambda: nc.vector.tensor_tensor(out=Y[:], in0=Gs[:], in1=E1[:], op=ALU.mult), reads=[Gs, E1], writes=[Y]))
            expmat(dgn, cst["msn"], wcol, wcol, lambda: k.op("dve", lambda: nc.vector.tensor_tensor(out=X[:], in0=Gs[:], in1=E1[:], op=ALU.mult), reads=[Gs, E1], writes=[X]))
            expmat(dgg, cst["mit"], ngc, ngc, lambda: k.op("dve", lambda: nc.vector.tensor_tensor(out=qkT[:], in0=pg[:, 128:256], in1=E1[:], op=ALU.mult), reads=[(pg, 1), E1], writes=[qkT]))
            expmat(dgg, None, None, None, lambda: k.op("dve", lambda: nc.vector.tensor_tensor(out=qgT[:], in0=qnb[:, cs], in1=E1[:], op=ALU.mult), reads=[qnb, E1], writes=[qgT]))
            k.op("dve", lambda: nc.vector.tensor_tensor(out=Pm[:], in0=self.identF[:], in1=Y[:], op=ALU.subtract), reads=[self.identF, Y], writes=[Pm])
            p1 = pp(); p2 = pp()
            k.op("pe", lambda: nc.tensor.matmul(p1[:, 0:128], lhsT=X[:], rhs=Y[:], start=True, stop=True), reads=[X, Y], writes=[p1])
            k.op("pe", lambda: nc.tensor.matmul(p2[:, 0:128], lhsT=Y[:], rhs=X[:], start=True, stop=True), reads=[X, Y], writes=[p2])
            zc, ztc, zn, ztn = Z, ZT, Z2, ZT2
            k.op("dve", lambda: nc.vector.tensor_copy(zc[:], p1[:, 0:128]), reads=[p1], writes=[zc])
            k.op("act", lambda: nc.scalar.copy(out=ztc[:], in_=p2[:, 0:128]), reads=[p2], writes=[ztc])
            for it in range(5):
                p3 = pp()
                k.op("pe", lambda: nc.tensor.matmul(p3[:, 0:128], lhsT=ztc[:], rhs=Pm[:], start=True, stop=True), reads=[ztc, Pm], writes=[p3])
                if it < 4:
                    p1 = pp(); p2 = pp()
                    k.op("pe", lambda: nc.tensor.matmul(p1[:, 0:128], lhsT=ztc[:], rhs=zc[:], start=True, stop=True), reads=[ztc, zc], writes=[p1])
                    k.op("pe", lambda: nc.tensor.matmul(p2[:, 0:128], lhsT=zc[:], rhs=ztc[:], start=True, stop=True), reads=[ztc, zc], writes=[p2])
                k.op("dve", lambda: nc.vector.tensor_tensor(out=Pm[:], in0=Pm[:], in1=p3[:, 0:128], op=ALU.add), reads=[Pm, p3], writes=[Pm])
                if it < 4:
                    k.op("dve", lambda: nc.vector.tensor_copy(zn[:], p1[:, 0:128]), reads=[p1], writes=[zn])
                    k.op("act", lambda: nc.scalar.copy(out=ztn[:], in_=p2[:, 0:128]), reads=[p2], writes=[ztn])
                    zc, ztc, zn, ztn = zn, ztn, zc, ztc
            k.op("act", lambda: nc.scalar.copy(out=PTb[:], in_=Pm[:]), reads=[Pm], writes=[PTb])
            pw = pp()
            k.op("pe", lambda: nc.tensor.matmul(pw[:, 0:128], lhsT=kbg[:], rhs=PTb[:], start=True, stop=True), reads=[kbg, PTb], writes=[pw])
            k.op("dve", lambda: nc.vector.tensor_scalar(out=nWT[:], in0=pw[:, 0:128], scalar1=-1.0, scalar2=None, op0=ALU.mult), reads=[pw], writes=[nWT])
            for c in range(2):
                ccs = slice(64 * c, 64 * c + 64)
                pv = pp()

                def mmv():
                    nc.tensor.matmul(pv[:, 0:128], lhsT=PTb[:], rhs=vb[:], start=True, stop=False)
                    return nc.tensor.matmul(pv[:, 0:128], lhsT=nWT[:], rhs=Sb[:], start=False, stop=True)
                k.op("pe", mmv, reads=[PTb, vb, nWT, Sb], writes=[pv])
                k.op("act", lambda: nc.scalar.copy(out=vnb[:], in_=pv[:, 0:128]), reads=[pv], writes=[vnb])

                def mmo():
                    nc.tensor.matmul(pOg[:, ccs], lhsT=Sb[:], rhs=qgT[:, ccs], start=True, stop=False)
                    return nc.tensor.matmul(pOg[:, ccs], lhsT=vnb[:], rhs=qkT[:, ccs], start=False, stop=True)
                k.op("pe", mmo, reads=[Sb, qgT, vnb, qkT], writes=[(pOg, c)])
                pu = pp()
                k.op("pe", lambda: nc.tensor.matmul(pu[:, 0:128], lhsT=kd[c][:], rhs=vnb[:], start=True, stop=True), reads=[kd[c], vnb], writes=[pu])
                k.op("dve", lambda: nc.vector.scalar_tensor_tensor(out=S[:], in0=S[:], scalar=egl[c][:, tc_], in1=pu[:, 0:128], op0=ALU.mult, op1=ALU.add),
                     reads=[S, egl[c], pu], writes=[S])
                k.op("act", lambda: nc.scalar.copy(out=Sb[:], in_=S[:]), reads=[S], writes=[Sb])
            k.op("dve", lambda: nc.vector.tensor_copy(oT[:, cs], pOg[:, 0:128]), reads=[pOg], writes=[(oT, t)])
        self.zfm(raw, raw.h, _OFF["gdn_z"], 0, stage=tA)
        k.op("act", lambda: nc.scalar.activation(out=raw[:], in_=raw[:], func=AF.Silu), reads=[raw], writes=[raw])
        k.op("dve", lambda: nc.vector.tensor_tensor(out=tA[:], in0=oT[:], in1=oT[:], op=ALU.mult), reads=[oT], writes=[tA])
        k.op("dve", lambda: nc.vector.tensor_scalar(out=tA[:], in0=tA[:], scalar1=1.0 / 128, scalar2=None, op0=ALU.mult), reads=[tA], writes=[tA])
        for tb in range(8):
            ts = slice(tb * 512, (tb + 1) * 512)
            ps = self.psS[tb % 3]
            k.op("pe", lambda: nc.tensor.matmul(ps[:], lhsT=self.onesF[:], rhs=tA[:, ts], start=True, stop=True), reads=[self.onesF, tA], writes=[ps])
            k.op("act", lambda: nc.scalar.activation(out=tB[:, ts], in_=ps[:], func=AF.Ln, bias=epsc[:, 0:1]), reads=[ps, epsc], writes=[(tB, tb)])
            k.op("act", lambda: nc.scalar.activation(out=tB[:, ts], in_=tB[:, ts], func=AF.Exp, scale=-0.5), reads=[(tB, tb)], writes=[(tB, tb)])
        k.op("dve", lambda: nc.vector.scalar_tensor_tensor(out=oT[:], in0=oT[:], scalar=ng[:, 0:1], in1=tB[:], op0=ALU.mult, op1=ALU.mult),
             reads=[oT, ng, tB], writes=[oT])
        k.op("dve", lambda: nc.vector.tensor_tensor(out=oT[:], in0=oT[:], in1=raw[:], op=ALU.mult), reads=[oT, raw], writes=[oT])
        k.dma("sp", yout.h, oT[:], reads=[oT], writes=[yout])


class Gathered:
    def __init__(self, nc, name, nrows, ncols, CR):
        self.nrows, self.ncols, self.CR = nrows, ncols, CR
        self.chunks = []
        r = 0
        while r < nrows:
            cr = min(CR, nrows - r)
            self.chunks.append((r, cr, T(nc.dram_tensor("%s_c%d" % (name, len(self.chunks)), [4 * cr, ncols], F32).ap(), "%s_c%d" % (name, len(self.chunks)))))
            r += cr

    def pieces(self, s_, r0, n):
        out = []
        r = r0
        while r < r0 + n:
            ci = r // self.CR
            c0, cr, t = self.chunks[ci]
            cnt = min(r0 + n, c0 + cr) - r
            out.append((t, t.h[s_ * cr + (r - c0):s_ * cr + (r - c0) + cnt, :], r - r0, cnt))
            r += cnt
        return out


def exchange(k, src, dst, sem):
    nc = k.nc
    k.barrier()
    sems = []
    for (c0, cr, t) in dst.chunks:
        cs = sem.enter_context(nc.semaphore("cc_%s" % t.name))
        sems.append(cs)
        nc.gpsimd.collective_compute("AllGather", ALU.bypass, replica_groups=[[0, 1, 2, 3], [4, 5, 6, 7]],
                                     ins=[src.h[c0:c0 + cr, :].opt()], outs=[t.h.opt()]).then_inc(cs)
    for cs in sems:
        for e in k.eng.values():
            e.wait_ge(cs, 1)


NBIG = {"fox": (2, 4), "lru": (6, 1), "gdn": (4, 3), "nsa": (0, 0)}


def build_fused(dbg=False):
    nc = bass.Bass("TRN2", target_bir_lowering=False)
    with ExitStack() as es:
        k = K(nc, es)
        x = k.din("x", [TOK, D])
        out = k.dout("out", [TOK, D])
        h = k.sb([128, NCH, TOK], F32, "h")
        vals = None
        zsh = [T(nc.dram_tensor("zsh%d" % l, [DIN, TOK], F32).ap(), "zsh%d" % l) for l in range(2)]
        zall = [Gathered(nc, "zall%d" % l, DIN, TOK, 256) for l in range(2)]
        ysh = [T(nc.dram_tensor("ysh%d" % l, [4 * 128, T_], F32).ap(), "ysh%d" % l) for l in range(2)]
        yall = [Gathered(nc, "yall%d" % l, 4 * 128, T_, 64) for l in range(2)]
        csem = [es, es, es, es]
        with k.scope():
            load_x(k, h, x)
        for l in range(2):
            with k.scope():
                dense_A(k, h, l, zsh[l], zall[l])
            for gi, nm in enumerate(("fox", "gdn", "lru", "nsa")):
                with k.scope():
                    m = Mix(k, l, zall[l], vals, NBIG[nm][0], NBIG[nm][1])
                    yout = T(ysh[l].h[gi * 128:(gi + 1) * 128, :], "y_%s%d" % (nm, l))
                    getattr(m, nm)(yout)
                    for (c0, cr, ct) in yall[l].chunks[2 * gi:2 * gi + 2]:
                        k.allgather(ysh[l].h[c0:c0 + cr, :], ct, [yout])
            with k.scope():
                dense_C(k, h, l, yall[l], None)
            if dbg and l == 0:
                dh = k.dout("dbg_h", [D, TOK])
                v = dh.h.rearrange("(c p) t -> p c t", p=128)
                for c in range(0, NCH, 4):
                    k.dma("sp", v[:, c:c + 4, :], h[:, c:c + 4, :], reads=[h], writes=[(dh, c)])
        with k.scope():
            final_out(k, h, out)
        k.wait_all("sp")
    return nc


def tm_tiles(a):
    t, d = a.shape
    return np.ascontiguousarray(a.reshape(t // 128, 128, d).transpose(1, 0, 2))


def col_tiles(v):
    return np.ascontiguousarray(v.reshape(-1, 128).T)


_OFF = {}
_o = 0
for _n, _w in (("fox_q", 512), ("fox_k", 512), ("fox_v", 512), ("fox_f", 4), ("gdn_q", 512), ("gdn_k", 512), ("gdn_v", 512),
               ("gdn_a", 4), ("gdn_b", 4), ("gdn_z", 512), ("lru_x", 512), ("lru_gate", 512), ("nsa_q", 512), ("nsa_kc", 128),
               ("nsa_vc", 128), ("nsa_ks", 128), ("nsa_vs", 128), ("nsa_kw", 128), ("nsa_vw", 128), ("nsa_g", 12)):
    _OFF[_n] = _o
    _o += _w


def consts_B():
    c = {}
    c["c_ident"] = np.eye(128, dtype=np.float32)
    p = np.arange(128)
    c["c_ut"] = (p[:, None] <= p[None, :]).astype(np.float32)
    q = np.arange(32)
    c["c_su"] = (q[:, None] < q[None, :]).astype(np.float32)
    col = np.arange(512)
    mb = np.zeros((128, 4, 512), np.float32)
    for m in range(4):
        mb[:, m, :] = np.where(p[:, None] + 128 * m <= col[None, :], 0.0, NEG)
    c["c_mbfox"] = mb
    ch = p // 64
    same = ch[:, None] == ch[None, :]
    c["c_ct"] = (same & (p[:, None] <= p[None, :])).astype(np.float32)
    c["c_sc"] = same.astype(np.float32)
    c["c_h0"] = np.broadcast_to((p < 64)[:, None], (128, 128)).astype(np.float32).copy()
    c["c_h1"] = np.broadcast_to((p >= 64)[:, None], (128, 128)).astype(np.float32).copy()
    c["c_mst"] = np.where(same & (p[None, :] > p[:, None]), 0.0, NEG).astype(np.float32)
    c["c_mit"] = np.where(same & (p[None, :] >= p[:, None]), 0.0, NEG).astype(np.float32)
    c["c_msn"] = np.where(same & (p[:, None] > p[None, :]), 0.0, NEG).astype(np.float32)
    c["c_cm"] = np.stack([(p < 64), (p >= 64)], axis=1).astype(np.float32)
    return c


def prep_mix(inp, l, j):
    L_ = "%d" % l
    m = {}
    hs = slice(j * 128, (j + 1) * 128)
    m["lru_cw" + L_] = np.ascontiguousarray(inp["lru_conv_w"][l][:, hs].T)
    m["lru_cb" + L_] = np.ascontiguousarray(inp["lru_conv_b"][l][hs].reshape(128, 1))
    for nm, src in (("lru_wa", "lru_w_a"), ("lru_wx", "lru_w_x")):
        bd = np.zeros((128, 128), np.float32)
        bd[0:64, 0:64] = inp[src][l][2 * j]
        bd[64:128, 64:128] = inp[src][l][2 * j + 1]
        m[nm + L_] = bd
    m["lru_ba" + L_] = np.ascontiguousarray(inp["lru_b_a"][l][hs].reshape(128, 1))
    m["lru_bx" + L_] = np.ascontiguousarray(inp["lru_b_x"][l][hs].reshape(128, 1))
    m["lru_lam" + L_] = np.ascontiguousarray(inp["lru_lambda"][l][hs].reshape(128, 1))
    cwf = inp["gdn_conv_w"][l]
    m["gdn_cw" + L_] = np.ascontiguousarray(np.stack([cwf[:, g0 * 512:(g0 + 1) * 512][:, hs].T for g0 in range(3)], axis=1))
    m["gdn_alog" + L_] = np.full((128, 1), inp["gdn_a_log"][l][j], np.float32)
    m["gdn_dtb" + L_] = np.full((128, 1), inp["gdn_dt_bias"][l][j], np.float32)
    m["gdn_ng" + L_] = np.ascontiguousarray(inp["gdn_norm_g"][l].reshape(128, 1))
    for nm, src in (("nsa_w1k", "nsa_w1_k"), ("nsa_w1v", "nsa_w1_v")):
        m[nm + L_] = np.ascontiguousarray(inp[src][l].reshape(32, 128, 128).transpose(1, 0, 2))
    m["nsa_w2k" + L_] = inp["nsa_w2_k"][l]; m["nsa_w2v" + L_] = inp["nsa_w2_v"][l]
    m["nsa_pek" + L_] = np.ascontiguousarray(inp["nsa_pe_k"][l].T); m["nsa_pev" + L_] = np.ascontiguousarray(inp["nsa_pe_v"][l].T)
    return m


def prep_shared(inp):
    m = dict(consts_B())
    for l in range(2):
        L_ = "%d" % l
        binp = np.zeros(NZC * 128, np.float32)
        binp[:DIN] = inp["b_in"][l]
        ong = np.ones((D,), np.float32)
        ong[0:512] = inp["out_norm_g"][l][0]
        ong[1024:1536] = inp["out_norm_g"][l][1]
        ong[1536:2048] = inp["out_norm_g"][l][2]
        m.update({"g1a" + L_: col16(inp["ffn1_norm_g"][l]), "g2a" + L_: col16(inp["mix_norm_g"][l]),
                  "f1wg" + L_: inp["ffn1_w_gate"][l], "f1wu" + L_: inp["ffn1_w_up"][l], "f1wd" + L_: inp["ffn1_w_down"][l],
                  "win" + L_: inp["w_in"][l], "bin" + L_: np.ascontiguousarray(binp.reshape(NZC, 128).T),
                  "ong" + L_: col16(ong), "wout" + L_: inp["w_out"][l], "g1c" + L_: col16(inp["ffn2_norm_g"][l]),
                  "f2wg" + L_: inp["ffn2_w_gate"][l], "f2wu" + L_: inp["ffn2_w_up"][l], "f2wd" + L_: inp["ffn2_w_down"][l]})
    m["gf"] = col16(inp["final_norm_g"])
    nsc = nsa_static()
    m["nsa_keep"] = nsc["keep"]; m["nsa_add"] = nsc["add"]; m["nsa_E"] = nsc["E"]; m["nsa_ov"] = nsc["ov"]
    return m


def prep_core(inp, core):
    j = core % 4
    m = {}
    for l in range(2):
        m.update(prep_mix(inp, l, j))
    order = [(j + 1 + hh) % 4 for hh in range(4)]
    rb = np.asarray(inp["rel_bias"], np.float32)
    nsc = nsa_static()
    bcs = np.where(nsc["bc_mask"][:, :, None, :], rb[nsc["bc_idx"]][..., order].transpose(0, 1, 3, 2), NEG)
    m["nsa_bc"] = np.ascontiguousarray(bcs.astype(np.float32))
    m["nsa_tbs"] = np.where(nsc["tbs_mask"], rb[nsc["tbs_idx"], j], NEG).astype(np.float32)
    m["nsa_tbw"] = np.where(nsc["tbw_mask"], rb[nsc["tbw_idx"], j], NEG).astype(np.float32)
    mk = np.zeros((128, 16), np.float32)
    for hh in range(4):
        mk[:, 4 * hh + (j + hh) % 4] = 1.0
    m["msk"] = mk
    return m


_PROG = {}


def run_fused(inputs, dbg=False):
    inp = {k_: np.asarray(v, np.float32) for k_, v in inputs.items()}
    x = inp["x"].reshape(8 * TOK, D)
    key = "dbg" if dbg else "main"
    if key not in _PROG:
        _PROG[key] = build_fused(dbg)
    nc = _PROG[key]
    shared = prep_shared(inp)
    maps = []
    for c in range(8):
        m = dict(shared)
        m.update(prep_core(inp, c))
        m["x"] = np.ascontiguousarray(x[c * TOK:(c + 1) * TOK])
        maps.append(m)
    res = run_bass_kernel_spmd(nc, maps, core_ids=list(range(8)))
    return res.results


def kernel(**inputs):
    res = run_fused(inputs)
    out = np.concatenate([r["out"] for r in res], axis=0).reshape(2, T_, D)
    return np.ascontiguousarray(out.astype(np.float32))


_NSC = {}


def t5_bucket_static(dist):
    import math
    import jax
    import jax.numpy as jnp
    with jax.default_device(jax.devices("cpu")[0]):
        n = jnp.maximum(jnp.asarray(dist, jnp.int32), 0)
        nf = jnp.maximum(n, 1).astype(jnp.float32)
        large = 16 + (jnp.log(nf / 16) / math.log(1024 / 16) * (32 - 16)).astype(jnp.int32)
        large = jnp.minimum(large, 31)
        return np.asarray(jnp.where(n < 16, n, large))


def nsa_static():
    if _NSC:
        return _NSC
    p = np.arange(128)
    col = np.arange(512)
    q = np.arange(T_)
    n = (np.arange(2)[:, None] * 128 + p[None, :])
    d = q[None, None, :] - (16 * n[:, :, None] + 31)
    msk = (d >= 0) & (n[:, :, None] < 255)
    _NSC["bc_idx"] = t5_bucket_static(d).transpose(1, 0, 2)
    _NSC["bc_mask"] = msk.transpose(1, 0, 2)
    ms = np.arange(12) - 3
    d = 128 * ms[None, :, None] + col[None, None, :] - p[:, None, None]
    _NSC["tbs_idx"] = t5_bucket_static(d); _NSC["tbs_mask"] = d >= 0
    mw = np.arange(8) - 3
    d = 128 * mw[None, :, None] + col[None, None, :] - p[:, None, None]
    _NSC["tbw_idx"] = t5_bucket_static(d); _NSC["tbw_mask"] = (d >= 0) & (d < 512)
    qpos = np.arange(NT)[None, :, None] * 128 + p[:, None, None]
    cur = qpos // 64
    jj = np.arange(64)[None, None, :]
    forced = (jj == 0) | (jj == cur) | (jj == cur - 1)
    fut = jj > cur
    _NSC["keep"] = np.where(forced | fut, 0.0, 1.0).astype(np.float32)
    _NSC["add"] = np.where(fut, -1.0, np.where(forced, 1.0e6, 0.0)).astype(np.float32)
    _NSC["E"] = (np.arange(T_)[None, :] // 64 == np.arange(64)[:, None]).astype(np.float32)
    nn = np.arange(256)
    cst, cen = nn * 16, nn * 16 + 31
    sst, sen = np.arange(64) * 64, np.arange(64) * 64 + 63
    ovl = ((cst[:, None] <= sen[None, :]) & (cen[:, None] >= sst[None, :]) & (nn[:, None] < 255)).astype(np.float32)
    _NSC["ov"] = np.ascontiguousarray(ovl.reshape(2, 128, 64).transpose(1, 0, 2))
    return _NSC
```

```python
import numpy as np
import ml_dtypes
from contextlib import ExitStack
import concourse.bass as bass
import concourse.mybir as mybir
from concourse.bass_utils import run_bass_kernel_spmd

F32 = mybir.dt.float32
BF16 = mybir.dt.bfloat16
I32 = mybir.dt.int32
SP_POOL = [mybir.EngineType.SP, mybir.EngineType.Pool]
AF = mybir.ActivationFunctionType
ALU = mybir.AluOpType
AX = mybir.AxisListType

D = 2048
NCH = 16
DFF = 5632
NF = 44
TOK = 1024
TG = 512
DIN = 5912
NZC = 47
EPS = 1e-6
NEG = -30000.0


class St:
    __slots__ = ("w", "r")

    def __init__(self, w=None, r=None):
        self.w = w
        self.r = list(r) if r else []

    def copy(self):
        return St(self.w, self.r)


class T:
    def __init__(self, handle, name):
        self.h = handle
        self.name = name
        self.whole = St()
        self.cells = {}

    def __getitem__(self, idx):
        return self.h[idx]

    def states(self, key):
        if key is None:
            return [self.whole] + list(self.cells.values())
        if key not in self.cells:
            self.cells[key] = self.whole.copy()
        return [self.cells[key]]


class K:
    NSLOT = 6

    def __init__(self, nc, es):
        self.nc = nc
        self.es = es
        self.eng = {"pe": nc.tensor, "dve": nc.vector, "act": nc.scalar, "pool": nc.gpsimd, "sp": nc.sync}
        self.sem = {}
        self.cnt = {}
        for e in self.eng:
            self.sem[e] = es.enter_context(nc.semaphore("s_" + e))
            self.cnt[e] = 0
        self.known = {e: {} for e in self.eng}
        self.dsem = {}
        self.duse = {}
        self.dnext = {}
        for q in ("sp", "pool"):
            self.dnext[q] = 0
            for s in range(self.NSLOT):
                key = ("d", q, s)
                self.dsem[key] = es.enter_context(nc.semaphore("d_%s_%d" % (q, s)))
                self.duse[key] = 0
        self.ntile = 0
        self.dins = {}
        self.es_root = es
        self.ncc = 0

    def sb(self, shape, dt, name=None):
        self.ntile += 1
        name = "%s_%d" % (name or "t", self.ntile)
        h = self.es.enter_context(self.nc.sbuf_tensor(name, list(shape), dt))
        return T(h, name)

    def ps(self, shape, dt, name=None):
        self.ntile += 1
        name = "%s_%d" % (name or "p", self.ntile)
        h = self.es.enter_context(self.nc.psum_tensor(name, list(shape), dt))
        return T(h, name)

    def din(self, name, shape, dt=F32):
        if name not in self.dins:
            self.dins[name] = T(self.nc.dram_tensor(name, list(shape), dt, kind="ExternalInput").ap(), name)
        return self.dins[name]

    def barrier(self):
        for e in self.eng:
            self.wait_all(e)

    def scope(self):
        return _Scope(self)

    def dout(self, name, shape, dt=F32):
        return T(self.nc.dram_tensor(name, list(shape), dt, kind="ExternalOutput").ap(), name)

    def semh(self, key):
        return self.sem[key] if key in self.sem else self.dsem[key]

    def _deps(self, reads, writes):
        deps = set()
        for (t, key) in reads:
            for st in t.states(key):
                if st.w is not None:
                    deps.add(st.w)
        for (t, key) in writes:
            for st in t.states(key):
                if st.w is not None:
                    deps.add(st.w)
                deps.update(st.r)
        return deps

    def _wait(self, eng, deps):
        need = {}
        kn = self.known[eng]
        for (sk, val) in deps:
            if sk == "pe" and eng == "pe":
                continue
            if kn.get(sk, 0) < val and need.get(sk, 0) < val:
                need[sk] = val
        for sk, val in need.items():
            self.eng[eng].wait_ge(self.semh(sk), val)
            kn[sk] = val

    def _commit(self, ev, reads, writes):
        for (t, key) in reads:
            for st in t.states(key):
                st.r.append(ev)
        for (t, key) in writes:
            if key is None:
                t.cells.clear()
                t.whole.w = ev
                t.whole.r = []
            else:
                st = t.states(key)[0]
                st.w = ev
                st.r = []

    @staticmethod
    def _norm(lst):
        out = []
        for x in lst or []:
            out.append(x if isinstance(x, tuple) else (x, None))
        return out

    def op(self, eng, fn, reads=None, writes=None):
        reads = self._norm(reads)
        writes = self._norm(writes)
        self._wait(eng, self._deps(reads, writes))
        ins = fn()
        self.cnt[eng] += 1
        ins.then_inc(self.sem[eng], 1)
        ev = (eng, self.cnt[eng])
        self._commit(ev, reads, writes)
        return ev

    def dma(self, q, out_ap, in_ap, reads=None, writes=None, **kw):
        reads = self._norm(reads)
        writes = self._norm(writes)
        deps = self._deps(reads, writes)
        s = self.dnext[q]
        self.dnext[q] = (s + 1) % self.NSLOT
        key = ("d", q, s)
        if self.duse[key] > 0:
            deps.add((key, 16 * self.duse[key]))
        self._wait(q, deps)
        ins = self.eng[q].dma_start(out=out_ap, in_=in_ap, **kw)
        self.duse[key] += 1
        ins.then_inc(self.dsem[key], 16)
        ev = (key, 16 * self.duse[key])
        self._commit(ev, reads, writes)
        return ev

    def allgather(self, src_ap, dst_t, reads):
        reads = self._norm(reads)
        writes = [(dst_t, None)]
        self._wait("pool", self._deps(reads, writes))
        cs = self.es_root.enter_context(self.nc.semaphore("cc%d" % self.ncc))
        key = ("c", self.ncc)
        self.ncc += 1
        self.dsem[key] = cs
        self.nc.gpsimd.collective_compute("AllGather", ALU.bypass, replica_groups=[[0, 1, 2, 3], [4, 5, 6, 7]],
                                          ins=[src_ap.opt()], outs=[dst_t.h.opt()]).then_inc(cs)
        ev = (key, 1)
        self._commit(ev, reads, writes)
        return ev

    def wait_all(self, eng="sp"):
        kn = self.known[eng]
        for e in self.eng:
            if self.cnt[e] > kn.get(e, 0):
                self.eng[eng].wait_ge(self.sem[e], self.cnt[e])
                kn[e] = self.cnt[e]
        for key, n in self.duse.items():
            if 16 * n > kn.get(key, 0):
                self.eng[eng].wait_ge(self.dsem[key], 16 * n)
                kn[key] = 16 * n


class _Scope:
    def __init__(self, k):
        self.k = k

    def __enter__(self):
        self.old = self.k.es
        self.sub = ExitStack()
        self.sub.__enter__()
        self.k.es = self.sub
        return self

    def __exit__(self, *a):
        self.k.barrier()
        self.k.es = self.old
        return self.sub.__exit__(*a)


class Dense:
    def __init__(self, k, h):
        self.k = k
        nc = k.nc
        self.nc = nc
        self.h = h
        self.hn = k.sb([128, NCH, TOK], BF16, "hn")
        self.sq = [k.sb([128, TG], BF16, "sq%d" % i) for i in range(2)]
        self.rstd = k.sb([128, TG], F32, "rstd")
        self.onesm = k.sb([128, 128], BF16, "onesm")
        self.ones4 = k.sb([128, 128], BF16, "ones4")
        self.gcol = k.sb([128, NCH], F32, "gcol")
        self.wg = [k.sb([128, NCH, 256], BF16, "wg%d" % i) for i in range(2)]
        self.wu = [k.sb([128, NCH, 256], BF16, "wu%d" % i) for i in range(2)]
        self.wd = [k.sb([128, 2, D], BF16, "wd%d" % i) for i in range(2)]
        self.act = [k.sb([128, 2, TOK], BF16, "act%d" % i) for i in range(2)]
        self.sg = [k.sb([128, TG], F32, "sg%d" % i) for i in range(2)]
        self.psA = [k.ps([128, TG], F32, "psA%d" % i) for i in range(4)]
        self.psB = [k.ps([128, TG], F32, "psB%d" % i) for i in range(3)]
        self.psN = k.ps([128, TG], F32, "psN")
        self.ia = 0
        self.ib = 0
        self.epsc = k.sb([128, 1], F32, "epsc")
        k.op("dve", lambda: nc.vector.memset(self.epsc[:], EPS), writes=[self.epsc])
        k.op("dve", lambda: nc.vector.memset(self.onesm[:], 1.0 / D), writes=[self.onesm])
        k.op("dve", lambda: nc.vector.memset(self.ones4[:], 1.0 / 512), writes=[self.ones4])

    def rstd_from(self, ps):
        k, nc = self.k, self.nc
        k.op("act", lambda: nc.scalar.activation(out=self.rstd[:], in_=ps[:], func=AF.Ln, bias=self.epsc[:]),
             reads=[ps, self.epsc], writes=[self.rstd])
        k.op("act", lambda: nc.scalar.activation(out=self.rstd[:], in_=self.rstd[:], func=AF.Exp, scale=-0.5),
             reads=[self.rstd], writes=[self.rstd])

    def load_h(self, src):
        k = self.k
        v = src.h.rearrange("(c p) t -> p c t", p=128)
        for c in range(0, NCH, 4):
            k.dma("sp", self.h[:, c:c + 4, :], v[:, c:c + 4, :], reads=[src],
                  writes=[(self.h, (cc, tg)) for cc in range(c, c + 4) for tg in range(2)])

    def store_h(self, dst):
        k = self.k
        v = dst.h.rearrange("(c p) t -> p c t", p=128)
        for c in range(0, NCH, 4):
            k.dma("sp", v[:, c:c + 4, :], self.h[:, c:c + 4, :],
                  reads=[(self.h, (cc, tg)) for cc in range(c, c + 4) for tg in range(2)], writes=[(dst, c)])

    def rmsnorm(self, gsrc, out_t=None, out_f32=None):
        k, nc = self.k, self.nc
        k.dma("sp", self.gcol[:], gsrc.h, reads=[gsrc], writes=[self.gcol])
        for tg in range(2):
            ts = slice(tg * TG, (tg + 1) * TG)
            for c in range(NCH):
                sq = self.sq[c % 2]
                k.op("act", lambda sq=sq, c=c: nc.scalar.activation(out=sq[:], in_=self.h[:, c, ts], func=AF.Square),
                     reads=[(self.h, (c, tg))], writes=[sq])
                k.op("pe", lambda sq=sq, c=c: nc.tensor.matmul(self.psN[:], lhsT=self.onesm[:], rhs=sq[:],
                                                               start=(c == 0), stop=(c == NCH - 1)),
                     reads=[sq, self.onesm], writes=[self.psN])
            self.rstd_from(self.psN)
            for c in range(NCH):
                if out_f32 is None:
                    k.op("dve", lambda c=c: nc.vector.scalar_tensor_tensor(
                        out=self.hn[:, c, ts], in0=self.h[:, c, ts], scalar=self.gcol[:, c:c + 1], in1=self.rstd[:],
                        op0=ALU.mult, op1=ALU.mult),
                        reads=[(self.h, (c, tg)), self.gcol, self.rstd], writes=[(self.hn, tg)])
                else:
                    k.op("dve", lambda c=c: nc.vector.scalar_tensor_tensor(
                        out=out_f32[:, c, ts], in0=self.h[:, c, ts], scalar=self.gcol[:, c:c + 1], in1=self.rstd[:],
                        op0=ALU.mult, op1=ALU.mult),
                        reads=[(self.h, (c, tg)), self.gcol, self.rstd], writes=[(out_f32, (c, tg))])

    def ffn(self, wg_d, wu_d, wd_d):
        k, nc = self.k, self.nc
        wgv = wg_d.h.rearrange("(c p) f -> p c f", p=128)
        wuv = wu_d.h.rearrange("(c p) f -> p c f", p=128)
        wdv = wd_d.h.rearrange("(c p) d -> p c d", p=128)
        NG = NF // 2

        def load(g):
            b = g % 2
            k.dma("pool", self.wg[b][:], wgv[:, :, g * 256:(g + 1) * 256], reads=[wg_d], writes=[self.wg[b]])
            k.dma("pool", self.wu[b][:], wuv[:, :, g * 256:(g + 1) * 256], reads=[wu_d], writes=[self.wu[b]])
            k.dma("pool", self.wd[b][:], wdv[:, 2 * g:2 * g + 2, :], reads=[wd_d], writes=[self.wd[b]])

        load(0)
        for g in range(NG):
            if g + 1 < NG:
                load(g + 1)
            b = g % 2
            wg, wu, wd, act = self.wg[b], self.wu[b], self.wd[b], self.act[b]
            for fcl in range(2):
                fs = slice(fcl * 128, (fcl + 1) * 128)
                for tg in range(2):
                    ts = slice(tg * TG, (tg + 1) * TG)
                    pg = self.psA[self.ia % 4]
                    pu = self.psA[(self.ia + 1) % 4]
                    self.ia += 2

                    def mm(p, w):
                        ins = None
                        for c in range(NCH):
                            ins = nc.tensor.matmul(p[:], lhsT=w[:, c, fs], rhs=self.hn[:, c, ts],
                                                   start=(c == 0), stop=(c == NCH - 1))
                        return ins
                    k.op("pe", lambda: mm(pg, wg), reads=[wg, (self.hn, tg)], writes=[pg])
                    k.op("pe", lambda: mm(pu, wu), reads=[wu, (self.hn, tg)], writes=[pu])
                    sg = self.sg[tg]
                    k.op("act", lambda: nc.scalar.activation(out=sg[:], in_=pg[:], func=AF.Silu), reads=[pg], writes=[sg])
                    k.op("dve", lambda: nc.vector.tensor_tensor(out=act[:, fcl, ts], in0=pu[:], in1=sg[:], op=ALU.mult),
                         reads=[pu, sg], writes=[(act, (fcl, tg))])
            for dc in range(NCH):
                ds = slice(dc * 128, (dc + 1) * 128)
                for tg in range(2):
                    ts = slice(tg * TG, (tg + 1) * TG)
                    pd = self.psB[self.ib % 3]
                    self.ib += 1

                    def mmd():
                        ins = None
                        for fcl in range(2):
                            ins = nc.tensor.matmul(pd[:], lhsT=wd[:, fcl, ds], rhs=act[:, fcl, ts],
                                                   start=(fcl == 0), stop=(fcl == 1))
                        return ins
                    k.op("pe", mmd, reads=[wd, (act, (0, tg)), (act, (1, tg))], writes=[pd])
                    k.op("dve", lambda: nc.vector.scalar_tensor_tensor(
                        out=self.h[:, dc, ts], in0=pd[:], scalar=0.5, in1=self.h[:, dc, ts], op0=ALU.mult, op1=ALU.add),
                        reads=[pd, (self.h, (dc, tg))], writes=[(self.h, (dc, tg))])

    def proj(self, w_d, ncols, rhs_t, emit):
        k, nc = self.k, self.nc
        wv = w_d.h.rearrange("(c p) f -> p c f", p=128)
        ngr = (ncols + 255) // 256

        def load(g):
            b = g % 2
            c0 = g * 256
            c1 = min(ncols, c0 + 256)
            k.dma("pool", self.wg[b][:, :, 0:c1 - c0], wv[:, :, c0:c1], reads=[w_d], writes=[self.wg[b]])

        load(0)
        for g in range(ngr):
            if g + 1 < ngr:
                load(g + 1)
            w = self.wg[g % 2]
            for ml in range(2):
                m = 2 * g + ml
                M = min(128, ncols - m * 128)
                if M <= 0:
                    continue
                for tg in range(2):
                    ts = slice(tg * TG, (tg + 1) * TG)
                    pd = self.psB[self.ib % 3]
                    self.ib += 1

                    def mm():
                        ins = None
                        for c in range(NCH):
                            ins = nc.tensor.matmul(pd[0:M, :], lhsT=w[:, c, ml * 128:ml * 128 + M], rhs=rhs_t[:, c, ts],
                                                   start=(c == 0), stop=(c == NCH - 1))
                        return ins
                    k.op("pe", mm, reads=[w, (rhs_t, tg)], writes=[pd])
                    emit(m, M, tg, ts, pd)


def dense_A(k, h, l, zsh, zall):
    nc = k.nc
    g1 = k.din("g1a%d" % l, [128, NCH]); g2 = k.din("g2a%d" % l, [128, NCH])
    wg = k.din("f1wg%d" % l, [D, DFF]); wu = k.din("f1wu%d" % l, [D, DFF]); wd = k.din("f1wd%d" % l, [DFF, D])
    win = k.din("win%d" % l, [D, DIN]); bin_ = k.din("bin%d" % l, [128, NZC])
    dn = Dense(k, h)
    bcol = k.sb([128, NZC], F32, "bcol")
    zs = [k.sb([128, TG], BF16, "zs%d" % i) for i in range(3)]
    k.dma("sp", bcol[:], bin_.h, reads=[bin_], writes=[bcol])
    dn.rmsnorm(g1)
    dn.ffn(wg, wu, wd)
    dn.rmsnorm(g2)
    cnt = [0]

    def emit(m, M, tg, ts, pd):
        z = zs[cnt[0] % 3]
        cnt[0] += 1
        k.op("act", lambda: nc.scalar.activation(out=z[0:M, :], in_=pd[0:M, :], func=AF.Identity,
                                                 bias=bcol[0:M, m:m + 1]), reads=[pd, bcol], writes=[z])
        k.dma("sp", zsh.h[m * 128:m * 128 + M, ts], z[0:M, :], reads=[z], writes=[(zsh, (m, tg))])
        if tg == 1 and (m % 4 == 3 or m == NZC - 1):
            c0, cr, ct = zall.chunks[m // 4]
            k.allgather(zsh.h[c0:c0 + cr, :], ct, [(zsh, (mm_, t_)) for mm_ in range(4 * (m // 4), 4 * (m // 4) + 4) for t_ in (0, 1) if mm_ < NZC])
    assert zall.CR == 512
    dn.proj(win, DIN, dn.hn, emit)


def dense_C(k, h, l, yall, rv):
    nc = k.nc
    ong = k.din("ong%d" % l, [128, NCH]); wout = k.din("wout%d" % l, [D, D]); g1 = k.din("g1c%d" % l, [128, NCH])
    wg = k.din("f2wg%d" % l, [D, DFF]); wu = k.din("f2wu%d" % l, [D, DFF]); wd = k.din("f2wd%d" % l, [DFF, D])
    dn = Dense(k, h)
    ys = [k.sb([128, 4, TG], F32, "ys%d" % i) for i in range(2)]
    ocol = k.sb([128, NCH], F32, "ocol")
    k.dma("sp", ocol[:], ong.h, reads=[ong], writes=[ocol])
    yst = [k.sb([128, 4, TG], F32, "yst%d" % i) for i in range(2)]
    mskc = k.sb([128, 16], F32, "mskc")
    dmsk = k.din("msk", [128, 16])
    k.dma("sp", mskc[:], dmsk.h, reads=[dmsk], writes=[mskc])
    i = 0
    for grp in range(4):
        for tg in range(2):
            ts = slice(tg * TG, (tg + 1) * TG)
            y = ys[i % 2]
            i += 1
            for rc in range(4):
                st_ = yst[rc % 2]
                for jp in range(4):
                    for (ct, sap_, d0, cnt) in yall.pieces(jp, grp * 128, 128):
                        k.dma("sp", st_[d0:d0 + cnt, jp, :], sap_[:, rc * TOK + tg * TG:rc * TOK + (tg + 1) * TG], reads=[ct], writes=[st_])
                mc = mskc[:, rc:rc + 1]
                if rc == 0:
                    k.op("dve", lambda: nc.vector.tensor_scalar(out=y[:], in0=st_[:], scalar1=mc, scalar2=None, op0=ALU.mult), reads=[st_, mskc], writes=[y])
                else:
                    k.op("dve", lambda: nc.vector.scalar_tensor_tensor(out=y[:], in0=st_[:], scalar=mc, in1=y[:], op0=ALU.mult, op1=ALU.add),
                         reads=[st_, mskc, y], writes=[y])
            if grp == 1:
                for cl in range(4):
                    k.op("dve", lambda cl=cl: nc.vector.tensor_copy(out=dn.hn[:, 4 + cl, ts], in_=y[:, cl, :]),
                         reads=[y], writes=[(dn.hn, tg)])
                continue
            for cl in range(4):
                sq = dn.sq[cl % 2]
                k.op("act", lambda sq=sq, cl=cl: nc.scalar.activation(out=sq[:], in_=y[:, cl, :], func=AF.Square),
                     reads=[y], writes=[sq])
                k.op("pe", lambda sq=sq, cl=cl: nc.tensor.matmul(dn.psN[:], lhsT=dn.ones4[:], rhs=sq[:],
                                                                 start=(cl == 0), stop=(cl == 3)),
                     reads=[sq, dn.ones4], writes=[dn.psN])
            dn.rstd_from(dn.psN)
            for cl in range(4):
                c = grp * 4 + cl
                k.op("dve", lambda cl=cl, c=c: nc.vector.scalar_tensor_tensor(
                    out=dn.hn[:, c, ts], in0=y[:, cl, :], scalar=ocol[:, c:c + 1], in1=dn.rstd[:],
                    op0=ALU.mult, op1=ALU.mult), reads=[y, ocol, dn.rstd], writes=[(dn.hn, tg)])

    def emit(m, M, tg, ts, pd):
        k.op("dve", lambda: nc.vector.tensor_tensor(out=h[:, m, ts], in0=pd[:], in1=h[:, m, ts], op=ALU.add),
             reads=[pd, (h, (m, tg))], writes=[(h, (m, tg))])
    dn.proj(wout, D, dn.hn, emit)
    dn.rmsnorm(g1)
    dn.ffn(wg, wu, wd)


def final_out(k, h, out):
    nc = k.nc
    sq_ = [k.sb([128, TG], BF16, "sq%d" % i) for i in range(2)]
    rstd = k.sb([128, TG], F32, "rstd")
    onesm = k.sb([128, 128], BF16, "onesm")
    gcol = k.sb([128, NCH], F32, "gcol")
    epsc = k.sb([128, 1], F32, "epsc")
    psN = k.ps([128, TG], F32, "psN")
    psB = [k.ps([128, TG], F32, "psB%d" % i) for i in range(3)]
    ib = [0]
    k.op("dve", lambda: nc.vector.memset(epsc[:], EPS), writes=[epsc])
    k.op("dve", lambda: nc.vector.memset(onesm[:], 1.0 / D), writes=[onesm])

    def rstd_from():
        k.op("act", lambda: nc.scalar.activation(out=rstd[:], in_=psN[:], func=AF.Ln, bias=epsc[:]), reads=[psN, epsc], writes=[rstd])
        k.op("act", lambda: nc.scalar.activation(out=rstd[:], in_=rstd[:], func=AF.Exp, scale=-0.5), reads=[rstd], writes=[rstd])
    gf = k.din("gf", [128, NCH])
    identF = k.sb([128, 128], F32, "identF")
    cid = k.din("c_ident", [128, 128])
    k.dma("sp", identF[:], cid.h, reads=[cid], writes=[identF])
    fin = k.sb([128, NCH, TG], F32, "fin")
    ot = [k.sb([128, D], F32, "ot%d" % i) for i in range(2)]
    k.dma("sp", gcol[:], gf.h, reads=[gf], writes=[gcol])
    for tg in range(2):
        ts = slice(tg * TG, (tg + 1) * TG)
        for c in range(NCH):
            sq = sq_[c % 2]
            k.op("act", lambda sq=sq, c=c: nc.scalar.activation(out=sq[:], in_=h[:, c, ts], func=AF.Square),
                 reads=[(h, (c, tg))], writes=[sq])
            k.op("pe", lambda sq=sq, c=c: nc.tensor.matmul(psN[:], lhsT=onesm[:], rhs=sq[:],
                                                           start=(c == 0), stop=(c == NCH - 1)),
                 reads=[sq, onesm], writes=[psN])
        rstd_from()
        for c in range(NCH):
            k.op("dve", lambda c=c: nc.vector.scalar_tensor_tensor(
                out=fin[:, c, :], in0=h[:, c, ts], scalar=gcol[:, c:c + 1], in1=rstd[:],
                op0=ALU.mult, op1=ALU.mult), reads=[(h, (c, tg)), gcol, rstd], writes=[(fin, c)])
        for tt in range(4):
            o = ot[tt % 2]
            for c4 in range(4):
                pd = psB[ib[0] % 3]
                ib[0] += 1

                def mmT():
                    ins = None
                    for cl in range(4):
                        c = c4 * 4 + cl
                        ins = nc.tensor.matmul(pd[:, cl * 128:(cl + 1) * 128], lhsT=fin[:, c, tt * 128:(tt + 1) * 128],
                                               rhs=identF[:], start=True, stop=True)
                    return ins
                k.op("pe", mmT, reads=[identF] + [(fin, c4 * 4 + cl) for cl in range(4)], writes=[pd])
                k.op("act", lambda: nc.scalar.copy(out=o[:, c4 * 512:(c4 + 1) * 512], in_=pd[:]), reads=[pd], writes=[(o, c4)])
            r0 = tg * TG + tt * 128
            k.dma("sp", out.h[r0:r0 + 128, :], o[:], reads=[o], writes=[(out, r0)])


def load_x(k, h, x):
    nc = k.nc
    identF = k.sb([128, 128], F32, "identF")
    cid = k.din("c_ident", [128, 128])
    k.dma("sp", identF[:], cid.h, reads=[cid], writes=[identF])
    xt = [k.sb([128, D], F32, "xt%d" % i) for i in range(4)]
    pst = [k.ps([128, TG], F32, "pst%d" % i) for i in range(4)]
    n = 0
    for tg in range(2):
        for tt in range(4):
            r0 = tg * TG + tt * 128
            k.dma("sp", xt[tt][:], x.h[r0:r0 + 128, :], reads=[x], writes=[xt[tt]])
        for c in range(NCH):
            pd = pst[n % 4]
            n += 1

            def mmT():
                ins = None
                for tt in range(4):
                    ins = nc.tensor.matmul(pd[:, tt * 128:(tt + 1) * 128], lhsT=xt[tt][:, c * 128:(c + 1) * 128], rhs=identF[:],
                                           start=True, stop=True)
                return ins
            k.op("pe", mmT, reads=[identF] + xt, writes=[pd])
            eng = "act" if c % 2 else "dve"
            if eng == "act":
                k.op("act", lambda: nc.scalar.copy(out=h[:, c, tg * TG:(tg + 1) * TG], in_=pd[:]), reads=[pd], writes=[(h, (c, tg))])
            else:
                k.op("dve", lambda: nc.vector.tensor_copy(h[:, c, tg * TG:(tg + 1) * TG], pd[:]), reads=[pd], writes=[(h, (c, tg))])


def col16(g):
    return np.ascontiguousarray(np.asarray(g, np.float32).reshape(NCH, 128).T)


T_ = 4096
NT = 32
SCALE = 128 ** -0.5


class Mix:
    def __init__(self, k, l, zall, vals, nbig=7, nbigb=4):
        self.k = k
        self.l = l
        self.zall = zall
        nc = self.nc = k.nc
        self.cident = k.din("c_ident", [128, 128])
        self.msk = k.sb([128, 16], F32, "msk")
        dmsk = k.din("msk", [128, 16])
        k.dma("sp", self.msk[:], dmsk.h, reads=[dmsk], writes=[self.msk])
        self.identF = k.sb([128, 128], F32, "identF")
        self.identB = k.sb([128, 128], BF16, "identB")
        self.onesF = k.sb([128, 128], F32, "onesF")
        self.onesB = k.sb([128, 128], BF16, "onesB")
        k.dma("sp", self.identF[:], self.cident.h, reads=[self.cident], writes=[self.identF])
        k.op("dve", lambda: nc.vector.tensor_copy(self.identB[:], self.identF[:]), reads=[self.identF], writes=[self.identB])
        k.op("dve", lambda: nc.vector.memset(self.onesF[:], 1.0), writes=[self.onesF])
        k.op("dve", lambda: nc.vector.memset(self.onesB[:], 1.0), writes=[self.onesB])
        self.big = [k.sb([128, T_], F32, "big%d" % i) for i in range(nbig)]
        self.bigb = [k.sb([128, T_], BF16, "bigb%d" % i) for i in range(nbigb)]
        self.psS = [k.ps([128, 512], F32, "psS%d" % i) for i in range(3)]
        self.psO = [k.ps([128, 512], F32, "psO%d" % i) for i in range(2)]
        self.psL = [k.ps([128, 512], F32, "psL%d" % i) for i in range(2)]
        self.psX = k.ps([128, 512], F32, "psX")
        self.iS = 0
        self.pT = [k.sb([128, 512], BF16, "pT%d" % i) for i in range(3)]
        self.tmpA = [k.sb([128, 512], F32, "tmpA%d" % i) for i in range(4)]

    def zfm(self, dst_t, dst_ap, off, dyn=None, q="sp", n=128, key=None, mul=128, stage=None):
        k, nc = self.k, self.nc
        dap = dst_ap[:] if not hasattr(dst_ap, "ap") else dst_ap
        if dyn is None:
            for s_ in range(4):
                for (ct, sap_, d0, cnt) in self.zall.pieces(s_, off, n):
                    k.dma(q, dap[d0:d0 + cnt, s_ * TOK:(s_ + 1) * TOK], sap_, reads=[ct], writes=[(dst_t, key)])
            return
        sap = stage[:]
        if sap.dtype != BF16:
            sap = sap.bitcast(BF16)[:, 0:T_]
        for jc in range(4):
            for s_ in range(4):
                for (ct, sap_, d0, cnt) in self.zall.pieces(s_, off + jc * mul, n):
                    k.dma(q, sap[d0:d0 + cnt, s_ * TOK:(s_ + 1) * TOK], sap_, reads=[ct], writes=[stage])
            mc = self.msk[0:n, dyn + jc:dyn + jc + 1]
            if jc == 0:
                k.op("dve", lambda: nc.vector.tensor_scalar(out=dap[0:n, :], in0=sap[0:n, :], scalar1=mc, scalar2=None, op0=ALU.mult),
                     reads=[stage, self.msk], writes=[(dst_t, key)])
            else:
                k.op("dve", lambda: nc.vector.scalar_tensor_tensor(out=dap[0:n, :], in0=sap[0:n, :], scalar=mc, in1=dap[0:n, :], op0=ALU.mult, op1=ALU.add),
                     reads=[stage, self.msk, (dst_t, key)], writes=[(dst_t, key)])

    def row2col(self, row, dst):
        k, nc = self.k, self.nc
        px = self.psX

        def mm():
            ins = None
            for kt in range(NT):
                ins = nc.tensor.matmul(px[:, kt:kt + 1], lhsT=row[0:1, kt * 128:(kt + 1) * 128], rhs=self.onesF[0:1, 0:1], start=True, stop=True)
            return ins
        k.op("pe", mm, reads=[row, self.onesF], writes=[px])
        k.op("dve", lambda: nc.vector.tensor_copy(dst[:], px[:, 0:NT]), reads=[px], writes=[dst])

    def fm2tm(self, src_ap, src_dep, dst_t, dst_ap, key=None):
        k, nc = self.k, self.nc
        for g4 in range(NT // 4):
            ps = self.psS[g4 % 3]

            def mm():
                ins = None
                for i in range(4):
                    kt = g4 * 4 + i
                    ins = nc.tensor.matmul(ps[:, i * 128:(i + 1) * 128], lhsT=src_ap[:, kt * 128:(kt + 1) * 128], rhs=self.identB[:], start=True, stop=True)
                return ins
            k.op("pe", mm, reads=[src_dep, self.identB], writes=[ps])
            if g4 % 2:
                k.op("act", lambda: nc.scalar.copy(out=dst_ap[:, g4 * 512:(g4 + 1) * 512], in_=ps[:]), reads=[ps], writes=[(dst_t, key)])
            else:
                k.op("dve", lambda: nc.vector.tensor_copy(dst_ap[:, g4 * 512:(g4 + 1) * 512], ps[:]), reads=[ps], writes=[(dst_t, key)])

    def ld(self, dst, src_t, src_ap=None, q="sp", dst_ap=None):
        self.k.dma(q, dst[:] if dst_ap is None else dst_ap, src_t.h if src_ap is None else src_ap, reads=[src_t], writes=[dst])

    def lru(self, yout):
        k, nc = self.k, self.nc
        L_ = "%d" % self.l
        cw = k.din("lru_cw" + L_, [128, 4]); cb = k.din("lru_cb" + L_, [128, 1])
        wa = k.din("lru_wa" + L_, [128, 128]); wx = k.din("lru_wx" + L_, [128, 128])
        ba = k.din("lru_ba" + L_, [128, 1]); bx = k.din("lru_bx" + L_, [128, 1]); lam = k.din("lru_lam" + L_, [128, 1])
        xs, xc, aa, uu, gg, tt = self.big[0:6]
        hs = gg
        xcb = self.bigb[0]
        cws = k.sb([128, 4], F32, "l_cw"); cbs = k.sb([128, 1], F32, "l_cb")
        was = k.sb([128, 128], BF16, "l_wa"); wxs = k.sb([128, 128], BF16, "l_wx")
        bas = k.sb([128, 1], F32, "l_ba"); bxs = k.sb([128, 1], F32, "l_bx"); lams = k.sb([128, 1], F32, "l_lam")
        nsp = k.sb([128, 1], F32, "l_nsp")
        self.zfm(xs, xs.h, _OFF["lru_x"], 0, stage=tt); self.zfm(gg, gg.h, _OFF["lru_gate"], 0, stage=tt); self.ld(cws, cw); self.ld(cbs, cb); self.ld(bas, ba); self.ld(bxs, bx); self.ld(lams, lam)
        self.ld(was, wa, q="pool"); self.ld(wxs, wx, q="pool")
        k.op("act", lambda: nc.scalar.activation(out=nsp[:], in_=lams[:], func=AF.Exp, scale=-1.0), reads=[lams], writes=[nsp])
        k.op("act", lambda: nc.scalar.activation(out=nsp[:], in_=nsp[:], func=AF.Ln, bias=self.onesF[:, 0:1]),
             reads=[nsp, self.onesF], writes=[nsp])
        k.op("dve", lambda: nc.vector.tensor_scalar(out=nsp[:], in0=nsp[:], scalar1=-8.0, scalar2=None, op0=ALU.mult),
             reads=[nsp], writes=[nsp])
        k.op("dve", lambda: nc.vector.tensor_scalar(out=xc[:], in0=xs[:], scalar1=cws[:, 3:4], scalar2=cbs[:, 0:1],
                                                    op0=ALU.mult, op1=ALU.add), reads=[xs, cws, cbs], writes=[xc])
        for i in range(3):
            sh = 3 - i
            k.op("dve", lambda i=i, sh=sh: nc.vector.scalar_tensor_tensor(
                out=xc[:, sh:], in0=xs[:, 0:T_ - sh], scalar=cws[:, i:i + 1], in1=xc[:, sh:], op0=ALU.mult, op1=ALU.add),
                reads=[xs, cws, xc], writes=[xc])
        k.op("act", lambda: nc.scalar.copy(out=xcb[:], in_=xc[:]), reads=[xc], writes=[xcb])
        for tb in range(8):
            ts = slice(tb * 512, (tb + 1) * 512)
            pr = self.psS[0]; pi = self.psS[1]
            k.op("pe", lambda: nc.tensor.matmul(pr[:], lhsT=was[:], rhs=xcb[:, ts], start=True, stop=True), reads=[was, xcb], writes=[pr])
            k.op("pe", lambda: nc.tensor.matmul(pi[:], lhsT=wxs[:], rhs=xcb[:, ts], start=True, stop=True), reads=[wxs, xcb], writes=[pi])
            r = self.tmpA[0]; ii = self.tmpA[1]
            k.op("act", lambda: nc.scalar.activation(out=r[:], in_=pr[:], func=AF.Sigmoid, bias=bas[:, 0:1]), reads=[pr, bas], writes=[r])
            k.op("act", lambda: nc.scalar.activation(out=ii[:], in_=pi[:], func=AF.Sigmoid, bias=bxs[:, 0:1]), reads=[pi, bxs], writes=[ii])
            k.op("act", lambda: nc.scalar.activation(out=aa[:, ts], in_=r[:], func=AF.Exp, scale=nsp[:, 0:1]),
                 reads=[r, nsp], writes=[(aa, tb)])
            t1 = self.tmpA[2]
            k.op("dve", lambda: nc.vector.tensor_tensor(out=t1[:], in0=aa[:, ts], in1=aa[:, ts], op=ALU.mult), reads=[(aa, tb)], writes=[t1])
            k.op("dve", lambda: nc.vector.tensor_scalar(out=t1[:], in0=t1[:], scalar1=-1.0, scalar2=1.0, op0=ALU.mult, op1=ALU.add),
                 reads=[t1], writes=[t1])
            k.op("act", lambda: nc.scalar.activation(out=t1[:], in_=t1[:], func=AF.Sqrt), reads=[t1], writes=[t1])
            k.op("dve", lambda: nc.vector.tensor_tensor(out=ii[:], in0=ii[:], in1=xc[:, ts], op=ALU.mult), reads=[ii, xc], writes=[ii])
            k.op("dve", lambda: nc.vector.tensor_tensor(out=uu[:, ts], in0=ii[:], in1=t1[:], op=ALU.mult), reads=[ii, t1], writes=[(uu, tb)])
        self.gelu_tanh(tt, gg, xs)
        k.op("dve", lambda: nc.vector.tensor_tensor_scan(out=hs[:], data0=aa[:], data1=uu[:], initial=0.0, op0=ALU.mult, op1=ALU.add),
             reads=[aa, uu], writes=[hs])
        k.op("dve", lambda: nc.vector.tensor_tensor(out=hs[:], in0=hs[:], in1=tt[:], op=ALU.mult), reads=[hs, tt], writes=[hs])
        k.dma("sp", yout.h, hs[:], reads=[hs], writes=[yout])

    def gelu_tanh(self, out, x, tmp):
        k, nc = self.k, self.nc
        k.op("dve", lambda: nc.vector.tensor_tensor(out=tmp[:], in0=x[:], in1=x[:], op=ALU.mult), reads=[x], writes=[tmp])
        k.op("dve", lambda: nc.vector.tensor_scalar(out=tmp[:], in0=tmp[:], scalar1=0.044715, scalar2=1.0, op0=ALU.mult, op1=ALU.add),
             reads=[tmp], writes=[tmp])
        k.op("dve", lambda: nc.vector.tensor_tensor(out=tmp[:], in0=tmp[:], in1=x[:], op=ALU.mult), reads=[tmp, x], writes=[tmp])
        k.op("act", lambda: nc.scalar.activation(out=tmp[:], in_=tmp[:], func=AF.Sigmoid, scale=1.5957691216057308),
             reads=[tmp], writes=[tmp])
        k.op("dve", lambda: nc.vector.tensor_tensor(out=out[:], in0=tmp[:], in1=x[:], op=ALU.mult), reads=[tmp, x], writes=[out])

    def attn_unit(self, kT, kt, qT, q0, extra, bias_ap, bias_reads, V, vslice, O, L, first, last, nkeys=128, kT_t=None, qT_t=None):
        k, nc = self.k, self.nc
        S = self.psS[self.iS % 3]
        P = self.pT[self.iS % 3]
        self.iS += 1
        nk = nkeys

        def mm():
            n = len(extra)
            ins = nc.tensor.matmul(S[0:nk, :], lhsT=kT[:, kt * 128:kt * 128 + nk], rhs=qT[:, q0:q0 + 512], start=True, stop=(n == 0))
            for i, (oa, l, r) in enumerate(extra):
                ins = nc.tensor.matmul(oa(S), lhsT=l, rhs=r, start=False, stop=(i == n - 1))
            return ins
        k.op("pe", mm, reads=[kT_t or kT, qT_t or qT] + self._xr, writes=[S])
        if bias_ap is None:
            k.op("act", lambda: nc.scalar.activation(out=P[0:nk, :], in_=S[0:nk, :], func=AF.Exp), reads=[S], writes=[P])
        else:
            k.op("act", lambda: nc.scalar.activation(out=P[0:nk, :], in_=S[0:nk, :], func=AF.Exp, bias=bias_ap),
                 reads=[S] + bias_reads, writes=[P])
        k.op("pe", lambda: nc.tensor.matmul(O[:], lhsT=vslice[0:nk], rhs=P[0:nk, :], start=first, stop=last), reads=[V, P], writes=[O])
        k.op("pe", lambda: nc.tensor.matmul(L[:], lhsT=self.onesB[0:nk, :], rhs=P[0:nk, :], start=first, stop=last),
             reads=[self.onesB, P], writes=[L])

    def fox(self, yout):
        k, nc = self.k, self.nc
        cut = k.din("c_ut", [128, 128]); csu = k.din("c_su", [32, 32]); cmb = k.din("c_mbfox", [128, 4, 512])
        qf = self.big[0]
        qb, kb = self.bigb[0], self.bigb[1]
        vb = self.bigb[2]
        mb = k.sb([128, 4, 512], BF16, "f_mb")
        ut = k.sb([128, 128], F32, "f_ut"); su = k.sb([32, 32], F32, "f_su")
        lf = k.sb([128, NT], F32, "f_lf"); negc = k.sb([128, NT], F32, "f_negc")
        totc = k.sb([32, 1], F32, "f_totc"); am = k.sb([32, 128], F32, "f_am")
        dg = self.big[1]
        frow = k.sb([1, T_], F32, "f_row")
        vT = self.bigb[3]
        frow2 = k.sb([1, T_], F32, "f_row2")
        self.zfm(kb, kb.h, _OFF["fox_k"], 0, q="pool", stage=qb)
        self.zfm(vT, vT.h, _OFF["fox_v"], 0, q="pool", stage=qb)
        self.zfm(qf, qf.h, _OFF["fox_q"], 0, stage=dg)
        self.zfm(frow, frow.h, _OFF["fox_f"], 0, n=1, mul=1, stage=frow2)
        self.fm2tm(vT.h, vT, vb, vb.h)
        k.dma("pool", mb[:], cmb.h, reads=[cmb], writes=[mb])
        self.ld(ut, cut); self.ld(su, csu)
        self.row2col(frow, lf)
        k.op("dve", lambda: nc.vector.tensor_scalar(out=qb[:], in0=qf[:], scalar1=SCALE, scalar2=None, op0=ALU.mult), reads=[qf], writes=[qb])
        k.op("act", lambda: nc.scalar.activation(out=lf[:], in_=lf[:], func=AF.Exp, scale=-1.0), reads=[lf], writes=[lf])
        k.op("act", lambda: nc.scalar.activation(out=lf[:], in_=lf[:], func=AF.Ln, bias=self.onesF[:, 0:1]), reads=[lf, self.onesF], writes=[lf])
        px = self.psX
        k.op("pe", lambda: nc.tensor.matmul(px[0:32, 0:1], lhsT=lf[:], rhs=self.onesF[:, 0:1], start=True, stop=True),
             reads=[lf, self.onesF], writes=[px])
        k.op("dve", lambda: nc.vector.tensor_copy(totc[:], px[0:32, 0:1]), reads=[px], writes=[totc])
        k.op("dve", lambda: nc.vector.tensor_scalar(out=am[:], in0=self.onesF[0:32, :], scalar1=totc[:, 0:1], scalar2=None, op0=ALU.mult),
             reads=[self.onesF, totc], writes=[am])

        def mmc():
            nc.tensor.matmul(px[:, 0:NT], lhsT=ut[:], rhs=lf[:], start=True, stop=False)
            return nc.tensor.matmul(px[:, 0:NT], lhsT=am[:], rhs=su[:], start=False, stop=True)
        k.op("pe", mmc, reads=[ut, lf, am, su], writes=[px])
        k.op("dve", lambda: nc.vector.tensor_copy(negc[:], px[:, 0:NT]), reads=[px], writes=[negc])
        for qt in range(NT):
            k.op("dve", lambda qt=qt: nc.vector.tensor_scalar(out=dg[:, qt * 128:(qt + 1) * 128], in0=self.identF[:],
                                                              scalar1=negc[:, qt:qt + 1], scalar2=-1.0, op0=ALU.mult, op1=ALU.mult),
                 reads=[self.identF, negc], writes=[(dg, qt)])
        for i in range(8):
            q0 = 512 * i
            O = self.psO[i % 2]; L = self.psL[i % 2]
            nk = 4 * i + 4
            for kt in range(nk):
                extra = []
                for jq in range(4):
                    qt = 4 * i + jq
                    extra.append((lambda S, jq=jq: S[:, jq * 128:(jq + 1) * 128], self.onesF[:], dg[:, qt * 128:(qt + 1) * 128]))
                self._xr = [self.onesF] + [(dg, 4 * i + jq) for jq in range(4)]
                if kt >= 4 * i:
                    extra.append((lambda S: S[:], self.identB[:], mb[:, kt - 4 * i, :]))
                    self._xr += [self.identB, mb]
                self.attn_unit(kb, kt, qb, q0, extra, negc[:, kt:kt + 1], [negc], vb, vb[:, kt * 128:(kt + 1) * 128], O, L,
                               kt == 0, kt == nk - 1)
            R = self.tmpA[i % 2]; ob = self.tmpA[2 + i % 2]
            k.op("dve", lambda: nc.vector.reciprocal(out=R[:], in_=L[:]), reads=[L], writes=[R])
            k.op("dve", lambda: nc.vector.tensor_tensor(out=ob[:], in0=O[:], in1=R[:], op=ALU.mult), reads=[O, R], writes=[ob])
            k.dma("sp", yout.h[:, q0:q0 + 512], ob[:], reads=[ob], writes=[(yout, i)])


    def bfv(self, t):
        return t.h[:].bitcast(BF16)

    def nsa(self, yout):
        k, nc = self.k, self.nc
        L_ = "%d" % self.l
        dw1k = k.din("nsa_w1k" + L_, [128, 32, 128]); dw1v = k.din("nsa_w1v" + L_, [128, 32, 128])
        dw2k = k.din("nsa_w2k" + L_, [128, 128]); dw2v = k.din("nsa_w2v" + L_, [128, 128])
        dpek = k.din("nsa_pek" + L_, [128, 32]); dpev = k.din("nsa_pev" + L_, [128, 32])
        dbc = k.din("nsa_bc", [128, 2, 4, T_]); dtbs = k.din("nsa_tbs", [128, 12, 512]); dtbw = k.din("nsa_tbw", [128, 8, 512])
        dkeep = k.din("nsa_keep", [128, NT, 64]); dadd = k.din("nsa_add", [128, NT, 64])
        dE = k.din("nsa_E", [64, T_]); dov = k.din("nsa_ov", [128, 2, 64])
        kcT = k.sb([128, 256], BF16, "n_kcT"); vc = k.sb([128, 2, 128], BF16, "n_vc")
        qown = k.sb([128, T_], BF16, "n_qown")
        ksb = k.sb([128, T_], BF16, "n_ks"); kwb = k.sb([128, T_], BF16, "n_kw")
        vsb = k.sb([128, T_], BF16, "n_vs"); vwb = k.sb([128, T_], BF16, "n_vw")
        grow = k.sb([3, T_], F32, "n_grow")
        with k.scope():
            gstg = k.sb([3, T_], BF16, "n_gstg")
            self.zfm(grow, grow.h, _OFF["nsa_g"], 0, n=3, mul=3, stage=gstg)
        gsel = [k.sb([3, 128], F32, "n_gsel%d" % i) for i in range(3)]
        for gi in range(3):
            k.op("dve", lambda gi=gi: nc.vector.tensor_scalar(out=gsel[gi][:], in0=self.onesF[0:3, :], scalar1=self.identF[0:3, gi:gi + 1], scalar2=None, op0=ALU.mult),
                 reads=[self.onesF, self.identF], writes=[gsel[gi]])
        self.zfm(qown, qown.h, _OFF["nsa_q"], 0, q="pool", stage=vsb)
        k.op("dve", lambda: nc.vector.tensor_scalar(out=qown[:], in0=qown[:], scalar1=SCALE, scalar2=None, op0=ALU.mult), reads=[qown], writes=[qown])
        self.zfm(ksb, ksb.h, _OFF["nsa_ks"], None, q="pool")
        self.zfm(kwb, kwb.h, _OFF["nsa_kw"], None, q="pool")
        with k.scope():
            stg = [k.sb([128, T_], BF16, "n_stg%d" % i) for i in range(2)]
            self.zfm(stg[0], stg[0].h, _OFF["nsa_vs"], None, q="pool")
            self.zfm(stg[1], stg[1].h, _OFF["nsa_vw"], None, q="pool")
            self.fm2tm(stg[0].h, stg[0], vsb, vsb.h)
            self.fm2tm(stg[1].h, stg[1], vwb, vwb.h)
        cscope = k.scope()
        cscope.__enter__()
        kcin_t = k.sb([128, T_], BF16, "n_kcin"); vcin_t = k.sb([128, T_], BF16, "n_vcin")
        kcin = kcin_t.h; vcin = vcin_t.h
        self.zfm(kcin_t, kcin, _OFF["nsa_kc"], None, q="pool")
        self.zfm(vcin_t, vcin, _OFF["nsa_vc"], None, q="pool")
        w1 = [k.sb([128, 32, 128], BF16, "n_w1k"), k.sb([128, 32, 128], BF16, "n_w1v")]
        w2 = [k.sb([128, 128], BF16, "n_w2k"), k.sb([128, 128], BF16, "n_w2v")]
        pe = [k.sb([128, 32], BF16, "n_pek"), k.sb([128, 32], BF16, "n_pev")]
        self.ld(w1[0], dw1k, q="pool"); self.ld(w1[1], dw1v, q="pool"); self.ld(w2[0], dw2k, q="pool"); self.ld(w2[1], dw2v, q="pool")
        self.ld(pe[0], dpek, q="pool"); self.ld(pe[1], dpev, q="pool")
        hidb = [k.sb([128, 256], BF16, "n_hidk"), k.sb([128, 256], BF16, "n_hidv")]
        pb = k.sb([128, 1], F32, "n_pb")
        hx = k.sb([128, 256], F32, "n_hx"); hy = k.sb([128, 256], F32, "n_hy"); hz = k.sb([128, 256], F32, "n_hz")
        srcs = [kcin_t, vcin_t]
        cin = [kcin, vcin]
        for w in range(2):
            px = self.psX

            def mmb():
                ins = None
                for i in range(32):
                    ins = nc.tensor.matmul(px[:, 0:1], lhsT=w1[w][:, i, :], rhs=pe[w][:, i:i + 1], start=(i == 0), stop=(i == 31))
                return ins
            k.op("pe", mmb, reads=[w1[w], pe[w]], writes=[px])
            k.op("dve", lambda: nc.vector.tensor_copy(pb[:], px[:, 0:1]), reads=[px], writes=[pb])
            ph = self.psS[w]

            def mmh():
                ins = None
                for i in range(32):
                    ins = nc.tensor.matmul(ph[:, 0:255], lhsT=w1[w][:, i, :], rhs=cin[w][:, i:i + 4065:16], start=(i == 0), stop=(i == 31))
                return ins
            k.op("pe", mmh, reads=[w1[w], srcs[w]], writes=[ph])
            k.op("dve", lambda: nc.vector.memset(hx[:], 0.0), writes=[hx])
            k.op("act", lambda: nc.scalar.activation(out=hx[:, 0:255], in_=ph[:, 0:255], func=AF.Identity, bias=pb[:, 0:1]),
                 reads=[ph, pb], writes=[hx])
            self.gelu_tanh(hz, hx, hy)
            k.op("dve", lambda: nc.vector.tensor_copy(hidb[w][:], hz[:]), reads=[hz], writes=[hidb[w]])
        pk = self.psS[2]
        k.op("pe", lambda: nc.tensor.matmul(pk[:, 0:256], lhsT=w2[0][:], rhs=hidb[0][:], start=True, stop=True), reads=[w2[0], hidb[0]], writes=[pk])
        k.op("dve", lambda: nc.vector.tensor_copy(kcT[:], pk[:, 0:256]), reads=[pk], writes=[kcT])
        for nt in range(2):
            pv = self.psO[nt]
            k.op("pe", lambda: nc.tensor.matmul(pv[:, 0:128], lhsT=hidb[1][:, nt * 128:(nt + 1) * 128], rhs=w2[1][:], start=True, stop=True),
                 reads=[hidb[1], w2[1]], writes=[pv])
            k.op("dve", lambda: nc.vector.tensor_copy(vc[:, nt, :], pv[:, 0:128]), reads=[pv], writes=[(vc, nt)])
        cscope.__exit__(None, None, None)
        tbs = k.sb([128, 12, 512], BF16, "n_tbs"); tbw = k.sb([128, 8, 512], BF16, "n_tbw")
        self.ld(tbs, dtbs, q="pool"); self.ld(tbw, dtbw, q="pool")
        keep = k.sb([128, 4, 64], F32, "n_keep"); addt = k.sb([128, 4, 64], F32, "n_add")
        Eb = k.sb([64, T_], BF16, "n_E"); ov = k.sb([128, 2, 64], BF16, "n_ov")
        self.ld(Eb, dE, q="pool"); self.ld(ov, dov, q="pool")
        qo = k.sb([128, 3, 512], BF16, "n_qo"); qst = k.sb([128, 4, 512], BF16, "n_qst")
        bc = [k.sb([128, 2, 4, 512], BF16, "n_bc0")] * 2
        gb = [k.sb([128, 3, 512], F32, "n_gb0")] * 2
        pc = [[k.sb([128, 512], BF16, "n_pc%d%d" % (h, nt)) for nt in range(2)] for h in range(4)]
        Rh = [k.sb([128, 512], F32, "n_R")] * 4
        acc = [k.sb([128, 512], F32, "n_acc%d" % i) for i in range(2)]
        impt = k.sb([128, 4, 64], F32, "n_imp"); wrk = k.sb([128, 64], F32, "n_wrk")
        m8 = k.sb([128, 8], F32, "n_m8"); thr = k.sb([128, 1], F32, "n_thr"); selb = k.sb([128, 64], F32, "n_selb")
        selT = k.sb([64, 512], BF16, "n_selT")
        Wt = k.sb([128, 512], F32, "n_W")
        NKC = [128, 127]
        for i in range(8):
            q0 = 512 * i
            bci = bc[i % 2]; gbi = gb[i % 2]; ac = acc[i % 2]
            k.dma("pool", bci[:], dbc.h[:, :, :, q0:q0 + 512], reads=[dbc], writes=[bci])
            for gi in range(3):
                pgx = self.psS[self.iS % 3]; self.iS += 1
                k.op("pe", lambda: nc.tensor.matmul(pgx[:], lhsT=gsel[gi][:], rhs=grow[0:3, q0:q0 + 512], start=True, stop=True),
                     reads=[gsel[gi], grow], writes=[pgx])
                k.op("act", lambda: nc.scalar.activation(out=gbi[:, gi, :], in_=pgx[:], func=AF.Sigmoid), reads=[pgx], writes=[(gbi, gi)])
            sq_ = q0 // TOK
            c0 = q0 - sq_ * TOK
            for jc in range(4):
                for (ct, sap_, d0, cnt) in self.zall.pieces(sq_, _OFF["nsa_q"] + jc * 128, 128):
                    k.dma("pool", qst[d0:d0 + cnt, jc, :], sap_[:, c0:c0 + 512], reads=[ct], writes=[qst])
            for hh in range(3):
                for jc in range(4):
                    mc = self.msk[:, 4 + 4 * hh + jc:5 + 4 * hh + jc]
                    if jc == 0:
                        k.op("dve", lambda: nc.vector.tensor_scalar(out=qo[:, hh, :], in0=qst[:, jc, :], scalar1=mc, scalar2=None, op0=ALU.mult),
                             reads=[qst, self.msk], writes=[qo])
                    else:
                        k.op("dve", lambda: nc.vector.scalar_tensor_tensor(out=qo[:, hh, :], in0=qst[:, jc, :], scalar=mc, in1=qo[:, hh, :], op0=ALU.mult, op1=ALU.add),
                             reads=[qst, self.msk, qo], writes=[qo])
            k.op("dve", lambda: nc.vector.tensor_scalar(out=qo[:], in0=qo[:], scalar1=SCALE, scalar2=None, op0=ALU.mult), reads=[qo], writes=[qo])
            k.dma("sp", keep[:], dkeep.h[:, 4 * i:4 * i + 4, :], reads=[dkeep], writes=[keep])
            k.dma("sp", addt[:], dadd.h[:, 4 * i:4 * i + 4, :], reads=[dadd], writes=[addt])
            for hh in range(4):
                h = hh
                own = (h == 3)
                L = self.psL[hh % 2]; O = self.psO[0]
                for nt in range(2):
                    nk = NKC[nt]
                    S = self.psS[self.iS % 3]; self.iS += 1
                    P = pc[h][nt]

                    def mm():
                        nc.tensor.matmul(S[0:nk, :], lhsT=kcT[:, nt * 128:nt * 128 + nk], rhs=(qown[:, q0:q0 + 512] if h == 3 else qo[:, h, :]), start=True, stop=False)
                        return nc.tensor.matmul(S[0:nk, :], lhsT=self.identB[:, 0:nk], rhs=bci[:, nt, h, :], start=False, stop=True)
                    k.op("pe", mm, reads=[kcT, qown, qo, self.identB, bci], writes=[S])
                    if nk < 128:
                        k.op("dve", lambda: nc.vector.memset(P[:], 0.0), writes=[P])
                    k.op("act", lambda: nc.scalar.activation(out=P[0:nk, :], in_=S[0:nk, :], func=AF.Exp), reads=[S], writes=[P])
                    k.op("pe", lambda: nc.tensor.matmul(L[:], lhsT=self.onesB[0:nk, :], rhs=P[0:nk, :], start=(nt == 0), stop=(nt == 1)),
                         reads=[self.onesB, P], writes=[L])
                    if own:
                        k.op("pe", lambda: nc.tensor.matmul(O[:], lhsT=vc[0:nk, nt, :], rhs=P[0:nk, :], start=(nt == 0), stop=(nt == 1)),
                             reads=[vc, P], writes=[O])
                k.op("dve", lambda: nc.vector.tensor_scalar(out=Rh[h][:], in0=L[:], scalar1=1e-30, scalar2=None, op0=ALU.max),
                     reads=[L], writes=[Rh[h]])
                k.op("dve", lambda: nc.vector.reciprocal(out=Rh[h][:], in_=Rh[h][:]), reads=[Rh[h]], writes=[Rh[h]])
                if own:
                    k.op("dve", lambda: nc.vector.tensor_tensor(out=Wt[:], in0=Rh[h][:], in1=gbi[:, 0, :], op=ALU.mult), reads=[Rh[h], gbi], writes=[Wt])
                    k.op("dve", lambda: nc.vector.tensor_tensor(out=ac[:], in0=O[:], in1=Wt[:], op=ALU.mult), reads=[O, Wt], writes=[ac])
                for nt in range(2):
                    k.op("dve", lambda: nc.vector.tensor_tensor(out=pc[h][nt][:], in0=pc[h][nt][:], in1=Rh[h][:], op=ALU.mult),
                         reads=[pc[h][nt], Rh[h]], writes=[pc[h][nt]])
            px = self.psX
            for jq in range(4):
                def mmi():
                    ins = None
                    n = 0
                    for h in range(4):
                        for nt in range(2):
                            ins = nc.tensor.matmul(px[:, jq * 64:(jq + 1) * 64], lhsT=pc[h][nt][:, jq * 128:(jq + 1) * 128], rhs=ov[:, nt, :],
                                                   start=(n == 0), stop=(n == 7))
                            n += 1
                    return ins
                k.op("pe", mmi, reads=[ov] + [pc[h][nt] for h in range(4) for nt in range(2)], writes=[(px, jq)])
            k.op("dve", lambda: nc.vector.tensor_tensor(out=impt[:].rearrange("p a b -> p (a b)"), in0=px[:, 0:256],
                                                        in1=keep[:].rearrange("p a b -> p (a b)"), op=ALU.mult),
                 reads=[px, keep], writes=[impt])
            k.op("dve", lambda: nc.vector.tensor_tensor(out=impt[:], in0=impt[:], in1=addt[:], op=ALU.add),
                 reads=[impt, addt], writes=[impt])
            pt = self.psL[0]
            for jq in range(4):
                k.op("dve", lambda: nc.vector.max(out=m8[:], in_=impt[:, jq, :]), reads=[impt], writes=[m8])
                k.op("dve", lambda: nc.vector.match_replace(out=wrk[:], in_to_replace=m8[:], in_values=impt[:, jq, :], imm_value=-1e30),
                     reads=[m8, impt], writes=[wrk])
                k.op("dve", lambda: nc.vector.max(out=m8[:], in_=wrk[:]), reads=[wrk], writes=[m8])
                k.op("dve", lambda: nc.vector.tensor_reduce(out=thr[:], in_=m8[:], axis=AX.X, op=ALU.min), reads=[m8], writes=[thr])
                k.op("dve", lambda: nc.vector.tensor_scalar(out=selb[:], in0=impt[:, jq, :], scalar1=thr[:, 0:1], scalar2=NEG,
                                                            op0=ALU.is_lt, op1=ALU.mult), reads=[impt, thr], writes=[selb])
                k.op("pe", lambda: nc.tensor.matmul(pt[0:64, jq * 128:(jq + 1) * 128], lhsT=selb[:], rhs=self.identF[:], start=True, stop=True),
                     reads=[selb, self.identF], writes=[(pt, jq)])
            k.op("dve", lambda: nc.vector.tensor_copy(selT[:], pt[0:64, :]), reads=[pt], writes=[selT])
            O = self.psO[1]; L = self.psL[1]
            nkt = 4 * i + 4
            for kt in range(nkt):
                idx = min(4 * i - kt + 3, 11)
                extra = [(lambda S: S[:], self.identB[:], tbs[:, idx, :]),
                         (lambda S: S[:], Eb[:, kt * 128:(kt + 1) * 128], selT[:])]
                self._xr = [self.identB, tbs, Eb, selT]
                self.attn_unit(ksb, kt, qown, q0, extra, None, [], vsb, vsb[:, kt * 128:(kt + 1) * 128], O, L, kt == 0, kt == nkt - 1)
            self.nsa_fin(O, L, gbi, 1, ac, Wt)
            O = self.psO[0]; L = self.psL[0]
            kts = list(range(max(0, 4 * i - 4), 4 * i + 4))
            for n, kt in enumerate(kts):
                idx = 4 * i - kt + 3
                extra = [(lambda S: S[:], self.identB[:], tbw[:, idx, :])]
                self._xr = [self.identB, tbw]
                self.attn_unit(kwb, kt, qown, q0, extra, None, [], vwb, vwb[:, kt * 128:(kt + 1) * 128], O, L, n == 0, n == len(kts) - 1)
            self.nsa_fin(O, L, gbi, 2, ac, Wt)
            k.dma("sp", yout.h[:, q0:q0 + 512], ac[:], reads=[ac], writes=[(yout, i)])

    def nsa_fin(self, O, L, gbi, gi, ac, Wt):
        k, nc = self.k, self.nc
        t2 = self.tmpA[0]
        k.op("dve", lambda: nc.vector.reciprocal(out=Wt[:], in_=L[:]), reads=[L], writes=[Wt])
        k.op("dve", lambda: nc.vector.tensor_tensor(out=Wt[:], in0=Wt[:], in1=gbi[:, gi, :], op=ALU.mult), reads=[Wt, gbi], writes=[Wt])
        k.op("dve", lambda: nc.vector.tensor_tensor(out=t2[:], in0=O[:], in1=Wt[:], op=ALU.mult), reads=[O, Wt], writes=[t2])
        k.op("dve", lambda: nc.vector.tensor_tensor(out=ac[:], in0=ac[:], in1=t2[:], op=ALU.add), reads=[ac, t2], writes=[ac])


    def gdn(self, yout):
        k, nc = self.k, self.nc
        L_ = "%d" % self.l
        dcw = k.din("gdn_cw" + L_, [128, 3, 4])
        dal = k.din("gdn_alog" + L_, [128, 1]); ddt = k.din("gdn_dtb" + L_, [128, 1]); dng = k.din("gdn_ng" + L_, [128, 1])
        dct = k.din("c_ct", [128, 128]); dsc = k.din("c_sc", [128, 128]); dh0 = k.din("c_h0", [128, 128]); dh1 = k.din("c_h1", [128, 128])
        dmst = k.din("c_mst", [128, 128]); dmit = k.din("c_mit", [128, 128]); dmsn = k.din("c_msn", [128, 128]); dcm = k.din("c_cm", [128, 2])
        B = self.big
        raw, W_, tA, tB = B[0], B[1], B[2], B[3]
        qs = ks = vs = oT = W_
        arow = k.sb([1, T_], F32, "g_arow")
        qnb, knb, vsb = self.bigb[0], self.bigb[1], self.bigb[2]
        cw = k.sb([128, 3, 4], F32, "g_cw"); self.ld(cw, dcw)
        cst = {}
        for nm, dd in (("ct", dct), ("sc", dsc), ("h0", dh0), ("h1", dh1), ("mst", dmst), ("mit", dmit), ("msn", dmsn)):
            cst[nm] = k.sb([128, 128], F32, "g_" + nm); self.ld(cst[nm], dd)
        cm = k.sb([128, 2], F32, "g_cm"); self.ld(cm, dcm)
        al = k.sb([128, 1], F32, "g_al"); dtb = k.sb([128, 1], F32, "g_dtb"); ng = k.sb([128, 1], F32, "g_ng")
        self.ld(al, dal); self.ld(dtb, ddt); self.ld(ng, dng)
        epsc = k.sb([128, 1], F32, "g_eps")
        k.op("dve", lambda: nc.vector.memset(epsc[:], EPS), writes=[epsc])
        for wi, (src, dst) in enumerate((("gdn_q", qs), ("gdn_k", ks), ("gdn_v", vs))):
            self.zfm(raw, raw.h, _OFF[src], 0, stage=tA)
            k.op("dve", lambda: nc.vector.tensor_scalar(out=dst[:], in0=raw[:], scalar1=cw[:, wi, 3:4], scalar2=None, op0=ALU.mult),
                 reads=[raw, cw], writes=[dst])
            for i in range(3):
                sh = 3 - i
                k.op("dve", lambda: nc.vector.scalar_tensor_tensor(out=dst[:, sh:], in0=raw[:, 0:T_ - sh], scalar=cw[:, wi, i:i + 1],
                                                                   in1=dst[:, sh:], op0=ALU.mult, op1=ALU.add), reads=[raw, cw, dst], writes=[dst])
            k.op("act", lambda: nc.scalar.activation(out=dst[:], in_=dst[:], func=AF.Silu), reads=[dst], writes=[dst])
            if wi == 2:
                k.op("act", lambda: nc.scalar.copy(out=vsb[:], in_=vs[:]), reads=[vs], writes=[vsb])
                continue
            src, dstb, sc = ((qs, qnb, SCALE), (ks, knb, 1.0))[wi]
            if True:
                k.op("dve", lambda: nc.vector.tensor_tensor(out=tA[:], in0=src[:], in1=src[:], op=ALU.mult), reads=[src], writes=[tA])
                for tb in range(8):
                    ts = slice(tb * 512, (tb + 1) * 512)
                    ps = self.psS[tb % 3]
                    k.op("pe", lambda: nc.tensor.matmul(ps[:], lhsT=self.onesF[:], rhs=tA[:, ts], start=True, stop=True), reads=[self.onesF, tA], writes=[ps])
                    k.op("act", lambda: nc.scalar.activation(out=tB[:, ts], in_=ps[:], func=AF.Ln, bias=epsc[:, 0:1]), reads=[ps, epsc], writes=[(tB, tb)])
                    k.op("act", lambda: nc.scalar.activation(out=tB[:, ts], in_=tB[:, ts], func=AF.Exp, scale=-0.5), reads=[(tB, tb)], writes=[(tB, tb)])
                k.op("dve", lambda: nc.vector.scalar_tensor_tensor(out=dstb[:], in0=src[:], scalar=sc, in1=tB[:], op0=ALU.mult, op1=ALU.mult),
                     reads=[src, tB], writes=[dstb])
        def col(nm):
            return k.sb([128, NT], F32, "g_c_" + nm)
        g = col("g"); beta = col("beta"); gc = col("gc"); ngc = col("ngc"); gl = col("gl"); wcol = col("w")
        skbg = col("skbg"); skd = [col("skd0"), col("skd1")]; egl = [col("egl0"), col("egl1")]; tmpc = col("tmp")
        self.zfm(arow, arow.h, _OFF["gdn_a"], 0, n=1, mul=1, stage=tB)
        self.row2col(arow, g)
        self.zfm(arow, arow.h, _OFF["gdn_b"], 0, n=1, mul=1, stage=tB)
        self.row2col(arow, beta)
        k.op("act", lambda: nc.scalar.activation(out=g[:], in_=g[:], func=AF.Exp, bias=dtb[:, 0:1]), reads=[g, dtb], writes=[g])
        k.op("act", lambda: nc.scalar.activation(out=g[:], in_=g[:], func=AF.Ln, bias=self.onesF[:, 0:1]), reads=[g, self.onesF], writes=[g])
        k.op("act", lambda: nc.scalar.activation(out=al[:], in_=al[:], func=AF.Exp), reads=[al], writes=[al])
        k.op("dve", lambda: nc.vector.tensor_scalar(out=g[:], in0=g[:], scalar1=al[:, 0:1], scalar2=-1.0, op0=ALU.mult, op1=ALU.mult),
             reads=[g, al], writes=[g])
        k.op("act", lambda: nc.scalar.activation(out=beta[:], in_=beta[:], func=AF.Sigmoid), reads=[beta], writes=[beta])
        px = self.psX

        def colmm(lhs, dst, func=None):
            k.op("pe", lambda: nc.tensor.matmul(px[:, 0:NT], lhsT=lhs[:], rhs=g[:], start=True, stop=True), reads=[lhs, g], writes=[px])
            if func is None:
                k.op("dve", lambda: nc.vector.tensor_copy(dst[:], px[:, 0:NT]), reads=[px], writes=[dst])
            else:
                k.op("act", lambda: nc.scalar.activation(out=dst[:], in_=px[:, 0:NT], func=func), reads=[px], writes=[dst])
        colmm(cst["ct"], gc)
        colmm(cst["sc"], gl)
        colmm(cst["h0"], egl[0], AF.Exp)
        colmm(cst["h1"], egl[1], AF.Exp)
        k.op("dve", lambda: nc.vector.tensor_scalar(out=ngc[:], in0=gc[:], scalar1=-1.0, scalar2=None, op0=ALU.mult), reads=[gc], writes=[ngc])
        k.op("act", lambda: nc.scalar.activation(out=wcol[:], in_=beta[:], func=AF.Ln), reads=[beta], writes=[wcol])
        k.op("dve", lambda: nc.vector.tensor_tensor(out=wcol[:], in0=wcol[:], in1=gc[:], op=ALU.add), reads=[wcol, gc], writes=[wcol])
        k.op("act", lambda: nc.scalar.activation(out=skbg[:], in_=gc[:], func=AF.Exp), reads=[gc], writes=[skbg])
        k.op("dve", lambda: nc.vector.tensor_tensor(out=skbg[:], in0=skbg[:], in1=beta[:], op=ALU.mult), reads=[skbg, beta], writes=[skbg])
        k.op("dve", lambda: nc.vector.tensor_tensor(out=tmpc[:], in0=gl[:], in1=gc[:], op=ALU.subtract), reads=[gl, gc], writes=[tmpc])
        k.op("act", lambda: nc.scalar.activation(out=tmpc[:], in_=tmpc[:], func=AF.Exp), reads=[tmpc], writes=[tmpc])
        for c in range(2):
            k.op("dve", lambda: nc.vector.tensor_scalar(out=skd[c][:], in0=tmpc[:], scalar1=cm[:, c:c + 1], scalar2=None, op0=ALU.mult),
                 reads=[tmpc, cm], writes=[skd[c]])
        S = k.sb([128, 128], F32, "g_S"); Sb = k.sb([128, 128], BF16, "g_Sb")
        k.op("dve", lambda: nc.vector.memset(S[:], 0.0), writes=[S])
        k.op("dve", lambda: nc.vector.memset(Sb[:], 0.0), writes=[Sb])

        def t128(nm, dt=F32):
            return k.sb([128, 128], dt, "g_t_" + nm)
        kbg = t128("kbg", BF16); kd = [t128("kd0", BF16), t128("kd1", BF16)]; vb = t128("vb", BF16)
        dgw = t128("dgw"); dgg = t128("dgg"); dgn = t128("dgn")
        Gs = t128("G"); Y = t128("Y"); X = t128("X"); Pm = t128("P"); Z = t128("Z"); ZT = t128("ZT"); Z2 = t128("Z2"); ZT2 = t128("ZT2")
        E1 = t128("E1"); qkT = t128("qkT", BF16); qgT = t128("qgT", BF16); PTb = t128("PTb", BF16); nWT = t128("nWT", BF16)
        vnb = t128("vnb", BF16)
        pool6 = [self.psS[0], self.psS[1], self.psS[2], self.psO[1], self.psL[0], self.psL[1]]
        ctr = [0]

        def pp():
            ctr[0] += 1
            return pool6[ctr[0] % 6]
        pOg = self.psO[0]
        for t in range(NT):
            cs = slice(t * 128, (t + 1) * 128)
            tc_ = slice(t, t + 1)
            pa = pp()
            k.op("pe", lambda: nc.tensor.matmul(pa[:, 0:128], lhsT=knb[:, cs], rhs=self.identB[:], start=True, stop=True), reads=[knb, self.identB], writes=[(pa, 0)])
            k.op("pe", lambda: nc.tensor.matmul(pa[:, 128:256], lhsT=vsb[:, cs], rhs=self.identB[:], start=True, stop=True), reads=[vsb, self.identB], writes=[(pa, 1)])
            k.op("dve", lambda: nc.vector.tensor_scalar(out=kbg[:], in0=pa[:, 0:128], scalar1=skbg[:, tc_], scalar2=None, op0=ALU.mult), reads=[(pa, 0), skbg], writes=[kbg])
            for c in range(2):
                k.op("dve", lambda: nc.vector.tensor_scalar(out=kd[c][:], in0=pa[:, 0:128], scalar1=skd[c][:, tc_], scalar2=None, op0=ALU.mult),
                     reads=[(pa, 0), skd[c]], writes=[kd[c]])
            k.op("dve", lambda: nc.vector.tensor_scalar(out=vb[:], in0=pa[:, 128:256], scalar1=beta[:, tc_], scalar2=None, op0=ALU.mult), reads=[(pa, 1), beta], writes=[vb])
            k.op("dve", lambda: nc.vector.tensor_scalar(out=dgw[:], in0=self.identF[:], scalar1=wcol[:, tc_], scalar2=None, op0=ALU.mult), reads=[self.identF, wcol], writes=[dgw])
            k.op("dve", lambda: nc.vector.tensor_scalar(out=dgg[:], in0=self.identF[:], scalar1=gc[:, tc_], scalar2=None, op0=ALU.mult), reads=[self.identF, gc], writes=[dgg])
            k.op("dve", lambda: nc.vector.tensor_scalar(out=dgn[:], in0=self.identF[:], scalar1=ngc[:, tc_], scalar2=None, op0=ALU.mult), reads=[self.identF, ngc], writes=[dgn])
            pg = pp()
            k.op("pe", lambda: nc.tensor.matmul(pg[:, 0:128], lhsT=knb[:, cs], rhs=knb[:, cs], start=True, stop=True), reads=[knb], writes=[(pg, 0)])
            k.op("pe", lambda: nc.tensor.matmul(pg[:, 128:256], lhsT=knb[:, cs], rhs=qnb[:, cs], start=True, stop=True), reads=[knb, qnb], writes=[(pg, 1)])
            k.op("dve", lambda: nc.vector.tensor_copy(Gs[:], pg[:, 0:128]), reads=[(pg, 0)], writes=[Gs])

            def expmat(diag, mask, bias_col, bias_t, dst_fn):
                pe_ = pp()

                def mm():
                    ins0 = nc.tensor.matmul(pe_[:, 0:128], lhsT=self.onesF[:], rhs=diag[:], start=True, stop=(mask is None))
                    if mask is None:
                        return ins0
                    return nc.tensor.matmul(pe_[:, 0:128], lhsT=self.identF[:], rhs=mask[:], start=False, stop=True)
                k.op("pe", mm, reads=[self.onesF, diag, self.identF] + ([mask] if mask is not None else []), writes=[pe_])
                if bias_col is None:
                    k.op("act", lambda: nc.scalar.activation(out=E1[:], in_=pe_[:, 0:128], func=AF.Exp), reads=[pe_], writes=[E1])
                else:
                    k.op("act", lambda: nc.scalar.activation(out=E1[:], in_=pe_[:, 0:128], func=AF.Exp, bias=bias_col[:, tc_]),
                         reads=[pe_, bias_t], writes=[E1])
                dst_fn()
            expmat(dgw, cst["mst"], ngc, ngc, lambda: k.op("dve", lambda: nc.vector.tensor_tensor(out=Y[:], in0=Gs[:], in1=E1[:], op=ALU.mult), reads=[Gs, E1], writes=[Y]))
            expmat(dgn, cst["msn"], wcol, wcol, lambda: k.op("dve", lambda: nc.vector.tensor_tensor(out=X[:], in0=Gs[:], in1=E1[:], op=ALU.mult), reads=[Gs, E1], writes=[X]))
            expmat(dgg, cst["mit"], ngc, ngc, lambda: k.op("dve", lambda: nc.vector.tensor_tensor(out=qkT[:], in0=pg[:, 128:256], in1=E1[:], op=ALU.mult), reads=[(pg, 1), E1], writes=[qkT]))
            expmat(dgg, None, None, None, lambda: k.op("dve", lambda: nc.vector.tensor_tensor(out=qgT[:], in0=qnb[:, cs], in1=E1[:], op=ALU.mult), reads=[qnb, E1], writes=[qgT]))
            k.op("dve", lambda: nc.vector.tensor_tensor(out=Pm[:], in0=self.identF[:], in1=Y[:], op=ALU.subtract), reads=[self.identF, Y], writes=[Pm])
            p1 = pp(); p2 = pp()
            k.op("pe", lambda: nc.tensor.matmul(p1[:, 0:128], lhsT=X[:], rhs=Y[:], start=True, stop=True), reads=[X, Y], writes=[p1])
            k.op("pe", lambda: nc.tensor.matmul(p2[:, 0:128], lhsT=Y[:], rhs=X[:], start=True, stop=True), reads=[X, Y], writes=[p2])
            zc, ztc, zn, ztn = Z, ZT, Z2, ZT2
            k.op("dve", lambda: nc.vector.tensor_copy(zc[:], p1[:, 0:128]), reads=[p1], writes=[zc])
            k.op("act", lambda: nc.scalar.copy(out=ztc[:], in_=p2[:, 0:128]), reads=[p2], writes=[ztc])
            for it in range(5):
                p3 = pp()
                k.op("pe", lambda: nc.tensor.matmul(p3[:, 0:128], lhsT=ztc[:], rhs=Pm[:], start=True, stop=True), reads=[ztc, Pm], writes=[p3])
                if it < 4:
                    p1 = pp(); p2 = pp()
                    k.op("pe", lambda: nc.tensor.matmul(p1[:, 0:128], lhsT=ztc[:], rhs=zc[:], start=True, stop=True), reads=[ztc, zc], writes=[p1])
                    k.op("pe", lambda: nc.tensor.matmul(p2[:, 0:128], lhsT=zc[:], rhs=ztc[:], start=True, stop=True), reads=[ztc, zc], writes=[p2])
                k.op("dve", lambda: nc.vector.tensor_tensor(out=Pm[:], in0=Pm[:], in1=p3[:, 0:128], op=ALU.add), reads=[Pm, p3], writes=[Pm])
                if it < 4:
                    k.op("dve", lambda: nc.vector.tensor_copy(zn[:], p1[:, 0:128]), reads=[p1], writes=[zn])
                    k.op("act", lambda: nc.scalar.copy(out=ztn[:], in_=p2[:, 0:128]), reads=[p2], writes=[ztn])
                    zc, ztc, zn, ztn = zn, ztn, zc, ztc
            k.op("act", lambda: nc.scalar.copy(out=PTb[:], in_=Pm[:]), reads=[Pm], writes=[PTb])
            pw = pp()
            k.op("pe", lambda: nc.tensor.matmul(pw[:, 0:128], lhsT=kbg[:], rhs=PTb[:], start=True, stop=True), reads=[kbg, PTb], writes=[pw])
            k.op("dve", lambda: nc.vector.tensor_scalar(out=nWT[:], in0=pw[:, 0:128], scalar1=-1.0, scalar2=None, op0=ALU.mult), reads=[pw], writes=[nWT])
            for c in range(2):
                ccs = slice(64 * c, 64 * c + 64)
                pv = pp()

                def mmv():
                    nc.tensor.matmul(pv[:, 0:128], lhsT=PTb[:], rhs=vb[:], start=True, stop=False)
                    return nc.tensor.matmul(pv[:, 0:128], lhsT=nWT[:], rhs=Sb[:], start=False, stop=True)
                k.op("pe", mmv, reads=[PTb, vb, nWT, Sb], writes=[pv])
                k.op("act", lambda: nc.scalar.copy(out=vnb[:], in_=pv[:, 0:128]), reads=[pv], writes=[vnb])

                def mmo():
                    nc.tensor.matmul(pOg[:, ccs], lhsT=Sb[:], rhs=qgT[:, ccs], start=True, stop=False)
                    return nc.tensor.matmul(pOg[:, ccs], lhsT=vnb[:], rhs=qkT[:, ccs], start=False, stop=True)
                k.op("pe", mmo, reads=[Sb, qgT, vnb, qkT], writes=[(pOg, c)])
                pu = pp()
                k.op("pe", lambda: nc.tensor.matmul(pu[:, 0:128], lhsT=kd[c][:], rhs=vnb[:], start=True, stop=True), reads=[kd[c], vnb], writes=[pu])
                k.op("dve", lambda: nc.vector.scalar_tensor_tensor(out=S[:], in0=S[:], scalar=egl[c][:, tc_], in1=pu[:, 0:128], op0=ALU.mult, op1=ALU.add),
                     reads=[S, egl[c], pu], writes=[S])
                k.op("act", lambda: nc.scalar.copy(out=Sb[:], in_=S[:]), reads=[S], writes=[Sb])
            k.op("dve", lambda: nc.vector.tensor_copy(oT[:, cs], pOg[:, 0:128]), reads=[pOg], writes=[(oT, t)])
        self.zfm(raw, raw.h, _OFF["gdn_z"], 0, stage=tA)
        k.op("act", lambda: nc.scalar.activation(out=raw[:], in_=raw[:], func=AF.Silu), reads=[raw], writes=[raw])
        k.op("dve", lambda: nc.vector.tensor_tensor(out=tA[:], in0=oT[:], in1=oT[:], op=ALU.mult), reads=[oT], writes=[tA])
        k.op("dve", lambda: nc.vector.tensor_scalar(out=tA[:], in0=tA[:], scalar1=1.0 / 128, scalar2=None, op0=ALU.mult), reads=[tA], writes=[tA])
        for tb in range(8):
            ts = slice(tb * 512, (tb + 1) * 512)
            ps = self.psS[tb % 3]
            k.op("pe", lambda: nc.tensor.matmul(ps[:], lhsT=self.onesF[:], rhs=tA[:, ts], start=True, stop=True), reads=[self.onesF, tA], writes=[ps])
            k.op("act", lambda: nc.scalar.activation(out=tB[:, ts], in_=ps[:], func=AF.Ln, bias=epsc[:, 0:1]), reads=[ps, epsc], writes=[(tB, tb)])
            k.op("act", lambda: nc.scalar.activation(out=tB[:, ts], in_=tB[:, ts], func=AF.Exp, scale=-0.5), reads=[(tB, tb)], writes=[(tB, tb)])
        k.op("dve", lambda: nc.vector.scalar_tensor_tensor(out=oT[:], in0=oT[:], scalar=ng[:, 0:1], in1=tB[:], op0=ALU.mult, op1=ALU.mult),
             reads=[oT, ng, tB], writes=[oT])
        k.op("dve", lambda: nc.vector.tensor_tensor(out=oT[:], in0=oT[:], in1=raw[:], op=ALU.mult), reads=[oT, raw], writes=[oT])
        k.dma("sp", yout.h, oT[:], reads=[oT], writes=[yout])


class Gathered:
    def __init__(self, nc, name, nrows, ncols, CR, dt=F32):
        self.nrows, self.ncols, self.CR = nrows, ncols, CR
        self.chunks = []
        r = 0
        while r < nrows:
            cr = min(CR, nrows - r)
            self.chunks.append((r, cr, T(nc.dram_tensor("%s_c%d" % (name, len(self.chunks)), [4 * cr, ncols], dt).ap(), "%s_c%d" % (name, len(self.chunks)))))
            r += cr

    def pieces(self, s_, r0, n):
        out = []
        r = r0
        while r < r0 + n:
            ci = r // self.CR
            c0, cr, t = self.chunks[ci]
            cnt = min(r0 + n, c0 + cr) - r
            out.append((t, t.h[s_ * cr + (r - c0):s_ * cr + (r - c0) + cnt, :], r - r0, cnt))
            r += cnt
        return out


def exchange(k, src, dst, sem):
    nc = k.nc
    k.barrier()
    sems = []
    for (c0, cr, t) in dst.chunks:
        cs = sem.enter_context(nc.semaphore("cc_%s" % t.name))
        sems.append(cs)
        nc.gpsimd.collective_compute("AllGather", ALU.bypass, replica_groups=[[0, 1, 2, 3], [4, 5, 6, 7]],
                                     ins=[src.h[c0:c0 + cr, :].opt()], outs=[t.h.opt()]).then_inc(cs)
    for cs in sems:
        for e in k.eng.values():
            e.wait_ge(cs, 1)


NBIG = {"fox": (2, 4), "lru": (6, 1), "gdn": (4, 3), "nsa": (0, 0)}


def build_fused(dbg=False):
    nc = bass.Bass("TRN2", target_bir_lowering=False)
    with ExitStack() as es:
        k = K(nc, es)
        x = k.din("x", [TOK, D])
        out = k.dout("out", [TOK, D])
        h = k.sb([128, NCH, TOK], F32, "h")
        vals = None
        zsh = [T(nc.dram_tensor("zsh%d" % l, [DIN, TOK], BF16).ap(), "zsh%d" % l) for l in range(2)]
        zall = [Gathered(nc, "zall%d" % l, DIN, TOK, 512, BF16) for l in range(2)]
        ysh = [T(nc.dram_tensor("ysh%d" % l, [4 * 128, T_], F32).ap(), "ysh%d" % l) for l in range(2)]
        yall = [Gathered(nc, "yall%d" % l, 4 * 128, T_, 64) for l in range(2)]
        csem = [es, es, es, es]
        with k.scope():
            load_x(k, h, x)
        for l in range(2):
            with k.scope():
                dense_A(k, h, l, zsh[l], zall[l])
            for gi, nm in enumerate(("fox", "gdn", "lru", "nsa")):
                with k.scope():
                    m = Mix(k, l, zall[l], vals, NBIG[nm][0], NBIG[nm][1])
                    yout = T(ysh[l].h[gi * 128:(gi + 1) * 128, :], "y_%s%d" % (nm, l))
                    getattr(m, nm)(yout)
                    for (c0, cr, ct) in yall[l].chunks[2 * gi:2 * gi + 2]:
                        k.allgather(ysh[l].h[c0:c0 + cr, :], ct, [yout])
            with k.scope():
                dense_C(k, h, l, yall[l], None)
            if dbg and l == 0:
                dh = k.dout("dbg_h", [D, TOK])
                v = dh.h.rearrange("(c p) t -> p c t", p=128)
                for c in range(0, NCH, 4):
                    k.dma("sp", v[:, c:c + 4, :], h[:, c:c + 4, :], reads=[h], writes=[(dh, c)])
        with k.scope():
            final_out(k, h, out)
        k.wait_all("sp")
    return nc


def tm_tiles(a):
    t, d = a.shape
    return np.ascontiguousarray(a.reshape(t // 128, 128, d).transpose(1, 0, 2))


def col_tiles(v):
    return np.ascontiguousarray(v.reshape(-1, 128).T)


_OFF = {}
_o = 0
for _n, _w in (("fox_q", 512), ("fox_k", 512), ("fox_v", 512), ("fox_f", 4), ("gdn_q", 512), ("gdn_k", 512), ("gdn_v", 512),
               ("gdn_a", 4), ("gdn_b", 4), ("gdn_z", 512), ("lru_x", 512), ("lru_gate", 512), ("nsa_q", 512), ("nsa_kc", 128),
               ("nsa_vc", 128), ("nsa_ks", 128), ("nsa_vs", 128), ("nsa_kw", 128), ("nsa_vw", 128), ("nsa_g", 12)):
    _OFF[_n] = _o
    _o += _w


def consts_B():
    c = {}
    c["c_ident"] = np.eye(128, dtype=np.float32)
    p = np.arange(128)
    c["c_ut"] = (p[:, None] <= p[None, :]).astype(np.float32)
    q = np.arange(32)
    c["c_su"] = (q[:, None] < q[None, :]).astype(np.float32)
    col = np.arange(512)
    mb = np.zeros((128, 4, 512), np.float32)
    for m in range(4):
        mb[:, m, :] = np.where(p[:, None] + 128 * m <= col[None, :], 0.0, NEG)
    c["c_mbfox"] = mb
    ch = p // 64
    same = ch[:, None] == ch[None, :]
    c["c_ct"] = (same & (p[:, None] <= p[None, :])).astype(np.float32)
    c["c_sc"] = same.astype(np.float32)
    c["c_h0"] = np.broadcast_to((p < 64)[:, None], (128, 128)).astype(np.float32).copy()
    c["c_h1"] = np.broadcast_to((p >= 64)[:, None], (128, 128)).astype(np.float32).copy()
    c["c_mst"] = np.where(same & (p[None, :] > p[:, None]), 0.0, NEG).astype(np.float32)
    c["c_mit"] = np.where(same & (p[None, :] >= p[:, None]), 0.0, NEG).astype(np.float32)
    c["c_msn"] = np.where(same & (p[:, None] > p[None, :]), 0.0, NEG).astype(np.float32)
    c["c_cm"] = np.stack([(p < 64), (p >= 64)], axis=1).astype(np.float32)
    return c


def prep_mix(inp, l, j):
    L_ = "%d" % l
    m = {}
    hs = slice(j * 128, (j + 1) * 128)
    m["lru_cw" + L_] = np.ascontiguousarray(inp["lru_conv_w"][l][:, hs].T)
    m["lru_cb" + L_] = np.ascontiguousarray(inp["lru_conv_b"][l][hs].reshape(128, 1))
    for nm, src in (("lru_wa", "lru_w_a"), ("lru_wx", "lru_w_x")):
        bd = np.zeros((128, 128), np.float32)
        bd[0:64, 0:64] = inp[src][l][2 * j]
        bd[64:128, 64:128] = inp[src][l][2 * j + 1]
        m[nm + L_] = bd
    m["lru_ba" + L_] = np.ascontiguousarray(inp["lru_b_a"][l][hs].reshape(128, 1))
    m["lru_bx" + L_] = np.ascontiguousarray(inp["lru_b_x"][l][hs].reshape(128, 1))
    m["lru_lam" + L_] = np.ascontiguousarray(inp["lru_lambda"][l][hs].reshape(128, 1))
    cwf = inp["gdn_conv_w"][l]
    m["gdn_cw" + L_] = np.ascontiguousarray(np.stack([cwf[:, g0 * 512:(g0 + 1) * 512][:, hs].T for g0 in range(3)], axis=1))
    m["gdn_alog" + L_] = np.full((128, 1), inp["gdn_a_log"][l][j], np.float32)
    m["gdn_dtb" + L_] = np.full((128, 1), inp["gdn_dt_bias"][l][j], np.float32)
    m["gdn_ng" + L_] = np.ascontiguousarray(inp["gdn_norm_g"][l].reshape(128, 1))
    for nm, src in (("nsa_w1k", "nsa_w1_k"), ("nsa_w1v", "nsa_w1_v")):
        m[nm + L_] = np.ascontiguousarray(inp[src][l].reshape(32, 128, 128).transpose(1, 0, 2))
    m["nsa_w2k" + L_] = inp["nsa_w2_k"][l]; m["nsa_w2v" + L_] = inp["nsa_w2_v"][l]
    m["nsa_pek" + L_] = np.ascontiguousarray(inp["nsa_pe_k"][l].T); m["nsa_pev" + L_] = np.ascontiguousarray(inp["nsa_pe_v"][l].T)
    return m


def prep_shared(inp):
    m = dict(consts_B())
    for l in range(2):
        L_ = "%d" % l
        binp = np.zeros(NZC * 128, np.float32)
        binp[:DIN] = inp["b_in"][l]
        ong = np.ones((D,), np.float32)
        ong[0:512] = inp["out_norm_g"][l][0]
        ong[1024:1536] = inp["out_norm_g"][l][1]
        ong[1536:2048] = inp["out_norm_g"][l][2]
        m.update({"g1a" + L_: col16(inp["ffn1_norm_g"][l]), "g2a" + L_: col16(inp["mix_norm_g"][l]),
                  "f1wg" + L_: inp["ffn1_w_gate"][l], "f1wu" + L_: inp["ffn1_w_up"][l], "f1wd" + L_: inp["ffn1_w_down"][l],
                  "win" + L_: inp["w_in"][l], "bin" + L_: np.ascontiguousarray(binp.reshape(NZC, 128).T),
                  "ong" + L_: col16(ong), "wout" + L_: inp["w_out"][l], "g1c" + L_: col16(inp["ffn2_norm_g"][l]),
                  "f2wg" + L_: inp["ffn2_w_gate"][l], "f2wu" + L_: inp["ffn2_w_up"][l], "f2wd" + L_: inp["ffn2_w_down"][l]})
    m["gf"] = col16(inp["final_norm_g"])
    nsc = nsa_static()
    m["nsa_keep"] = nsc["keep"]; m["nsa_add"] = nsc["add"]; m["nsa_E"] = nsc["E"]; m["nsa_ov"] = nsc["ov"]
    return m


def prep_core(inp, core):
    j = core % 4
    m = {}
    for l in range(2):
        m.update(prep_mix(inp, l, j))
    order = [(j + 1 + hh) % 4 for hh in range(4)]
    rb = np.asarray(inp["rel_bias"], np.float32)
    nsc = nsa_static()
    bcs = np.where(nsc["bc_mask"][:, :, None, :], rb[nsc["bc_idx"]][..., order].transpose(0, 1, 3, 2), NEG)
    m["nsa_bc"] = np.ascontiguousarray(bcs.astype(np.float32))
    m["nsa_tbs"] = np.where(nsc["tbs_mask"], rb[nsc["tbs_idx"], j], NEG).astype(np.float32)
    m["nsa_tbw"] = np.where(nsc["tbw_mask"], rb[nsc["tbw_idx"], j], NEG).astype(np.float32)
    mk = np.zeros((128, 16), np.float32)
    for hh in range(4):
        mk[:, 4 * hh + (j + hh) % 4] = 1.0
    m["msk"] = mk
    return m


_PROG = {}


def run_fused(inputs, dbg=False):
    inp = {k_: np.asarray(v, np.float32) for k_, v in inputs.items()}
    x = inp["x"].reshape(8 * TOK, D)
    key = "dbg" if dbg else "main"
    if key not in _PROG:
        _PROG[key] = build_fused(dbg)
    nc = _PROG[key]
    shared = prep_shared(inp)
    maps = []
    for c in range(8):
        m = dict(shared)
        m.update(prep_core(inp, c))
        m["x"] = np.ascontiguousarray(x[c * TOK:(c + 1) * TOK])
        maps.append(m)
    res = run_bass_kernel_spmd(nc, maps, core_ids=list(range(8)))
    return res.results


def kernel(**inputs):
    res = run_fused(inputs)
    out = np.concatenate([r["out"] for r in res], axis=0).reshape(2, T_, D)
    return np.ascontiguousarray(out.astype(np.float32))


_NSC = {}


def t5_bucket_static(dist):
    import math
    import jax
    import jax.numpy as jnp
    with jax.default_device(jax.devices("cpu")[0]):
        n = jnp.maximum(jnp.asarray(dist, jnp.int32), 0)
        nf = jnp.maximum(n, 1).astype(jnp.float32)
        large = 16 + (jnp.log(nf / 16) / math.log(1024 / 16) * (32 - 16)).astype(jnp.int32)
        large = jnp.minimum(large, 31)
        return np.asarray(jnp.where(n < 16, n, large))


def nsa_static():
    if _NSC:
        return _NSC
    p = np.arange(128)
    col = np.arange(512)
    q = np.arange(T_)
    n = (np.arange(2)[:, None] * 128 + p[None, :])
    d = q[None, None, :] - (16 * n[:, :, None] + 31)
    msk = (d >= 0) & (n[:, :, None] < 255)
    _NSC["bc_idx"] = t5_bucket_static(d).transpose(1, 0, 2)
    _NSC["bc_mask"] = msk.transpose(1, 0, 2)
    ms = np.arange(12) - 3
    d = 128 * ms[None, :, None] + col[None, None, :] - p[:, None, None]
    _NSC["tbs_idx"] = t5_bucket_static(d); _NSC["tbs_mask"] = d >= 0
    mw = np.arange(8) - 3
    d = 128 * mw[None, :, None] + col[None, None, :] - p[:, None, None]
    _NSC["tbw_idx"] = t5_bucket_static(d); _NSC["tbw_mask"] = (d >= 0) & (d < 512)
    qpos = np.arange(NT)[None, :, None] * 128 + p[:, None, None]
    cur = qpos // 64
    jj = np.arange(64)[None, None, :]
    forced = (jj == 0) | (jj == cur) | (jj == cur - 1)
    fut = jj > cur
    _NSC["keep"] = np.where(forced | fut, 0.0, 1.0).astype(np.float32)
    _NSC["add"] = np.where(fut, -1.0, np.where(forced, 1.0e6, 0.0)).astype(np.float32)
    _NSC["E"] = (np.arange(T_)[None, :] // 64 == np.arange(64)[:, None]).astype(np.float32)
    nn = np.arange(256)
    cst, cen = nn * 16, nn * 16 + 31
    sst, sen = np.arange(64) * 64, np.arange(64) * 64 + 63
    ovl = ((cst[:, None] <= sen[None, :]) & (cen[:, None] >= sst[None, :]) & (nn[:, None] < 255)).astype(np.float32)
    _NSC["ov"] = np.ascontiguousarray(ovl.reshape(2, 128, 64).transpose(1, 0, 2))
    return _NSC
```

```python
import numpy as np
import ml_dtypes
from contextlib import ExitStack
import concourse.bass as bass
import concourse.mybir as mybir
from concourse.bass_utils import run_bass_kernel_spmd

F32 = mybir.dt.float32
BF16 = mybir.dt.bfloat16
I32 = mybir.dt.int32
SP_POOL = [mybir.EngineType.SP, mybir.EngineType.Pool]
AF = mybir.ActivationFunctionType
ALU = mybir.AluOpType
AX = mybir.AxisListType

D = 2048
NCH = 16
DFF = 5632
NF = 44
TOK = 1024
TG = 512
DIN = 5912
NZC = 47
EPS = 1e-6
NEG = -30000.0


class St:
    __slots__ = ("w", "r")

    def __init__(self, w=None, r=None):
        self.w = w
        self.r = list(r) if r else []

    def copy(self):
        return St(self.w, self.r)


class T:
    def __init__(self, handle, name):
        self.h = handle
        self.name = name
        self.whole = St()
        self.cells = {}

    def __getitem__(self, idx):
        return self.h[idx]

    def states(self, key):
        if key is None:
            return [self.whole] + list(self.cells.values())
        if key not in self.cells:
            self.cells[key] = self.whole.copy()
        return [self.cells[key]]


class K:
    NSLOT = 6

    def __init__(self, nc, es):
        self.nc = nc
        self.es = es
        self.eng = {"pe": nc.tensor, "dve": nc.vector, "act": nc.scalar, "pool": nc.gpsimd, "sp": nc.sync}
        self.sem = {}
        self.cnt = {}
        for e in self.eng:
            self.sem[e] = es.enter_context(nc.semaphore("s_" + e))
            self.cnt[e] = 0
        self.known = {e: {} for e in self.eng}
        self.dsem = {}
        self.duse = {}
        self.dnext = {}
        for q in ("sp", "pool"):
            self.dnext[q] = 0
            for s in range(self.NSLOT):
                key = ("d", q, s)
                self.dsem[key] = es.enter_context(nc.semaphore("d_%s_%d" % (q, s)))
                self.duse[key] = 0
        self.ntile = 0
        self.dins = {}
        self.es_root = es
        self.ncc = 0

    def sb(self, shape, dt, name=None):
        self.ntile += 1
        name = "%s_%d" % (name or "t", self.ntile)
        h = self.es.enter_context(self.nc.sbuf_tensor(name, list(shape), dt))
        return T(h, name)

    def ps(self, shape, dt, name=None):
        self.ntile += 1
        name = "%s_%d" % (name or "p", self.ntile)
        h = self.es.enter_context(self.nc.psum_tensor(name, list(shape), dt))
        return T(h, name)

    def din(self, name, shape, dt=F32):
        if name not in self.dins:
            self.dins[name] = T(self.nc.dram_tensor(name, list(shape), dt, kind="ExternalInput").ap(), name)
        return self.dins[name]

    def barrier(self):
        for e in self.eng:
            self.wait_all(e)

    def scope(self):
        return _Scope(self)

    def dout(self, name, shape, dt=F32):
        return T(self.nc.dram_tensor(name, list(shape), dt, kind="ExternalOutput").ap(), name)

    def semh(self, key):
        return self.sem[key] if key in self.sem else self.dsem[key]

    def _deps(self, reads, writes):
        deps = set()
        for (t, key) in reads:
            for st in t.states(key):
                if st.w is not None:
                    deps.add(st.w)
        for (t, key) in writes:
            for st in t.states(key):
                if st.w is not None:
                    deps.add(st.w)
                deps.update(st.r)
        return deps

    def _wait(self, eng, deps):
        need = {}
        kn = self.known[eng]
        for (sk, val) in deps:
            if sk == "pe" and eng == "pe":
                continue
            if kn.get(sk, 0) < val and need.get(sk, 0) < val:
                need[sk] = val
        for sk, val in need.items():
            self.eng[eng].wait_ge(self.semh(sk), val)
            kn[sk] = val

    def _commit(self, ev, reads, writes):
        for (t, key) in reads:
            for st in t.states(key):
                st.r.append(ev)
        for (t, key) in writes:
            if key is None:
                t.cells.clear()
                t.whole.w = ev
                t.whole.r = []
            else:
                st = t.states(key)[0]
                st.w = ev
                st.r = []

    @staticmethod
    def _norm(lst):
        out = []
        for x in lst or []:
            out.append(x if isinstance(x, tuple) else (x, None))
        return out

    def op(self, eng, fn, reads=None, writes=None):
        reads = self._norm(reads)
        writes = self._norm(writes)
        self._wait(eng, self._deps(reads, writes))
        ins = fn()
        self.cnt[eng] += 1
        ins.then_inc(self.sem[eng], 1)
        ev = (eng, self.cnt[eng])
        self._commit(ev, reads, writes)
        return ev

    def dma(self, q, out_ap, in_ap, reads=None, writes=None, **kw):
        reads = self._norm(reads)
        writes = self._norm(writes)
        deps = self._deps(reads, writes)
        s = self.dnext[q]
        self.dnext[q] = (s + 1) % self.NSLOT
        key = ("d", q, s)
        if self.duse[key] > 0:
            deps.add((key, 16 * self.duse[key]))
        self._wait(q, deps)
        ins = self.eng[q].dma_start(out=out_ap, in_=in_ap, **kw)
        self.duse[key] += 1
        ins.then_inc(self.dsem[key], 16)
        ev = (key, 16 * self.duse[key])
        self._commit(ev, reads, writes)
        return ev

    def allgather(self, src_ap, dst_t, reads):
        reads = self._norm(reads)
        writes = [(dst_t, None)]
        self._wait("pool", self._deps(reads, writes))
        cs = self.es_root.enter_context(self.nc.semaphore("cc%d" % self.ncc))
        key = ("c", self.ncc)
        self.ncc += 1
        self.dsem[key] = cs
        self.nc.gpsimd.collective_compute("AllGather", ALU.bypass, replica_groups=[[0, 1, 2, 3], [4, 5, 6, 7]],
                                          ins=[src_ap.opt()], outs=[dst_t.h.opt()]).then_inc(cs)
        ev = (key, 1)
        self._commit(ev, reads, writes)
        return ev

    def wait_all(self, eng="sp"):
        kn = self.known[eng]
        for e in self.eng:
            if self.cnt[e] > kn.get(e, 0):
                self.eng[eng].wait_ge(self.sem[e], self.cnt[e])
                kn[e] = self.cnt[e]
        for key, n in self.duse.items():
            if 16 * n > kn.get(key, 0):
                self.eng[eng].wait_ge(self.dsem[key], 16 * n)
                kn[key] = 16 * n


class _Scope:
    def __init__(self, k):
        self.k = k

    def __enter__(self):
        self.old = self.k.es
        self.sub = ExitStack()
        self.sub.__enter__()
        self.k.es = self.sub
        return self

    def __exit__(self, *a):
        self.k.barrier()
        self.k.es = self.old
        return self.sub.__exit__(*a)


class Dense:
    def __init__(self, k, h):
        self.k = k
        nc = k.nc
        self.nc = nc
        self.h = h
        self.hn = k.sb([128, NCH, TOK], BF16, "hn")
        self.sq = [k.sb([128, TG], BF16, "sq%d" % i) for i in range(2)]
        self.rstd = k.sb([128, TG], F32, "rstd")
        self.onesm = k.sb([128, 128], BF16, "onesm")
        self.ones4 = k.sb([128, 128], BF16, "ones4")
        self.gcol = k.sb([128, NCH], F32, "gcol")
        self.wg = [k.sb([128, NCH, 256], BF16, "wg%d" % i) for i in range(2)]
        self.wu = [k.sb([128, NCH, 256], BF16, "wu%d" % i) for i in range(2)]
        self.wd = [k.sb([128, 2, D], BF16, "wd%d" % i) for i in range(2)]
        self.act = [k.sb([128, 2, TOK], BF16, "act%d" % i) for i in range(2)]
        self.sg = [k.sb([128, TG], F32, "sg%d" % i) for i in range(2)]
        self.psA = [k.ps([128, TG], F32, "psA%d" % i) for i in range(4)]
        self.psB = [k.ps([128, TG], F32, "psB%d" % i) for i in range(3)]
        self.psN = k.ps([128, TG], F32, "psN")
        self.ia = 0
        self.ib = 0
        self.epsc = k.sb([128, 1], F32, "epsc")
        k.op("dve", lambda: nc.vector.memset(self.epsc[:], EPS), writes=[self.epsc])
        k.op("dve", lambda: nc.vector.memset(self.onesm[:], 1.0 / D), writes=[self.onesm])
        k.op("dve", lambda: nc.vector.memset(self.ones4[:], 1.0 / 512), writes=[self.ones4])

    def rstd_from(self, ps):
        k, nc = self.k, self.nc
        k.op("act", lambda: nc.scalar.activation(out=self.rstd[:], in_=ps[:], func=AF.Ln, bias=self.epsc[:]),
             reads=[ps, self.epsc], writes=[self.rstd])
        k.op("act", lambda: nc.scalar.activation(out=self.rstd[:], in_=self.rstd[:], func=AF.Exp, scale=-0.5),
             reads=[self.rstd], writes=[self.rstd])

    def load_h(self, src):
        k = self.k
        v = src.h.rearrange("(c p) t -> p c t", p=128)
        for c in range(0, NCH, 4):
            k.dma("sp", self.h[:, c:c + 4, :], v[:, c:c + 4, :], reads=[src],
                  writes=[(self.h, (cc, tg)) for cc in range(c, c + 4) for tg in range(2)])

    def store_h(self, dst):
        k = self.k
        v = dst.h.rearrange("(c p) t -> p c t", p=128)
        for c in range(0, NCH, 4):
            k.dma("sp", v[:, c:c + 4, :], self.h[:, c:c + 4, :],
                  reads=[(self.h, (cc, tg)) for cc in range(c, c + 4) for tg in range(2)], writes=[(dst, c)])

    def rmsnorm(self, gsrc, out_t=None, out_f32=None):
        k, nc = self.k, self.nc
        k.dma("sp", self.gcol[:], gsrc.h, reads=[gsrc], writes=[self.gcol])
        for tg in range(2):
            ts = slice(tg * TG, (tg + 1) * TG)
            for c in range(NCH):
                sq = self.sq[c % 2]
                k.op("act", lambda sq=sq, c=c: nc.scalar.activation(out=sq[:], in_=self.h[:, c, ts], func=AF.Square),
                     reads=[(self.h, (c, tg))], writes=[sq])
                k.op("pe", lambda sq=sq, c=c: nc.tensor.matmul(self.psN[:], lhsT=self.onesm[:], rhs=sq[:],
                                                               start=(c == 0), stop=(c == NCH - 1)),
                     reads=[sq, self.onesm], writes=[self.psN])
            self.rstd_from(self.psN)
            for c in range(NCH):
                if out_f32 is None:
                    k.op("dve", lambda c=c: nc.vector.scalar_tensor_tensor(
                        out=self.hn[:, c, ts], in0=self.h[:, c, ts], scalar=self.gcol[:, c:c + 1], in1=self.rstd[:],
                        op0=ALU.mult, op1=ALU.mult),
                        reads=[(self.h, (c, tg)), self.gcol, self.rstd], writes=[(self.hn, tg)])
                else:
                    k.op("dve", lambda c=c: nc.vector.scalar_tensor_tensor(
                        out=out_f32[:, c, ts], in0=self.h[:, c, ts], scalar=self.gcol[:, c:c + 1], in1=self.rstd[:],
                        op0=ALU.mult, op1=ALU.mult),
                        reads=[(self.h, (c, tg)), self.gcol, self.rstd], writes=[(out_f32, (c, tg))])

    def ffn(self, wg_d, wu_d, wd_d):
        k, nc = self.k, self.nc
        wgv = wg_d.h.rearrange("(c p) f -> p c f", p=128)
        wuv = wu_d.h.rearrange("(c p) f -> p c f", p=128)
        wdv = wd_d.h.rearrange("(c p) d -> p c d", p=128)
        NG = NF // 2

        def load(g):
            b = g % 2
            k.dma("pool", self.wg[b][:], wgv[:, :, g * 256:(g + 1) * 256], reads=[wg_d], writes=[self.wg[b]])
            k.dma("pool", self.wu[b][:], wuv[:, :, g * 256:(g + 1) * 256], reads=[wu_d], writes=[self.wu[b]])
            k.dma("pool", self.wd[b][:], wdv[:, 2 * g:2 * g + 2, :], reads=[wd_d], writes=[self.wd[b]])

        load(0)
        for g in range(NG):
            if g + 1 < NG:
                load(g + 1)
            b = g % 2
            wg, wu, wd, act = self.wg[b], self.wu[b], self.wd[b], self.act[b]
            for fcl in range(2):
                fs = slice(fcl * 128, (fcl + 1) * 128)
                for tg in range(2):
                    ts = slice(tg * TG, (tg + 1) * TG)
                    pg = self.psA[self.ia % 4]
                    pu = self.psA[(self.ia + 1) % 4]
                    self.ia += 2

                    def mm(p, w):
                        ins = None
                        for c in range(NCH):
                            ins = nc.tensor.matmul(p[:], lhsT=w[:, c, fs], rhs=self.hn[:, c, ts],
                                                   start=(c == 0), stop=(c == NCH - 1))
                        return ins
                    k.op("pe", lambda: mm(pg, wg), reads=[wg, (self.hn, tg)], writes=[pg])
                    k.op("pe", lambda: mm(pu, wu), reads=[wu, (self.hn, tg)], writes=[pu])
                    sg = self.sg[tg]
                    k.op("act", lambda: nc.scalar.activation(out=sg[:], in_=pg[:], func=AF.Silu), reads=[pg], writes=[sg])
                    k.op("dve", lambda: nc.vector.tensor_tensor(out=act[:, fcl, ts], in0=pu[:], in1=sg[:], op=ALU.mult),
                         reads=[pu, sg], writes=[(act, (fcl, tg))])
            for dc in range(NCH):
                ds = slice(dc * 128, (dc + 1) * 128)
                for tg in range(2):
                    ts = slice(tg * TG, (tg + 1) * TG)
                    pd = self.psB[self.ib % 3]
                    self.ib += 1

                    def mmd():
                        ins = None
                        for fcl in range(2):
                            ins = nc.tensor.matmul(pd[:], lhsT=wd[:, fcl, ds], rhs=act[:, fcl, ts],
                                                   start=(fcl == 0), stop=(fcl == 1))
                        return ins
                    k.op("pe", mmd, reads=[wd, (act, (0, tg)), (act, (1, tg))], writes=[pd])
                    k.op("dve", lambda: nc.vector.scalar_tensor_tensor(
                        out=self.h[:, dc, ts], in0=pd[:], scalar=0.5, in1=self.h[:, dc, ts], op0=ALU.mult, op1=ALU.add),
                        reads=[pd, (self.h, (dc, tg))], writes=[(self.h, (dc, tg))])

    def proj(self, w_d, ncols, rhs_t, emit):
        k, nc = self.k, self.nc
        wv = w_d.h.rearrange("(c p) f -> p c f", p=128)
        ngr = (ncols + 255) // 256

        def load(g):
            b = g % 2
            c0 = g * 256
            c1 = min(ncols, c0 + 256)
            k.dma("pool", self.wg[b][:, :, 0:c1 - c0], wv[:, :, c0:c1], reads=[w_d], writes=[self.wg[b]])

        load(0)
        for g in range(ngr):
            if g + 1 < ngr:
                load(g + 1)
            w = self.wg[g % 2]
            for ml in range(2):
                m = 2 * g + ml
                M = min(128, ncols - m * 128)
                if M <= 0:
                    continue
                for tg in range(2):
                    ts = slice(tg * TG, (tg + 1) * TG)
                    pd = self.psB[self.ib % 3]
                    self.ib += 1

                    def mm():
                        ins = None
                        for c in range(NCH):
                            ins = nc.tensor.matmul(pd[0:M, :], lhsT=w[:, c, ml * 128:ml * 128 + M], rhs=rhs_t[:, c, ts],
                                                   start=(c == 0), stop=(c == NCH - 1))
                        return ins
                    k.op("pe", mm, reads=[w, (rhs_t, tg)], writes=[pd])
                    emit(m, M, tg, ts, pd)


def dense_A(k, h, l, zsh, zall):
    nc = k.nc
    g1 = k.din("g1a%d" % l, [128, NCH]); g2 = k.din("g2a%d" % l, [128, NCH])
    wg = k.din("f1wg%d" % l, [D, DFF]); wu = k.din("f1wu%d" % l, [D, DFF]); wd = k.din("f1wd%d" % l, [DFF, D])
    win = k.din("win%d" % l, [D, DIN]); bin_ = k.din("bin%d" % l, [128, NZC])
    dn = Dense(k, h)
    bcol = k.sb([128, NZC], F32, "bcol")
    zs = [k.sb([128, TG], BF16, "zs%d" % i) for i in range(3)]
    k.dma("sp", bcol[:], bin_.h, reads=[bin_], writes=[bcol])
    dn.rmsnorm(g1)
    dn.ffn(wg, wu, wd)
    dn.rmsnorm(g2)
    cnt = [0]

    def emit(m, M, tg, ts, pd):
        z = zs[cnt[0] % 3]
        cnt[0] += 1
        k.op("act", lambda: nc.scalar.activation(out=z[0:M, :], in_=pd[0:M, :], func=AF.Identity,
                                                 bias=bcol[0:M, m:m + 1]), reads=[pd, bcol], writes=[z])
        k.dma("sp", zsh.h[m * 128:m * 128 + M, ts], z[0:M, :], reads=[z], writes=[(zsh, (m, tg))])
        if tg == 1 and (m % 4 == 3 or m == NZC - 1):
            c0, cr, ct = zall.chunks[m // 4]
            k.allgather(zsh.h[c0:c0 + cr, :], ct, [(zsh, (mm_, t_)) for mm_ in range(4 * (m // 4), 4 * (m // 4) + 4) for t_ in (0, 1) if mm_ < NZC])
    assert zall.CR == 512
    dn.proj(win, DIN, dn.hn, emit)


def dense_C(k, h, l, yall, rv):
    nc = k.nc
    ong = k.din("ong%d" % l, [128, NCH]); wout = k.din("wout%d" % l, [D, D]); g1 = k.din("g1c%d" % l, [128, NCH])
    wg = k.din("f2wg%d" % l, [D, DFF]); wu = k.din("f2wu%d" % l, [D, DFF]); wd = k.din("f2wd%d" % l, [DFF, D])
    dn = Dense(k, h)
    ys = [k.sb([128, 4, TG], F32, "ys%d" % i) for i in range(2)]
    ocol = k.sb([128, NCH], F32, "ocol")
    k.dma("sp", ocol[:], ong.h, reads=[ong], writes=[ocol])
    yst = [k.sb([128, 4, TG], F32, "yst%d" % i) for i in range(2)]
    mskc = k.sb([128, 16], F32, "mskc")
    dmsk = k.din("msk", [128, 16])
    k.dma("sp", mskc[:], dmsk.h, reads=[dmsk], writes=[mskc])
    i = 0
    for grp in range(4):
        for tg in range(2):
            ts = slice(tg * TG, (tg + 1) * TG)
            y = ys[i % 2]
            i += 1
            for rc in range(4):
                st_ = yst[rc % 2]
                for jp in range(4):
                    for (ct, sap_, d0, cnt) in yall.pieces(jp, grp * 128, 128):
                        k.dma("sp", st_[d0:d0 + cnt, jp, :], sap_[:, rc * TOK + tg * TG:rc * TOK + (tg + 1) * TG], reads=[ct], writes=[st_])
                mc = mskc[:, rc:rc + 1]
                if rc == 0:
                    k.op("dve", lambda: nc.vector.tensor_scalar(out=y[:], in0=st_[:], scalar1=mc, scalar2=None, op0=ALU.mult), reads=[st_, mskc], writes=[y])
                else:
                    k.op("dve", lambda: nc.vector.scalar_tensor_tensor(out=y[:], in0=st_[:], scalar=mc, in1=y[:], op0=ALU.mult, op1=ALU.add),
                         reads=[st_, mskc, y], writes=[y])
            if grp == 1:
                for cl in range(4):
                    k.op("dve", lambda cl=cl: nc.vector.tensor_copy(out=dn.hn[:, 4 + cl, ts], in_=y[:, cl, :]),
                         reads=[y], writes=[(dn.hn, tg)])
                continue
            for cl in range(4):
                sq = dn.sq[cl % 2]
                k.op("act", lambda sq=sq, cl=cl: nc.scalar.activation(out=sq[:], in_=y[:, cl, :], func=AF.Square),
                     reads=[y], writes=[sq])
                k.op("pe", lambda sq=sq, cl=cl: nc.tensor.matmul(dn.psN[:], lhsT=dn.ones4[:], rhs=sq[:],
                                                                 start=(cl == 0), stop=(cl == 3)),
                     reads=[sq, dn.ones4], writes=[dn.psN])
            dn.rstd_from(dn.psN)
            for cl in range(4):
                c = grp * 4 + cl
                k.op("dve", lambda cl=cl, c=c: nc.vector.scalar_tensor_tensor(
                    out=dn.hn[:, c, ts], in0=y[:, cl, :], scalar=ocol[:, c:c + 1], in1=dn.rstd[:],
                    op0=ALU.mult, op1=ALU.mult), reads=[y, ocol, dn.rstd], writes=[(dn.hn, tg)])

    def emit(m, M, tg, ts, pd):
        k.op("dve", lambda: nc.vector.tensor_tensor(out=h[:, m, ts], in0=pd[:], in1=h[:, m, ts], op=ALU.add),
             reads=[pd, (h, (m, tg))], writes=[(h, (m, tg))])
    dn.proj(wout, D, dn.hn, emit)
    dn.rmsnorm(g1)
    dn.ffn(wg, wu, wd)


def final_out(k, h, out):
    nc = k.nc
    sq_ = [k.sb([128, TG], BF16, "sq%d" % i) for i in range(2)]
    rstd = k.sb([128, TG], F32, "rstd")
    onesm = k.sb([128, 128], BF16, "onesm")
    gcol = k.sb([128, NCH], F32, "gcol")
    epsc = k.sb([128, 1], F32, "epsc")
    psN = k.ps([128, TG], F32, "psN")
    psB = [k.ps([128, TG], F32, "psB%d" % i) for i in range(3)]
    ib = [0]
    k.op("dve", lambda: nc.vector.memset(epsc[:], EPS), writes=[epsc])
    k.op("dve", lambda: nc.vector.memset(onesm[:], 1.0 / D), writes=[onesm])

    def rstd_from():
        k.op("act", lambda: nc.scalar.activation(out=rstd[:], in_=psN[:], func=AF.Ln, bias=epsc[:]), reads=[psN, epsc], writes=[rstd])
        k.op("act", lambda: nc.scalar.activation(out=rstd[:], in_=rstd[:], func=AF.Exp, scale=-0.5), reads=[rstd], writes=[rstd])
    gf = k.din("gf", [128, NCH])
    identF = k.sb([128, 128], F32, "identF")
    cid = k.din("c_ident", [128, 128])
    k.dma("sp", identF[:], cid.h, reads=[cid], writes=[identF])
    fin = k.sb([128, NCH, TG], F32, "fin")
    ot = [k.sb([128, D], F32, "ot%d" % i) for i in range(2)]
    k.dma("sp", gcol[:], gf.h, reads=[gf], writes=[gcol])
    for tg in range(2):
        ts = slice(tg * TG, (tg + 1) * TG)
        for c in range(NCH):
            sq = sq_[c % 2]
            k.op("act", lambda sq=sq, c=c: nc.scalar.activation(out=sq[:], in_=h[:, c, ts], func=AF.Square),
                 reads=[(h, (c, tg))], writes=[sq])
            k.op("pe", lambda sq=sq, c=c: nc.tensor.matmul(psN[:], lhsT=onesm[:], rhs=sq[:],
                                                           start=(c == 0), stop=(c == NCH - 1)),
                 reads=[sq, onesm], writes=[psN])
        rstd_from()
        for c in range(NCH):
            k.op("dve", lambda c=c: nc.vector.scalar_tensor_tensor(
                out=fin[:, c, :], in0=h[:, c, ts], scalar=gcol[:, c:c + 1], in1=rstd[:],
                op0=ALU.mult, op1=ALU.mult), reads=[(h, (c, tg)), gcol, rstd], writes=[(fin, c)])
        for tt in range(4):
            o = ot[tt % 2]
            for c4 in range(4):
                pd = psB[ib[0] % 3]
                ib[0] += 1

                def mmT():
                    ins = None
                    for cl in range(4):
                        c = c4 * 4 + cl
                        ins = nc.tensor.matmul(pd[:, cl * 128:(cl + 1) * 128], lhsT=fin[:, c, tt * 128:(tt + 1) * 128],
                                               rhs=identF[:], start=True, stop=True)
                    return ins
                k.op("pe", mmT, reads=[identF] + [(fin, c4 * 4 + cl) for cl in range(4)], writes=[pd])
                k.op("act", lambda: nc.scalar.copy(out=o[:, c4 * 512:(c4 + 1) * 512], in_=pd[:]), reads=[pd], writes=[(o, c4)])
            r0 = tg * TG + tt * 128
            k.dma("sp", out.h[r0:r0 + 128, :], o[:], reads=[o], writes=[(out, r0)])


def load_x(k, h, x):
    nc = k.nc
    identF = k.sb([128, 128], F32, "identF")
    cid = k.din("c_ident", [128, 128])
    k.dma("sp", identF[:], cid.h, reads=[cid], writes=[identF])
    xt = [k.sb([128, D], F32, "xt%d" % i) for i in range(4)]
    pst = [k.ps([128, TG], F32, "pst%d" % i) for i in range(4)]
    n = 0
    for tg in range(2):
        for tt in range(4):
            r0 = tg * TG + tt * 128
            k.dma("sp", xt[tt][:], x.h[r0:r0 + 128, :], reads=[x], writes=[xt[tt]])
        for c in range(NCH):
            pd = pst[n % 4]
            n += 1

            def mmT():
                ins = None
                for tt in range(4):
                    ins = nc.tensor.matmul(pd[:, tt * 128:(tt + 1) * 128], lhsT=xt[tt][:, c * 128:(c + 1) * 128], rhs=identF[:],
                                           start=True, stop=True)
                return ins
            k.op("pe", mmT, reads=[identF] + xt, writes=[pd])
            eng = "act" if c % 2 else "dve"
            if eng == "act":
                k.op("act", lambda: nc.scalar.copy(out=h[:, c, tg * TG:(tg + 1) * TG], in_=pd[:]), reads=[pd], writes=[(h, (c, tg))])
            else:
                k.op("dve", lambda: nc.vector.tensor_copy(h[:, c, tg * TG:(tg + 1) * TG], pd[:]), reads=[pd], writes=[(h, (c, tg))])


def col16(g):
    return np.ascontiguousarray(np.asarray(g, np.float32).reshape(NCH, 128).T)


T_ = 4096
NT = 32
SCALE = 128 ** -0.5


class Mix:
    def __init__(self, k, l, zall, vals, nbig=7, nbigb=4):
        self.k = k
        self.l = l
        self.zall = zall
        nc = self.nc = k.nc
        self.cident = k.din("c_ident", [128, 128])
        self.msk = k.sb([128, 16], F32, "msk")
        dmsk = k.din("msk", [128, 16])
        k.dma("sp", self.msk[:], dmsk.h, reads=[dmsk], writes=[self.msk])
        self.identF = k.sb([128, 128], F32, "identF")
        self.identB = k.sb([128, 128], BF16, "identB")
        self.onesF = k.sb([128, 128], F32, "onesF")
        self.onesB = k.sb([128, 128], BF16, "onesB")
        k.dma("sp", self.identF[:], self.cident.h, reads=[self.cident], writes=[self.identF])
        k.op("dve", lambda: nc.vector.tensor_copy(self.identB[:], self.identF[:]), reads=[self.identF], writes=[self.identB])
        k.op("dve", lambda: nc.vector.memset(self.onesF[:], 1.0), writes=[self.onesF])
        k.op("dve", lambda: nc.vector.memset(self.onesB[:], 1.0), writes=[self.onesB])
        self.big = [k.sb([128, T_], F32, "big%d" % i) for i in range(nbig)]
        self.bigb = [k.sb([128, T_], BF16, "bigb%d" % i) for i in range(nbigb)]
        self.psS = [k.ps([128, 512], F32, "psS%d" % i) for i in range(3)]
        self.psO = [k.ps([128, 512], F32, "psO%d" % i) for i in range(2)]
        self.psL = [k.ps([128, 512], F32, "psL%d" % i) for i in range(2)]
        self.psX = k.ps([128, 512], F32, "psX")
        self.iS = 0
        self.pT = [k.sb([128, 512], BF16, "pT%d" % i) for i in range(3)]
        self.tmpA = [k.sb([128, 512], F32, "tmpA%d" % i) for i in range(4)]

    def zfm(self, dst_t, dst_ap, off, dyn=None, q="sp", n=128, key=None, mul=128, stage=None):
        k, nc = self.k, self.nc
        dap = dst_ap[:] if not hasattr(dst_ap, "ap") else dst_ap
        if dyn is None:
            for s_ in range(4):
                for (ct, sap_, d0, cnt) in self.zall.pieces(s_, off, n):
                    k.dma(q, dap[d0:d0 + cnt, s_ * TOK:(s_ + 1) * TOK], sap_, reads=[ct], writes=[(dst_t, key)])
            return
        sap = stage[:]
        if sap.dtype != BF16:
            sap = sap.bitcast(BF16)[:, 0:T_]
        for jc in range(4):
            for s_ in range(4):
                for (ct, sap_, d0, cnt) in self.zall.pieces(s_, off + jc * mul, n):
                    k.dma(q, sap[d0:d0 + cnt, s_ * TOK:(s_ + 1) * TOK], sap_, reads=[ct], writes=[stage])
            mc = self.msk[0:n, dyn + jc:dyn + jc + 1]
            if jc == 0:
                k.op("dve", lambda: nc.vector.tensor_scalar(out=dap[0:n, :], in0=sap[0:n, :], scalar1=mc, scalar2=None, op0=ALU.mult),
                     reads=[stage, self.msk], writes=[(dst_t, key)])
            else:
                k.op("dve", lambda: nc.vector.scalar_tensor_tensor(out=dap[0:n, :], in0=sap[0:n, :], scalar=mc, in1=dap[0:n, :], op0=ALU.mult, op1=ALU.add),
                     reads=[stage, self.msk, (dst_t, key)], writes=[(dst_t, key)])

    def row2col(self, row, dst):
        k, nc = self.k, self.nc
        px = self.psX

        def mm():
            ins = None
            for kt in range(NT):
                ins = nc.tensor.matmul(px[:, kt:kt + 1], lhsT=row[0:1, kt * 128:(kt + 1) * 128], rhs=self.onesF[0:1, 0:1], start=True, stop=True)
            return ins
        k.op("pe", mm, reads=[row, self.onesF], writes=[px])
        k.op("dve", lambda: nc.vector.tensor_copy(dst[:], px[:, 0:NT]), reads=[px], writes=[dst])

    def fm2tm(self, src_ap, src_dep, dst_t, dst_ap, key=None):
        k, nc = self.k, self.nc
        for g4 in range(NT // 4):
            ps = self.psS[g4 % 3]

            def mm():
                ins = None
                for i in range(4):
                    kt = g4 * 4 + i
                    ins = nc.tensor.matmul(ps[:, i * 128:(i + 1) * 128], lhsT=src_ap[:, kt * 128:(kt + 1) * 128], rhs=self.identB[:], start=True, stop=True)
                return ins
            k.op("pe", mm, reads=[src_dep, self.identB], writes=[ps])
            if g4 % 2:
                k.op("act", lambda: nc.scalar.copy(out=dst_ap[:, g4 * 512:(g4 + 1) * 512], in_=ps[:]), reads=[ps], writes=[(dst_t, key)])
            else:
                k.op("dve", lambda: nc.vector.tensor_copy(dst_ap[:, g4 * 512:(g4 + 1) * 512], ps[:]), reads=[ps], writes=[(dst_t, key)])

    def ld(self, dst, src_t, src_ap=None, q="sp", dst_ap=None):
        self.k.dma(q, dst[:] if dst_ap is None else dst_ap, src_t.h if src_ap is None else src_ap, reads=[src_t], writes=[dst])

    def lru(self, yout):
        k, nc = self.k, self.nc
        L_ = "%d" % self.l
        cw = k.din("lru_cw" + L_, [128, 4]); cb = k.din("lru_cb" + L_, [128, 1])
        wa = k.din("lru_wa" + L_, [128, 128]); wx = k.din("lru_wx" + L_, [128, 128])
        ba = k.din("lru_ba" + L_, [128, 1]); bx = k.din("lru_bx" + L_, [128, 1]); lam = k.din("lru_lam" + L_, [128, 1])
        xs, xc, aa, uu, gg, tt = self.big[0:6]
        hs = gg
        xcb = self.bigb[0]
        cws = k.sb([128, 4], F32, "l_cw"); cbs = k.sb([128, 1], F32, "l_cb")
        was = k.sb([128, 128], BF16, "l_wa"); wxs = k.sb([128, 128], BF16, "l_wx")
        bas = k.sb([128, 1], F32, "l_ba"); bxs = k.sb([128, 1], F32, "l_bx"); lams = k.sb([128, 1], F32, "l_lam")
        nsp = k.sb([128, 1], F32, "l_nsp")
        self.zfm(xs, xs.h, _OFF["lru_x"], 0, stage=tt); self.zfm(gg, gg.h, _OFF["lru_gate"], 0, stage=tt); self.ld(cws, cw); self.ld(cbs, cb); self.ld(bas, ba); self.ld(bxs, bx); self.ld(lams, lam)
        self.ld(was, wa, q="pool"); self.ld(wxs, wx, q="pool")
        k.op("act", lambda: nc.scalar.activation(out=nsp[:], in_=lams[:], func=AF.Exp, scale=-1.0), reads=[lams], writes=[nsp])
        k.op("act", lambda: nc.scalar.activation(out=nsp[:], in_=nsp[:], func=AF.Ln, bias=self.onesF[:, 0:1]),
             reads=[nsp, self.onesF], writes=[nsp])
        k.op("dve", lambda: nc.vector.tensor_scalar(out=nsp[:], in0=nsp[:], scalar1=-8.0, scalar2=None, op0=ALU.mult),
             reads=[nsp], writes=[nsp])
        k.op("dve", lambda: nc.vector.tensor_scalar(out=xc[:], in0=xs[:], scalar1=cws[:, 3:4], scalar2=cbs[:, 0:1],
                                                    op0=ALU.mult, op1=ALU.add), reads=[xs, cws, cbs], writes=[xc])
        for i in range(3):
            sh = 3 - i
            k.op("dve", lambda i=i, sh=sh: nc.vector.scalar_tensor_tensor(
                out=xc[:, sh:], in0=xs[:, 0:T_ - sh], scalar=cws[:, i:i + 1], in1=xc[:, sh:], op0=ALU.mult, op1=ALU.add),
                reads=[xs, cws, xc], writes=[xc])
        k.op("act", lambda: nc.scalar.copy(out=xcb[:], in_=xc[:]), reads=[xc], writes=[xcb])
        for tb in range(8):
            ts = slice(tb * 512, (tb + 1) * 512)
            pr = self.psS[0]; pi = self.psS[1]
            k.op("pe", lambda: nc.tensor.matmul(pr[:], lhsT=was[:], rhs=xcb[:, ts], start=True, stop=True), reads=[was, xcb], writes=[pr])
            k.op("pe", lambda: nc.tensor.matmul(pi[:], lhsT=wxs[:], rhs=xcb[:, ts], start=True, stop=True), reads=[wxs, xcb], writes=[pi])
            r = self.tmpA[0]; ii = self.tmpA[1]
            k.op("act", lambda: nc.scalar.activation(out=r[:], in_=pr[:], func=AF.Sigmoid, bias=bas[:, 0:1]), reads=[pr, bas], writes=[r])
            k.op("act", lambda: nc.scalar.activation(out=ii[:], in_=pi[:], func=AF.Sigmoid, bias=bxs[:, 0:1]), reads=[pi, bxs], writes=[ii])
            k.op("act", lambda: nc.scalar.activation(out=aa[:, ts], in_=r[:], func=AF.Exp, scale=nsp[:, 0:1]),
                 reads=[r, nsp], writes=[(aa, tb)])
            t1 = self.tmpA[2]
            k.op("dve", lambda: nc.vector.tensor_tensor(out=t1[:], in0=aa[:, ts], in1=aa[:, ts], op=ALU.mult), reads=[(aa, tb)], writes=[t1])
            k.op("dve", lambda: nc.vector.tensor_scalar(out=t1[:], in0=t1[:], scalar1=-1.0, scalar2=1.0, op0=ALU.mult, op1=ALU.add),
                 reads=[t1], writes=[t1])
            k.op("act", lambda: nc.scalar.activation(out=t1[:], in_=t1[:], func=AF.Sqrt), reads=[t1], writes=[t1])
            k.op("dve", lambda: nc.vector.tensor_tensor(out=ii[:], in0=ii[:], in1=xc[:, ts], op=ALU.mult), reads=[ii, xc], writes=[ii])
            k.op("dve", lambda: nc.vector.tensor_tensor(out=uu[:, ts], in0=ii[:], in1=t1[:], op=ALU.mult), reads=[ii, t1], writes=[(uu, tb)])
        self.gelu_tanh(tt, gg, xs)
        k.op("dve", lambda: nc.vector.tensor_tensor_scan(out=hs[:], data0=aa[:], data1=uu[:], initial=0.0, op0=ALU.mult, op1=ALU.add),
             reads=[aa, uu], writes=[hs])
        k.op("dve", lambda: nc.vector.tensor_tensor(out=hs[:], in0=hs[:], in1=tt[:], op=ALU.mult), reads=[hs, tt], writes=[hs])
        k.dma("sp", yout.h, hs[:], reads=[hs], writes=[yout])

    def gelu_tanh(self, out, x, tmp):
        k, nc = self.k, self.nc
        k.op("dve", lambda: nc.vector.tensor_tensor(out=tmp[:], in0=x[:], in1=x[:], op=ALU.mult), reads=[x], writes=[tmp])
        k.op("dve", lambda: nc.vector.tensor_scalar(out=tmp[:], in0=tmp[:], scalar1=0.044715, scalar2=1.0, op0=ALU.mult, op1=ALU.add),
             reads=[tmp], writes=[tmp])
        k.op("dve", lambda: nc.vector.tensor_tensor(out=tmp[:], in0=tmp[:], in1=x[:], op=ALU.mult), reads=[tmp, x], writes=[tmp])
        k.op("act", lambda: nc.scalar.activation(out=tmp[:], in_=tmp[:], func=AF.Sigmoid, scale=1.5957691216057308),
             reads=[tmp], writes=[tmp])
        k.op("dve", lambda: nc.vector.tensor_tensor(out=out[:], in0=tmp[:], in1=x[:], op=ALU.mult), reads=[tmp, x], writes=[out])

    def attn_unit(self, kT, kt, qT, q0, extra, bias_ap, bias_reads, V, vslice, O, L, first, last, nkeys=128, kT_t=None, qT_t=None):
        k, nc = self.k, self.nc
        S = self.psS[self.iS % 3]
        P = self.pT[self.iS % 3]
        self.iS += 1
        nk = nkeys

        def mm():
            n = len(extra)
            ins = nc.tensor.matmul(S[0:nk, :], lhsT=kT[:, kt * 128:kt * 128 + nk], rhs=qT[:, q0:q0 + 512], start=True, stop=(n == 0))
            for i, (oa, l, r) in enumerate(extra):
                ins = nc.tensor.matmul(oa(S), lhsT=l, rhs=r, start=False, stop=(i == n - 1))
            return ins
        k.op("pe", mm, reads=[kT_t or kT, qT_t or qT] + self._xr, writes=[S])
        if bias_ap is None:
            k.op("act", lambda: nc.scalar.activation(out=P[0:nk, :], in_=S[0:nk, :], func=AF.Exp), reads=[S], writes=[P])
        else:
            k.op("act", lambda: nc.scalar.activation(out=P[0:nk, :], in_=S[0:nk, :], func=AF.Exp, bias=bias_ap),
                 reads=[S] + bias_reads, writes=[P])
        def pv():
            k.op("pe", lambda: nc.tensor.matmul(O[:], lhsT=vslice[0:nk], rhs=P[0:nk, :], start=first, stop=last), reads=[V, P], writes=[O])
            k.op("pe", lambda: nc.tensor.matmul(L[:], lhsT=self.onesB[0:nk, :], rhs=P[0:nk, :], start=first, stop=last),
                 reads=[self.onesB, P], writes=[L])
        pend = getattr(self, "_pend", None)
        self._pend = pv
        if pend is not None:
            pend()

    def flush_attn(self):
        pend = getattr(self, "_pend", None)
        self._pend = None
        if pend is not None:
            pend()

    def fox(self, yout):
        k, nc = self.k, self.nc
        cut = k.din("c_ut", [128, 128]); csu = k.din("c_su", [32, 32]); cmb = k.din("c_mbfox", [128, 4, 512])
        qf = self.big[0]
        qb, kb = self.bigb[0], self.bigb[1]
        vb = self.bigb[2]
        mb = k.sb([128, 4, 512], BF16, "f_mb")
        ut = k.sb([128, 128], F32, "f_ut"); su = k.sb([32, 32], F32, "f_su")
        lf = k.sb([128, NT], F32, "f_lf"); negc = k.sb([128, NT], F32, "f_negc")
        totc = k.sb([32, 1], F32, "f_totc"); am = k.sb([32, 128], F32, "f_am")
        dg = self.big[1]
        frow = k.sb([1, T_], F32, "f_row")
        vT = self.bigb[3]
        frow2 = k.sb([1, T_], F32, "f_row2")
        self.zfm(kb, kb.h, _OFF["fox_k"], 0, q="pool", stage=qb)
        self.zfm(vT, vT.h, _OFF["fox_v"], 0, q="pool", stage=qb)
        self.zfm(qf, qf.h, _OFF["fox_q"], 0, stage=dg)
        self.zfm(frow, frow.h, _OFF["fox_f"], 0, n=1, mul=1, stage=frow2)
        self.fm2tm(vT.h, vT, vb, vb.h)
        k.dma("pool", mb[:], cmb.h, reads=[cmb], writes=[mb])
        self.ld(ut, cut); self.ld(su, csu)
        self.row2col(frow, lf)
        k.op("dve", lambda: nc.vector.tensor_scalar(out=qb[:], in0=qf[:], scalar1=SCALE, scalar2=None, op0=ALU.mult), reads=[qf], writes=[qb])
        k.op("act", lambda: nc.scalar.activation(out=lf[:], in_=lf[:], func=AF.Exp, scale=-1.0), reads=[lf], writes=[lf])
        k.op("act", lambda: nc.scalar.activation(out=lf[:], in_=lf[:], func=AF.Ln, bias=self.onesF[:, 0:1]), reads=[lf, self.onesF], writes=[lf])
        px = self.psX
        k.op("pe", lambda: nc.tensor.matmul(px[0:32, 0:1], lhsT=lf[:], rhs=self.onesF[:, 0:1], start=True, stop=True),
             reads=[lf, self.onesF], writes=[px])
        k.op("dve", lambda: nc.vector.tensor_copy(totc[:], px[0:32, 0:1]), reads=[px], writes=[totc])
        k.op("dve", lambda: nc.vector.tensor_scalar(out=am[:], in0=self.onesF[0:32, :], scalar1=totc[:, 0:1], scalar2=None, op0=ALU.mult),
             reads=[self.onesF, totc], writes=[am])

        def mmc():
            nc.tensor.matmul(px[:, 0:NT], lhsT=ut[:], rhs=lf[:], start=True, stop=False)
            return nc.tensor.matmul(px[:, 0:NT], lhsT=am[:], rhs=su[:], start=False, stop=True)
        k.op("pe", mmc, reads=[ut, lf, am, su], writes=[px])
        k.op("dve", lambda: nc.vector.tensor_copy(negc[:], px[:, 0:NT]), reads=[px], writes=[negc])
        for qt in range(NT):
            k.op("dve", lambda qt=qt: nc.vector.tensor_scalar(out=dg[:, qt * 128:(qt + 1) * 128], in0=self.identF[:],
                                                              scalar1=negc[:, qt:qt + 1], scalar2=-1.0, op0=ALU.mult, op1=ALU.mult),
                 reads=[self.identF, negc], writes=[(dg, qt)])
        for i in range(8):
            q0 = 512 * i
            O = self.psO[i % 2]; L = self.psL[i % 2]
            nk = 4 * i + 4
            for kt in range(nk):
                extra = []
                for jq in range(4):
                    qt = 4 * i + jq
                    extra.append((lambda S, jq=jq: S[:, jq * 128:(jq + 1) * 128], self.onesF[:], dg[:, qt * 128:(qt + 1) * 128]))
                self._xr = [self.onesF] + [(dg, 4 * i + jq) for jq in range(4)]
                if kt >= 4 * i:
                    extra.append((lambda S: S[:], self.identB[:], mb[:, kt - 4 * i, :]))
                    self._xr += [self.identB, mb]
                self.attn_unit(kb, kt, qb, q0, extra, negc[:, kt:kt + 1], [negc], vb, vb[:, kt * 128:(kt + 1) * 128], O, L,
                               kt == 0, kt == nk - 1)
            self.flush_attn()
            R = self.tmpA[i % 2]; ob = self.tmpA[2 + i % 2]
            k.op("dve", lambda: nc.vector.reciprocal(out=R[:], in_=L[:]), reads=[L], writes=[R])
            k.op("dve", lambda: nc.vector.tensor_tensor(out=ob[:], in0=O[:], in1=R[:], op=ALU.mult), reads=[O, R], writes=[ob])
            k.dma("sp", yout.h[:, q0:q0 + 512], ob[:], reads=[ob], writes=[(yout, i)])


    def bfv(self, t):
        return t.h[:].bitcast(BF16)

    def nsa(self, yout):
        k, nc = self.k, self.nc
        L_ = "%d" % self.l
        dw1k = k.din("nsa_w1k" + L_, [128, 32, 128]); dw1v = k.din("nsa_w1v" + L_, [128, 32, 128])
        dw2k = k.din("nsa_w2k" + L_, [128, 128]); dw2v = k.din("nsa_w2v" + L_, [128, 128])
        dpek = k.din("nsa_pek" + L_, [128, 32]); dpev = k.din("nsa_pev" + L_, [128, 32])
        dbc = k.din("nsa_bc", [128, 2, 4, T_]); dtbs = k.din("nsa_tbs", [128, 12, 512]); dtbw = k.din("nsa_tbw", [128, 8, 512])
        dkeep = k.din("nsa_keep", [128, NT, 64]); dadd = k.din("nsa_add", [128, NT, 64])
        dE = k.din("nsa_E", [64, T_]); dov = k.din("nsa_ov", [128, 2, 64])
        kcT = k.sb([128, 256], BF16, "n_kcT"); vc = k.sb([128, 2, 128], BF16, "n_vc")
        qown = k.sb([128, T_], BF16, "n_qown")
        ksb = k.sb([128, T_], BF16, "n_ks"); kwb = k.sb([128, T_], BF16, "n_kw")
        vsb = k.sb([128, T_], BF16, "n_vs"); vwb = k.sb([128, T_], BF16, "n_vw")
        grow = k.sb([3, T_], F32, "n_grow")
        with k.scope():
            gstg = k.sb([3, T_], BF16, "n_gstg")
            self.zfm(grow, grow.h, _OFF["nsa_g"], 0, n=3, mul=3, stage=gstg)
        gsel = [k.sb([3, 128], F32, "n_gsel%d" % i) for i in range(3)]
        for gi in range(3):
            k.op("dve", lambda gi=gi: nc.vector.tensor_scalar(out=gsel[gi][:], in0=self.onesF[0:3, :], scalar1=self.identF[0:3, gi:gi + 1], scalar2=None, op0=ALU.mult),
                 reads=[self.onesF, self.identF], writes=[gsel[gi]])
        self.zfm(qown, qown.h, _OFF["nsa_q"], 0, q="pool", stage=vsb)
        k.op("dve", lambda: nc.vector.tensor_scalar(out=qown[:], in0=qown[:], scalar1=SCALE, scalar2=None, op0=ALU.mult), reads=[qown], writes=[qown])
        self.zfm(ksb, ksb.h, _OFF["nsa_ks"], None, q="pool")
        self.zfm(kwb, kwb.h, _OFF["nsa_kw"], None, q="pool")
        with k.scope():
            stg = [k.sb([128, T_], BF16, "n_stg%d" % i) for i in range(2)]
            self.zfm(stg[0], stg[0].h, _OFF["nsa_vs"], None, q="pool")
            self.zfm(stg[1], stg[1].h, _OFF["nsa_vw"], None, q="pool")
            self.fm2tm(stg[0].h, stg[0], vsb, vsb.h)
            self.fm2tm(stg[1].h, stg[1], vwb, vwb.h)
        cscope = k.scope()
        cscope.__enter__()
        kcin_t = k.sb([128, T_], BF16, "n_kcin"); vcin_t = k.sb([128, T_], BF16, "n_vcin")
        kcin = kcin_t.h; vcin = vcin_t.h
        self.zfm(kcin_t, kcin, _OFF["nsa_kc"], None, q="pool")
        self.zfm(vcin_t, vcin, _OFF["nsa_vc"], None, q="pool")
        w1 = [k.sb([128, 32, 128], BF16, "n_w1k"), k.sb([128, 32, 128], BF16, "n_w1v")]
        w2 = [k.sb([128, 128], BF16, "n_w2k"), k.sb([128, 128], BF16, "n_w2v")]
        pe = [k.sb([128, 32], BF16, "n_pek"), k.sb([128, 32], BF16, "n_pev")]
        self.ld(w1[0], dw1k, q="pool"); self.ld(w1[1], dw1v, q="pool"); self.ld(w2[0], dw2k, q="pool"); self.ld(w2[1], dw2v, q="pool")
        self.ld(pe[0], dpek, q="pool"); self.ld(pe[1], dpev, q="pool")
        hidb = [k.sb([128, 256], BF16, "n_hidk"), k.sb([128, 256], BF16, "n_hidv")]
        pb = k.sb([128, 1], F32, "n_pb")
        hx = k.sb([128, 256], F32, "n_hx"); hy = k.sb([128, 256], F32, "n_hy"); hz = k.sb([128, 256], F32, "n_hz")
        srcs = [kcin_t, vcin_t]
        cin = [kcin, vcin]
        for w in range(2):
            px = self.psX

            def mmb():
                ins = None
                for i in range(32):
                    ins = nc.tensor.matmul(px[:, 0:1], lhsT=w1[w][:, i, :], rhs=pe[w][:, i:i + 1], start=(i == 0), stop=(i == 31))
                return ins
            k.op("pe", mmb, reads=[w1[w], pe[w]], writes=[px])
            k.op("dve", lambda: nc.vector.tensor_copy(pb[:], px[:, 0:1]), reads=[px], writes=[pb])
            ph = self.psS[w]

            def mmh():
                ins = None
                for i in range(32):
                    ins = nc.tensor.matmul(ph[:, 0:255], lhsT=w1[w][:, i, :], rhs=cin[w][:, i:i + 4065:16], start=(i == 0), stop=(i == 31))
                return ins
            k.op("pe", mmh, reads=[w1[w], srcs[w]], writes=[ph])
            k.op("dve", lambda: nc.vector.memset(hx[:], 0.0), writes=[hx])
            k.op("act", lambda: nc.scalar.activation(out=hx[:, 0:255], in_=ph[:, 0:255], func=AF.Identity, bias=pb[:, 0:1]),
                 reads=[ph, pb], writes=[hx])
            self.gelu_tanh(hz, hx, hy)
            k.op("dve", lambda: nc.vector.tensor_copy(hidb[w][:], hz[:]), reads=[hz], writes=[hidb[w]])
        pk = self.psS[2]
        k.op("pe", lambda: nc.tensor.matmul(pk[:, 0:256], lhsT=w2[0][:], rhs=hidb[0][:], start=True, stop=True), reads=[w2[0], hidb[0]], writes=[pk])
        k.op("dve", lambda: nc.vector.tensor_copy(kcT[:], pk[:, 0:256]), reads=[pk], writes=[kcT])
        for nt in range(2):
            pv = self.psO[nt]
            k.op("pe", lambda: nc.tensor.matmul(pv[:, 0:128], lhsT=hidb[1][:, nt * 128:(nt + 1) * 128], rhs=w2[1][:], start=True, stop=True),
                 reads=[hidb[1], w2[1]], writes=[pv])
            k.op("dve", lambda: nc.vector.tensor_copy(vc[:, nt, :], pv[:, 0:128]), reads=[pv], writes=[(vc, nt)])
        cscope.__exit__(None, None, None)
        tbs = k.sb([128, 12, 512], BF16, "n_tbs"); tbw = k.sb([128, 8, 512], BF16, "n_tbw")
        self.ld(tbs, dtbs, q="pool"); self.ld(tbw, dtbw, q="pool")
        keep = k.sb([128, 4, 64], F32, "n_keep"); addt = k.sb([128, 4, 64], F32, "n_add")
        Eb = k.sb([64, T_], BF16, "n_E"); ov = k.sb([128, 2, 64], BF16, "n_ov")
        self.ld(Eb, dE, q="pool"); self.ld(ov, dov, q="pool")
        qo = k.sb([128, 3, 512], BF16, "n_qo"); qst = k.sb([128, 4, 512], BF16, "n_qst")
        bc = [k.sb([128, 2, 4, 512], BF16, "n_bc0")] * 2
        gb = [k.sb([128, 3, 512], F32, "n_gb0")] * 2
        pc = [[k.sb([128, 512], BF16, "n_pc%d%d" % (h, nt)) for nt in range(2)] for h in range(4)]
        Rh = [k.sb([128, 512], F32, "n_R")] * 4
        acc = [k.sb([128, 512], F32, "n_acc%d" % i) for i in range(2)]
        impt = k.sb([128, 4, 64], F32, "n_imp"); wrk = k.sb([128, 64], F32, "n_wrk")
        m8 = k.sb([128, 8], F32, "n_m8"); thr = k.sb([128, 1], F32, "n_thr"); selb = k.sb([128, 64], F32, "n_selb")
        selT = k.sb([64, 512], BF16, "n_selT")
        Wt = k.sb([128, 512], F32, "n_W")
        NKC = [128, 127]
        for i in range(8):
            q0 = 512 * i
            bci = bc[i % 2]; gbi = gb[i % 2]; ac = acc[i % 2]
            k.dma("pool", bci[:], dbc.h[:, :, :, q0:q0 + 512], reads=[dbc], writes=[bci])
            for gi in range(3):
                pgx = self.psS[self.iS % 3]; self.iS += 1
                k.op("pe", lambda: nc.tensor.matmul(pgx[:], lhsT=gsel[gi][:], rhs=grow[0:3, q0:q0 + 512], start=True, stop=True),
                     reads=[gsel[gi], grow], writes=[pgx])
                k.op("act", lambda: nc.scalar.activation(out=gbi[:, gi, :], in_=pgx[:], func=AF.Sigmoid), reads=[pgx], writes=[(gbi, gi)])
            sq_ = q0 // TOK
            c0 = q0 - sq_ * TOK
            for jc in range(4):
                for (ct, sap_, d0, cnt) in self.zall.pieces(sq_, _OFF["nsa_q"] + jc * 128, 128):
                    k.dma("pool", qst[d0:d0 + cnt, jc, :], sap_[:, c0:c0 + 512], reads=[ct], writes=[qst])
            for hh in range(3):
                for jc in range(4):
                    mc = self.msk[:, 4 + 4 * hh + jc:5 + 4 * hh + jc]
                    if jc == 0:
                        k.op("dve", lambda: nc.vector.tensor_scalar(out=qo[:, hh, :], in0=qst[:, jc, :], scalar1=mc, scalar2=None, op0=ALU.mult),
                             reads=[qst, self.msk], writes=[qo])
                    else:
                        k.op("dve", lambda: nc.vector.scalar_tensor_tensor(out=qo[:, hh, :], in0=qst[:, jc, :], scalar=mc, in1=qo[:, hh, :], op0=ALU.mult, op1=ALU.add),
                             reads=[qst, self.msk, qo], writes=[qo])
            k.op("dve", lambda: nc.vector.tensor_scalar(out=qo[:], in0=qo[:], scalar1=SCALE, scalar2=None, op0=ALU.mult), reads=[qo], writes=[qo])
            k.dma("sp", keep[:], dkeep.h[:, 4 * i:4 * i + 4, :], reads=[dkeep], writes=[keep])
            k.dma("sp", addt[:], dadd.h[:, 4 * i:4 * i + 4, :], reads=[dadd], writes=[addt])
            for hh in range(4):
                h = hh
                own = (h == 3)
                L = self.psL[hh % 2]; O = self.psO[0]
                for nt in range(2):
                    nk = NKC[nt]
                    S = self.psS[self.iS % 3]; self.iS += 1
                    P = pc[h][nt]

                    def mm():
                        nc.tensor.matmul(S[0:nk, :], lhsT=kcT[:, nt * 128:nt * 128 + nk], rhs=(qown[:, q0:q0 + 512] if h == 3 else qo[:, h, :]), start=True, stop=False)
                        return nc.tensor.matmul(S[0:nk, :], lhsT=self.identB[:, 0:nk], rhs=bci[:, nt, h, :], start=False, stop=True)
                    k.op("pe", mm, reads=[kcT, qown, qo, self.identB, bci], writes=[S])
                    if nk < 128:
                        k.op("dve", lambda: nc.vector.memset(P[:], 0.0), writes=[P])
                    k.op("act", lambda: nc.scalar.activation(out=P[0:nk, :], in_=S[0:nk, :], func=AF.Exp), reads=[S], writes=[P])
                    k.op("pe", lambda: nc.tensor.matmul(L[:], lhsT=self.onesB[0:nk, :], rhs=P[0:nk, :], start=(nt == 0), stop=(nt == 1)),
                         reads=[self.onesB, P], writes=[L])
                    if own:
                        k.op("pe", lambda: nc.tensor.matmul(O[:], lhsT=vc[0:nk, nt, :], rhs=P[0:nk, :], start=(nt == 0), stop=(nt == 1)),
                             reads=[vc, P], writes=[O])
                k.op("dve", lambda: nc.vector.tensor_scalar(out=Rh[h][:], in0=L[:], scalar1=1e-30, scalar2=None, op0=ALU.max),
                     reads=[L], writes=[Rh[h]])
                k.op("dve", lambda: nc.vector.reciprocal(out=Rh[h][:], in_=Rh[h][:]), reads=[Rh[h]], writes=[Rh[h]])
                if own:
                    k.op("dve", lambda: nc.vector.tensor_tensor(out=Wt[:], in0=Rh[h][:], in1=gbi[:, 0, :], op=ALU.mult), reads=[Rh[h], gbi], writes=[Wt])
                    k.op("dve", lambda: nc.vector.tensor_tensor(out=ac[:], in0=O[:], in1=Wt[:], op=ALU.mult), reads=[O, Wt], writes=[ac])
                for nt in range(2):
                    k.op("dve", lambda: nc.vector.tensor_tensor(out=pc[h][nt][:], in0=pc[h][nt][:], in1=Rh[h][:], op=ALU.mult),
                         reads=[pc[h][nt], Rh[h]], writes=[pc[h][nt]])
            px = self.psX
            for jq in range(4):
                def mmi():
                    ins = None
                    n = 0
                    for h in range(4):
                        for nt in range(2):
                            ins = nc.tensor.matmul(px[:, jq * 64:(jq + 1) * 64], lhsT=pc[h][nt][:, jq * 128:(jq + 1) * 128], rhs=ov[:, nt, :],
                                                   start=(n == 0), stop=(n == 7))
                            n += 1
                    return ins
                k.op("pe", mmi, reads=[ov] + [pc[h][nt] for h in range(4) for nt in range(2)], writes=[(px, jq)])
            k.op("dve", lambda: nc.vector.tensor_tensor(out=impt[:].rearrange("p a b -> p (a b)"), in0=px[:, 0:256],
                                                        in1=keep[:].rearrange("p a b -> p (a b)"), op=ALU.mult),
                 reads=[px, keep], writes=[impt])
            k.op("dve", lambda: nc.vector.tensor_tensor(out=impt[:], in0=impt[:], in1=addt[:], op=ALU.add),
                 reads=[impt, addt], writes=[impt])
            pt = self.psL[0]
            for jq in range(4):
                k.op("dve", lambda: nc.vector.max(out=m8[:], in_=impt[:, jq, :]), reads=[impt], writes=[m8])
                k.op("dve", lambda: nc.vector.match_replace(out=wrk[:], in_to_replace=m8[:], in_values=impt[:, jq, :], imm_value=-1e30),
                     reads=[m8, impt], writes=[wrk])
                k.op("dve", lambda: nc.vector.max(out=m8[:], in_=wrk[:]), reads=[wrk], writes=[m8])
                k.op("dve", lambda: nc.vector.tensor_reduce(out=thr[:], in_=m8[:], axis=AX.X, op=ALU.min), reads=[m8], writes=[thr])
                k.op("dve", lambda: nc.vector.tensor_scalar(out=selb[:], in0=impt[:, jq, :], scalar1=thr[:, 0:1], scalar2=NEG,
                                                            op0=ALU.is_lt, op1=ALU.mult), reads=[impt, thr], writes=[selb])
                k.op("pe", lambda: nc.tensor.matmul(pt[0:64, jq * 128:(jq + 1) * 128], lhsT=selb[:], rhs=self.identF[:], start=True, stop=True),
                     reads=[selb, self.identF], writes=[(pt, jq)])
            k.op("dve", lambda: nc.vector.tensor_copy(selT[:], pt[0:64, :]), reads=[pt], writes=[selT])
            O = self.psO[1]; L = self.psL[1]
            nkt = 4 * i + 4
            for kt in range(nkt):
                idx = min(4 * i - kt + 3, 11)
                extra = [(lambda S: S[:], self.identB[:], tbs[:, idx, :]),
                         (lambda S: S[:], Eb[:, kt * 128:(kt + 1) * 128], selT[:])]
                self._xr = [self.identB, tbs, Eb, selT]
                self.attn_unit(ksb, kt, qown, q0, extra, None, [], vsb, vsb[:, kt * 128:(kt + 1) * 128], O, L, kt == 0, kt == nkt - 1)
            self.flush_attn()
            self.nsa_fin(O, L, gbi, 1, ac, Wt)
            O = self.psO[0]; L = self.psL[0]
            kts = list(range(max(0, 4 * i - 4), 4 * i + 4))
            for n, kt in enumerate(kts):
                idx = 4 * i - kt + 3
                extra = [(lambda S: S[:], self.identB[:], tbw[:, idx, :])]
                self._xr = [self.identB, tbw]
                self.attn_unit(kwb, kt, qown, q0, extra, None, [], vwb, vwb[:, kt * 128:(kt + 1) * 128], O, L, n == 0, n == len(kts) - 1)
            self.flush_attn()
            self.nsa_fin(O, L, gbi, 2, ac, Wt)
            k.dma("sp", yout.h[:, q0:q0 + 512], ac[:], reads=[ac], writes=[(yout, i)])

    def nsa_fin(self, O, L, gbi, gi, ac, Wt):
        k, nc = self.k, self.nc
        t2 = self.tmpA[0]
        k.op("dve", lambda: nc.vector.reciprocal(out=Wt[:], in_=L[:]), reads=[L], writes=[Wt])
        k.op("dve", lambda: nc.vector.tensor_tensor(out=Wt[:], in0=Wt[:], in1=gbi[:, gi, :], op=ALU.mult), reads=[Wt, gbi], writes=[Wt])
        k.op("dve", lambda: nc.vector.tensor_tensor(out=t2[:], in0=O[:], in1=Wt[:], op=ALU.mult), reads=[O, Wt], writes=[t2])
        k.op("dve", lambda: nc.vector.tensor_tensor(out=ac[:], in0=ac[:], in1=t2[:], op=ALU.add), reads=[ac, t2], writes=[ac])


    def gdn(self, yout):
        k, nc = self.k, self.nc
        L_ = "%d" % self.l
        dcw = k.din("gdn_cw" + L_, [128, 3, 4])
        dal = k.din("gdn_alog" + L_, [128, 1]); ddt = k.din("gdn_dtb" + L_, [128, 1]); dng = k.din("gdn_ng" + L_, [128, 1])
        dct = k.din("c_ct", [128, 128]); dsc = k.din("c_sc", [128, 128]); dh0 = k.din("c_h0", [128, 128]); dh1 = k.din("c_h1", [128, 128])
        dmst = k.din("c_mst", [128, 128]); dmit = k.din("c_mit", [128, 128]); dmsn = k.din("c_msn", [128, 128]); dcm = k.din("c_cm", [128, 2])
        B = self.big
        raw, W_, tA, tB = B[0], B[1], B[2], B[3]
        qs = ks = vs = oT = W_
        arow = k.sb([1, T_], F32, "g_arow")
        qnb, knb, vsb = self.bigb[0], self.bigb[1], self.bigb[2]
        cw = k.sb([128, 3, 4], F32, "g_cw"); self.ld(cw, dcw)
        cst = {}
        for nm, dd in (("ct", dct), ("sc", dsc), ("h0", dh0), ("h1", dh1), ("mst", dmst), ("mit", dmit), ("msn", dmsn)):
            cst[nm] = k.sb([128, 128], F32, "g_" + nm); self.ld(cst[nm], dd)
        cm = k.sb([128, 2], F32, "g_cm"); self.ld(cm, dcm)
        al = k.sb([128, 1], F32, "g_al"); dtb = k.sb([128, 1], F32, "g_dtb"); ng = k.sb([128, 1], F32, "g_ng")
        self.ld(al, dal); self.ld(dtb, ddt); self.ld(ng, dng)
        epsc = k.sb([128, 1], F32, "g_eps")
        k.op("dve", lambda: nc.vector.memset(epsc[:], EPS), writes=[epsc])
        for wi, (src, dst) in enumerate((("gdn_q", qs), ("gdn_k", ks), ("gdn_v", vs))):
            self.zfm(raw, raw.h, _OFF[src], 0, stage=tA)
            k.op("dve", lambda: nc.vector.tensor_scalar(out=dst[:], in0=raw[:], scalar1=cw[:, wi, 3:4], scalar2=None, op0=ALU.mult),
                 reads=[raw, cw], writes=[dst])
            for i in range(3):
                sh = 3 - i
                k.op("dve", lambda: nc.vector.scalar_tensor_tensor(out=dst[:, sh:], in0=raw[:, 0:T_ - sh], scalar=cw[:, wi, i:i + 1],
                                                                   in1=dst[:, sh:], op0=ALU.mult, op1=ALU.add), reads=[raw, cw, dst], writes=[dst])
            k.op("act", lambda: nc.scalar.activation(out=dst[:], in_=dst[:], func=AF.Silu), reads=[dst], writes=[dst])
            if wi == 2:
                k.op("act", lambda: nc.scalar.copy(out=vsb[:], in_=vs[:]), reads=[vs], writes=[vsb])
                continue
            src, dstb, sc = ((qs, qnb, SCALE), (ks, knb, 1.0))[wi]
            if True:
                k.op("dve", lambda: nc.vector.tensor_tensor(out=tA[:], in0=src[:], in1=src[:], op=ALU.mult), reads=[src], writes=[tA])
                for tb in range(8):
                    ts = slice(tb * 512, (tb + 1) * 512)
                    ps = self.psS[tb % 3]
                    k.op("pe", lambda: nc.tensor.matmul(ps[:], lhsT=self.onesF[:], rhs=tA[:, ts], start=True, stop=True), reads=[self.onesF, tA], writes=[ps])
                    k.op("act", lambda: nc.scalar.activation(out=tB[:, ts], in_=ps[:], func=AF.Ln, bias=epsc[:, 0:1]), reads=[ps, epsc], writes=[(tB, tb)])
                    k.op("act", lambda: nc.scalar.activation(out=tB[:, ts], in_=tB[:, ts], func=AF.Exp, scale=-0.5), reads=[(tB, tb)], writes=[(tB, tb)])
                k.op("dve", lambda: nc.vector.scalar_tensor_tensor(out=dstb[:], in0=src[:], scalar=sc, in1=tB[:], op0=ALU.mult, op1=ALU.mult),
                     reads=[src, tB], writes=[dstb])
        def col(nm):
            return k.sb([128, NT], F32, "g_c_" + nm)
        g = col("g"); beta = col("beta"); gc = col("gc"); ngc = col("ngc"); gl = col("gl"); wcol = col("w")
        skbg = col("skbg"); skd = [col("skd0"), col("skd1")]; egl = [col("egl0"), col("egl1")]; tmpc = col("tmp")
        self.zfm(arow, arow.h, _OFF["gdn_a"], 0, n=1, mul=1, stage=tB)
        self.row2col(arow, g)
        self.zfm(arow, arow.h, _OFF["gdn_b"], 0, n=1, mul=1, stage=tB)
        self.row2col(arow, beta)
        k.op("act", lambda: nc.scalar.activation(out=g[:], in_=g[:], func=AF.Exp, bias=dtb[:, 0:1]), reads=[g, dtb], writes=[g])
        k.op("act", lambda: nc.scalar.activation(out=g[:], in_=g[:], func=AF.Ln, bias=self.onesF[:, 0:1]), reads=[g, self.onesF], writes=[g])
        k.op("act", lambda: nc.scalar.activation(out=al[:], in_=al[:], func=AF.Exp), reads=[al], writes=[al])
        k.op("dve", lambda: nc.vector.tensor_scalar(out=g[:], in0=g[:], scalar1=al[:, 0:1], scalar2=-1.0, op0=ALU.mult, op1=ALU.mult),
             reads=[g, al], writes=[g])
        k.op("act", lambda: nc.scalar.activation(out=beta[:], in_=beta[:], func=AF.Sigmoid), reads=[beta], writes=[beta])
        px = self.psX

        def colmm(lhs, dst, func=None):
            k.op("pe", lambda: nc.tensor.matmul(px[:, 0:NT], lhsT=lhs[:], rhs=g[:], start=True, stop=True), reads=[lhs, g], writes=[px])
            if func is None:
                k.op("dve", lambda: nc.vector.tensor_copy(dst[:], px[:, 0:NT]), reads=[px], writes=[dst])
            else:
                k.op("act", lambda: nc.scalar.activation(out=dst[:], in_=px[:, 0:NT], func=func), reads=[px], writes=[dst])
        colmm(cst["ct"], gc)
        colmm(cst["sc"], gl)
        colmm(cst["h0"], egl[0], AF.Exp)
        colmm(cst["h1"], egl[1], AF.Exp)
        k.op("dve", lambda: nc.vector.tensor_scalar(out=ngc[:], in0=gc[:], scalar1=-1.0, scalar2=None, op0=ALU.mult), reads=[gc], writes=[ngc])
        k.op("act", lambda: nc.scalar.activation(out=wcol[:], in_=beta[:], func=AF.Ln), reads=[beta], writes=[wcol])
        k.op("dve", lambda: nc.vector.tensor_tensor(out=wcol[:], in0=wcol[:], in1=gc[:], op=ALU.add), reads=[wcol, gc], writes=[wcol])
        k.op("act", lambda: nc.scalar.activation(out=skbg[:], in_=gc[:], func=AF.Exp), reads=[gc], writes=[skbg])
        k.op("dve", lambda: nc.vector.tensor_tensor(out=skbg[:], in0=skbg[:], in1=beta[:], op=ALU.mult), reads=[skbg, beta], writes=[skbg])
        k.op("dve", lambda: nc.vector.tensor_tensor(out=tmpc[:], in0=gl[:], in1=gc[:], op=ALU.subtract), reads=[gl, gc], writes=[tmpc])
        k.op("act", lambda: nc.scalar.activation(out=tmpc[:], in_=tmpc[:], func=AF.Exp), reads=[tmpc], writes=[tmpc])
        for c in range(2):
            k.op("dve", lambda: nc.vector.tensor_scalar(out=skd[c][:], in0=tmpc[:], scalar1=cm[:, c:c + 1], scalar2=None, op0=ALU.mult),
                 reads=[tmpc, cm], writes=[skd[c]])
        S = k.sb([128, 128], F32, "g_S"); Sb = k.sb([128, 128], BF16, "g_Sb")
        k.op("dve", lambda: nc.vector.memset(S[:], 0.0), writes=[S])
        k.op("dve", lambda: nc.vector.memset(Sb[:], 0.0), writes=[Sb])

        def t128(nm, dt=F32):
            return k.sb([128, 128], dt, "g_t_" + nm)
        kbg = t128("kbg", BF16); kd = [t128("kd0", BF16), t128("kd1", BF16)]; vb = t128("vb", BF16)
        dgw = t128("dgw"); dgg = t128("dgg"); dgn = t128("dgn")
        Gs = t128("G"); Y = t128("Y"); X = t128("X"); Pm = t128("P"); Z = t128("Z"); ZT = t128("ZT"); Z2 = t128("Z2"); ZT2 = t128("ZT2")
        E1 = t128("E1"); qkT = t128("qkT", BF16); qgT = t128("qgT", BF16); PTb = t128("PTb", BF16); nWT = t128("nWT", BF16)
        vnb = t128("vnb", BF16)
        pool6 = [self.psS[0], self.psS[1], self.psS[2], self.psO[1], self.psL[0], self.psL[1]]
        ctr = [0]

        def pp():
            ctr[0] += 1
            return pool6[ctr[0] % 6]
        pOg = self.psO[0]
        for t in range(NT):
            cs = slice(t * 128, (t + 1) * 128)
            tc_ = slice(t, t + 1)
            pa = pp()
            k.op("pe", lambda: nc.tensor.matmul(pa[:, 0:128], lhsT=knb[:, cs], rhs=self.identB[:], start=True, stop=True), reads=[knb, self.identB], writes=[(pa, 0)])
            k.op("pe", lambda: nc.tensor.matmul(pa[:, 128:256], lhsT=vsb[:, cs], rhs=self.identB[:], start=True, stop=True), reads=[vsb, self.identB], writes=[(pa, 1)])
            k.op("dve", lambda: nc.vector.tensor_scalar(out=kbg[:], in0=pa[:, 0:128], scalar1=skbg[:, tc_], scalar2=None, op0=ALU.mult), reads=[(pa, 0), skbg], writes=[kbg])
            for c in range(2):
                k.op("dve", lambda: nc.vector.tensor_scalar(out=kd[c][:], in0=pa[:, 0:128], scalar1=skd[c][:, tc_], scalar2=None, op0=ALU.mult),
                     reads=[(pa, 0), skd[c]], writes=[kd[c]])
            k.op("dve", lambda: nc.vector.tensor_scalar(out=vb[:], in0=pa[:, 128:256], scalar1=beta[:, tc_], scalar2=None, op0=ALU.mult), reads=[(pa, 1), beta], writes=[vb])
            k.op("dve", lambda: nc.vector.tensor_scalar(out=dgw[:], in0=self.identF[:], scalar1=wcol[:, tc_], scalar2=None, op0=ALU.mult), reads=[self.identF, wcol], writes=[dgw])
            k.op("dve", lambda: nc.vector.tensor_scalar(out=dgg[:], in0=self.identF[:], scalar1=gc[:, tc_], scalar2=None, op0=ALU.mult), reads=[self.identF, gc], writes=[dgg])
            k.op("dve", lambda: nc.vector.tensor_scalar(out=dgn[:], in0=self.identF[:], scalar1=ngc[:, tc_], scalar2=None, op0=ALU.mult), reads=[self.identF, ngc], writes=[dgn])
            pg = pp()
            k.op("pe", lambda: nc.tensor.matmul(pg[:, 0:128], lhsT=knb[:, cs], rhs=knb[:, cs], start=True, stop=True), reads=[knb], writes=[(pg, 0)])
            k.op("pe", lambda: nc.tensor.matmul(pg[:, 128:256], lhsT=knb[:, cs], rhs=qnb[:, cs], start=True, stop=True), reads=[knb, qnb], writes=[(pg, 1)])
            k.op("dve", lambda: nc.vector.tensor_copy(Gs[:], pg[:, 0:128]), reads=[(pg, 0)], writes=[Gs])

            def expmat(diag, mask, bias_col, bias_t, dst_fn):
                pe_ = pp()

                def mm():
                    ins0 = nc.tensor.matmul(pe_[:, 0:128], lhsT=self.onesF[:], rhs=diag[:], start=True, stop=(mask is None))
                    if mask is None:
                        return ins0
                    return nc.tensor.matmul(pe_[:, 0:128], lhsT=self.identF[:], rhs=mask[:], start=False, stop=True)
                k.op("pe", mm, reads=[self.onesF, diag, self.identF] + ([mask] if mask is not None else []), writes=[pe_])
                if bias_col is None:
                    k.op("act", lambda: nc.scalar.activation(out=E1[:], in_=pe_[:, 0:128], func=AF.Exp), reads=[pe_], writes=[E1])
                else:
                    k.op("act", lambda: nc.scalar.activation(out=E1[:], in_=pe_[:, 0:128], func=AF.Exp, bias=bias_col[:, tc_]),
                         reads=[pe_, bias_t], writes=[E1])
                dst_fn()
            expmat(dgw, cst["mst"], ngc, ngc, lambda: k.op("dve", lambda: nc.vector.tensor_tensor(out=Y[:], in0=Gs[:], in1=E1[:], op=ALU.mult), reads=[Gs, E1], writes=[Y]))
            expmat(dgn, cst["msn"], wcol, wcol, lambda: k.op("dve", lambda: nc.vector.tensor_tensor(out=X[:], in0=Gs[:], in1=E1[:], op=ALU.mult), reads=[Gs, E1], writes=[X]))
            expmat(dgg, cst["mit"], ngc, ngc, lambda: k.op("dve", lambda: nc.vector.tensor_tensor(out=qkT[:], in0=pg[:, 128:256], in1=E1[:], op=ALU.mult), reads=[(pg, 1), E1], writes=[qkT]))
            expmat(dgg, None, None, None, lambda: k.op("dve", lambda: nc.vector.tensor_tensor(out=qgT[:], in0=qnb[:, cs], in1=E1[:], op=ALU.mult), reads=[qnb, E1], writes=[qgT]))
            k.op("dve", lambda: nc.vector.tensor_tensor(out=Pm[:], in0=self.identF[:], in1=Y[:], op=ALU.subtract), reads=[self.identF, Y], writes=[Pm])
            p1 = pp(); p2 = pp()
            k.op("pe", lambda: nc.tensor.matmul(p1[:, 0:128], lhsT=X[:], rhs=Y[:], start=True, stop=True), reads=[X, Y], writes=[p1])
            k.op("pe", lambda: nc.tensor.matmul(p2[:, 0:128], lhsT=Y[:], rhs=X[:], start=True, stop=True), reads=[X, Y], writes=[p2])
            zc, ztc, zn, ztn = Z, ZT, Z2, ZT2
            k.op("dve", lambda: nc.vector.tensor_copy(zc[:], p1[:, 0:128]), reads=[p1], writes=[zc])
            k.op("act", lambda: nc.scalar.copy(out=ztc[:], in_=p2[:, 0:128]), reads=[p2], writes=[ztc])
            for it in range(5):
                p3 = pp()
                k.op("pe", lambda: nc.tensor.matmul(p3[:, 0:128], lhsT=ztc[:], rhs=Pm[:], start=True, stop=True), reads=[ztc, Pm], writes=[p3])
                if it < 4:
                    p1 = pp(); p2 = pp()
                    k.op("pe", lambda: nc.tensor.matmul(p1[:, 0:128], lhsT=ztc[:], rhs=zc[:], start=True, stop=True), reads=[ztc, zc], writes=[p1])
                    k.op("pe", lambda: nc.tensor.matmul(p2[:, 0:128], lhsT=zc[:], rhs=ztc[:], start=True, stop=True), reads=[ztc, zc], writes=[p2])
                k.op("dve", lambda: nc.vector.tensor_tensor(out=Pm[:], in0=Pm[:], in1=p3[:, 0:128], op=ALU.add), reads=[Pm, p3], writes=[Pm])
                if it < 4:
                    k.op("dve", lambda: nc.vector.tensor_copy(zn[:], p1[:, 0:128]), reads=[p1], writes=[zn])
                    k.op("act", lambda: nc.scalar.copy(out=ztn[:], in_=p2[:, 0:128]), reads=[p2], writes=[ztn])
                    zc, ztc, zn, ztn = zn, ztn, zc, ztc
            k.op("act", lambda: nc.scalar.copy(out=PTb[:], in_=Pm[:]), reads=[Pm], writes=[PTb])
            pw = pp()
            k.op("pe", lambda: nc.tensor.matmul(pw[:, 0:128], lhsT=kbg[:], rhs=PTb[:], start=True, stop=True), reads=[kbg, PTb], writes=[pw])
            k.op("dve", lambda: nc.vector.tensor_scalar(out=nWT[:], in0=pw[:, 0:128], scalar1=-1.0, scalar2=None, op0=ALU.mult), reads=[pw], writes=[nWT])
            for c in range(2):
                ccs = slice(64 * c, 64 * c + 64)
                pv = pp()

                def mmv():
                    nc.tensor.matmul(pv[:, 0:128], lhsT=PTb[:], rhs=vb[:], start=True, stop=False)
                    return nc.tensor.matmul(pv[:, 0:128], lhsT=nWT[:], rhs=Sb[:], start=False, stop=True)
                k.op("pe", mmv, reads=[PTb, vb, nWT, Sb], writes=[pv])
                k.op("act", lambda: nc.scalar.copy(out=vnb[:], in_=pv[:, 0:128]), reads=[pv], writes=[vnb])

                def mmo():
                    nc.tensor.matmul(pOg[:, ccs], lhsT=Sb[:], rhs=qgT[:, ccs], start=True, stop=False)
                    return nc.tensor.matmul(pOg[:, ccs], lhsT=vnb[:], rhs=qkT[:, ccs], start=False, stop=True)
                k.op("pe", mmo, reads=[Sb, qgT, vnb, qkT], writes=[(pOg, c)])
                pu = pp()
                k.op("pe", lambda: nc.tensor.matmul(pu[:, 0:128], lhsT=kd[c][:], rhs=vnb[:], start=True, stop=True), reads=[kd[c], vnb], writes=[pu])
                k.op("dve", lambda: nc.vector.scalar_tensor_tensor(out=S[:], in0=S[:], scalar=egl[c][:, tc_], in1=pu[:, 0:128], op0=ALU.mult, op1=ALU.add),
                     reads=[S, egl[c], pu], writes=[S])
                k.op("act", lambda: nc.scalar.copy(out=Sb[:], in_=S[:]), reads=[S], writes=[Sb])
            k.op("dve", lambda: nc.vector.tensor_copy(oT[:, cs], pOg[:, 0:128]), reads=[pOg], writes=[(oT, t)])
        self.zfm(raw, raw.h, _OFF["gdn_z"], 0, stage=tA)
        k.op("act", lambda: nc.scalar.activation(out=raw[:], in_=raw[:], func=AF.Silu), reads=[raw], writes=[raw])
        k.op("dve", lambda: nc.vector.tensor_tensor(out=tA[:], in0=oT[:], in1=oT[:], op=ALU.mult), reads=[oT], writes=[tA])
        k.op("dve", lambda: nc.vector.tensor_scalar(out=tA[:], in0=tA[:], scalar1=1.0 / 128, scalar2=None, op0=ALU.mult), reads=[tA], writes=[tA])
        for tb in range(8):
            ts = slice(tb * 512, (tb + 1) * 512)
            ps = self.psS[tb % 3]
            k.op("pe", lambda: nc.tensor.matmul(ps[:], lhsT=self.onesF[:], rhs=tA[:, ts], start=True, stop=True), reads=[self.onesF, tA], writes=[ps])
            k.op("act", lambda: nc.scalar.activation(out=tB[:, ts], in_=ps[:], func=AF.Ln, bias=epsc[:, 0:1]), reads=[ps, epsc], writes=[(tB, tb)])
            k.op("act", lambda: nc.scalar.activation(out=tB[:, ts], in_=tB[:, ts], func=AF.Exp, scale=-0.5), reads=[(tB, tb)], writes=[(tB, tb)])
        k.op("dve", lambda: nc.vector.scalar_tensor_tensor(out=oT[:], in0=oT[:], scalar=ng[:, 0:1], in1=tB[:], op0=ALU.mult, op1=ALU.mult),
             reads=[oT, ng, tB], writes=[oT])
        k.op("dve", lambda: nc.vector.tensor_tensor(out=oT[:], in0=oT[:], in1=raw[:], op=ALU.mult), reads=[oT, raw], writes=[oT])
        k.dma("sp", yout.h, oT[:], reads=[oT], writes=[yout])


class Gathered:
    def __init__(self, nc, name, nrows, ncols, CR, dt=F32):
        self.nrows, self.ncols, self.CR = nrows, ncols, CR
        self.chunks = []
        r = 0
        while r < nrows:
            cr = min(CR, nrows - r)
            self.chunks.append((r, cr, T(nc.dram_tensor("%s_c%d" % (name, len(self.chunks)), [4 * cr, ncols], dt).ap(), "%s_c%d" % (name, len(self.chunks)))))
            r += cr

    def pieces(self, s_, r0, n):
        out = []
        r = r0
        while r < r0 + n:
            ci = r // self.CR
            c0, cr, t = self.chunks[ci]
            cnt = min(r0 + n, c0 + cr) - r
            out.append((t, t.h[s_ * cr + (r - c0):s_ * cr + (r - c0) + cnt, :], r - r0, cnt))
            r += cnt
        return out


def exchange(k, src, dst, sem):
    nc = k.nc
    k.barrier()
    sems = []
    for (c0, cr, t) in dst.chunks:
        cs = sem.enter_context(nc.semaphore("cc_%s" % t.name))
        sems.append(cs)
        nc.gpsimd.collective_compute("AllGather", ALU.bypass, replica_groups=[[0, 1, 2, 3], [4, 5, 6, 7]],
                                     ins=[src.h[c0:c0 + cr, :].opt()], outs=[t.h.opt()]).then_inc(cs)
    for cs in sems:
        for e in k.eng.values():
            e.wait_ge(cs, 1)


NBIG = {"fox": (2, 4), "lru": (6, 1), "gdn": (4, 3), "nsa": (0, 0)}


def build_fused(dbg=False):
    nc = bass.Bass("TRN2", target_bir_lowering=False)
    with ExitStack() as es:
        k = K(nc, es)
        x = k.din("x", [TOK, D])
        out = k.dout("out", [TOK, D])
        h = k.sb([128, NCH, TOK], F32, "h")
        vals = None
        zsh = [T(nc.dram_tensor("zsh%d" % l, [DIN, TOK], BF16).ap(), "zsh%d" % l) for l in range(2)]
        zall = [Gathered(nc, "zall%d" % l, DIN, TOK, 512, BF16) for l in range(2)]
        ysh = [T(nc.dram_tensor("ysh%d" % l, [4 * 128, T_], F32).ap(), "ysh%d" % l) for l in range(2)]
        yall = [Gathered(nc, "yall%d" % l, 4 * 128, T_, 64) for l in range(2)]
        csem = [es, es, es, es]
        with k.scope():
            load_x(k, h, x)
        for l in range(2):
            with k.scope():
                dense_A(k, h, l, zsh[l], zall[l])
            for gi, nm in enumerate(("fox", "gdn", "lru", "nsa")):
                with k.scope():
                    m = Mix(k, l, zall[l], vals, NBIG[nm][0], NBIG[nm][1])
                    yout = T(ysh[l].h[gi * 128:(gi + 1) * 128, :], "y_%s%d" % (nm, l))
                    getattr(m, nm)(yout)
                    for (c0, cr, ct) in yall[l].chunks[2 * gi:2 * gi + 2]:
                        k.allgather(ysh[l].h[c0:c0 + cr, :], ct, [yout])
            with k.scope():
                dense_C(k, h, l, yall[l], None)
            if dbg and l == 0:
                dh = k.dout("dbg_h", [D, TOK])
                v = dh.h.rearrange("(c p) t -> p c t", p=128)
                for c in range(0, NCH, 4):
                    k.dma("sp", v[:, c:c + 4, :], h[:, c:c + 4, :], reads=[h], writes=[(dh, c)])
        with k.scope():
            final_out(k, h, out)
        k.wait_all("sp")
    return nc


def tm_tiles(a):
    t, d = a.shape
    return np.ascontiguousarray(a.reshape(t // 128, 128, d).transpose(1, 0, 2))


def col_tiles(v):
    return np.ascontiguousarray(v.reshape(-1, 128).T)


_OFF = {}
_o = 0
for _n, _w in (("fox_q", 512), ("fox_k", 512), ("fox_v", 512), ("fox_f", 4), ("gdn_q", 512), ("gdn_k", 512), ("gdn_v", 512),
               ("gdn_a", 4), ("gdn_b", 4), ("gdn_z", 512), ("lru_x", 512), ("lru_gate", 512), ("nsa_q", 512), ("nsa_kc", 128),
               ("nsa_vc", 128), ("nsa_ks", 128), ("nsa_vs", 128), ("nsa_kw", 128), ("nsa_vw", 128), ("nsa_g", 12)):
    _OFF[_n] = _o
    _o += _w


def consts_B():
    c = {}
    c["c_ident"] = np.eye(128, dtype=np.float32)
    p = np.arange(128)
    c["c_ut"] = (p[:, None] <= p[None, :]).astype(np.float32)
    q = np.arange(32)
    c["c_su"] = (q[:, None] < q[None, :]).astype(np.float32)
    col = np.arange(512)
    mb = np.zeros((128, 4, 512), np.float32)
    for m in range(4):
        mb[:, m, :] = np.where(p[:, None] + 128 * m <= col[None, :], 0.0, NEG)
    c["c_mbfox"] = mb
    ch = p // 64
    same = ch[:, None] == ch[None, :]
    c["c_ct"] = (same & (p[:, None] <= p[None, :])).astype(np.float32)
    c["c_sc"] = same.astype(np.float32)
    c["c_h0"] = np.broadcast_to((p < 64)[:, None], (128, 128)).astype(np.float32).copy()
    c["c_h1"] = np.broadcast_to((p >= 64)[:, None], (128, 128)).astype(np.float32).copy()
    c["c_mst"] = np.where(same & (p[None, :] > p[:, None]), 0.0, NEG).astype(np.float32)
    c["c_mit"] = np.where(same & (p[None, :] >= p[:, None]), 0.0, NEG).astype(np.float32)
    c["c_msn"] = np.where(same & (p[:, None] > p[None, :]), 0.0, NEG).astype(np.float32)
    c["c_cm"] = np.stack([(p < 64), (p >= 64)], axis=1).astype(np.float32)
    return c


def prep_mix(inp, l, j):
    L_ = "%d" % l
    m = {}
    hs = slice(j * 128, (j + 1) * 128)
    m["lru_cw" + L_] = np.ascontiguousarray(inp["lru_conv_w"][l][:, hs].T)
    m["lru_cb" + L_] = np.ascontiguousarray(inp["lru_conv_b"][l][hs].reshape(128, 1))
    for nm, src in (("lru_wa", "lru_w_a"), ("lru_wx", "lru_w_x")):
        bd = np.zeros((128, 128), np.float32)
        bd[0:64, 0:64] = inp[src][l][2 * j]
        bd[64:128, 64:128] = inp[src][l][2 * j + 1]
        m[nm + L_] = bd
    m["lru_ba" + L_] = np.ascontiguousarray(inp["lru_b_a"][l][hs].reshape(128, 1))
    m["lru_bx" + L_] = np.ascontiguousarray(inp["lru_b_x"][l][hs].reshape(128, 1))
    m["lru_lam" + L_] = np.ascontiguousarray(inp["lru_lambda"][l][hs].reshape(128, 1))
    cwf = inp["gdn_conv_w"][l]
    m["gdn_cw" + L_] = np.ascontiguousarray(np.stack([cwf[:, g0 * 512:(g0 + 1) * 512][:, hs].T for g0 in range(3)], axis=1))
    m["gdn_alog" + L_] = np.full((128, 1), inp["gdn_a_log"][l][j], np.float32)
    m["gdn_dtb" + L_] = np.full((128, 1), inp["gdn_dt_bias"][l][j], np.float32)
    m["gdn_ng" + L_] = np.ascontiguousarray(inp["gdn_norm_g"][l].reshape(128, 1))
    for nm, src in (("nsa_w1k", "nsa_w1_k"), ("nsa_w1v", "nsa_w1_v")):
        m[nm + L_] = np.ascontiguousarray(inp[src][l].reshape(32, 128, 128).transpose(1, 0, 2))
    m["nsa_w2k" + L_] = inp["nsa_w2_k"][l]; m["nsa_w2v" + L_] = inp["nsa_w2_v"][l]
    m["nsa_pek" + L_] = np.ascontiguousarray(inp["nsa_pe_k"][l].T); m["nsa_pev" + L_] = np.ascontiguousarray(inp["nsa_pe_v"][l].T)
    return m


def prep_shared(inp):
    m = dict(consts_B())
    for l in range(2):
        L_ = "%d" % l
        binp = np.zeros(NZC * 128, np.float32)
        binp[:DIN] = inp["b_in"][l]
        ong = np.ones((D,), np.float32)
        ong[0:512] = inp["out_norm_g"][l][0]
        ong[1024:1536] = inp["out_norm_g"][l][1]
        ong[1536:2048] = inp["out_norm_g"][l][2]
        m.update({"g1a" + L_: col16(inp["ffn1_norm_g"][l]), "g2a" + L_: col16(inp["mix_norm_g"][l]),
                  "f1wg" + L_: inp["ffn1_w_gate"][l], "f1wu" + L_: inp["ffn1_w_up"][l], "f1wd" + L_: inp["ffn1_w_down"][l],
                  "win" + L_: inp["w_in"][l], "bin" + L_: np.ascontiguousarray(binp.reshape(NZC, 128).T),
                  "ong" + L_: col16(ong), "wout" + L_: inp["w_out"][l], "g1c" + L_: col16(inp["ffn2_norm_g"][l]),
                  "f2wg" + L_: inp["ffn2_w_gate"][l], "f2wu" + L_: inp["ffn2_w_up"][l], "f2wd" + L_: inp["ffn2_w_down"][l]})
    m["gf"] = col16(inp["final_norm_g"])
    nsc = nsa_static()
    m["nsa_keep"] = nsc["keep"]; m["nsa_add"] = nsc["add"]; m["nsa_E"] = nsc["E"]; m["nsa_ov"] = nsc["ov"]
    return m


def prep_core(inp, core):
    j = core % 4
    m = {}
    for l in range(2):
        m.update(prep_mix(inp, l, j))
    order = [(j + 1 + hh) % 4 for hh in range(4)]
    rb = np.asarray(inp["rel_bias"], np.float32)
    nsc = nsa_static()
    bcs = np.where(nsc["bc_mask"][:, :, None, :], rb[nsc["bc_idx"]][..., order].transpose(0, 1, 3, 2), NEG)
    m["nsa_bc"] = np.ascontiguousarray(bcs.astype(np.float32))
    m["nsa_tbs"] = np.where(nsc["tbs_mask"], rb[nsc["tbs_idx"], j], NEG).astype(np.float32)
    m["nsa_tbw"] = np.where(nsc["tbw_mask"], rb[nsc["tbw_idx"], j], NEG).astype(np.float32)
    mk = np.zeros((128, 16), np.float32)
    for hh in range(4):
        mk[:, 4 * hh + (j + hh) % 4] = 1.0
    m["msk"] = mk
    return m


_PROG = {}


def run_fused(inputs, dbg=False):
    inp = {k_: np.asarray(v, np.float32) for k_, v in inputs.items()}
    x = inp["x"].reshape(8 * TOK, D)
    key = "dbg" if dbg else "main"
    if key not in _PROG:
        _PROG[key] = build_fused(dbg)
    nc = _PROG[key]
    shared = prep_shared(inp)
    maps = []
    for c in range(8):
        m = dict(shared)
        m.update(prep_core(inp, c))
        m["x"] = np.ascontiguousarray(x[c * TOK:(c + 1) * TOK])
        maps.append(m)
    res = run_bass_kernel_spmd(nc, maps, core_ids=list(range(8)))
    return res.results


def kernel(**inputs):
    res = run_fused(inputs)
    out = np.concatenate([r["out"] for r in res], axis=0).reshape(2, T_, D)
    return np.ascontiguousarray(out.astype(np.float32))


_NSC = {}


def t5_bucket_static(dist):
    import math
    import jax
    import jax.numpy as jnp
    with jax.default_device(jax.devices("cpu")[0]):
        n = jnp.maximum(jnp.asarray(dist, jnp.int32), 0)
        nf = jnp.maximum(n, 1).astype(jnp.float32)
        large = 16 + (jnp.log(nf / 16) / math.log(1024 / 16) * (32 - 16)).astype(jnp.int32)
        large = jnp.minimum(large, 31)
        return np.asarray(jnp.where(n < 16, n, large))


def nsa_static():
    if _NSC:
        return _NSC
    p = np.arange(128)
    col = np.arange(512)
    q = np.arange(T_)
    n = (np.arange(2)[:, None] * 128 + p[None, :])
    d = q[None, None, :] - (16 * n[:, :, None] + 31)
    msk = (d >= 0) & (n[:, :, None] < 255)
    _NSC["bc_idx"] = t5_bucket_static(d).transpose(1, 0, 2)
    _NSC["bc_mask"] = msk.transpose(1, 0, 2)
    ms = np.arange(12) - 3
    d = 128 * ms[None, :, None] + col[None, None, :] - p[:, None, None]
    _NSC["tbs_idx"] = t5_bucket_static(d); _NSC["tbs_mask"] = d >= 0
    mw = np.arange(8) - 3
    d = 128 * mw[None, :, None] + col[None, None, :] - p[:, None, None]
    _NSC["tbw_idx"] = t5_bucket_static(d); _NSC["tbw_mask"] = (d >= 0) & (d < 512)
    qpos = np.arange(NT)[None, :, None] * 128 + p[:, None, None]
    cur = qpos // 64
    jj = np.arange(64)[None, None, :]
    forced = (jj == 0) | (jj == cur) | (jj == cur - 1)
    fut = jj > cur
    _NSC["keep"] = np.where(forced | fut, 0.0, 1.0).astype(np.float32)
    _NSC["add"] = np.where(fut, -1.0, np.where(forced, 1.0e6, 0.0)).astype(np.float32)
    _NSC["E"] = (np.arange(T_)[None, :] // 64 == np.arange(64)[:, None]).astype(np.float32)
    nn = np.arange(256)
    cst, cen = nn * 16, nn * 16 + 31
    sst, sen = np.arange(64) * 64, np.arange(64) * 64 + 63
    ovl = ((cst[:, None] <= sen[None, :]) & (cen[:, None] >= sst[None, :]) & (nn[:, None] < 255)).astype(np.float32)
    _NSC["ov"] = np.ascontiguousarray(ovl.reshape(2, 128, 64).transpose(1, 0, 2))
    return _NSC
```

```python
import numpy as np
import ml_dtypes
from contextlib import ExitStack
import concourse.bass as bass
import concourse.mybir as mybir
from concourse.bass_utils import run_bass_kernel_spmd

F32 = mybir.dt.float32
BF16 = mybir.dt.bfloat16
I32 = mybir.dt.int32
SP_POOL = [mybir.EngineType.SP, mybir.EngineType.Pool]
AF = mybir.ActivationFunctionType
ALU = mybir.AluOpType
AX = mybir.AxisListType

D = 2048
NCH = 16
DFF = 5632
NF = 44
TOK = 1024
TG = 512
DIN = 5912
NZC = 47
EPS = 1e-6
NEG = -30000.0


class St:
    __slots__ = ("w", "r")

    def __init__(self, w=None, r=None):
        self.w = w
        self.r = list(r) if r else []

    def copy(self):
        return St(self.w, self.r)


class T:
    def __init__(self, handle, name):
        self.h = handle
        self.name = name
        self.whole = St()
        self.cells = {}

    def __getitem__(self, idx):
        return self.h[idx]

    def states(self, key):
        if key is None:
            return [self.whole] + list(self.cells.values())
        if key not in self.cells:
            self.cells[key] = self.whole.copy()
        return [self.cells[key]]


class K:
    NSLOT = 6

    def __init__(self, nc, es):
        self.nc = nc
        self.es = es
        self.eng = {"pe": nc.tensor, "dve": nc.vector, "act": nc.scalar, "pool": nc.gpsimd, "sp": nc.sync}
        self.sem = {}
        self.cnt = {}
        for e in self.eng:
            self.sem[e] = es.enter_context(nc.semaphore("s_" + e))
            self.cnt[e] = 0
        self.known = {e: {} for e in self.eng}
        self.dsem = {}
        self.duse = {}
        self.dnext = {}
        for q in ("sp", "pool"):
            self.dnext[q] = 0
            for s in range(self.NSLOT):
                key = ("d", q, s)
                self.dsem[key] = es.enter_context(nc.semaphore("d_%s_%d" % (q, s)))
                self.duse[key] = 0
        self.ntile = 0
        self.dins = {}
        self.es_root = es
        self.ncc = 0

    def sb(self, shape, dt, name=None):
        self.ntile += 1
        name = "%s_%d" % (name or "t", self.ntile)
        h = self.es.enter_context(self.nc.sbuf_tensor(name, list(shape), dt))
        return T(h, name)

    def ps(self, shape, dt, name=None):
        self.ntile += 1
        name = "%s_%d" % (name or "p", self.ntile)
        h = self.es.enter_context(self.nc.psum_tensor(name, list(shape), dt))
        return T(h, name)

    def din(self, name, shape, dt=F32):
        if name not in self.dins:
            self.dins[name] = T(self.nc.dram_tensor(name, list(shape), dt, kind="ExternalInput").ap(), name)
        return self.dins[name]

    def barrier(self):
        for e in self.eng:
            self.wait_all(e)

    def scope(self):
        return _Scope(self)

    def dout(self, name, shape, dt=F32):
        return T(self.nc.dram_tensor(name, list(shape), dt, kind="ExternalOutput").ap(), name)

    def semh(self, key):
        return self.sem[key] if key in self.sem else self.dsem[key]

    def _deps(self, reads, writes):
        deps = set()
        for (t, key) in reads:
            for st in t.states(key):
                if st.w is not None:
                    deps.add(st.w)
        for (t, key) in writes:
            for st in t.states(key):
                if st.w is not None:
                    deps.add(st.w)
                deps.update(st.r)
        return deps

    def _wait(self, eng, deps):
        need = {}
        kn = self.known[eng]
        for (sk, val) in deps:
            if sk == "pe" and eng == "pe":
                continue
            if kn.get(sk, 0) < val and need.get(sk, 0) < val:
                need[sk] = val
        for sk, val in need.items():
            self.eng[eng].wait_ge(self.semh(sk), val)
            kn[sk] = val

    def _commit(self, ev, reads, writes):
        for (t, key) in reads:
            for st in t.states(key):
                st.r.append(ev)
        for (t, key) in writes:
            if key is None:
                t.cells.clear()
                t.whole.w = ev
                t.whole.r = []
            else:
                st = t.states(key)[0]
                st.w = ev
                st.r = []

    @staticmethod
    def _norm(lst):
        out = []
        for x in lst or []:
            out.append(x if isinstance(x, tuple) else (x, None))
        return out

    def op(self, eng, fn, reads=None, writes=None):
        reads = self._norm(reads)
        writes = self._norm(writes)
        self._wait(eng, self._deps(reads, writes))
        ins = fn()
        self.cnt[eng] += 1
        ins.then_inc(self.sem[eng], 1)
        ev = (eng, self.cnt[eng])
        self._commit(ev, reads, writes)
        return ev

    def dma(self, q, out_ap, in_ap, reads=None, writes=None, **kw):
        reads = self._norm(reads)
        writes = self._norm(writes)
        deps = self._deps(reads, writes)
        s = self.dnext[q]
        self.dnext[q] = (s + 1) % self.NSLOT
        key = ("d", q, s)
        if self.duse[key] > 0:
            deps.add((key, 16 * self.duse[key]))
        self._wait(q, deps)
        ins = self.eng[q].dma_start(out=out_ap, in_=in_ap, **kw)
        self.duse[key] += 1
        ins.then_inc(self.dsem[key], 16)
        ev = (key, 16 * self.duse[key])
        self._commit(ev, reads, writes)
        return ev

    def allgather(self, src_ap, dst_t, reads):
        reads = self._norm(reads)
        writes = [(dst_t, None)]
        self._wait("pool", self._deps(reads, writes))
        cs = self.es_root.enter_context(self.nc.semaphore("cc%d" % self.ncc))
        key = ("c", self.ncc)
        self.ncc += 1
        self.dsem[key] = cs
        self.nc.gpsimd.collective_compute("AllGather", ALU.bypass, replica_groups=[[0, 1, 2, 3], [4, 5, 6, 7]],
                                          ins=[src_ap.opt()], outs=[dst_t.h.opt()]).then_inc(cs)
        ev = (key, 1)
        self._commit(ev, reads, writes)
        return ev

    def wait_all(self, eng="sp"):
        kn = self.known[eng]
        for e in self.eng:
            if self.cnt[e] > kn.get(e, 0):
                self.eng[eng].wait_ge(self.sem[e], self.cnt[e])
                kn[e] = self.cnt[e]
        for key, n in self.duse.items():
            if 16 * n > kn.get(key, 0):
                self.eng[eng].wait_ge(self.dsem[key], 16 * n)
                kn[key] = 16 * n


class _Scope:
    def __init__(self, k):
        self.k = k

    def __enter__(self):
        self.old = self.k.es
        self.sub = ExitStack()
        self.sub.__enter__()
        self.k.es = self.sub
        return self

    def __exit__(self, *a):
        self.k.barrier()
        self.k.es = self.old
        return self.sub.__exit__(*a)


class Dense:
    def __init__(self, k, h):
        self.k = k
        nc = k.nc
        self.nc = nc
        self.h = h
        self.hn = k.sb([128, NCH, TOK], BF16, "hn")
        self.sq = [k.sb([128, TG], BF16, "sq%d" % i) for i in range(2)]
        self.rstd = k.sb([128, TG], F32, "rstd")
        self.onesm = k.sb([128, 128], BF16, "onesm")
        self.ones4 = k.sb([128, 128], BF16, "ones4")
        self.gcol = k.sb([128, NCH], F32, "gcol")
        self.wg = [k.sb([128, NCH, 256], BF16, "wg%d" % i) for i in range(2)]
        self.wu = [k.sb([128, NCH, 256], BF16, "wu%d" % i) for i in range(2)]
        self.wd = [k.sb([128, 2, D], BF16, "wd%d" % i) for i in range(2)]
        self.act = [k.sb([128, 2, TOK], BF16, "act%d" % i) for i in range(2)]
        self.sg = [k.sb([128, TG], F32, "sg%d" % i) for i in range(2)]
        self.psA = [k.ps([128, TG], F32, "psA%d" % i) for i in range(4)]
        self.psB = [k.ps([128, TG], F32, "psB%d" % i) for i in range(3)]
        self.psN = k.ps([128, TG], F32, "psN")
        self.ia = 0
        self.ib = 0
        self.epsc = k.sb([128, 1], F32, "epsc")
        k.op("dve", lambda: nc.vector.memset(self.epsc[:], EPS), writes=[self.epsc])
        k.op("dve", lambda: nc.vector.memset(self.onesm[:], 1.0 / D), writes=[self.onesm])
        k.op("dve", lambda: nc.vector.memset(self.ones4[:], 1.0 / 512), writes=[self.ones4])

    def rstd_from(self, ps):
        k, nc = self.k, self.nc
        k.op("act", lambda: nc.scalar.activation(out=self.rstd[:], in_=ps[:], func=AF.Ln, bias=self.epsc[:]),
             reads=[ps, self.epsc], writes=[self.rstd])
        k.op("act", lambda: nc.scalar.activation(out=self.rstd[:], in_=self.rstd[:], func=AF.Exp, scale=-0.5),
             reads=[self.rstd], writes=[self.rstd])

    def load_h(self, src):
        k = self.k
        v = src.h.rearrange("(c p) t -> p c t", p=128)
        for c in range(0, NCH, 4):
            k.dma("sp", self.h[:, c:c + 4, :], v[:, c:c + 4, :], reads=[src],
                  writes=[(self.h, (cc, tg)) for cc in range(c, c + 4) for tg in range(2)])

    def store_h(self, dst):
        k = self.k
        v = dst.h.rearrange("(c p) t -> p c t", p=128)
        for c in range(0, NCH, 4):
            k.dma("sp", v[:, c:c + 4, :], self.h[:, c:c + 4, :],
                  reads=[(self.h, (cc, tg)) for cc in range(c, c + 4) for tg in range(2)], writes=[(dst, c)])

    def rmsnorm(self, gsrc, out_t=None, out_f32=None):
        k, nc = self.k, self.nc
        k.dma("sp", self.gcol[:], gsrc.h, reads=[gsrc], writes=[self.gcol])
        for tg in range(2):
            ts = slice(tg * TG, (tg + 1) * TG)
            for c in range(NCH):
                sq = self.sq[c % 2]
                k.op("act", lambda sq=sq, c=c: nc.scalar.activation(out=sq[:], in_=self.h[:, c, ts], func=AF.Square),
                     reads=[(self.h, (c, tg))], writes=[sq])
                k.op("pe", lambda sq=sq, c=c: nc.tensor.matmul(self.psN[:], lhsT=self.onesm[:], rhs=sq[:],
                                                               start=(c == 0), stop=(c == NCH - 1)),
                     reads=[sq, self.onesm], writes=[self.psN])
            self.rstd_from(self.psN)
            for c in range(NCH):
                if out_f32 is None:
                    k.op("dve", lambda c=c: nc.vector.scalar_tensor_tensor(
                        out=self.hn[:, c, ts], in0=self.h[:, c, ts], scalar=self.gcol[:, c:c + 1], in1=self.rstd[:],
                        op0=ALU.mult, op1=ALU.mult),
                        reads=[(self.h, (c, tg)), self.gcol, self.rstd], writes=[(self.hn, tg)])
                else:
                    k.op("dve", lambda c=c: nc.vector.scalar_tensor_tensor(
                        out=out_f32[:, c, ts], in0=self.h[:, c, ts], scalar=self.gcol[:, c:c + 1], in1=self.rstd[:],
                        op0=ALU.mult, op1=ALU.mult),
                        reads=[(self.h, (c, tg)), self.gcol, self.rstd], writes=[(out_f32, (c, tg))])

    def ffn(self, wg_d, wu_d, wd_d):
        k, nc = self.k, self.nc
        wgv = wg_d.h.rearrange("(c p) f -> p c f", p=128)
        wuv = wu_d.h.rearrange("(c p) f -> p c f", p=128)
        wdv = wd_d.h.rearrange("(c p) d -> p c d", p=128)
        NG = NF // 2

        def load(g):
            b = g % 2
            k.dma("pool", self.wg[b][:], wgv[:, :, g * 256:(g + 1) * 256], reads=[wg_d], writes=[self.wg[b]])
            k.dma("pool", self.wu[b][:], wuv[:, :, g * 256:(g + 1) * 256], reads=[wu_d], writes=[self.wu[b]])
            k.dma("pool", self.wd[b][:], wdv[:, 2 * g:2 * g + 2, :], reads=[wd_d], writes=[self.wd[b]])

        load(0)
        for g in range(NG):
            if g + 1 < NG:
                load(g + 1)
            b = g % 2
            wg, wu, wd, act = self.wg[b], self.wu[b], self.wd[b], self.act[b]
            for fcl in range(2):
                fs = slice(fcl * 128, (fcl + 1) * 128)
                for tg in range(2):
                    ts = slice(tg * TG, (tg + 1) * TG)
                    pg = self.psA[self.ia % 4]
                    pu = self.psA[(self.ia + 1) % 4]
                    self.ia += 2

                    def mm(p, w):
                        ins = None
                        for c in range(NCH):
                            ins = nc.tensor.matmul(p[:], lhsT=w[:, c, fs], rhs=self.hn[:, c, ts],
                                                   start=(c == 0), stop=(c == NCH - 1))
                        return ins
                    k.op("pe", lambda: mm(pg, wg), reads=[wg, (self.hn, tg)], writes=[pg])
                    k.op("pe", lambda: mm(pu, wu), reads=[wu, (self.hn, tg)], writes=[pu])
                    sg = self.sg[tg]
                    k.op("act", lambda: nc.scalar.activation(out=sg[:], in_=pg[:], func=AF.Silu), reads=[pg], writes=[sg])
                    k.op("dve", lambda: nc.vector.tensor_tensor(out=act[:, fcl, ts], in0=pu[:], in1=sg[:], op=ALU.mult),
                         reads=[pu, sg], writes=[(act, (fcl, tg))])
            for dc in range(NCH):
                ds = slice(dc * 128, (dc + 1) * 128)
                for tg in range(2):
                    ts = slice(tg * TG, (tg + 1) * TG)
                    pd = self.psB[self.ib % 3]
                    self.ib += 1

                    def mmd():
                        ins = None
                        for fcl in range(2):
                            ins = nc.tensor.matmul(pd[:], lhsT=wd[:, fcl, ds], rhs=act[:, fcl, ts],
                                                   start=(fcl == 0), stop=(fcl == 1))
                        return ins
                    k.op("pe", mmd, reads=[wd, (act, (0, tg)), (act, (1, tg))], writes=[pd])
                    k.op("dve", lambda: nc.vector.scalar_tensor_tensor(
                        out=self.h[:, dc, ts], in0=pd[:], scalar=0.5, in1=self.h[:, dc, ts], op0=ALU.mult, op1=ALU.add),
                        reads=[pd, (self.h, (dc, tg))], writes=[(self.h, (dc, tg))])

    def proj(self, w_d, ncols, rhs_t, emit):
        k, nc = self.k, self.nc
        wv = w_d.h.rearrange("(c p) f -> p c f", p=128)
        ngr = (ncols + 255) // 256

        def load(g):
            b = g % 2
            c0 = g * 256
            c1 = min(ncols, c0 + 256)
            k.dma("pool", self.wg[b][:, :, 0:c1 - c0], wv[:, :, c0:c1], reads=[w_d], writes=[self.wg[b]])

        load(0)
        for g in range(ngr):
            if g + 1 < ngr:
                load(g + 1)
            w = self.wg[g % 2]
            for ml in range(2):
                m = 2 * g + ml
                M = min(128, ncols - m * 128)
                if M <= 0:
                    continue
                for tg in range(2):
                    ts = slice(tg * TG, (tg + 1) * TG)
                    pd = self.psB[self.ib % 3]
                    self.ib += 1

                    def mm():
                        ins = None
                        for c in range(NCH):
                            ins = nc.tensor.matmul(pd[0:M, :], lhsT=w[:, c, ml * 128:ml * 128 + M], rhs=rhs_t[:, c, ts],
                                                   start=(c == 0), stop=(c == NCH - 1))
                        return ins
                    k.op("pe", mm, reads=[w, (rhs_t, tg)], writes=[pd])
                    emit(m, M, tg, ts, pd)


def dense_A(k, h, l, zsh, zall):
    nc = k.nc
    g1 = k.din("g1a%d" % l, [128, NCH]); g2 = k.din("g2a%d" % l, [128, NCH])
    wg = k.din("f1wg%d" % l, [D, DFF]); wu = k.din("f1wu%d" % l, [D, DFF]); wd = k.din("f1wd%d" % l, [DFF, D])
    win = k.din("win%d" % l, [D, DIN]); bin_ = k.din("bin%d" % l, [128, NZC])
    dn = Dense(k, h)
    bcol = k.sb([128, NZC], F32, "bcol")
    zs = [k.sb([128, TG], BF16, "zs%d" % i) for i in range(3)]
    k.dma("sp", bcol[:], bin_.h, reads=[bin_], writes=[bcol])
    dn.rmsnorm(g1)
    dn.ffn(wg, wu, wd)
    dn.rmsnorm(g2)
    cnt = [0]

    def emit(m, M, tg, ts, pd):
        z = zs[cnt[0] % 3]
        cnt[0] += 1
        k.op("act", lambda: nc.scalar.activation(out=z[0:M, :], in_=pd[0:M, :], func=AF.Identity,
                                                 bias=bcol[0:M, m:m + 1]), reads=[pd, bcol], writes=[z])
        k.dma("sp", zsh.h[m * 128:m * 128 + M, ts], z[0:M, :], reads=[z], writes=[(zsh, (m, tg))])
        if tg == 1 and (m % 4 == 3 or m == NZC - 1):
            c0, cr, ct = zall.chunks[m // 4]
            k.allgather(zsh.h[c0:c0 + cr, :], ct, [(zsh, (mm_, t_)) for mm_ in range(4 * (m // 4), 4 * (m // 4) + 4) for t_ in (0, 1) if mm_ < NZC])
    assert zall.CR == 512
    dn.proj(win, DIN, dn.hn, emit)


def dense_C(k, h, l, yall, rv):
    nc = k.nc
    ong = k.din("ong%d" % l, [128, NCH]); wout = k.din("wout%d" % l, [D, D]); g1 = k.din("g1c%d" % l, [128, NCH])
    wg = k.din("f2wg%d" % l, [D, DFF]); wu = k.din("f2wu%d" % l, [D, DFF]); wd = k.din("f2wd%d" % l, [DFF, D])
    dn = Dense(k, h)
    ys = [k.sb([128, 4, TG], F32, "ys%d" % i) for i in range(2)]
    ocol = k.sb([128, NCH], F32, "ocol")
    k.dma("sp", ocol[:], ong.h, reads=[ong], writes=[ocol])
    yst = [k.sb([128, 4, TG], F32, "yst%d" % i) for i in range(2)]
    mskc = k.sb([128, 16], F32, "mskc")
    dmsk = k.din("msk", [128, 16])
    k.dma("sp", mskc[:], dmsk.h, reads=[dmsk], writes=[mskc])
    i = 0
    for grp in range(4):
        for tg in range(2):
            ts = slice(tg * TG, (tg + 1) * TG)
            y = ys[i % 2]
            i += 1
            for rc in range(4):
                st_ = yst[rc % 2]
                for jp in range(4):
                    for (ct, sap_, d0, cnt) in yall.pieces(jp, grp * 128, 128):
                        k.dma("sp", st_[d0:d0 + cnt, jp, :], sap_[:, rc * TOK + tg * TG:rc * TOK + (tg + 1) * TG], reads=[ct], writes=[st_])
                mc = mskc[:, rc:rc + 1]
                if rc == 0:
                    k.op("dve", lambda: nc.vector.tensor_scalar(out=y[:], in0=st_[:], scalar1=mc, scalar2=None, op0=ALU.mult), reads=[st_, mskc], writes=[y])
                else:
                    k.op("dve", lambda: nc.vector.scalar_tensor_tensor(out=y[:], in0=st_[:], scalar=mc, in1=y[:], op0=ALU.mult, op1=ALU.add),
                         reads=[st_, mskc, y], writes=[y])
            if grp == 1:
                for cl in range(4):
                    k.op("dve", lambda cl=cl: nc.vector.tensor_copy(out=dn.hn[:, 4 + cl, ts], in_=y[:, cl, :]),
                         reads=[y], writes=[(dn.hn, tg)])
                continue
            for cl in range(4):
                sq = dn.sq[cl % 2]
                k.op("act", lambda sq=sq, cl=cl: nc.scalar.activation(out=sq[:], in_=y[:, cl, :], func=AF.Square),
                     reads=[y], writes=[sq])
                k.op("pe", lambda sq=sq, cl=cl: nc.tensor.matmul(dn.psN[:], lhsT=dn.ones4[:], rhs=sq[:],
                                                                 start=(cl == 0), stop=(cl == 3)),
                     reads=[sq, dn.ones4], writes=[dn.psN])
            dn.rstd_from(dn.psN)
            for cl in range(4):
                c = grp * 4 + cl
                k.op("dve", lambda cl=cl, c=c: nc.vector.scalar_tensor_tensor(
                    out=dn.hn[:, c, ts], in0=y[:, cl, :], scalar=ocol[:, c:c + 1], in1=dn.rstd[:],
                    op0=ALU.mult, op1=ALU.mult), reads=[y, ocol, dn.rstd], writes=[(dn.hn, tg)])

    def emit(m, M, tg, ts, pd):
        k.op("dve", lambda: nc.vector.tensor_tensor(out=h[:, m, ts], in0=pd[:], in1=h[:, m, ts], op=ALU.add),
             reads=[pd, (h, (m, tg))], writes=[(h, (m, tg))])
    dn.proj(wout, D, dn.hn, emit)
    dn.rmsnorm(g1)
    dn.ffn(wg, wu, wd)


def final_out(k, h, out):
    nc = k.nc
    sq_ = [k.sb([128, TG], BF16, "sq%d" % i) for i in range(2)]
    rstd = k.sb([128, TG], F32, "rstd")
    onesm = k.sb([128, 128], BF16, "onesm")
    gcol = k.sb([128, NCH], F32, "gcol")
    epsc = k.sb([128, 1], F32, "epsc")
    psN = k.ps([128, TG], F32, "psN")
    psB = [k.ps([128, TG], F32, "psB%d" % i) for i in range(3)]
    ib = [0]
    k.op("dve", lambda: nc.vector.memset(epsc[:], EPS), writes=[epsc])
    k.op("dve", lambda: nc.vector.memset(onesm[:], 1.0 / D), writes=[onesm])

    def rstd_from():
        k.op("act", lambda: nc.scalar.activation(out=rstd[:], in_=psN[:], func=AF.Ln, bias=epsc[:]), reads=[psN, epsc], writes=[rstd])
        k.op("act", lambda: nc.scalar.activation(out=rstd[:], in_=rstd[:], func=AF.Exp, scale=-0.5), reads=[rstd], writes=[rstd])
    gf = k.din("gf", [128, NCH])
    identF = k.sb([128, 128], F32, "identF")
    cid = k.din("c_ident", [128, 128])
    k.dma("sp", identF[:], cid.h, reads=[cid], writes=[identF])
    fin = k.sb([128, NCH, TG], F32, "fin")
    ot = [k.sb([128, D], F32, "ot%d" % i) for i in range(2)]
    k.dma("sp", gcol[:], gf.h, reads=[gf], writes=[gcol])
    for tg in range(2):
        ts = slice(tg * TG, (tg + 1) * TG)
        for c in range(NCH):
            sq = sq_[c % 2]
            k.op("act", lambda sq=sq, c=c: nc.scalar.activation(out=sq[:], in_=h[:, c, ts], func=AF.Square),
                 reads=[(h, (c, tg))], writes=[sq])
            k.op("pe", lambda sq=sq, c=c: nc.tensor.matmul(psN[:], lhsT=onesm[:], rhs=sq[:],
                                                           start=(c == 0), stop=(c == NCH - 1)),
                 reads=[sq, onesm], writes=[psN])
        rstd_from()
        for c in range(NCH):
            k.op("dve", lambda c=c: nc.vector.scalar_tensor_tensor(
                out=fin[:, c, :], in0=h[:, c, ts], scalar=gcol[:, c:c + 1], in1=rstd[:],
                op0=ALU.mult, op1=ALU.mult), reads=[(h, (c, tg)), gcol, rstd], writes=[(fin, c)])
        for tt in range(4):
            o = ot[tt % 2]
            for c4 in range(4):
                pd = psB[ib[0] % 3]
                ib[0] += 1

                def mmT():
                    ins = None
                    for cl in range(4):
                        c = c4 * 4 + cl
                        ins = nc.tensor.matmul(pd[:, cl * 128:(cl + 1) * 128], lhsT=fin[:, c, tt * 128:(tt + 1) * 128],
                                               rhs=identF[:], start=True, stop=True)
                    return ins
                k.op("pe", mmT, reads=[identF] + [(fin, c4 * 4 + cl) for cl in range(4)], writes=[pd])
                k.op("act", lambda: nc.scalar.copy(out=o[:, c4 * 512:(c4 + 1) * 512], in_=pd[:]), reads=[pd], writes=[(o, c4)])
            r0 = tg * TG + tt * 128
            k.dma("sp", out.h[r0:r0 + 128, :], o[:], reads=[o], writes=[(out, r0)])


def load_x(k, h, x):
    nc = k.nc
    identF = k.sb([128, 128], F32, "identF")
    cid = k.din("c_ident", [128, 128])
    k.dma("sp", identF[:], cid.h, reads=[cid], writes=[identF])
    xt = [k.sb([128, D], F32, "xt%d" % i) for i in range(4)]
    pst = [k.ps([128, TG], F32, "pst%d" % i) for i in range(4)]
    n = 0
    for tg in range(2):
        for tt in range(4):
            r0 = tg * TG + tt * 128
            k.dma("sp", xt[tt][:], x.h[r0:r0 + 128, :], reads=[x], writes=[xt[tt]])
        for c in range(NCH):
            pd = pst[n % 4]
            n += 1

            def mmT():
                ins = None
                for tt in range(4):
                    ins = nc.tensor.matmul(pd[:, tt * 128:(tt + 1) * 128], lhsT=xt[tt][:, c * 128:(c + 1) * 128], rhs=identF[:],
                                           start=True, stop=True)
                return ins
            k.op("pe", mmT, reads=[identF] + xt, writes=[pd])
            eng = "act" if c % 2 else "dve"
            if eng == "act":
                k.op("act", lambda: nc.scalar.copy(out=h[:, c, tg * TG:(tg + 1) * TG], in_=pd[:]), reads=[pd], writes=[(h, (c, tg))])
            else:
                k.op("dve", lambda: nc.vector.tensor_copy(h[:, c, tg * TG:(tg + 1) * TG], pd[:]), reads=[pd], writes=[(h, (c, tg))])


def col16(g):
    return np.ascontiguousarray(np.asarray(g, np.float32).reshape(NCH, 128).T)


T_ = 4096
NT = 32
SCALE = 128 ** -0.5


class Mix:
    def __init__(self, k, l, zall, vals, nbig=7, nbigb=4):
        self.k = k
        self.l = l
        self.zall = zall
        nc = self.nc = k.nc
        self.cident = k.din("c_ident", [128, 128])
        self.msk = k.sb([128, 16], F32, "msk")
        dmsk = k.din("msk", [128, 16])
        k.dma("sp", self.msk[:], dmsk.h, reads=[dmsk], writes=[self.msk])
        self.identF = k.sb([128, 128], F32, "identF")
        self.identB = k.sb([128, 128], BF16, "identB")
        self.onesF = k.sb([128, 128], F32, "onesF")
        self.onesB = k.sb([128, 128], BF16, "onesB")
        k.dma("sp", self.identF[:], self.cident.h, reads=[self.cident], writes=[self.identF])
        k.op("dve", lambda: nc.vector.tensor_copy(self.identB[:], self.identF[:]), reads=[self.identF], writes=[self.identB])
        k.op("dve", lambda: nc.vector.memset(self.onesF[:], 1.0), writes=[self.onesF])
        k.op("dve", lambda: nc.vector.memset(self.onesB[:], 1.0), writes=[self.onesB])
        self.big = [k.sb([128, T_], F32, "big%d" % i) for i in range(nbig)]
        self.bigb = [k.sb([128, T_], BF16, "bigb%d" % i) for i in range(nbigb)]
        self.psS = [k.ps([128, 512], F32, "psS%d" % i) for i in range(3)]
        self.psO = [k.ps([128, 512], F32, "psO%d" % i) for i in range(2)]
        self.psL = [k.ps([128, 512], F32, "psL%d" % i) for i in range(2)]
        self.psX = k.ps([128, 512], F32, "psX")
        self.iS = 0
        self.pT = [k.sb([128, 512], BF16, "pT%d" % i) for i in range(3)]
        self.tmpA = [k.sb([128, 512], F32, "tmpA%d" % i) for i in range(4)]

    def zfm(self, dst_t, dst_ap, off, dyn=None, q="sp", n=128, key=None, mul=128, stage=None):
        k, nc = self.k, self.nc
        dap = dst_ap[:] if not hasattr(dst_ap, "ap") else dst_ap
        if dyn is None:
            for s_ in range(4):
                for (ct, sap_, d0, cnt) in self.zall.pieces(s_, off, n):
                    k.dma(q, dap[d0:d0 + cnt, s_ * TOK:(s_ + 1) * TOK], sap_, reads=[ct], writes=[(dst_t, key)])
            return
        sap = stage[:]
        if sap.dtype != BF16:
            sap = sap.bitcast(BF16)[:, 0:T_]
        for jc in range(4):
            for s_ in range(4):
                for (ct, sap_, d0, cnt) in self.zall.pieces(s_, off + jc * mul, n):
                    k.dma(q, sap[d0:d0 + cnt, s_ * TOK:(s_ + 1) * TOK], sap_, reads=[ct], writes=[stage])
            mc = self.msk[0:n, dyn + jc:dyn + jc + 1]
            if jc == 0:
                k.op("dve", lambda: nc.vector.tensor_scalar(out=dap[0:n, :], in0=sap[0:n, :], scalar1=mc, scalar2=None, op0=ALU.mult),
                     reads=[stage, self.msk], writes=[(dst_t, key)])
            else:
                k.op("dve", lambda: nc.vector.scalar_tensor_tensor(out=dap[0:n, :], in0=sap[0:n, :], scalar=mc, in1=dap[0:n, :], op0=ALU.mult, op1=ALU.add),
                     reads=[stage, self.msk, (dst_t, key)], writes=[(dst_t, key)])

    def row2col(self, row, dst):
        k, nc = self.k, self.nc
        px = self.psX

        def mm():
            ins = None
            for kt in range(NT):
                ins = nc.tensor.matmul(px[:, kt:kt + 1], lhsT=row[0:1, kt * 128:(kt + 1) * 128], rhs=self.onesF[0:1, 0:1], start=True, stop=True)
            return ins
        k.op("pe", mm, reads=[row, self.onesF], writes=[px])
        k.op("dve", lambda: nc.vector.tensor_copy(dst[:], px[:, 0:NT]), reads=[px], writes=[dst])

    def fm2tm(self, src_ap, src_dep, dst_t, dst_ap, key=None):
        k, nc = self.k, self.nc
        for g4 in range(NT // 4):
            ps = self.psS[g4 % 3]

            def mm():
                ins = None
                for i in range(4):
                    kt = g4 * 4 + i
                    ins = nc.tensor.matmul(ps[:, i * 128:(i + 1) * 128], lhsT=src_ap[:, kt * 128:(kt + 1) * 128], rhs=self.identB[:], start=True, stop=True)
                return ins
            k.op("pe", mm, reads=[src_dep, self.identB], writes=[ps])
            if g4 % 2:
                k.op("act", lambda: nc.scalar.copy(out=dst_ap[:, g4 * 512:(g4 + 1) * 512], in_=ps[:]), reads=[ps], writes=[(dst_t, key)])
            else:
                k.op("dve", lambda: nc.vector.tensor_copy(dst_ap[:, g4 * 512:(g4 + 1) * 512], ps[:]), reads=[ps], writes=[(dst_t, key)])

    def ld(self, dst, src_t, src_ap=None, q="sp", dst_ap=None):
        self.k.dma(q, dst[:] if dst_ap is None else dst_ap, src_t.h if src_ap is None else src_ap, reads=[src_t], writes=[dst])

    def lru(self, yout):
        k, nc = self.k, self.nc
        L_ = "%d" % self.l
        cw = k.din("lru_cw" + L_, [128, 4]); cb = k.din("lru_cb" + L_, [128, 1])
        wa = k.din("lru_wa" + L_, [128, 128]); wx = k.din("lru_wx" + L_, [128, 128])
        ba = k.din("lru_ba" + L_, [128, 1]); bx = k.din("lru_bx" + L_, [128, 1]); lam = k.din("lru_lam" + L_, [128, 1])
        xs, xc, aa, uu, gg, tt = self.big[0:6]
        hs = gg
        xcb = self.bigb[0]
        cws = k.sb([128, 4], F32, "l_cw"); cbs = k.sb([128, 1], F32, "l_cb")
        was = k.sb([128, 128], BF16, "l_wa"); wxs = k.sb([128, 128], BF16, "l_wx")
        bas = k.sb([128, 1], F32, "l_ba"); bxs = k.sb([128, 1], F32, "l_bx"); lams = k.sb([128, 1], F32, "l_lam")
        nsp = k.sb([128, 1], F32, "l_nsp")
        self.zfm(xs, xs.h, _OFF["lru_x"], 0, stage=tt); self.zfm(gg, gg.h, _OFF["lru_gate"], 0, stage=tt); self.ld(cws, cw); self.ld(cbs, cb); self.ld(bas, ba); self.ld(bxs, bx); self.ld(lams, lam)
        self.ld(was, wa, q="pool"); self.ld(wxs, wx, q="pool")
        k.op("act", lambda: nc.scalar.activation(out=nsp[:], in_=lams[:], func=AF.Exp, scale=-1.0), reads=[lams], writes=[nsp])
        k.op("act", lambda: nc.scalar.activation(out=nsp[:], in_=nsp[:], func=AF.Ln, bias=self.onesF[:, 0:1]),
             reads=[nsp, self.onesF], writes=[nsp])
        k.op("dve", lambda: nc.vector.tensor_scalar(out=nsp[:], in0=nsp[:], scalar1=-8.0, scalar2=None, op0=ALU.mult),
             reads=[nsp], writes=[nsp])
        k.op("dve", lambda: nc.vector.tensor_scalar(out=xc[:], in0=xs[:], scalar1=cws[:, 3:4], scalar2=cbs[:, 0:1],
                                                    op0=ALU.mult, op1=ALU.add), reads=[xs, cws, cbs], writes=[xc])
        for i in range(3):
            sh = 3 - i
            k.op("dve", lambda i=i, sh=sh: nc.vector.scalar_tensor_tensor(
                out=xc[:, sh:], in0=xs[:, 0:T_ - sh], scalar=cws[:, i:i + 1], in1=xc[:, sh:], op0=ALU.mult, op1=ALU.add),
                reads=[xs, cws, xc], writes=[xc])
        k.op("act", lambda: nc.scalar.copy(out=xcb[:], in_=xc[:]), reads=[xc], writes=[xcb])
        for tb in range(8):
            ts = slice(tb * 512, (tb + 1) * 512)
            pr = self.psS[0]; pi = self.psS[1]
            k.op("pe", lambda: nc.tensor.matmul(pr[:], lhsT=was[:], rhs=xcb[:, ts], start=True, stop=True), reads=[was, xcb], writes=[pr])
            k.op("pe", lambda: nc.tensor.matmul(pi[:], lhsT=wxs[:], rhs=xcb[:, ts], start=True, stop=True), reads=[wxs, xcb], writes=[pi])
            r = self.tmpA[0]; ii = self.tmpA[1]
            k.op("act", lambda: nc.scalar.activation(out=r[:], in_=pr[:], func=AF.Sigmoid, bias=bas[:, 0:1]), reads=[pr, bas], writes=[r])
            k.op("act", lambda: nc.scalar.activation(out=ii[:], in_=pi[:], func=AF.Sigmoid, bias=bxs[:, 0:1]), reads=[pi, bxs], writes=[ii])
            k.op("act", lambda: nc.scalar.activation(out=aa[:, ts], in_=r[:], func=AF.Exp, scale=nsp[:, 0:1]),
                 reads=[r, nsp], writes=[(aa, tb)])
            t1 = self.tmpA[2]
            k.op("dve", lambda: nc.vector.tensor_tensor(out=t1[:], in0=aa[:, ts], in1=aa[:, ts], op=ALU.mult), reads=[(aa, tb)], writes=[t1])
            k.op("dve", lambda: nc.vector.tensor_scalar(out=t1[:], in0=t1[:], scalar1=-1.0, scalar2=1.0, op0=ALU.mult, op1=ALU.add),
                 reads=[t1], writes=[t1])
            k.op("act", lambda: nc.scalar.activation(out=t1[:], in_=t1[:], func=AF.Sqrt), reads=[t1], writes=[t1])
            k.op("dve", lambda: nc.vector.tensor_tensor(out=ii[:], in0=ii[:], in1=xc[:, ts], op=ALU.mult), reads=[ii, xc], writes=[ii])
            k.op("dve", lambda: nc.vector.tensor_tensor(out=uu[:, ts], in0=ii[:], in1=t1[:], op=ALU.mult), reads=[ii, t1], writes=[(uu, tb)])
        self.gelu_tanh(tt, gg, xs)
        k.op("dve", lambda: nc.vector.tensor_tensor_scan(out=hs[:], data0=aa[:], data1=uu[:], initial=0.0, op0=ALU.mult, op1=ALU.add),
             reads=[aa, uu], writes=[hs])
        k.op("dve", lambda: nc.vector.tensor_tensor(out=hs[:], in0=hs[:], in1=tt[:], op=ALU.mult), reads=[hs, tt], writes=[hs])
        k.dma("sp", yout.h, hs[:], reads=[hs], writes=[yout])

    def gelu_tanh(self, out, x, tmp):
        k, nc = self.k, self.nc
        k.op("dve", lambda: nc.vector.tensor_tensor(out=tmp[:], in0=x[:], in1=x[:], op=ALU.mult), reads=[x], writes=[tmp])
        k.op("dve", lambda: nc.vector.tensor_scalar(out=tmp[:], in0=tmp[:], scalar1=0.044715, scalar2=1.0, op0=ALU.mult, op1=ALU.add),
             reads=[tmp], writes=[tmp])
        k.op("dve", lambda: nc.vector.tensor_tensor(out=tmp[:], in0=tmp[:], in1=x[:], op=ALU.mult), reads=[tmp, x], writes=[tmp])
        k.op("act", lambda: nc.scalar.activation(out=tmp[:], in_=tmp[:], func=AF.Sigmoid, scale=1.5957691216057308),
             reads=[tmp], writes=[tmp])
        k.op("dve", lambda: nc.vector.tensor_tensor(out=out[:], in0=tmp[:], in1=x[:], op=ALU.mult), reads=[tmp, x], writes=[out])

    def attn_unit(self, kT, kt, qT, q0, extra, bias_ap, bias_reads, V, vslice, O, L, first, last, nkeys=128, kT_t=None, qT_t=None):
        k, nc = self.k, self.nc
        S = self.psS[self.iS % 3]
        P = self.pT[self.iS % 3]
        self.iS += 1
        nk = nkeys

        def mm():
            n = len(extra)
            ins = nc.tensor.matmul(S[0:nk, :], lhsT=kT[:, kt * 128:kt * 128 + nk], rhs=qT[:, q0:q0 + 512], start=True, stop=(n == 0))
            for i, (oa, l, r) in enumerate(extra):
                ins = nc.tensor.matmul(oa(S), lhsT=l, rhs=r, start=False, stop=(i == n - 1))
            return ins
        k.op("pe", mm, reads=[kT_t or kT, qT_t or qT] + self._xr, writes=[S])
        if bias_ap is None:
            k.op("act", lambda: nc.scalar.activation(out=P[0:nk, :], in_=S[0:nk, :], func=AF.Exp), reads=[S], writes=[P])
        else:
            k.op("act", lambda: nc.scalar.activation(out=P[0:nk, :], in_=S[0:nk, :], func=AF.Exp, bias=bias_ap),
                 reads=[S] + bias_reads, writes=[P])
        def pv():
            k.op("pe", lambda: nc.tensor.matmul(O[:], lhsT=vslice[0:nk], rhs=P[0:nk, :], start=first, stop=last), reads=[V, P], writes=[O])
            k.op("pe", lambda: nc.tensor.matmul(L[:], lhsT=self.onesB[0:nk, :], rhs=P[0:nk, :], start=first, stop=last),
                 reads=[self.onesB, P], writes=[L])
        pend = getattr(self, "_pend", None)
        self._pend = pv
        if pend is not None:
            pend()

    def flush_attn(self):
        pend = getattr(self, "_pend", None)
        self._pend = None
        if pend is not None:
            pend()

    def fox(self, yout):
        k, nc = self.k, self.nc
        cut = k.din("c_ut", [128, 128]); csu = k.din("c_su", [32, 32]); cmb = k.din("c_mbfox", [128, 4, 512])
        qf = self.big[0]
        qb, kb = self.bigb[0], self.bigb[1]
        vb = self.bigb[2]
        mb = k.sb([128, 4, 512], BF16, "f_mb")
        ut = k.sb([128, 128], F32, "f_ut"); su = k.sb([32, 32], F32, "f_su")
        lf = k.sb([128, NT], F32, "f_lf"); negc = k.sb([128, NT], F32, "f_negc")
        totc = k.sb([32, 1], F32, "f_totc"); am = k.sb([32, 128], F32, "f_am")
        dg = self.big[1]
        frow = k.sb([1, T_], F32, "f_row")
        vT = self.bigb[3]
        frow2 = k.sb([1, T_], F32, "f_row2")
        self.zfm(kb, kb.h, _OFF["fox_k"], 0, q="pool", stage=qb)
        self.zfm(vT, vT.h, _OFF["fox_v"], 0, q="pool", stage=qb)
        self.zfm(qf, qf.h, _OFF["fox_q"], 0, stage=dg)
        self.zfm(frow, frow.h, _OFF["fox_f"], 0, n=1, mul=1, stage=frow2)
        self.fm2tm(vT.h, vT, vb, vb.h)
        k.dma("pool", mb[:], cmb.h, reads=[cmb], writes=[mb])
        self.ld(ut, cut); self.ld(su, csu)
        self.row2col(frow, lf)
        k.op("dve", lambda: nc.vector.tensor_scalar(out=qb[:], in0=qf[:], scalar1=SCALE, scalar2=None, op0=ALU.mult), reads=[qf], writes=[qb])
        k.op("act", lambda: nc.scalar.activation(out=lf[:], in_=lf[:], func=AF.Exp, scale=-1.0), reads=[lf], writes=[lf])
        k.op("act", lambda: nc.scalar.activation(out=lf[:], in_=lf[:], func=AF.Ln, bias=self.onesF[:, 0:1]), reads=[lf, self.onesF], writes=[lf])
        px = self.psX
        k.op("pe", lambda: nc.tensor.matmul(px[0:32, 0:1], lhsT=lf[:], rhs=self.onesF[:, 0:1], start=True, stop=True),
             reads=[lf, self.onesF], writes=[px])
        k.op("dve", lambda: nc.vector.tensor_copy(totc[:], px[0:32, 0:1]), reads=[px], writes=[totc])
        k.op("dve", lambda: nc.vector.tensor_scalar(out=am[:], in0=self.onesF[0:32, :], scalar1=totc[:, 0:1], scalar2=None, op0=ALU.mult),
             reads=[self.onesF, totc], writes=[am])

        def mmc():
            nc.tensor.matmul(px[:, 0:NT], lhsT=ut[:], rhs=lf[:], start=True, stop=False)
            return nc.tensor.matmul(px[:, 0:NT], lhsT=am[:], rhs=su[:], start=False, stop=True)
        k.op("pe", mmc, reads=[ut, lf, am, su], writes=[px])
        k.op("dve", lambda: nc.vector.tensor_copy(negc[:], px[:, 0:NT]), reads=[px], writes=[negc])
        for qt in range(NT):
            k.op("dve", lambda qt=qt: nc.vector.tensor_scalar(out=dg[:, qt * 128:(qt + 1) * 128], in0=self.identF[:],
                                                              scalar1=negc[:, qt:qt + 1], scalar2=-1.0, op0=ALU.mult, op1=ALU.mult),
                 reads=[self.identF, negc], writes=[(dg, qt)])
        for i in range(8):
            q0 = 512 * i
            O = self.psO[i % 2]; L = self.psL[i % 2]
            nk = 4 * i + 4
            for kt in range(nk):
                extra = []
                for jq in range(4):
                    qt = 4 * i + jq
                    extra.append((lambda S, jq=jq: S[:, jq * 128:(jq + 1) * 128], self.onesF[:], dg[:, qt * 128:(qt + 1) * 128]))
                self._xr = [self.onesF] + [(dg, 4 * i + jq) for jq in range(4)]
                if kt >= 4 * i:
                    extra.append((lambda S: S[:], self.identB[:], mb[:, kt - 4 * i, :]))
                    self._xr += [self.identB, mb]
                self.attn_unit(kb, kt, qb, q0, extra, negc[:, kt:kt + 1], [negc], vb, vb[:, kt * 128:(kt + 1) * 128], O, L,
                               kt == 0, kt == nk - 1)
            self.flush_attn()
            R = self.tmpA[i % 2]; ob = self.tmpA[2 + i % 2]
            k.op("dve", lambda: nc.vector.reciprocal(out=R[:], in_=L[:]), reads=[L], writes=[R])
            k.op("dve", lambda: nc.vector.tensor_tensor(out=ob[:], in0=O[:], in1=R[:], op=ALU.mult), reads=[O, R], writes=[ob])
            k.dma("sp", yout.h[:, q0:q0 + 512], ob[:], reads=[ob], writes=[(yout, i)])


    def bfv(self, t):
        return t.h[:].bitcast(BF16)

    def nsa(self, yout):
        k, nc = self.k, self.nc
        L_ = "%d" % self.l
        dw1k = k.din("nsa_w1k" + L_, [128, 32, 128]); dw1v = k.din("nsa_w1v" + L_, [128, 32, 128])
        dw2k = k.din("nsa_w2k" + L_, [128, 128]); dw2v = k.din("nsa_w2v" + L_, [128, 128])
        dpek = k.din("nsa_pek" + L_, [128, 32]); dpev = k.din("nsa_pev" + L_, [128, 32])
        dbc = k.din("nsa_bc", [128, 2, 4, T_]); dtbs = k.din("nsa_tbs", [128, 12, 512]); dtbw = k.din("nsa_tbw", [128, 8, 512])
        dkeep = k.din("nsa_keep", [128, NT, 64]); dadd = k.din("nsa_add", [128, NT, 64])
        dE = k.din("nsa_E", [64, T_]); dov = k.din("nsa_ov", [128, 2, 64])
        kcT = k.sb([128, 256], BF16, "n_kcT"); vc = k.sb([128, 2, 128], BF16, "n_vc")
        qown = k.sb([128, T_], BF16, "n_qown")
        ksb = k.sb([128, T_], BF16, "n_ks"); kwb = k.sb([128, T_], BF16, "n_kw")
        vsb = k.sb([128, T_], BF16, "n_vs"); vwb = k.sb([128, T_], BF16, "n_vw")
        grow = k.sb([3, T_], F32, "n_grow")
        with k.scope():
            gstg = k.sb([3, T_], BF16, "n_gstg")
            self.zfm(grow, grow.h, _OFF["nsa_g"], 0, n=3, mul=3, stage=gstg)
        gsel = [k.sb([3, 128], F32, "n_gsel%d" % i) for i in range(3)]
        for gi in range(3):
            k.op("dve", lambda gi=gi: nc.vector.tensor_scalar(out=gsel[gi][:], in0=self.onesF[0:3, :], scalar1=self.identF[0:3, gi:gi + 1], scalar2=None, op0=ALU.mult),
                 reads=[self.onesF, self.identF], writes=[gsel[gi]])
        self.zfm(qown, qown.h, _OFF["nsa_q"], 0, q="pool", stage=vsb)
        k.op("dve", lambda: nc.vector.tensor_scalar(out=qown[:], in0=qown[:], scalar1=SCALE, scalar2=None, op0=ALU.mult), reads=[qown], writes=[qown])
        self.zfm(ksb, ksb.h, _OFF["nsa_ks"], None, q="pool")
        self.zfm(kwb, kwb.h, _OFF["nsa_kw"], None, q="pool")
        with k.scope():
            stg = [k.sb([128, T_], BF16, "n_stg%d" % i) for i in range(2)]
            self.zfm(stg[0], stg[0].h, _OFF["nsa_vs"], None, q="pool")
            self.zfm(stg[1], stg[1].h, _OFF["nsa_vw"], None, q="pool")
            self.fm2tm(stg[0].h, stg[0], vsb, vsb.h)
            self.fm2tm(stg[1].h, stg[1], vwb, vwb.h)
        cscope = k.scope()
        cscope.__enter__()
        kcin_t = k.sb([128, T_], BF16, "n_kcin"); vcin_t = k.sb([128, T_], BF16, "n_vcin")
        kcin = kcin_t.h; vcin = vcin_t.h
        self.zfm(kcin_t, kcin, _OFF["nsa_kc"], None, q="pool")
        self.zfm(vcin_t, vcin, _OFF["nsa_vc"], None, q="pool")
        w1 = [k.sb([128, 32, 128], BF16, "n_w1k"), k.sb([128, 32, 128], BF16, "n_w1v")]
        w2 = [k.sb([128, 128], BF16, "n_w2k"), k.sb([128, 128], BF16, "n_w2v")]
        pe = [k.sb([128, 32], BF16, "n_pek"), k.sb([128, 32], BF16, "n_pev")]
        self.ld(w1[0], dw1k, q="pool"); self.ld(w1[1], dw1v, q="pool"); self.ld(w2[0], dw2k, q="pool"); self.ld(w2[1], dw2v, q="pool")
        self.ld(pe[0], dpek, q="pool"); self.ld(pe[1], dpev, q="pool")
        hidb = [k.sb([128, 256], BF16, "n_hidk"), k.sb([128, 256], BF16, "n_hidv")]
        pb = k.sb([128, 1], F32, "n_pb")
        hx = k.sb([128, 256], F32, "n_hx"); hy = k.sb([128, 256], F32, "n_hy"); hz = k.sb([128, 256], F32, "n_hz")
        srcs = [kcin_t, vcin_t]
        cin = [kcin, vcin]
        for w in range(2):
            px = self.psX

            def mmb():
                ins = None
                for i in range(32):
                    ins = nc.tensor.matmul(px[:, 0:1], lhsT=w1[w][:, i, :], rhs=pe[w][:, i:i + 1], start=(i == 0), stop=(i == 31))
                return ins
            k.op("pe", mmb, reads=[w1[w], pe[w]], writes=[px])
            k.op("dve", lambda: nc.vector.tensor_copy(pb[:], px[:, 0:1]), reads=[px], writes=[pb])
            ph = self.psS[w]

            def mmh():
                ins = None
                for i in range(32):
                    ins = nc.tensor.matmul(ph[:, 0:255], lhsT=w1[w][:, i, :], rhs=cin[w][:, i:i + 4065:16], start=(i == 0), stop=(i == 31))
                return ins
            k.op("pe", mmh, reads=[w1[w], srcs[w]], writes=[ph])
            k.op("dve", lambda: nc.vector.memset(hx[:], 0.0), writes=[hx])
            k.op("act", lambda: nc.scalar.activation(out=hx[:, 0:255], in_=ph[:, 0:255], func=AF.Identity, bias=pb[:, 0:1]),
                 reads=[ph, pb], writes=[hx])
            self.gelu_tanh(hz, hx, hy)
            k.op("dve", lambda: nc.vector.tensor_copy(hidb[w][:], hz[:]), reads=[hz], writes=[hidb[w]])
        pk = self.psS[2]
        k.op("pe", lambda: nc.tensor.matmul(pk[:, 0:256], lhsT=w2[0][:], rhs=hidb[0][:], start=True, stop=True), reads=[w2[0], hidb[0]], writes=[pk])
        k.op("dve", lambda: nc.vector.tensor_copy(kcT[:], pk[:, 0:256]), reads=[pk], writes=[kcT])
        for nt in range(2):
            pv = self.psO[nt]
            k.op("pe", lambda: nc.tensor.matmul(pv[:, 0:128], lhsT=hidb[1][:, nt * 128:(nt + 1) * 128], rhs=w2[1][:], start=True, stop=True),
                 reads=[hidb[1], w2[1]], writes=[pv])
            k.op("dve", lambda: nc.vector.tensor_copy(vc[:, nt, :], pv[:, 0:128]), reads=[pv], writes=[(vc, nt)])
        cscope.__exit__(None, None, None)
        tbs = k.sb([128, 12, 512], BF16, "n_tbs"); tbw = k.sb([128, 8, 512], BF16, "n_tbw")
        self.ld(tbs, dtbs, q="pool"); self.ld(tbw, dtbw, q="pool")
        keep = k.sb([128, 4, 64], F32, "n_keep"); addt = k.sb([128, 4, 64], F32, "n_add")
        Eb = k.sb([64, T_], BF16, "n_E"); ov = k.sb([128, 2, 64], BF16, "n_ov")
        self.ld(Eb, dE, q="pool"); self.ld(ov, dov, q="pool")
        qo = k.sb([128, 3, 512], BF16, "n_qo"); qst = k.sb([128, 4, 512], BF16, "n_qst")
        bc = [k.sb([128, 2, 4, 512], BF16, "n_bc0")] * 2
        gb = [k.sb([128, 3, 512], F32, "n_gb0")] * 2
        pc = [[k.sb([128, 512], BF16, "n_pc%d%d" % (h, nt)) for nt in range(2)] for h in range(4)]
        Rh = [k.sb([128, 512], F32, "n_R")] * 4
        acc = [k.sb([128, 512], F32, "n_acc%d" % i) for i in range(2)]
        impt = k.sb([128, 4, 64], F32, "n_imp"); wrk = k.sb([128, 64], F32, "n_wrk")
        m8 = k.sb([128, 8], F32, "n_m8"); thr = k.sb([128, 1], F32, "n_thr"); selb = k.sb([128, 64], F32, "n_selb")
        selT = k.sb([64, 512], BF16, "n_selT")
        Wt = k.sb([128, 512], F32, "n_W")
        NKC = [128, 127]
        for i in range(8):
            q0 = 512 * i
            bci = bc[i % 2]; gbi = gb[i % 2]; ac = acc[i % 2]
            k.dma("pool", bci[:], dbc.h[:, :, :, q0:q0 + 512], reads=[dbc], writes=[bci])
            for gi in range(3):
                pgx = self.psS[self.iS % 3]; self.iS += 1
                k.op("pe", lambda: nc.tensor.matmul(pgx[:], lhsT=gsel[gi][:], rhs=grow[0:3, q0:q0 + 512], start=True, stop=True),
                     reads=[gsel[gi], grow], writes=[pgx])
                k.op("act", lambda: nc.scalar.activation(out=gbi[:, gi, :], in_=pgx[:], func=AF.Sigmoid), reads=[pgx], writes=[(gbi, gi)])
            sq_ = q0 // TOK
            c0 = q0 - sq_ * TOK
            for jc in range(4):
                for (ct, sap_, d0, cnt) in self.zall.pieces(sq_, _OFF["nsa_q"] + jc * 128, 128):
                    k.dma("pool", qst[d0:d0 + cnt, jc, :], sap_[:, c0:c0 + 512], reads=[ct], writes=[qst])
            for hh in range(3):
                for jc in range(4):
                    mc = self.msk[:, 4 + 4 * hh + jc:5 + 4 * hh + jc]
                    if jc == 0:
                        k.op("dve", lambda: nc.vector.tensor_scalar(out=qo[:, hh, :], in0=qst[:, jc, :], scalar1=mc, scalar2=None, op0=ALU.mult),
                             reads=[qst, self.msk], writes=[qo])
                    else:
                        k.op("dve", lambda: nc.vector.scalar_tensor_tensor(out=qo[:, hh, :], in0=qst[:, jc, :], scalar=mc, in1=qo[:, hh, :], op0=ALU.mult, op1=ALU.add),
                             reads=[qst, self.msk, qo], writes=[qo])
            k.op("dve", lambda: nc.vector.tensor_scalar(out=qo[:], in0=qo[:], scalar1=SCALE, scalar2=None, op0=ALU.mult), reads=[qo], writes=[qo])
            k.dma("sp", keep[:], dkeep.h[:, 4 * i:4 * i + 4, :], reads=[dkeep], writes=[keep])
            k.dma("sp", addt[:], dadd.h[:, 4 * i:4 * i + 4, :], reads=[dadd], writes=[addt])
            for hh in range(4):
                h = hh
                own = (h == 3)
                L = self.psL[hh % 2]; O = self.psO[0]
                for nt in range(2):
                    nk = NKC[nt]
                    S = self.psS[self.iS % 3]; self.iS += 1
                    P = pc[h][nt]

                    def mm():
                        nc.tensor.matmul(S[0:nk, :], lhsT=kcT[:, nt * 128:nt * 128 + nk], rhs=(qown[:, q0:q0 + 512] if h == 3 else qo[:, h, :]), start=True, stop=False)
                        return nc.tensor.matmul(S[0:nk, :], lhsT=self.identB[:, 0:nk], rhs=bci[:, nt, h, :], start=False, stop=True)
                    k.op("pe", mm, reads=[kcT, qown, qo, self.identB, bci], writes=[S])
                    if nk < 128:
                        k.op("dve", lambda: nc.vector.memset(P[:], 0.0), writes=[P])
                    k.op("act", lambda: nc.scalar.activation(out=P[0:nk, :], in_=S[0:nk, :], func=AF.Exp), reads=[S], writes=[P])
                    k.op("pe", lambda: nc.tensor.matmul(L[:], lhsT=self.onesB[0:nk, :], rhs=P[0:nk, :], start=(nt == 0), stop=(nt == 1)),
                         reads=[self.onesB, P], writes=[L])
                    if own:
                        k.op("pe", lambda: nc.tensor.matmul(O[:], lhsT=vc[0:nk, nt, :], rhs=P[0:nk, :], start=(nt == 0), stop=(nt == 1)),
                             reads=[vc, P], writes=[O])
                k.op("dve", lambda: nc.vector.tensor_scalar(out=Rh[h][:], in0=L[:], scalar1=1e-30, scalar2=None, op0=ALU.max),
                     reads=[L], writes=[Rh[h]])
                k.op("dve", lambda: nc.vector.reciprocal(out=Rh[h][:], in_=Rh[h][:]), reads=[Rh[h]], writes=[Rh[h]])
                if own:
                    k.op("dve", lambda: nc.vector.tensor_tensor(out=Wt[:], in0=Rh[h][:], in1=gbi[:, 0, :], op=ALU.mult), reads=[Rh[h], gbi], writes=[Wt])
                    k.op("dve", lambda: nc.vector.tensor_tensor(out=ac[:], in0=O[:], in1=Wt[:], op=ALU.mult), reads=[O, Wt], writes=[ac])
                for nt in range(2):
                    k.op("dve", lambda: nc.vector.tensor_tensor(out=pc[h][nt][:], in0=pc[h][nt][:], in1=Rh[h][:], op=ALU.mult),
                         reads=[pc[h][nt], Rh[h]], writes=[pc[h][nt]])
            px = self.psX
            for jq in range(4):
                def mmi():
                    ins = None
                    n = 0
                    for h in range(4):
                        for nt in range(2):
                            ins = nc.tensor.matmul(px[:, jq * 64:(jq + 1) * 64], lhsT=pc[h][nt][:, jq * 128:(jq + 1) * 128], rhs=ov[:, nt, :],
                                                   start=(n == 0), stop=(n == 7))
                            n += 1
                    return ins
                k.op("pe", mmi, reads=[ov] + [pc[h][nt] for h in range(4) for nt in range(2)], writes=[(px, jq)])
            k.op("dve", lambda: nc.vector.tensor_tensor(out=impt[:].rearrange("p a b -> p (a b)"), in0=px[:, 0:256],
                                                        in1=keep[:].rearrange("p a b -> p (a b)"), op=ALU.mult),
                 reads=[px, keep], writes=[impt])
            k.op("dve", lambda: nc.vector.tensor_tensor(out=impt[:], in0=impt[:], in1=addt[:], op=ALU.add),
                 reads=[impt, addt], writes=[impt])
            pt = self.psL[0]
            for jq in range(4):
                k.op("dve", lambda: nc.vector.max(out=m8[:], in_=impt[:, jq, :]), reads=[impt], writes=[m8])
                k.op("dve", lambda: nc.vector.match_replace(out=wrk[:], in_to_replace=m8[:], in_values=impt[:, jq, :], imm_value=-1e30),
                     reads=[m8, impt], writes=[wrk])
                k.op("dve", lambda: nc.vector.max(out=m8[:], in_=wrk[:]), reads=[wrk], writes=[m8])
                k.op("dve", lambda: nc.vector.tensor_reduce(out=thr[:], in_=m8[:], axis=AX.X, op=ALU.min), reads=[m8], writes=[thr])
                k.op("dve", lambda: nc.vector.tensor_scalar(out=selb[:], in0=impt[:, jq, :], scalar1=thr[:, 0:1], scalar2=NEG,
                                                            op0=ALU.is_lt, op1=ALU.mult), reads=[impt, thr], writes=[selb])
                k.op("pe", lambda: nc.tensor.matmul(pt[0:64, jq * 128:(jq + 1) * 128], lhsT=selb[:], rhs=self.identF[:], start=True, stop=True),
                     reads=[selb, self.identF], writes=[(pt, jq)])
            k.op("dve", lambda: nc.vector.tensor_copy(selT[:], pt[0:64, :]), reads=[pt], writes=[selT])
            O = self.psO[1]; L = self.psL[1]
            nkt = 4 * i + 4
            for kt in range(nkt):
                idx = min(4 * i - kt + 3, 11)
                extra = [(lambda S: S[:], self.identB[:], tbs[:, idx, :]),
                         (lambda S: S[:], Eb[:, kt * 128:(kt + 1) * 128], selT[:])]
                self._xr = [self.identB, tbs, Eb, selT]
                self.attn_unit(ksb, kt, qown, q0, extra, None, [], vsb, vsb[:, kt * 128:(kt + 1) * 128], O, L, kt == 0, kt == nkt - 1)
            self.flush_attn()
            self.nsa_fin(O, L, gbi, 1, ac, Wt)
            O = self.psO[0]; L = self.psL[0]
            kts = list(range(max(0, 4 * i - 4), 4 * i + 4))
            for n, kt in enumerate(kts):
                idx = 4 * i - kt + 3
                extra = [(lambda S: S[:], self.identB[:], tbw[:, idx, :])]
                self._xr = [self.identB, tbw]
                self.attn_unit(kwb, kt, qown, q0, extra, None, [], vwb, vwb[:, kt * 128:(kt + 1) * 128], O, L, n == 0, n == len(kts) - 1)
            self.flush_attn()
            self.nsa_fin(O, L, gbi, 2, ac, Wt)
            k.dma("sp", yout.h[:, q0:q0 + 512], ac[:], reads=[ac], writes=[(yout, i)])

    def nsa_fin(self, O, L, gbi, gi, ac, Wt):
        k, nc = self.k, self.nc
        t2 = self.tmpA[0]
        k.op("dve", lambda: nc.vector.reciprocal(out=Wt[:], in_=L[:]), reads=[L], writes=[Wt])
        k.op("dve", lambda: nc.vector.tensor_tensor(out=Wt[:], in0=Wt[:], in1=gbi[:, gi, :], op=ALU.mult), reads=[Wt, gbi], writes=[Wt])
        k.op("dve", lambda: nc.vector.tensor_tensor(out=t2[:], in0=O[:], in1=Wt[:], op=ALU.mult), reads=[O, Wt], writes=[t2])
        k.op("dve", lambda: nc.vector.tensor_tensor(out=ac[:], in0=ac[:], in1=t2[:], op=ALU.add), reads=[ac, t2], writes=[ac])


    def gdn(self, yout):
        k, nc = self.k, self.nc
        L_ = "%d" % self.l
        dcw = k.din("gdn_cw" + L_, [128, 3, 4])
        dal = k.din("gdn_alog" + L_, [128, 1]); ddt = k.din("gdn_dtb" + L_, [128, 1]); dng = k.din("gdn_ng" + L_, [128, 1])
        dct = k.din("c_ct", [128, 128]); dsc = k.din("c_sc", [128, 128]); dh0 = k.din("c_h0", [128, 128]); dh1 = k.din("c_h1", [128, 128])
        dmst = k.din("c_mst", [128, 128]); dmit = k.din("c_mit", [128, 128]); dmsn = k.din("c_msn", [128, 128]); dcm = k.din("c_cm", [128, 2])
        B = self.big
        raw, W_, tA, tB = B[0], B[1], B[2], B[3]
        qs = ks = vs = oT = W_
        arow = k.sb([1, T_], F32, "g_arow")
        qnb, knb, vsb = self.bigb[0], self.bigb[1], self.bigb[2]
        cw = k.sb([128, 3, 4], F32, "g_cw"); self.ld(cw, dcw)
        cst = {}
        for nm, dd in (("ct", dct), ("sc", dsc), ("h0", dh0), ("h1", dh1), ("mst", dmst), ("mit", dmit), ("msn", dmsn)):
            cst[nm] = k.sb([128, 128], F32, "g_" + nm); self.ld(cst[nm], dd)
        cm = k.sb([128, 2], F32, "g_cm"); self.ld(cm, dcm)
        al = k.sb([128, 1], F32, "g_al"); dtb = k.sb([128, 1], F32, "g_dtb"); ng = k.sb([128, 1], F32, "g_ng")
        self.ld(al, dal); self.ld(dtb, ddt); self.ld(ng, dng)
        epsc = k.sb([128, 1], F32, "g_eps")
        k.op("dve", lambda: nc.vector.memset(epsc[:], EPS), writes=[epsc])
        for wi, (src, dst) in enumerate((("gdn_q", qs), ("gdn_k", ks), ("gdn_v", vs))):
            self.zfm(raw, raw.h, _OFF[src], 0, stage=tA)
            k.op("dve", lambda: nc.vector.tensor_scalar(out=dst[:], in0=raw[:], scalar1=cw[:, wi, 3:4], scalar2=None, op0=ALU.mult),
                 reads=[raw, cw], writes=[dst])
            for i in range(3):
                sh = 3 - i
                k.op("dve", lambda: nc.vector.scalar_tensor_tensor(out=dst[:, sh:], in0=raw[:, 0:T_ - sh], scalar=cw[:, wi, i:i + 1],
                                                                   in1=dst[:, sh:], op0=ALU.mult, op1=ALU.add), reads=[raw, cw, dst], writes=[dst])
            k.op("act", lambda: nc.scalar.activation(out=dst[:], in_=dst[:], func=AF.Silu), reads=[dst], writes=[dst])
            if wi == 2:
                k.op("act", lambda: nc.scalar.copy(out=vsb[:], in_=vs[:]), reads=[vs], writes=[vsb])
                continue
            src, dstb, sc = ((qs, qnb, SCALE), (ks, knb, 1.0))[wi]
            if True:
                k.op("dve", lambda: nc.vector.tensor_tensor(out=tA[:], in0=src[:], in1=src[:], op=ALU.mult), reads=[src], writes=[tA])
                for tb in range(8):
                    ts = slice(tb * 512, (tb + 1) * 512)
                    ps = self.psS[tb % 3]
                    k.op("pe", lambda: nc.tensor.matmul(ps[:], lhsT=self.onesF[:], rhs=tA[:, ts], start=True, stop=True), reads=[self.onesF, tA], writes=[ps])
                    k.op("act", lambda: nc.scalar.activation(out=tB[:, ts], in_=ps[:], func=AF.Ln, bias=epsc[:, 0:1]), reads=[ps, epsc], writes=[(tB, tb)])
                    k.op("act", lambda: nc.scalar.activation(out=tB[:, ts], in_=tB[:, ts], func=AF.Exp, scale=-0.5), reads=[(tB, tb)], writes=[(tB, tb)])
                k.op("dve", lambda: nc.vector.scalar_tensor_tensor(out=dstb[:], in0=src[:], scalar=sc, in1=tB[:], op0=ALU.mult, op1=ALU.mult),
                     reads=[src, tB], writes=[dstb])
        def col(nm):
            return k.sb([128, NT], F32, "g_c_" + nm)
        g = col("g"); beta = col("beta"); gc = col("gc"); ngc = col("ngc"); gl = col("gl"); wcol = col("w")
        skbg = col("skbg"); skd = [col("skd0"), col("skd1")]; egl = [col("egl0"), col("egl1")]; tmpc = col("tmp")
        self.zfm(arow, arow.h, _OFF["gdn_a"], 0, n=1, mul=1, stage=tB)
        self.row2col(arow, g)
        self.zfm(arow, arow.h, _OFF["gdn_b"], 0, n=1, mul=1, stage=tB)
        self.row2col(arow, beta)
        k.op("act", lambda: nc.scalar.activation(out=g[:], in_=g[:], func=AF.Exp, bias=dtb[:, 0:1]), reads=[g, dtb], writes=[g])
        k.op("act", lambda: nc.scalar.activation(out=g[:], in_=g[:], func=AF.Ln, bias=self.onesF[:, 0:1]), reads=[g, self.onesF], writes=[g])
        k.op("act", lambda: nc.scalar.activation(out=al[:], in_=al[:], func=AF.Exp), reads=[al], writes=[al])
        k.op("dve", lambda: nc.vector.tensor_scalar(out=g[:], in0=g[:], scalar1=al[:, 0:1], scalar2=-1.0, op0=ALU.mult, op1=ALU.mult),
             reads=[g, al], writes=[g])
        k.op("act", lambda: nc.scalar.activation(out=beta[:], in_=beta[:], func=AF.Sigmoid), reads=[beta], writes=[beta])
        px = self.psX

        def colmm(lhs, dst, func=None):
            k.op("pe", lambda: nc.tensor.matmul(px[:, 0:NT], lhsT=lhs[:], rhs=g[:], start=True, stop=True), reads=[lhs, g], writes=[px])
            if func is None:
                k.op("dve", lambda: nc.vector.tensor_copy(dst[:], px[:, 0:NT]), reads=[px], writes=[dst])
            else:
                k.op("act", lambda: nc.scalar.activation(out=dst[:], in_=px[:, 0:NT], func=func), reads=[px], writes=[dst])
        colmm(cst["ct"], gc)
        colmm(cst["sc"], gl)
        colmm(cst["h0"], egl[0], AF.Exp)
        colmm(cst["h1"], egl[1], AF.Exp)
        k.op("dve", lambda: nc.vector.tensor_scalar(out=ngc[:], in0=gc[:], scalar1=-1.0, scalar2=None, op0=ALU.mult), reads=[gc], writes=[ngc])
        k.op("act", lambda: nc.scalar.activation(out=wcol[:], in_=beta[:], func=AF.Ln), reads=[beta], writes=[wcol])
        k.op("dve", lambda: nc.vector.tensor_tensor(out=wcol[:], in0=wcol[:], in1=gc[:], op=ALU.add), reads=[wcol, gc], writes=[wcol])
        k.op("act", lambda: nc.scalar.activation(out=skbg[:], in_=gc[:], func=AF.Exp), reads=[gc], writes=[skbg])
        k.op("dve", lambda: nc.vector.tensor_tensor(out=skbg[:], in0=skbg[:], in1=beta[:], op=ALU.mult), reads=[skbg, beta], writes=[skbg])
        k.op("dve", lambda: nc.vector.tensor_tensor(out=tmpc[:], in0=gl[:], in1=gc[:], op=ALU.subtract), reads=[gl, gc], writes=[tmpc])
        k.op("act", lambda: nc.scalar.activation(out=tmpc[:], in_=tmpc[:], func=AF.Exp), reads=[tmpc], writes=[tmpc])
        for c in range(2):
            k.op("dve", lambda: nc.vector.tensor_scalar(out=skd[c][:], in0=tmpc[:], scalar1=cm[:, c:c + 1], scalar2=None, op0=ALU.mult),
                 reads=[tmpc, cm], writes=[skd[c]])
        S = k.sb([128, 128], F32, "g_S"); Sb = k.sb([128, 128], BF16, "g_Sb")
        k.op("dve", lambda: nc.vector.memset(S[:], 0.0), writes=[S])
        k.op("dve", lambda: nc.vector.memset(Sb[:], 0.0), writes=[Sb])

        def t128(nm, dt=F32):
            return k.sb([128, 128], dt, "g_t_" + nm)
        kbg = t128("kbg", BF16)
        kd2 = [[t128("kd0a", BF16), t128("kd1a", BF16)], [t128("kd0b", BF16), t128("kd1b", BF16)]]
        vb2 = [t128("vba", BF16), t128("vbb", BF16)]
        dgw = t128("dgw"); dgg = t128("dgg"); dgn = t128("dgn")
        Gs = t128("G"); Y = t128("Y"); X = t128("X"); Pm = t128("P"); Z = t128("Z"); ZT = t128("ZT"); Z2 = t128("Z2"); ZT2 = t128("ZT2")
        E1 = t128("E1")
        qkT2 = [t128("qkTa", BF16), t128("qkTb", BF16)]; qgT2 = [t128("qgTa", BF16), t128("qgTb", BF16)]
        PTb2 = [t128("PTba", BF16), t128("PTbb", BF16)]; nWT2 = [t128("nWTa", BF16), t128("nWTb", BF16)]
        vnb = t128("vnb", BF16)
        poolA = [self.psS[0], self.psS[1], self.psS[2], self.psL[0]]
        poolB = [self.psO[1], self.psL[1]]
        ctr = [0, 0]

        def pp():
            ctr[0] += 1
            return poolA[ctr[0] % 4]

        def ppB():
            ctr[1] += 1
            return poolB[ctr[1] % 2]
        pOg = self.psO[0]

        def pre(t):
            cs = slice(t * 128, (t + 1) * 128)
            tc_ = slice(t, t + 1)
            kd = kd2[t % 2]; vb = vb2[t % 2]; qkT = qkT2[t % 2]; qgT = qgT2[t % 2]; PTb = PTb2[t % 2]; nWT = nWT2[t % 2]
            pa = pp()
            k.op("pe", lambda: nc.tensor.matmul(pa[:, 0:128], lhsT=knb[:, cs], rhs=self.identB[:], start=True, stop=True), reads=[knb, self.identB], writes=[(pa, 0)])
            k.op("pe", lambda: nc.tensor.matmul(pa[:, 128:256], lhsT=vsb[:, cs], rhs=self.identB[:], start=True, stop=True), reads=[vsb, self.identB], writes=[(pa, 1)])
            k.op("dve", lambda: nc.vector.tensor_scalar(out=kbg[:], in0=pa[:, 0:128], scalar1=skbg[:, tc_], scalar2=None, op0=ALU.mult), reads=[(pa, 0), skbg], writes=[kbg])
            for c in range(2):
                k.op("dve", lambda: nc.vector.tensor_scalar(out=kd[c][:], in0=pa[:, 0:128], scalar1=skd[c][:, tc_], scalar2=None, op0=ALU.mult),
                     reads=[(pa, 0), skd[c]], writes=[kd[c]])
            k.op("dve", lambda: nc.vector.tensor_scalar(out=vb[:], in0=pa[:, 128:256], scalar1=beta[:, tc_], scalar2=None, op0=ALU.mult), reads=[(pa, 1), beta], writes=[vb])
            k.op("dve", lambda: nc.vector.tensor_scalar(out=dgw[:], in0=self.identF[:], scalar1=wcol[:, tc_], scalar2=None, op0=ALU.mult), reads=[self.identF, wcol], writes=[dgw])
            k.op("dve", lambda: nc.vector.tensor_scalar(out=dgg[:], in0=self.identF[:], scalar1=gc[:, tc_], scalar2=None, op0=ALU.mult), reads=[self.identF, gc], writes=[dgg])
            k.op("dve", lambda: nc.vector.tensor_scalar(out=dgn[:], in0=self.identF[:], scalar1=ngc[:, tc_], scalar2=None, op0=ALU.mult), reads=[self.identF, ngc], writes=[dgn])
            pg = pp()
            k.op("pe", lambda: nc.tensor.matmul(pg[:, 0:128], lhsT=knb[:, cs], rhs=knb[:, cs], start=True, stop=True), reads=[knb], writes=[(pg, 0)])
            k.op("pe", lambda: nc.tensor.matmul(pg[:, 128:256], lhsT=knb[:, cs], rhs=qnb[:, cs], start=True, stop=True), reads=[knb, qnb], writes=[(pg, 1)])
            k.op("dve", lambda: nc.vector.tensor_copy(Gs[:], pg[:, 0:128]), reads=[(pg, 0)], writes=[Gs])
            yield

            def expmat(diag, mask, bias_col, bias_t, dst_fn):
                pe_ = pp()

                def mm():
                    ins0 = nc.tensor.matmul(pe_[:, 0:128], lhsT=self.onesF[:], rhs=diag[:], start=True, stop=(mask is None))
                    if mask is None:
                        return ins0
                    return nc.tensor.matmul(pe_[:, 0:128], lhsT=self.identF[:], rhs=mask[:], start=False, stop=True)
                k.op("pe", mm, reads=[self.onesF, diag, self.identF] + ([mask] if mask is not None else []), writes=[pe_])
                if bias_col is None:
                    k.op("act", lambda: nc.scalar.activation(out=E1[:], in_=pe_[:, 0:128], func=AF.Exp), reads=[pe_], writes=[E1])
                else:
                    k.op("act", lambda: nc.scalar.activation(out=E1[:], in_=pe_[:, 0:128], func=AF.Exp, bias=bias_col[:, tc_]),
                         reads=[pe_, bias_t], writes=[E1])
                dst_fn()
            expmat(dgw, cst["mst"], ngc, ngc, lambda: k.op("dve", lambda: nc.vector.tensor_tensor(out=Y[:], in0=Gs[:], in1=E1[:], op=ALU.mult), reads=[Gs, E1], writes=[Y]))
            yield
            expmat(dgn, cst["msn"], wcol, wcol, lambda: k.op("dve", lambda: nc.vector.tensor_tensor(out=X[:], in0=Gs[:], in1=E1[:], op=ALU.mult), reads=[Gs, E1], writes=[X]))
            yield
            expmat(dgg, cst["mit"], ngc, ngc, lambda: k.op("dve", lambda: nc.vector.tensor_tensor(out=qkT[:], in0=pg[:, 128:256], in1=E1[:], op=ALU.mult), reads=[(pg, 1), E1], writes=[qkT]))
            yield
            expmat(dgg, None, None, None, lambda: k.op("dve", lambda: nc.vector.tensor_tensor(out=qgT[:], in0=qnb[:, cs], in1=E1[:], op=ALU.mult), reads=[qnb, E1], writes=[qgT]))
            yield
            k.op("dve", lambda: nc.vector.tensor_tensor(out=Pm[:], in0=self.identF[:], in1=Y[:], op=ALU.subtract), reads=[self.identF, Y], writes=[Pm])
            p1 = pp(); p2 = pp()
            k.op("pe", lambda: nc.tensor.matmul(p1[:, 0:128], lhsT=X[:], rhs=Y[:], start=True, stop=True), reads=[X, Y], writes=[p1])
            k.op("pe", lambda: nc.tensor.matmul(p2[:, 0:128], lhsT=Y[:], rhs=X[:], start=True, stop=True), reads=[X, Y], writes=[p2])
            zc, ztc, zn, ztn = Z, ZT, Z2, ZT2
            k.op("dve", lambda: nc.vector.tensor_copy(zc[:], p1[:, 0:128]), reads=[p1], writes=[zc])
            k.op("act", lambda: nc.scalar.copy(out=ztc[:], in_=p2[:, 0:128]), reads=[p2], writes=[ztc])
            yield
            for it in range(5):
                p3 = pp()
                k.op("pe", lambda: nc.tensor.matmul(p3[:, 0:128], lhsT=ztc[:], rhs=Pm[:], start=True, stop=True), reads=[ztc, Pm], writes=[p3])
                if it < 4:
                    p1 = pp(); p2 = pp()
                    k.op("pe", lambda: nc.tensor.matmul(p1[:, 0:128], lhsT=ztc[:], rhs=zc[:], start=True, stop=True), reads=[ztc, zc], writes=[p1])
                    k.op("pe", lambda: nc.tensor.matmul(p2[:, 0:128], lhsT=zc[:], rhs=ztc[:], start=True, stop=True), reads=[ztc, zc], writes=[p2])
                k.op("dve", lambda: nc.vector.tensor_tensor(out=Pm[:], in0=Pm[:], in1=p3[:, 0:128], op=ALU.add), reads=[Pm, p3], writes=[Pm])
                if it < 4:
                    k.op("dve", lambda: nc.vector.tensor_copy(zn[:], p1[:, 0:128]), reads=[p1], writes=[zn])
                    k.op("act", lambda: nc.scalar.copy(out=ztn[:], in_=p2[:, 0:128]), reads=[p2], writes=[ztn])
                    zc, ztc, zn, ztn = zn, ztn, zc, ztc
                yield
            k.op("act", lambda: nc.scalar.copy(out=PTb[:], in_=Pm[:]), reads=[Pm], writes=[PTb])
            pw = pp()
            k.op("pe", lambda: nc.tensor.matmul(pw[:, 0:128], lhsT=kbg[:], rhs=PTb[:], start=True, stop=True), reads=[kbg, PTb], writes=[pw])
            k.op("dve", lambda: nc.vector.tensor_scalar(out=nWT[:], in0=pw[:, 0:128], scalar1=-1.0, scalar2=None, op0=ALU.mult), reads=[pw], writes=[nWT])
            yield

        def rec(t):
            cs = slice(t * 128, (t + 1) * 128)
            tc_ = slice(t, t + 1)
            kd = kd2[t % 2]; vb = vb2[t % 2]; qkT = qkT2[t % 2]; qgT = qgT2[t % 2]; PTb = PTb2[t % 2]; nWT = nWT2[t % 2]
            for c in range(2):
                ccs = slice(64 * c, 64 * c + 64)
                pv = ppB()

                def mmv():
                    nc.tensor.matmul(pv[:, 0:128], lhsT=PTb[:], rhs=vb[:], start=True, stop=False)
                    return nc.tensor.matmul(pv[:, 0:128], lhsT=nWT[:], rhs=Sb[:], start=False, stop=True)
                k.op("pe", mmv, reads=[PTb, vb, nWT, Sb], writes=[pv])
                yield
                k.op("act", lambda: nc.scalar.copy(out=vnb[:], in_=pv[:, 0:128]), reads=[pv], writes=[vnb])
                yield

                def mmo():
                    nc.tensor.matmul(pOg[:, ccs], lhsT=Sb[:], rhs=qgT[:, ccs], start=True, stop=False)
                    return nc.tensor.matmul(pOg[:, ccs], lhsT=vnb[:], rhs=qkT[:, ccs], start=False, stop=True)
                k.op("pe", mmo, reads=[Sb, qgT, vnb, qkT], writes=[(pOg, c)])
                pu = ppB()
                k.op("pe", lambda: nc.tensor.matmul(pu[:, 0:128], lhsT=kd[c][:], rhs=vnb[:], start=True, stop=True), reads=[kd[c], vnb], writes=[pu])
                yield
                k.op("dve", lambda: nc.vector.scalar_tensor_tensor(out=S[:], in0=S[:], scalar=egl[c][:, tc_], in1=pu[:, 0:128], op0=ALU.mult, op1=ALU.add),
                     reads=[S, egl[c], pu], writes=[S])
                yield
                k.op("act", lambda: nc.scalar.copy(out=Sb[:], in_=S[:]), reads=[S], writes=[Sb])
                yield
            k.op("dve", lambda: nc.vector.tensor_copy(oT[:, cs], pOg[:, 0:128]), reads=[pOg], writes=[(oT, t)])

        def drive(*gens):
            gens = [g for g in gens if g is not None]
            while gens:
                for g in list(gens):
                    try:
                        next(g)
                    except StopIteration:
                        gens.remove(g)
        drive(pre(0))
        for t in range(NT):
            drive(rec(t), pre(t + 1) if t + 1 < NT else None)
        self.zfm(raw, raw.h, _OFF["gdn_z"], 0, stage=tA)
        k.op("act", lambda: nc.scalar.activation(out=raw[:], in_=raw[:], func=AF.Silu), reads=[raw], writes=[raw])
        k.op("dve", lambda: nc.vector.tensor_tensor(out=tA[:], in0=oT[:], in1=oT[:], op=ALU.mult), reads=[oT], writes=[tA])
        k.op("dve", lambda: nc.vector.tensor_scalar(out=tA[:], in0=tA[:], scalar1=1.0 / 128, scalar2=None, op0=ALU.mult), reads=[tA], writes=[tA])
        for tb in range(8):
            ts = slice(tb * 512, (tb + 1) * 512)
            ps = self.psS[tb % 3]
            k.op("pe", lambda: nc.tensor.matmul(ps[:], lhsT=self.onesF[:], rhs=tA[:, ts], start=True, stop=True), reads=[self.onesF, tA], writes=[ps])
            k.op("act", lambda: nc.scalar.activation(out=tB[:, ts], in_=ps[:], func=AF.Ln, bias=epsc[:, 0:1]), reads=[ps, epsc], writes=[(tB, tb)])
            k.op("act", lambda: nc.scalar.activation(out=tB[:, ts], in_=tB[:, ts], func=AF.Exp, scale=-0.5), reads=[(tB, tb)], writes=[(tB, tb)])
        k.op("dve", lambda: nc.vector.scalar_tensor_tensor(out=oT[:], in0=oT[:], scalar=ng[:, 0:1], in1=tB[:], op0=ALU.mult, op1=ALU.mult),
             reads=[oT, ng, tB], writes=[oT])
        k.op("dve", lambda: nc.vector.tensor_tensor(out=oT[:], in0=oT[:], in1=raw[:], op=ALU.mult), reads=[oT, raw], writes=[oT])
        k.dma("sp", yout.h, oT[:], reads=[oT], writes=[yout])


class Gathered:
    def __init__(self, nc, name, nrows, ncols, CR, dt=F32):
        self.nrows, self.ncols, self.CR = nrows, ncols, CR
        self.chunks = []
        r = 0
        while r < nrows:
            cr = min(CR, nrows - r)
            self.chunks.append((r, cr, T(nc.dram_tensor("%s_c%d" % (name, len(self.chunks)), [4 * cr, ncols], dt).ap(), "%s_c%d" % (name, len(self.chunks)))))
            r += cr

    def pieces(self, s_, r0, n):
        out = []
        r = r0
        while r < r0 + n:
            ci = r // self.CR
            c0, cr, t = self.chunks[ci]
            cnt = min(r0 + n, c0 + cr) - r
            out.append((t, t.h[s_ * cr + (r - c0):s_ * cr + (r - c0) + cnt, :], r - r0, cnt))
            r += cnt
        return out


def exchange(k, src, dst, sem):
    nc = k.nc
    k.barrier()
    sems = []
    for (c0, cr, t) in dst.chunks:
        cs = sem.enter_context(nc.semaphore("cc_%s" % t.name))
        sems.append(cs)
        nc.gpsimd.collective_compute("AllGather", ALU.bypass, replica_groups=[[0, 1, 2, 3], [4, 5, 6, 7]],
                                     ins=[src.h[c0:c0 + cr, :].opt()], outs=[t.h.opt()]).then_inc(cs)
    for cs in sems:
        for e in k.eng.values():
            e.wait_ge(cs, 1)


NBIG = {"fox": (2, 4), "lru": (6, 1), "gdn": (4, 3), "nsa": (0, 0)}


def build_fused(dbg=False):
    nc = bass.Bass("TRN2", target_bir_lowering=False)
    with ExitStack() as es:
        k = K(nc, es)
        x = k.din("x", [TOK, D])
        out = k.dout("out", [TOK, D])
        h = k.sb([128, NCH, TOK], F32, "h")
        vals = None
        zsh = [T(nc.dram_tensor("zsh%d" % l, [DIN, TOK], BF16).ap(), "zsh%d" % l) for l in range(2)]
        zall = [Gathered(nc, "zall%d" % l, DIN, TOK, 512, BF16) for l in range(2)]
        ysh = [T(nc.dram_tensor("ysh%d" % l, [4 * 128, T_], F32).ap(), "ysh%d" % l) for l in range(2)]
        yall = [Gathered(nc, "yall%d" % l, 4 * 128, T_, 64) for l in range(2)]
        csem = [es, es, es, es]
        with k.scope():
            load_x(k, h, x)
        for l in range(2):
            with k.scope():
                dense_A(k, h, l, zsh[l], zall[l])
            for gi, nm in enumerate(("fox", "gdn", "lru", "nsa")):
                with k.scope():
                    m = Mix(k, l, zall[l], vals, NBIG[nm][0], NBIG[nm][1])
                    yout = T(ysh[l].h[gi * 128:(gi + 1) * 128, :], "y_%s%d" % (nm, l))
                    getattr(m, nm)(yout)
                    for (c0, cr, ct) in yall[l].chunks[2 * gi:2 * gi + 2]:
                        k.allgather(ysh[l].h[c0:c0 + cr, :], ct, [yout])
            with k.scope():
                dense_C(k, h, l, yall[l], None)
            if dbg and l == 0:
                dh = k.dout("dbg_h", [D, TOK])
                v = dh.h.rearrange("(c p) t -> p c t", p=128)
                for c in range(0, NCH, 4):
                    k.dma("sp", v[:, c:c + 4, :], h[:, c:c + 4, :], reads=[h], writes=[(dh, c)])
        with k.scope():
            final_out(k, h, out)
        k.wait_all("sp")
    return nc


def tm_tiles(a):
    t, d = a.shape
    return np.ascontiguousarray(a.reshape(t // 128, 128, d).transpose(1, 0, 2))


def col_tiles(v):
    return np.ascontiguousarray(v.reshape(-1, 128).T)


_OFF = {}
_o = 0
for _n, _w in (("fox_q", 512), ("fox_k", 512), ("fox_v", 512), ("fox_f", 4), ("gdn_q", 512), ("gdn_k", 512), ("gdn_v", 512),
               ("gdn_a", 4), ("gdn_b", 4), ("gdn_z", 512), ("lru_x", 512), ("lru_gate", 512), ("nsa_q", 512), ("nsa_kc", 128),
               ("nsa_vc", 128), ("nsa_ks", 128), ("nsa_vs", 128), ("nsa_kw", 128), ("nsa_vw", 128), ("nsa_g", 12)):
    _OFF[_n] = _o
    _o += _w


def consts_B():
    c = {}
    c["c_ident"] = np.eye(128, dtype=np.float32)
    p = np.arange(128)
    c["c_ut"] = (p[:, None] <= p[None, :]).astype(np.float32)
    q = np.arange(32)
    c["c_su"] = (q[:, None] < q[None, :]).astype(np.float32)
    col = np.arange(512)
    mb = np.zeros((128, 4, 512), np.float32)
    for m in range(4):
        mb[:, m, :] = np.where(p[:, None] + 128 * m <= col[None, :], 0.0, NEG)
    c["c_mbfox"] = mb
    ch = p // 64
    same = ch[:, None] == ch[None, :]
    c["c_ct"] = (same & (p[:, None] <= p[None, :])).astype(np.float32)
    c["c_sc"] = same.astype(np.float32)
    c["c_h0"] = np.broadcast_to((p < 64)[:, None], (128, 128)).astype(np.float32).copy()
    c["c_h1"] = np.broadcast_to((p >= 64)[:, None], (128, 128)).astype(np.float32).copy()
    c["c_mst"] = np.where(same & (p[None, :] > p[:, None]), 0.0, NEG).astype(np.float32)
    c["c_mit"] = np.where(same & (p[None, :] >= p[:, None]), 0.0, NEG).astype(np.float32)
    c["c_msn"] = np.where(same & (p[:, None] > p[None, :]), 0.0, NEG).astype(np.float32)
    c["c_cm"] = np.stack([(p < 64), (p >= 64)], axis=1).astype(np.float32)
    return c


def prep_mix(inp, l, j):
    L_ = "%d" % l
    m = {}
    hs = slice(j * 128, (j + 1) * 128)
    m["lru_cw" + L_] = np.ascontiguousarray(inp["lru_conv_w"][l][:, hs].T)
    m["lru_cb" + L_] = np.ascontiguousarray(inp["lru_conv_b"][l][hs].reshape(128, 1))
    for nm, src in (("lru_wa", "lru_w_a"), ("lru_wx", "lru_w_x")):
        bd = np.zeros((128, 128), np.float32)
        bd[0:64, 0:64] = inp[src][l][2 * j]
        bd[64:128, 64:128] = inp[src][l][2 * j + 1]
        m[nm + L_] = bd
    m["lru_ba" + L_] = np.ascontiguousarray(inp["lru_b_a"][l][hs].reshape(128, 1))
    m["lru_bx" + L_] = np.ascontiguousarray(inp["lru_b_x"][l][hs].reshape(128, 1))
    m["lru_lam" + L_] = np.ascontiguousarray(inp["lru_lambda"][l][hs].reshape(128, 1))
    cwf = inp["gdn_conv_w"][l]
    m["gdn_cw" + L_] = np.ascontiguousarray(np.stack([cwf[:, g0 * 512:(g0 + 1) * 512][:, hs].T for g0 in range(3)], axis=1))
    m["gdn_alog" + L_] = np.full((128, 1), inp["gdn_a_log"][l][j], np.float32)
    m["gdn_dtb" + L_] = np.full((128, 1), inp["gdn_dt_bias"][l][j], np.float32)
    m["gdn_ng" + L_] = np.ascontiguousarray(inp["gdn_norm_g"][l].reshape(128, 1))
    for nm, src in (("nsa_w1k", "nsa_w1_k"), ("nsa_w1v", "nsa_w1_v")):
        m[nm + L_] = np.ascontiguousarray(inp[src][l].reshape(32, 128, 128).transpose(1, 0, 2))
    m["nsa_w2k" + L_] = inp["nsa_w2_k"][l]; m["nsa_w2v" + L_] = inp["nsa_w2_v"][l]
    m["nsa_pek" + L_] = np.ascontiguousarray(inp["nsa_pe_k"][l].T); m["nsa_pev" + L_] = np.ascontiguousarray(inp["nsa_pe_v"][l].T)
    return m


def prep_shared(inp):
    m = dict(consts_B())
    for l in range(2):
        L_ = "%d" % l
        binp = np.zeros(NZC * 128, np.float32)
        binp[:DIN] = inp["b_in"][l]
        ong = np.ones((D,), np.float32)
        ong[0:512] = inp["out_norm_g"][l][0]
        ong[1024:1536] = inp["out_norm_g"][l][1]
        ong[1536:2048] = inp["out_norm_g"][l][2]
        m.update({"g1a" + L_: col16(inp["ffn1_norm_g"][l]), "g2a" + L_: col16(inp["mix_norm_g"][l]),
                  "f1wg" + L_: inp["ffn1_w_gate"][l], "f1wu" + L_: inp["ffn1_w_up"][l], "f1wd" + L_: inp["ffn1_w_down"][l],
                  "win" + L_: inp["w_in"][l], "bin" + L_: np.ascontiguousarray(binp.reshape(NZC, 128).T),
                  "ong" + L_: col16(ong), "wout" + L_: inp["w_out"][l], "g1c" + L_: col16(inp["ffn2_norm_g"][l]),
                  "f2wg" + L_: inp["ffn2_w_gate"][l], "f2wu" + L_: inp["ffn2_w_up"][l], "f2wd" + L_: inp["ffn2_w_down"][l]})
    m["gf"] = col16(inp["final_norm_g"])
    nsc = nsa_static()
    m["nsa_keep"] = nsc["keep"]; m["nsa_add"] = nsc["add"]; m["nsa_E"] = nsc["E"]; m["nsa_ov"] = nsc["ov"]
    return m


def prep_core(inp, core):
    j = core % 4
    m = {}
    for l in range(2):
        m.update(prep_mix(inp, l, j))
    order = [(j + 1 + hh) % 4 for hh in range(4)]
    rb = np.asarray(inp["rel_bias"], np.float32)
    nsc = nsa_static()
    bcs = np.where(nsc["bc_mask"][:, :, None, :], rb[nsc["bc_idx"]][..., order].transpose(0, 1, 3, 2), NEG)
    m["nsa_bc"] = np.ascontiguousarray(bcs.astype(np.float32))
    m["nsa_tbs"] = np.where(nsc["tbs_mask"], rb[nsc["tbs_idx"], j], NEG).astype(np.float32)
    m["nsa_tbw"] = np.where(nsc["tbw_mask"], rb[nsc["tbw_idx"], j], NEG).astype(np.float32)
    mk = np.zeros((128, 16), np.float32)
    for hh in range(4):
        mk[:, 4 * hh + (j + hh) % 4] = 1.0
    m["msk"] = mk
    return m


_PROG = {}


def run_fused(inputs, dbg=False):
    inp = {k_: np.asarray(v, np.float32) for k_, v in inputs.items()}
    x = inp["x"].reshape(8 * TOK, D)
    key = "dbg" if dbg else "main"
    if key not in _PROG:
        _PROG[key] = build_fused(dbg)
    nc = _PROG[key]
    shared = prep_shared(inp)
    maps = []
    for c in range(8):
        m = dict(shared)
        m.update(prep_core(inp, c))
        m["x"] = np.ascontiguousarray(x[c * TOK:(c + 1) * TOK])
        maps.append(m)
    res = run_bass_kernel_spmd(nc, maps, core_ids=list(range(8)))
    return res.results


def kernel(**inputs):
    res = run_fused(inputs)
    out = np.concatenate([r["out"] for r in res], axis=0).reshape(2, T_, D)
    return np.ascontiguousarray(out.astype(np.float32))


_NSC = {}


def t5_bucket_static(dist):
    import math
    import jax
    import jax.numpy as jnp
    with jax.default_device(jax.devices("cpu")[0]):
        n = jnp.maximum(jnp.asarray(dist, jnp.int32), 0)
        nf = jnp.maximum(n, 1).astype(jnp.float32)
        large = 16 + (jnp.log(nf / 16) / math.log(1024 / 16) * (32 - 16)).astype(jnp.int32)
        large = jnp.minimum(large, 31)
        return np.asarray(jnp.where(n < 16, n, large))


def nsa_static():
    if _NSC:
        return _NSC
    p = np.arange(128)
    col = np.arange(512)
    q = np.arange(T_)
    n = (np.arange(2)[:, None] * 128 + p[None, :])
    d = q[None, None, :] - (16 * n[:, :, None] + 31)
    msk = (d >= 0) & (n[:, :, None] < 255)
    _NSC["bc_idx"] = t5_bucket_static(d).transpose(1, 0, 2)
    _NSC["bc_mask"] = msk.transpose(1, 0, 2)
    ms = np.arange(12) - 3
    d = 128 * ms[None, :, None] + col[None, None, :] - p[:, None, None]
    _NSC["tbs_idx"] = t5_bucket_static(d); _NSC["tbs_mask"] = d >= 0
    mw = np.arange(8) - 3
    d = 128 * mw[None, :, None] + col[None, None, :] - p[:, None, None]
    _NSC["tbw_idx"] = t5_bucket_static(d); _NSC["tbw_mask"] = (d >= 0) & (d < 512)
    qpos = np.arange(NT)[None, :, None] * 128 + p[:, None, None]
    cur = qpos // 64
    jj = np.arange(64)[None, None, :]
    forced = (jj == 0) | (jj == cur) | (jj == cur - 1)
    fut = jj > cur
    _NSC["keep"] = np.where(forced | fut, 0.0, 1.0).astype(np.float32)
    _NSC["add"] = np.where(fut, -1.0, np.where(forced, 1.0e6, 0.0)).astype(np.float32)
    _NSC["E"] = (np.arange(T_)[None, :] // 64 == np.arange(64)[:, None]).astype(np.float32)
    nn = np.arange(256)
    cst, cen = nn * 16, nn * 16 + 31
    sst, sen = np.arange(64) * 64, np.arange(64) * 64 + 63
    ovl = ((cst[:, None] <= sen[None, :]) & (cen[:, None] >= sst[None, :]) & (nn[:, None] < 255)).astype(np.float32)
    _NSC["ov"] = np.ascontiguousarray(ovl.reshape(2, 128, 64).transpose(1, 0, 2))
    return _NSC
```
